# Optimizing a Trainium2 kernel written in Bass

```python
import math
import jax, jax.numpy as jnp
from jax import lax
import numpy as np

D_MODEL = 1024
BATCH = 16
SEQ = 2048
DEPTH = 2

CHUNK = 64
Q_BLOCK = 128

SSD_HEADS = 16
SSD_HEAD_DIM = 64
SSD_INNER = SSD_HEADS * SSD_HEAD_DIM
SSD_GROUPS = 2
SSD_STATE = 128
SSD_CONV = 4
SSD_CONV_DIM = SSD_INNER + 2 * SSD_GROUPS * SSD_STATE

SB_HEADS = 8
SB_HEAD_DIM = 64
SB_WIDTH = SB_HEADS * SB_HEAD_DIM

MLA_HEADS = 8
MLA_Q_RANK = 256
MLA_KV_RANK = 128
MLA_NOPE = 64
MLA_ROPE = 32
MLA_V = 64
ROPE_THETA = 10000.0

FOX_HEADS = 8
FOX_HEAD_DIM = 64
FOX_WIDTH = FOX_HEADS * FOX_HEAD_DIM

EVEN_IN = SSD_INNER + SSD_CONV_DIM + SSD_HEADS + 3 * SB_WIDTH
EVEN_OUT = SSD_INNER + SB_WIDTH
ODD_IN = MLA_Q_RANK + MLA_KV_RANK + MLA_ROPE + 3 * FOX_WIDTH + FOX_HEADS
ODD_OUT = MLA_HEADS * MLA_V + FOX_WIDTH

MOE_GROUPS = 4
MOE_EXPERTS_PER_GROUP = 4
MOE_EXPERTS = MOE_GROUPS * MOE_EXPERTS_PER_GROUP
MOE_TOP_K = 2
MOE_FF = 256

DEEPNORM_ALPHA = (2.0 * DEPTH) ** 0.25
DEEPNORM_BETA = (8.0 * DEPTH) ** -0.25
LN_EPS = 1e-5
RMS_EPS = 1e-6

N_EVEN = (DEPTH + 1) // 2
N_ODD = DEPTH // 2

kernel_name = 'hybrid_ssd_stickbreak_mla_fox_hmoe'


def _layer_norm(x, g, b):
    x32 = x.astype(jnp.float32)
    mu = jnp.mean(x32, -1, keepdims=True)
    var = jnp.mean(jnp.square(x32 - mu), -1, keepdims=True)
    return ((x32 - mu) * lax.rsqrt(var + LN_EPS) * g + b).astype(x.dtype)


def _rms_norm(x, g):
    x32 = x.astype(jnp.float32)
    return (x32 * lax.rsqrt(jnp.mean(x32 * x32, -1, keepdims=True) + RMS_EPS) * g).astype(x.dtype)


def _split(h, sizes):
    idx = [int(i) for i in np.cumsum(sizes)[:-1]]
    return jnp.split(h, idx, axis=-1)


def _rope(x, cos, sin):
    x1, x2 = jnp.split(x, 2, axis=-1)
    return jnp.concatenate([x1 * cos - x2 * sin, x2 * cos + x1 * sin], axis=-1).astype(x.dtype)


def _causal_depthwise_conv(x, w, b):
    k, c = w.shape
    y = lax.conv_general_dilated(x, w[:, None, :], window_strides=(1,), padding=[(k - 1, 0)],
                                 dimension_numbers=('NWC', 'WIO', 'NWC'), feature_group_count=c)
    return y + b


def _ssd_scan(x, dt, a, b_in, c_in):
    f32 = jnp.float32
    bsz, s, h, p = x.shape
    g, n = b_in.shape[2], b_in.shape[3]
    r = h // g
    nc = s // CHUNK
    xs = (x.astype(f32) * dt[..., None]).reshape(bsz, nc, CHUNK, g, r, p)
    da = (dt * a).reshape(bsz, nc, CHUNK, g, r).transpose(0, 3, 4, 1, 2)
    a_cum = jnp.cumsum(da, axis=-1)
    bc = b_in.astype(f32).reshape(bsz, nc, CHUNK, g, n)
    cc = c_in.astype(f32).reshape(bsz, nc, CHUNK, g, n)
    causal = jnp.tril(jnp.ones((CHUNK, CHUNK), dtype=bool))
    seg = a_cum[..., :, None] - a_cum[..., None, :]
    decay_in = jnp.exp(jnp.where(causal, seg, -jnp.inf))
    cb = jnp.einsum('bclgn,bcsgn->bgcls', cc, bc)
    y_diag = jnp.einsum('bgcls,bgrcls,bcsgrp->bclgrp', cb, decay_in, xs)
    decay_to_end = jnp.exp(a_cum[..., -1:] - a_cum)
    states = jnp.einsum('bclgn,bgrcl,bclgrp->bcgrpn', bc, decay_to_end, xs)
    chunk_decay = jnp.exp(a_cum[..., -1])

    def step(hstate, inp):
        st, dec = inp
        return hstate * dec[..., None, None] + st, hstate

    h0 = jnp.zeros((bsz, g, r, p, n), f32)
    _, prev = lax.scan(step, h0, (jnp.moveaxis(states, 1, 0), jnp.moveaxis(chunk_decay, -1, 0)))
    y_off = jnp.einsum('bclgn,cbgrpn,bgrcl->bclgrp', cc, prev, jnp.exp(a_cum))
    return (y_diag + y_off).reshape(bsz, s, h, p)


def _stick_breaking_attention(q, k, v):
    s, d = q.shape[1], q.shape[3]
    scale = 1.0 / math.sqrt(d)
    outs = []
    for q0 in range(0, s, Q_BLOCK):
        end = q0 + Q_BLOCK
        z = jnp.einsum('bthd,bshd->bhts', q[:, q0:end], k[:, :end]).astype(jnp.float32) * scale
        t_pos = q0 + jnp.arange(Q_BLOCK)[:, None]
        s_pos = jnp.arange(end)[None, :]
        strict = s_pos < t_pos
        log_1m = jnp.where(strict, jax.nn.log_sigmoid(-z), 0.0)
        later = lax.cumsum(log_1m, axis=3, reverse=True) - log_1m
        w = jnp.where(strict, jnp.exp(jax.nn.log_sigmoid(z) + later), 0.0)
        outs.append(jnp.einsum('bhts,bshd->bthd', w.astype(v.dtype), v[:, :end]))
    return jnp.concatenate(outs, axis=1)


def _mla_attention(q_nope, q_pe, k_nope, k_pe, v):
    s = q_nope.shape[1]
    scale = 1.0 / math.sqrt(MLA_NOPE + MLA_ROPE)
    outs = []
    for q0 in range(0, s, Q_BLOCK):
        end = q0 + Q_BLOCK
        z = (jnp.einsum('bthd,bshd->bhts', q_nope[:, q0:end], k_nope[:, :end])
             + jnp.einsum('bthd,bsd->bhts', q_pe[:, q0:end], k_pe[:, :end])).astype(jnp.float32) * scale
        t_pos = q0 + jnp.arange(Q_BLOCK)[:, None]
        s_pos = jnp.arange(end)[None, :]
        allowed = (s_pos // CHUNK) <= (t_pos // CHUNK)
        prob = jax.nn.softmax(jnp.where(allowed, z, -jnp.inf), axis=-1)
        outs.append(jnp.einsum('bhts,bshd->bthd', prob.astype(v.dtype), v[:, :end]))
    return jnp.concatenate(outs, axis=1)


def _forgetting_attention(q, k, v, log_f):
    s, d = q.shape[1], q.shape[3]
    scale = 1.0 / math.sqrt(d)
    fh = jnp.cumsum(log_f, axis=1).transpose(0, 2, 1)
    outs = []
    for q0 in range(0, s, Q_BLOCK):
        end = q0 + Q_BLOCK
        z = jnp.einsum('bthd,bshd->bhts', q[:, q0:end], k[:, :end]).astype(jnp.float32) * scale
        z = z + fh[:, :, q0:end, None] - fh[:, :, None, :end]
        t_pos = q0 + jnp.arange(Q_BLOCK)[:, None]
        s_pos = jnp.arange(end)[None, :]
        prob = jax.nn.softmax(jnp.where(s_pos <= t_pos, z, -jnp.inf), axis=-1)
        outs.append(jnp.einsum('bhts,bshd->bthd', prob.astype(v.dtype), v[:, :end]))
    return jnp.concatenate(outs, axis=1)


def _even_mixer(x, w_in, conv_w, conv_b, dt_bias, a_log, d_skip, norm_g, w_out):
    f32 = jnp.float32
    bsz, s, _ = x.shape
    h = x @ w_in
    z, xbc, dt_raw, q, k, v = _split(h, [SSD_INNER, SSD_CONV_DIM, SSD_HEADS, SB_WIDTH, SB_WIDTH, SB_WIDTH])
    xbc = jax.nn.silu(_causal_depthwise_conv(xbc, conv_w, conv_b))
    xs, bs, cs = _split(xbc, [SSD_INNER, SSD_GROUPS * SSD_STATE, SSD_GROUPS * SSD_STATE])
    xs = xs.reshape(bsz, s, SSD_HEADS, SSD_HEAD_DIM)
    dt = jax.nn.softplus((dt_raw + dt_bias).astype(f32))
    a = -jnp.exp(a_log.astype(f32))
    y = _ssd_scan(xs, dt, a, bs.reshape(bsz, s, SSD_GROUPS, SSD_STATE), cs.reshape(bsz, s, SSD_GROUPS, SSD_STATE))
    y = (y + d_skip[:, None] * xs).reshape(bsz, s, SSD_INNER).astype(x.dtype)
    y = _rms_norm(y * jax.nn.silu(z), norm_g)
    sb = _stick_breaking_attention(q.reshape(bsz, s, SB_HEADS, SB_HEAD_DIM), k.reshape(bsz, s, SB_HEADS, SB_HEAD_DIM),
                                   v.reshape(bsz, s, SB_HEADS, SB_HEAD_DIM)).reshape(bsz, s, SB_WIDTH)
    return jnp.concatenate([y, sb], axis=-1) @ w_out


def _odd_mixer(x, w_in, q_norm_g, w_q_up, kv_norm_g, w_kv_up, f_bias, w_out, cos, sin):
    bsz, s, _ = x.shape
    h = x @ w_in
    c_q, c_kv, k_pe, q, k, v, f_raw = _split(h, [MLA_Q_RANK, MLA_KV_RANK, MLA_ROPE, FOX_WIDTH, FOX_WIDTH, FOX_WIDTH, FOX_HEADS])
    q_m = (_rms_norm(c_q, q_norm_g) @ w_q_up).reshape(bsz, s, MLA_HEADS, MLA_NOPE + MLA_ROPE)
    q_nope = q_m[..., :MLA_NOPE]
    q_pe = _rope(q_m[..., MLA_NOPE:], cos[:, None, :], sin[:, None, :])
    kv = (_rms_norm(c_kv, kv_norm_g) @ w_kv_up).reshape(bsz, s, MLA_HEADS, MLA_NOPE + MLA_V)
    k_nope, v_m = kv[..., :MLA_NOPE], kv[..., MLA_NOPE:]
    k_pe = _rope(k_pe, cos, sin)
    mla = _mla_attention(q_nope, q_pe, k_nope, k_pe, v_m).reshape(bsz, s, MLA_HEADS * MLA_V)
    log_f = jax.nn.log_sigmoid((f_raw + f_bias).astype(jnp.float32))
    fox = _forgetting_attention(q.reshape(bsz, s, FOX_HEADS, FOX_HEAD_DIM), k.reshape(bsz, s, FOX_HEADS, FOX_HEAD_DIM),
                                v.reshape(bsz, s, FOX_HEADS, FOX_HEAD_DIM), log_f).reshape(bsz, s, FOX_WIDTH)
    return jnp.concatenate([mla, fox], axis=-1) @ w_out


def _hier_moe(x, w_group, b_group, w_expert, b_expert, w_gate, w_up, w_down):
    f32 = jnp.float32
    bsz, s, d = x.shape
    t = x.reshape(bsz * s, d)
    n_tok = t.shape[0]
    g_prob = jax.nn.softmax((t @ w_group + b_group).astype(f32), axis=-1)
    g_p, g_sel = lax.top_k(g_prob, 1)
    e_logits = (t @ w_expert + b_expert).astype(f32).reshape(n_tok, MOE_GROUPS, MOE_EXPERTS_PER_GROUP)
    idx = jnp.broadcast_to(g_sel[:, :, None], (n_tok, 1, MOE_EXPERTS_PER_GROUP))
    e_prob = jax.nn.softmax(jnp.take_along_axis(e_logits, idx, axis=1)[:, 0], axis=-1)
    top_p, top_i = lax.top_k(e_prob, MOE_TOP_K)
    weights = g_p * top_p / jnp.sum(top_p, axis=-1, keepdims=True)
    expert_id = g_sel * MOE_EXPERTS_PER_GROUP + top_i
    gate = jnp.sum(jax.nn.one_hot(expert_id, MOE_EXPERTS, dtype=f32) * weights[..., None], axis=1)
    y = jnp.zeros((n_tok, d), f32)
    for e in range(MOE_EXPERTS):
        hid = jax.nn.silu(t @ w_gate[e]) * (t @ w_up[e])
        y = y + gate[:, e:e + 1] * (hid @ w_down[e])
    return y.astype(x.dtype).reshape(bsz, s, d)


def setup_inputs(seed: int = 0) -> dict:
    key = jax.random.key(seed)
    ks = iter(jax.random.split(key, 40))
    nrm = lambda shape, scale: jax.random.normal(next(ks), shape, jnp.float32) * scale
    gain = lambda shape: 1.0 + nrm(shape, 0.02)
    dt0 = jnp.exp(jax.random.uniform(next(ks), (N_EVEN, SSD_HEADS), jnp.float32, math.log(1e-3), math.log(1e-1)))
    return {
        'x': nrm((BATCH, SEQ, D_MODEL), 1.0),
        'ev_w_in': nrm((N_EVEN, D_MODEL, EVEN_IN), D_MODEL ** -0.5),
        'ev_conv_w': nrm((N_EVEN, SSD_CONV, SSD_CONV_DIM), SSD_CONV ** -0.5),
        'ev_conv_b': nrm((N_EVEN, SSD_CONV_DIM), 0.02),
        'ev_dt_bias': dt0 + jnp.log(-jnp.expm1(-dt0)),
        'ev_a_log': jnp.log(jax.random.uniform(next(ks), (N_EVEN, SSD_HEADS), jnp.float32, 1.0, 16.0)),
        'ev_d_skip': gain((N_EVEN, SSD_HEADS)),
        'ev_norm_g': gain((N_EVEN, SSD_INNER)),
        'ev_w_out': nrm((N_EVEN, EVEN_OUT, D_MODEL), EVEN_OUT ** -0.5 * DEEPNORM_BETA),
        'od_w_in': nrm((N_ODD, D_MODEL, ODD_IN), D_MODEL ** -0.5),
        'od_q_norm_g': gain((N_ODD, MLA_Q_RANK)),
        'od_w_q_up': nrm((N_ODD, MLA_Q_RANK, MLA_HEADS * (MLA_NOPE + MLA_ROPE)), MLA_Q_RANK ** -0.5),
        'od_kv_norm_g': gain((N_ODD, MLA_KV_RANK)),
        'od_w_kv_up': nrm((N_ODD, MLA_KV_RANK, MLA_HEADS * (MLA_NOPE + MLA_V)), MLA_KV_RANK ** -0.5),
        'od_f_bias': nrm((N_ODD, FOX_HEADS), 0.1),
        'od_w_out': nrm((N_ODD, ODD_OUT, D_MODEL), ODD_OUT ** -0.5 * DEEPNORM_BETA),
        'ln1_g': gain((DEPTH, D_MODEL)),
        'ln1_b': nrm((DEPTH, D_MODEL), 0.02),
        'ln2_g': gain((DEPTH, D_MODEL)),
        'ln2_b': nrm((DEPTH, D_MODEL), 0.02),
        'moe_w_group': nrm((DEPTH, D_MODEL, MOE_GROUPS), D_MODEL ** -0.5),
        'moe_b_group': nrm((DEPTH, MOE_GROUPS), 0.01),
        'moe_w_expert': nrm((DEPTH, D_MODEL, MOE_EXPERTS), D_MODEL ** -0.5),
        'moe_b_expert': nrm((DEPTH, MOE_EXPERTS), 0.01),
        'moe_w_gate': nrm((DEPTH, MOE_EXPERTS, D_MODEL, MOE_FF), D_MODEL ** -0.5),
        'moe_w_up': nrm((DEPTH, MOE_EXPERTS, D_MODEL, MOE_FF), D_MODEL ** -0.5 * DEEPNORM_BETA),
        'moe_w_down': nrm((DEPTH, MOE_EXPERTS, MOE_FF, D_MODEL), MOE_FF ** -0.5 * DEEPNORM_BETA),
    }


def reference(x, ev_w_in, ev_conv_w, ev_conv_b, ev_dt_bias, ev_a_log, ev_d_skip, ev_norm_g, ev_w_out,
              od_w_in, od_q_norm_g, od_w_q_up, od_kv_norm_g, od_w_kv_up, od_f_bias, od_w_out,
              ln1_g, ln1_b, ln2_g, ln2_b, moe_w_group, moe_b_group, moe_w_expert, moe_b_expert,
              moe_w_gate, moe_w_up, moe_w_down):
    s = x.shape[1]
    inv_freq = ROPE_THETA ** (-(jnp.arange(0, MLA_ROPE, 2, dtype=jnp.float32) / MLA_ROPE))
    ang = jnp.arange(s, dtype=jnp.float32)[:, None] * inv_freq[None, :]
    cos, sin = jnp.cos(ang), jnp.sin(ang)
    for i in range(DEPTH):
        j = i // 2
        if i % 2 == 0:
            mix = _even_mixer(x, ev_w_in[j], ev_conv_w[j], ev_conv_b[j], ev_dt_bias[j], ev_a_log[j],
                              ev_d_skip[j], ev_norm_g[j], ev_w_out[j])
        else:
            mix = _odd_mixer(x, od_w_in[j], od_q_norm_g[j], od_w_q_up[j], od_kv_norm_g[j], od_w_kv_up[j],
                             od_f_bias[j], od_w_out[j], cos, sin)
        x = _layer_norm(DEEPNORM_ALPHA * x + mix, ln1_g[i], ln1_b[i])
        ffn = _hier_moe(x, moe_w_group[i], moe_b_group[i], moe_w_expert[i], moe_b_expert[i],
                        moe_w_gate[i], moe_w_up[i], moe_w_down[i])
        x = _layer_norm(DEEPNORM_ALPHA * x + ffn, ln2_g[i], ln2_b[i])
    return x
```

```python
import math
from contextlib import ExitStack
import numpy as np
import ml_dtypes
import concourse.bass as bass
import concourse.mybir as mybir
from concourse.bass_utils import run_bass_kernel_spmd

F32 = mybir.dt.float32
BF16 = mybir.dt.bfloat16
AF = mybir.ActivationFunctionType
ALU = mybir.AluOpType

D = 1024
NCORES = 8
ALPHA = (2.0 * 2) ** 0.25
LN_EPS = 1e-5
RMS_EPS = 1e-6
EVEN_IN = 4112
ODD_IN = 1960

ENGS = ("pe", "act", "dve", "pool", "sp")


class Buf:
    _n = 0

    def __init__(self, name, t):
        self.name = name
        self.t = t
        self.w = None
        self.r = {}
        self.excl = False
        self.sem = None
        self.semcnt = 0
        Buf._n += 1
        self.id = Buf._n

    def __getitem__(self, k):
        return self.t[k]


class Sched:
    def __init__(self, nc, es):
        self.nc = nc
        self.es = es
        self.ops = {e: [] for e in ENGS}
        self.cnt = {e: 0 for e in ENGS}
        self.known = {e: {} for e in ENGS}
        self.snap = {e: [None] for e in ENGS}
        self.sems = {e: es.enter_context(nc.semaphore("c_" + e)) for e in ENGS}
        self.nsem = len(ENGS)

    def _dma_sem(self, b):
        if b.sem is None:
            b.sem = self.es.enter_context(self.nc.semaphore("d_%d" % b.id))
            self.nsem += 1
        return b.sem

    def _waits(self, eng, reads, writes):
        waits = {}

        def need(ev, same_ok):
            if ev is None:
                return
            k, v = ev
            if k == eng and (same_ok or eng in ("pe", "sp")):
                return
            if v > waits.get(k, 0):
                waits[k] = v
        for b in reads:
            need(b.w, False)
            if b.excl:
                for k, v in b.r.items():
                    need((k, v), True)
        for b in writes:
            need(b.w, False)
            for k, v in b.r.items():
                need((k, v), True)
        kn = self.known[eng]
        out = []
        for k, v in waits.items():
            if kn.get(k, 0) >= v:
                continue
            out.append((k, v))
            kn[k] = v
            if isinstance(k, str):
                sn = self.snap[k][v]
                if sn is not None:
                    for k2, v2 in sn.items():
                        if k2 != eng and kn.get(k2, 0) < v2:
                            kn[k2] = v2
        return out

    def op(self, eng, emit, reads=(), writes=()):
        waits = self._waits(eng, reads, writes)
        self.cnt[eng] += 1
        idx = self.cnt[eng]
        ev = (eng, idx)
        for b in reads:
            b.r[eng] = idx
        for b in writes:
            b.w = ev
            b.r = {}
        self.snap[eng].append({k: v for k, v in self.known[eng].items() if isinstance(k, str)})
        self.ops[eng].append((waits, emit, None))

    def dma(self, q, out_ap, in_ap, dst, src, extra_reads=(), **kw):
        reads = [src] + list(extra_reads)
        waits = self._waits(q, reads, [dst])
        sem = self._dma_sem(dst)
        dst.semcnt += 16
        ev = (("dma", dst.id, sem), dst.semcnt)
        src.r[ev[0]] = ev[1]
        for b in extra_reads:
            b.r[ev[0]] = ev[1]
        dst.w = ev
        dst.r = {}

        def emit(e):
            return e.dma_start(out=out_ap, in_=in_ap, **kw)
        self.ops[q].append((waits, emit, sem))

    def finish_waits(self, eng, bufs):
        waits = self._waits(eng, bufs, [])
        self.ops[eng].append((waits, None, None))

    def emit_all(self):
        nc = self.nc
        handles = {"pe": "tensor", "act": "scalar", "dve": "vector", "pool": "gpsimd", "sp": "sync"}
        with nc.Block() as block:
            for e in ENGS:
                ops = self.ops[e]
                esem = self.sems[e]

                def body(eng, ops=ops, esem=esem):
                    for waits, emit, dsem in ops:
                        for k, v in waits:
                            s = self.sems[k] if isinstance(k, str) else k[2]
                            eng.wait_ge(s, v)
                        if emit is None:
                            continue
                        ins = emit(eng)
                        if dsem is not None:
                            ins.then_inc(dsem, 16)
                        else:
                            ins.then_inc(esem, 1)
                getattr(block, handles[e])(body)


def _consts(S):
    c = {}
    p = np.arange(128)
    c["ident"] = np.eye(128, dtype=np.float32)
    c["identb"] = np.eye(128, dtype=np.float32).astype(ml_dtypes.bfloat16)
    c["onesb"] = np.ones((128, 128), np.float32).astype(ml_dtypes.bfloat16)
    c["uincl"] = (p[:, None] >= p[None, :]).astype(np.float32).astype(ml_dtypes.bfloat16)
    s = p[None, :, None]
    t = np.arange(512)[None, None, :]
    m = np.arange(4)[:, None, None]
    c["mask_sb"] = ((s + 128 * m) < t).astype(np.float32).astype(ml_dtypes.bfloat16)
    c["mask_mla"] = (((s + 128 * m) // 64) <= (t // 64)).astype(np.float32).astype(ml_dtypes.bfloat16)
    c["negm_fox"] = np.where((s + 128 * m) <= t, 0.0, -30000.0).astype(np.float32).astype(ml_dtypes.bfloat16)
    c2 = p // 64
    l = p % 64
    c["tri2"] = ((c2[:, None] == c2[None, :]) & (l[:, None] <= l[None, :])).astype(np.float32)
    c["bd"] = (c2[:, None] == c2[None, :]).astype(np.float32)
    c["lastsel"] = (p[:, None] == (64 * c2[None, :] + 63)).astype(np.float32)
    sl = np.zeros((2, 128, 128), np.float32)
    sl[0, 63, :] = 1.0
    sl[1, 127, :] = 1.0
    c["sellast"] = sl
    c["i64x2"] = (l[:, None] == np.arange(64)[None, :]).astype(np.float32)
    c["trimask"] = (np.arange(64)[None, :] >= l[:, None]).astype(np.float32)
    c["sel8"] = np.repeat(np.eye(8, dtype=np.float32), 128, axis=1)
    ng = np.where(np.arange(64)[None, :] < l[:, None], -30000.0, 0.0).astype(np.float32)
    c["negm_ssd"] = np.tile(ng[:, None, :], (1, 16, 1)).reshape(128, 1024).astype(ml_dtypes.bfloat16)
    inv = 10000.0 ** (-(np.arange(0, 32, 2, dtype=np.float32) / 32.0))
    ang = np.arange(S, dtype=np.float32)[None, :] * inv[:, None]
    cos = np.cos(ang).astype(np.float32)
    sin = np.sin(ang).astype(np.float32)
    cosT = np.concatenate([cos, cos], 0)
    sinT = np.concatenate([-sin, sin], 0)
    c["ropec"] = np.concatenate([cosT, cosT, cosT, cosT], 0).astype(np.float32)
    c["ropes"] = np.concatenate([sinT, sinT, sinT, sinT], 0).astype(np.float32)
    return c


class K:
    def __init__(self, S, NSEQ, layers=(0, 1), parts=("mix", "moe"), taps=()):
        self.S, self.NSEQ = S, NSEQ
        self.NT = S // 128
        self.GW = 512
        self.NG = S // 512
        self.layers, self.parts, self.taps = layers, parts, taps
        self.nc = bass.Bass("TRN2", target_bir_lowering=False)
        self.es = ExitStack()
        self.sc = Sched(self.nc, self.es)
        self.psi = 0
        self.evi = 0
        self.dbg = {}
        self.tap_out = {}

    def dram_in(self, name, shape, dt=F32):
        t = self.nc.dram_tensor(name, list(shape), dt, kind="ExternalInput")
        return Buf(name, t)

    def dram_out(self, name, shape, dt=F32):
        t = self.nc.dram_tensor(name, list(shape), dt, kind="ExternalOutput")
        return Buf(name, t)

    def sb(self, name, shape, dt=F32):
        t = self.es.enter_context(self.nc.sbuf_tensor("s_" + name, list(shape), dt))
        return Buf(name, t)

    def PS(self):
        b = self.ps[self.psi % len(self.ps)]
        self.psi += 1
        return b

    def mm(self, out, lhsT, rhs, start, stop, reads, writes):
        self.sc.op("pe", lambda e: e.matmul(out, lhsT, rhs, start=start, stop=stop), reads, writes)

    def tr(self, out, in_, ident, reads, writes):
        self.sc.op("pe", lambda e: e.transpose(out, in_, ident), reads, writes)

    def act(self, out, in_, func, reads, writes, bias=None, scale=1.0):
        kw = {}
        if bias is not None:
            kw["bias"] = bias
        self.sc.op("act", lambda e: e.activation(out=out, in_=in_, func=func, scale=scale, **kw), reads, writes)

    def tt(self, eng, out, in0, in1, op, reads, writes):
        self.sc.op(eng, lambda e: e.tensor_tensor(out=out, in0=in0, in1=in1, op=op), reads, writes)

    def ts(self, eng, out, in0, s1, s2, op0, op1, reads, writes):
        if op1 is None:
            self.sc.op(eng, lambda e: e.tensor_scalar(out=out, in0=in0, scalar1=s1, scalar2=None, op0=op0), reads, writes)
        else:
            self.sc.op(eng, lambda e: e.tensor_scalar(out=out, in0=in0, scalar1=s1, scalar2=s2, op0=op0, op1=op1), reads, writes)

    def stt(self, out, in0, scalar, in1, op0, op1, reads, writes):
        self.sc.op("dve", lambda e: e.scalar_tensor_tensor(out=out, in0=in0, scalar=scalar, in1=in1, op0=op0, op1=op1), reads, writes)

    def cp(self, eng, out, in_, reads, writes):
        if eng == "act":
            self.sc.op("act", lambda e: e.activation(out=out, in_=in_, func=AF.Copy), reads, writes)
        else:
            self.sc.op(eng, lambda e: e.tensor_copy(out=out, in_=in_), reads, writes)

    def evac(self, out, in_, reads, writes):
        self.evi += 1
        self.cp("act" if self.evi % 2 else "dve", out, in_, reads, writes)

    def load(self, q, dstbuf, out_ap, srcbuf, in_ap, **kw):
        self.sc.dma(q, out_ap, in_ap, dstbuf, srcbuf, **kw)

    def setup(self):
        S, NSEQ, NT = self.S, self.NSEQ, self.NT
        self.x = self.dram_in("x", [NSEQ, S, D])
        self.out = self.dram_out("out", [NSEQ, S, D])
        self.xscr = Buf("xscr", self.nc.dram_tensor("xscr", [S, D], F32, kind="Internal"))
        W = {}
        W["ev_w_in"] = self.dram_in("ev_w_in", [D, EVEN_IN])
        W["ev_w_out"] = self.dram_in("ev_w_out", [1536, D])
        W["od_w_in"] = self.dram_in("od_w_in", [D, ODD_IN + 128])
        W["od_w_q_up"] = self.dram_in("od_w_q_up", [256, 512 + 256 + 256])
        W["od_w_kv_up"] = self.dram_in("od_w_kv_up", [128, 1024])
        W["od_w_out"] = self.dram_in("od_w_out", [D, D])
        W["moe_w_gate"] = self.dram_in("moe_w_gate", [2, 16, D, 256])
        W["moe_w_up"] = self.dram_in("moe_w_up", [2, 16, D, 256])
        W["moe_w_down"] = self.dram_in("moe_w_down", [2, 16, 256, D])
        W["moe_wr"] = self.dram_in("moe_wr", [2, 128, 8, 20])
        W["rows"] = self.dram_in("rows", [NROWS, D])
        W["cols"] = self.dram_in("cols", [128, NCOLS])
        self.W = W
        cs = _consts(S)
        self.cdram = {}
        for k, v in cs.items():
            dt = BF16 if v.dtype == ml_dtypes.bfloat16 else F32
            self.cdram[k] = self.dram_in("c_" + k, v.shape, dt)
        self.slab = [self.sb("slab%d" % i, [128, 2048], BF16) for i in range(28)]
        self.XT = self.slab[0:8]
        self.wslot = [self.sb("wslot%d" % i, [128, 4096], BF16) for i in range(4)]
        self.wi = 0
        self.ps = [Buf("ps%d" % i, self.es.enter_context(self.nc.psum_tensor("ps%d" % i, [128, 512], F32)))
                   for i in range(8)]
        for b in self.ps:
            b.excl = True
        self.acc = self.ps[6:8]
        self.ps = self.ps[0:6]
        c = {}
        for k in ("ident", "identb", "onesb", "uincl", "tri2", "bd", "lastsel", "i64x2", "trimask"):
            v = cs[k]
            c[k] = self.sb("k_" + k, v.shape, BF16 if v.dtype == ml_dtypes.bfloat16 else F32)
            self.load("sp", c[k], c[k][:, :], self.cdram[k], self.cdram[k][:, :])
        c["sellast"] = self.sb("k_sellast", [128, 2, 128])
        for i in range(2):
            self.load("sp", c["sellast"], c["sellast"][:, i, :], self.cdram["sellast"], self.cdram["sellast"][i, :, :])
        self.c = c
        self.rows = self.sb("rows", [128, NROWS_SB, D])
        self.srow = self.sb("srow", [128, 512])
        self.maskbuf = self.sb("maskbuf", [128, 4, 512], BF16)
        self.P2 = [self.sb("p2_%d" % i, [128, 512]) for i in range(10)]
        self.cols = self.sb("cols", [128, NCOLS])
        self.load("sp", self.cols, self.cols[:, :], W["cols"], W["cols"][:, :])
        self.wr = self.sb("wr", [128, 2, 8, 20])
        for l in range(2):
            self.load("sp", self.wr, self.wr[:, l, :, :], W["moe_wr"], W["moe_wr"][l, :, :, :])
        self.gate = self.sb("gate", [128, NT, 16])
        self.xres = [self.sb("xres0", [128, D])] * 2
        self.tmpA = [self.sb("tmpA%d" % i, [128, D]) for i in range(2)]
        self.T4 = [Buf("t4_%d" % i, None) for i in range(4)]
        for i in range(4):
            self.T4[i].t = self.slab[24 + i].t
        self.T4 = self.slab[24:28]
        self.small = self.sb("small", [128, 64])
        self.small2 = self.sb("small2", [128, 8])
        self.small2b = self.sb("small2b", [128, 16])
        self.dtb = self.sb("dtb", [128, self.NT, 16])
        self.dab = self.sb("dab", [128, self.NT, 16])
        self.ssm = self.sb("ssm", [128, 80])
        self.cbm = self.sb("cbm", [128, 128])
        self.zscr = Buf("zscr", self.nc.dram_tensor("zscr", [S, D], F32, kind="Internal"))
        self.cspt = self.sb("cspt", [128, self.NT, 8])
        self.xi = 0

    def row(self, r):
        return self.rows[:, r, :]

    def load_srow(self, r, off, n):
        src = self.W["rows"][r:r + 1, 0:n].broadcast_to([128, n])
        self.load("sp", self.srow, self.srow[:, off:off + n], self.W["rows"], src)

    def load_mask(self, name):
        cd = self.cdram[name]
        for m_ in range(4):
            self.load("sp", self.maskbuf, self.maskbuf[:, m_, :], cd, cd[m_, :, :])

    def load_rows(self, idx_list):
        for slot, r in enumerate(idx_list):
            src = self.W["rows"][r:r + 1, :].broadcast_to([128, D])
            self.load("sp", self.rows, self.rows[:, slot, :], self.W["rows"], src)

    def transpose_tile(self, xb, xap, j, router_layer=None):
        c = self.c
        for half in range(2):
            ps = self.PS()
            for kk in range(4):
                k = half * 4 + kk
                self.tr(ps[:, kk * 128:(kk + 1) * 128], xap[:, k * 128:(k + 1) * 128], c["ident"][:, :],
                        [xb, c["ident"]], [ps])
            self.evi += 1
            for kk in range(4):
                k = half * 4 + kk
                self.cp("act" if self.evi % 2 else "dve", self.XT[k][:, j * 128:(j + 1) * 128],
                        ps[:, kk * 128:(kk + 1) * 128], [ps], [self.XT[k]])
            if router_layer is not None and not self.dbg.get('noxtf'):
                xtf = self.xtf[half]
                self.cp("dve", xtf[:, :], ps[:, :], [ps], [xtf])
        if router_layer is not None and not self.dbg.get('norouter'):
            self.router(j, router_layer)

    def router(self, j, l):
        sm = self.small
        ps = self.PS()
        for k in range(8):
            xtf = self.xtf[k // 4]
            self.mm(ps[:, 0:20], xtf[:, (k % 4) * 128:(k % 4 + 1) * 128], self.wr[:, l, k, :], k == 0, k == 7,
                    [xtf, self.wr], [ps])
        lg = sm[:, 0:20]
        rb = self.srow[:, 0:20]
        self.tt("dve", lg, ps[:, 0:20], rb, ALU.add, [ps, self.srow], [sm])
        R, Wr = [sm], [sm]
        self.sc.op("dve", lambda e: e.reduce_max(out=sm[:, 20:21], in_=sm[:, 0:4], axis=mybir.AxisListType.X), R, Wr)
        self.ts("dve", sm[:, 21:25], sm[:, 0:4], sm[:, 20:21], None, ALU.is_equal, None, R, Wr)
        self.ts("dve", sm[:, 25:29], sm[:, 0:4], sm[:, 20:21], None, ALU.subtract, None, R, Wr)
        self.act(sm[:, 25:29], sm[:, 25:29], AF.Exp, R, Wr)
        self.sc.op("dve", lambda e: e.reduce_sum(out=sm[:, 29:30], in_=sm[:, 25:29], axis=mybir.AxisListType.X), R, Wr)
        self.sc.op("dve", lambda e: e.reciprocal(out=sm[:, 30:31], in_=sm[:, 29:30]), R, Wr)
        el = sm[:, 4:20].rearrange("p (g e) -> p g e", e=4)
        ohg_b = sm[:, 21:25].unsqueeze(2).broadcast_to([128, 4, 4])
        prod = sm[:, 32:48].rearrange("p (g e) -> p g e", e=4)
        self.tt("dve", prod, el, ohg_b, ALU.mult, R, Wr)
        prod_t = sm[:, 32:48].rearrange("p (g e) -> p e g", e=4)
        self.sc.op("dve", lambda e: e.reduce_sum(out=sm[:, 48:52], in_=prod_t, axis=mybir.AxisListType.X), R, Wr)
        self.sc.op("dve", lambda e: e.reduce_max(out=sm[:, 52:53], in_=sm[:, 48:52], axis=mybir.AxisListType.X), R, Wr)
        self.ts("dve", sm[:, 53:57], sm[:, 48:52], sm[:, 52:53], None, ALU.is_equal, None, R, Wr)
        self.stt(sm[:, 57:61], sm[:, 53:57], -1e30, sm[:, 48:52], ALU.mult, ALU.add, R, Wr)
        self.sc.op("dve", lambda e: e.reduce_max(out=sm[:, 61:62], in_=sm[:, 57:61], axis=mybir.AxisListType.X), R, Wr)
        self.ts("dve", sm[:, 25:29], sm[:, 57:61], sm[:, 61:62], None, ALU.is_equal, None, R, Wr)
        self.ts("dve", sm[:, 62:63], sm[:, 61:62], sm[:, 52:53], None, ALU.subtract, None, R, Wr)
        self.act(sm[:, 62:63], sm[:, 62:63], AF.Exp, R, Wr)
        self.ts("dve", sm[:, 63:64], sm[:, 62:63], 1.0, None, ALU.add, None, R, Wr)
        self.sc.op("dve", lambda e: e.reciprocal(out=sm[:, 63:64], in_=sm[:, 63:64]), R, Wr)
        self.tt("dve", sm[:, 63:64], sm[:, 63:64], sm[:, 30:31], ALU.mult, R, Wr)
        self.tt("dve", sm[:, 62:63], sm[:, 62:63], sm[:, 63:64], ALU.mult, R, Wr)
        self.ts("dve", sm[:, 53:57], sm[:, 53:57], sm[:, 63:64], None, ALU.mult, None, R, Wr)
        self.stt(sm[:, 53:57], sm[:, 25:29], sm[:, 62:63], sm[:, 53:57], ALU.mult, ALU.add, R, Wr)
        g4_b = sm[:, 53:57].unsqueeze(1).broadcast_to([128, 4, 4])
        gout = self.gate[:, j, :].rearrange("p (g e) -> p g e", e=4)
        self.tt("dve", gout, ohg_b, g4_b, ALU.mult, R, [self.gate])

    def ln_tile(self, xb, xap, g_row, b_row):
        sm = self.lnsm
        R, Wr = [sm], [sm]
        for h in range(2):
            self.sc.op("dve", lambda e, h=h: e.bn_stats(out=sm[:, 6 * h:6 * h + 6], in_=xap[:, h * 512:(h + 1) * 512]),
                       [xb], Wr)
        self.sc.op("dve", lambda e: e.bn_aggr(out=sm[:, 12:14], in_=sm[:, 0:12]), R, Wr)
        self.ts("dve", sm[:, 14:15], sm[:, 13:14], LN_EPS, None, ALU.add, None, R, Wr)
        self.act(sm[:, 14:15], sm[:, 14:15], AF.Sqrt, R, Wr)
        self.sc.op("dve", lambda e: e.reciprocal(out=sm[:, 15:16], in_=sm[:, 14:15]), R, Wr)
        self.ts("dve", xap, xap, sm[:, 12:13], sm[:, 15:16], ALU.subtract, ALU.mult, [xb, sm], [xb])
        self.tt("pool", xap, xap, g_row, ALU.mult, [xb, self.rows], [xb])
        self.tt("pool", xap, xap, b_row, ALU.add, [xb, self.rows], [xb])

    def wnext(self):
        b = self.wslot[self.wi % len(self.wslot)]
        self.wi += 1
        return b

    def wload_k(self, slot, col0, ncols, wbuf, wap2d, nk=8):
        raise NotImplementedError

    def moe(self, l, src, dst, last):
        S, NT, NG = self.S, self.NT, self.NG
        W = self.W
        XA = [self.slab[8 + j] for j in range(NT)]
        xa = [b.t[:, :].bitcast(F32) for b in XA]
        self.load_rows([2 + 4 * l, 3 + 4 * l])
        for j in range(NT):
            sb_, sap = src(j)
            self.load("sp", XA[j], xa[j], sb_, sap)
            self.ts("pool", xa[j], xa[j], ALPHA, None, ALU.mult, None, [XA[j]], [XA[j]])
        hid = self.hid
        hidv = [h.t[:, :].bitcast(BF16)[:, 0:512] for h in hid]
        for e in range(self.dbg.get('nexp', 16)):
            sa = self.wnext()
            sv = sa.t[:, :].rearrange("p (k c) -> p k c", c=512)
            self.load("pool", sa, sv[:, :, 0:256], W["moe_w_gate"],
                      W["moe_w_gate"][l, e, :, :].rearrange("(k p) f -> p k f", p=128))
            self.load("pool", sa, sv[:, :, 256:512], W["moe_w_up"],
                      W["moe_w_up"][l, e, :, :].rearrange("(k p) f -> p k f", p=128))
            sd = self.wnext()
            dv = sd.t[:, 0:2048].rearrange("p (k c) -> p k c", c=1024)
            self.load("pool", sd, dv, W["moe_w_down"],
                      W["moe_w_down"][l, e, :, :].rearrange("(k p) f -> p k f", p=128))
            for g in range(NG):
                tsl = slice(g * 512, (g + 1) * 512)
                for f in range(2):
                    gps = self.PS()
                    for k in range(8):
                        self.mm(gps[:, :], sv[:, k, f * 128:(f + 1) * 128], self.XT[k][:, tsl], k == 0, k == 7,
                                [sa, self.XT[k]], [gps])
                    ups = self.PS()
                    for k in range(8):
                        self.mm(ups[:, :], sv[:, k, 256 + f * 128:256 + (f + 1) * 128], self.XT[k][:, tsl], k == 0, k == 7,
                                [sa, self.XT[k]], [ups])
                    sg = self.sg[f]
                    self.act(sg[:, :], gps[:, :], AF.Silu, [gps], [sg])
                    self.tt("dve", hidv[f], sg[:, :], ups[:, :], ALU.mult, [sg, ups], [hid[f]])
                for tt_ in range(4):
                    j = g * 4 + tt_
                    for half in range(2):
                        ops = self.PS()
                        for f in range(2):
                            self.mm(ops[:, :], hidv[f][:, tt_ * 128:(tt_ + 1) * 128], dv[:, f, half * 512:(half + 1) * 512],
                                    f == 0, f == 1, [hid[f], sd], [ops])
                        xs_ = xa[j][:, half * 512:(half + 1) * 512]
                        self.stt(xs_, ops[:, :], self.gate[:, j, e:e + 1], xs_, ALU.mult, ALU.add,
                                 [ops, self.gate, XA[j]], [XA[j]])
        for j in range(NT):
            if not self.dbg.get('noln'):
                self.ln_tile(XA[j], xa[j], self.row(0), self.row(1))
            db, dap = dst(j)
            self.load("sp", db, dap, XA[j], xa[j])
            if not last:
                self.transpose_tile(XA[j], xa[j], j)

    def prologue(self, seq, router_layer=None):
        for j in range(self.NT):
            xb = self.xres[j % 2]
            self.load("sp", xb, xb[:, :], self.x, self.x[seq, j * 128:(j + 1) * 128, :])
            self.transpose_tile(xb, xb[:, :], j, router_layer)

    def alloc_misc(self):
        self.xtf = self.P2[0:2]
        self.lnsm = self.sb("lnsm", [128, 16])
        self.sg = self.P2[2:4]
        self.hid = self.P2[4:6]

    def run(self):
        self.setup()
        self.alloc_misc()
        NT = self.NT
        for seq in range(self.NSEQ):
            def src_x(j, seq=seq):
                return self.x, self.x[seq, j * 128:(j + 1) * 128, :]

            def src_scr(j):
                return self.xscr, self.xscr[j * 128:(j + 1) * 128, :]

            def dst_out(j, seq=seq):
                return self.out, self.out[seq, j * 128:(j + 1) * 128, :]
            cur = src_x
            first = True
            subl = [(l, p) for l in self.layers for p in self.parts]
            for i, (l, p) in enumerate(subl):
                last = i == len(subl) - 1
                dst = dst_out if last else src_scr
                if p == "moe":
                    if first:
                        self.load_srow(9 + l, 0, 20)
                        self.prologue(seq, router_layer=l)
                    self.moe(l, cur, dst, last)
                else:
                    if first:
                        self.prologue(seq)
                    if l == 0:
                        self.even_mixer(cur, dst)
                    else:
                        self.odd_mixer(cur, dst)
                cur = src_scr
                first = False
        self.sc.finish_waits("sp", [self.out])
        self.sc.emit_all()
        return self.nc


NROWS = 16
NROWS_SB = 3
NCOLS = 64


def _host_inputs(S, inputs):
    f = lambda a: np.ascontiguousarray(np.asarray(a, dtype=np.float32))
    m = {}
    m["ev_w_in"] = f(inputs["ev_w_in"][0])
    m["ev_w_out"] = f(inputs["ev_w_out"][0])
    wi = f(inputs["od_w_in"][0])
    kpe = wi[:, 384:416]
    kpe_sw = np.concatenate([kpe[:, 16:32], kpe[:, 0:16]], 1)
    m["od_w_in"] = np.ascontiguousarray(np.concatenate([wi, kpe, kpe, kpe_sw, kpe_sw], 1))
    wq = f(inputs["od_w_q_up"][0]).reshape(256, 8, 96)
    nope = wq[:, :, :64].reshape(256, 512)
    pe = wq[:, :, 64:]
    pe_sw = np.concatenate([pe[:, :, 16:], pe[:, :, :16]], 2)
    m["od_w_q_up"] = np.ascontiguousarray(np.concatenate([nope, pe.reshape(256, 256), pe_sw.reshape(256, 256)], 1))
    wkv = f(inputs["od_w_kv_up"][0]).reshape(128, 8, 128)
    m["od_w_kv_up"] = np.ascontiguousarray(np.concatenate([wkv[:, :, :64].reshape(128, 512), wkv[:, :, 64:].reshape(128, 512)], 1))
    m["od_w_out"] = f(inputs["od_w_out"][0])
    m["moe_w_gate"] = f(inputs["moe_w_gate"])
    m["moe_w_up"] = f(inputs["moe_w_up"])
    m["moe_w_down"] = f(inputs["moe_w_down"])
    wr = np.concatenate([f(inputs["moe_w_group"]), f(inputs["moe_w_expert"])], 2)
    m["moe_wr"] = np.ascontiguousarray(wr.reshape(2, 8, 128, 20).transpose(0, 2, 1, 3))
    rows = np.zeros((NROWS, D), np.float32)
    for l in range(2):
        rows[4 * l + 0] = inputs["ln1_g"][l]
        rows[4 * l + 1] = inputs["ln1_b"][l]
        rows[4 * l + 2] = inputs["ln2_g"][l]
        rows[4 * l + 3] = inputs["ln2_b"][l]
        rows[9 + l, 0:4] = inputs["moe_b_group"][l]
        rows[9 + l, 4:20] = inputs["moe_b_expert"][l]
    rows[8] = inputs["ev_norm_g"][0]
    rows[11, 0:16] = inputs["ev_dt_bias"][0]
    rows[11, 16:32] = inputs["ev_a_log"][0]
    rows[11, 32:48] = inputs["ev_d_skip"][0]
    rows[12, 0:256] = inputs["od_q_norm_g"][0]
    rows[12, 256:384] = inputs["od_kv_norm_g"][0]
    rows[13, 0:8] = inputs["od_f_bias"][0]
    m["rows"] = rows
    cols = np.zeros((128, NCOLS), np.float32)
    cw = f(inputs["ev_conv_w"][0])
    cols[:, 0:48] = cw.T.reshape(12, 128, 4).transpose(1, 0, 2).reshape(128, 48)
    cols[:, 48:60] = f(inputs["ev_conv_b"][0]).reshape(12, 128).T
    cols[0:8, 60] = f(inputs["od_f_bias"][0])
    m["cols"] = cols
    for k, v in _consts(S).items():
        m["c_" + k] = v
    return m


_CACHE = {}


def kernel(**inputs):
    x = np.ascontiguousarray(np.asarray(inputs["x"], dtype=np.float32))
    B, S, _ = x.shape
    nseq = B // NCORES
    key = (S, nseq)
    if key not in _CACHE:
        _CACHE[key] = K(S, nseq).run()
    nc = _CACHE[key]
    shared = _host_inputs(S, inputs)
    in_maps = []
    for c in range(NCORES):
        m = dict(shared)
        m["x"] = np.ascontiguousarray(x[c * nseq:(c + 1) * nseq])
        in_maps.append(m)
    res = run_bass_kernel_spmd(nc, in_maps, core_ids=list(range(NCORES)))
    return np.concatenate([np.asarray(r["out"]) for r in res.results], axis=0).astype(np.float32)


def _f32v(b):
    return b.t[:, :].bitcast(F32)


def _bfv(b):
    return b.t[:, :].bitcast(BF16)


def _wview(slot, c):
    return slot.t[:, :].rearrange("p (k c) -> p k c", c=c)


def _kp(ap2d):
    return ap2d.rearrange("(k p) c -> p k c", p=128)


def _rope_tables(self, g):
    rc, rs = self.P2[3], self.P2[4]
    cc, cs_ = self.cdram["ropec"], self.cdram["ropes"]
    self.load("sp", rc, rc[0:64, :], cc, cc[0:64, g * 512:(g + 1) * 512])
    self.load("sp", rs, rs[0:64, :], cs_, cs_[0:64, g * 512:(g + 1) * 512])
    return rc, rs


def _rope(self, A, B, rc, rs, dst, dst_ap):
    t1, t2 = self.P2[5], self.P2[6]
    self.tt("dve", t1[0:64, :], A[0:64, :], rc[0:64, :], ALU.mult, [A, rc], [t1])
    self.tt("dve", t2[0:64, :], B[0:64, :], rs[0:64, :], ALU.mult, [B, rs], [t2])
    self.tt("pool", dst_ap, t1[0:64, :], t2[0:64, :], ALU.add, [t1, t2], [dst])


def _attn_finish(self, OT, DEN, hh, dst, dst_ap):
    rec = self.P2[9]
    lo, hi = hh * 64, hh * 64 + 64
    self.sc.op("dve", lambda e: e.reciprocal(out=rec[lo:hi, :], in_=DEN[lo:hi, :]), [DEN], [rec])
    self.tt("dve", dst_ap, OT[lo:hi, :], rec[lo:hi, :], ALU.mult, [OT, rec], [dst])


def odd_mixer(self, src, dst):
    S, NT, NG = self.S, self.NT, self.NG
    W = self.W
    Win = W["od_w_in"]
    sl = self.slab
    P2 = self.P2
    XT = self.XT
    c = self.c
    sc_mla = 1.0 / math.sqrt(96.0)
    sc_fox = 1.0 / 8.0
    self.load_rows([4, 5])
    self.load_srow(10, 0, 20)
    self.load_srow(12, 96, 384)
    CQNT, CKVNT, KPE = sl[8:10], sl[10], sl[11]
    OTF = sl[12:16]
    sm = self.small
    wl = self.wnext()
    wlv = _wview(wl, 512)
    self.load("pool", wl, wlv[:, :, 0:384], Win, _kp(Win[:, 0:384]))
    self.load("pool", wl, wlv[:, :, 384:512], Win, _kp(Win[:, 1960:2088]))
    lat, sq, latn_b = P2[0], P2[1], P2[2]
    latn = _bfv(latn_b)
    for j in range(NT):
        tsl = slice(j * 128, (j + 1) * 128)
        ps = self.PS()
        for k in range(8):
            self.mm(ps[:, 0:384], XT[k][:, tsl], wlv[:, k, 0:384], k == 0, k == 7, [XT[k], wl], [ps])
        self.cp("act", lat[:, 0:384], ps[:, 0:384], [ps], [lat])
        self.tt("pool", sq[:, 0:384], lat[:, 0:384], lat[:, 0:384], ALU.mult, [lat], [sq])
        self.sc.op("dve", lambda e: e.reduce_sum(out=sm[:, 0:1], in_=sq[:, 0:256], axis=mybir.AxisListType.X), [sq], [sm])
        self.sc.op("dve", lambda e: e.reduce_sum(out=sm[:, 1:2], in_=sq[:, 256:384], axis=mybir.AxisListType.X), [sq], [sm])
        self.ts("dve", sm[:, 0:1], sm[:, 0:1], 1.0 / 256, RMS_EPS, ALU.mult, ALU.add, [sm], [sm])
        self.ts("dve", sm[:, 1:2], sm[:, 1:2], 1.0 / 128, RMS_EPS, ALU.mult, ALU.add, [sm], [sm])
        self.act(sm[:, 0:2], sm[:, 0:2], AF.Sqrt, [sm], [sm])
        self.sc.op("dve", lambda e: e.reciprocal(out=sm[:, 2:4], in_=sm[:, 0:2]), [sm], [sm])
        self.stt(latn[:, 0:256], lat[:, 0:256], sm[:, 2:3], self.srow[:, 96:352], ALU.mult, ALU.mult,
                 [lat, sm, self.srow], [latn_b])
        self.stt(latn[:, 256:384], lat[:, 256:384], sm[:, 3:4], self.srow[:, 352:480], ALU.mult, ALU.mult,
                 [lat, sm, self.srow], [latn_b])
        pb = self.PS()
        pbv = _bfv(pb)
        for i in range(3):
            self.tr(pbv[:, i * 128:(i + 1) * 128], latn[:, i * 128:(i + 1) * 128], c["identb"][:, :], [latn_b, c["identb"]], [pb])
        for i, dstb in enumerate((CQNT[0], CQNT[1], CKVNT)):
            self.cp("dve", dstb[:, tsl], pbv[:, i * 128:(i + 1) * 128], [pb], [dstb])
    for g in range(NG):
        gsl = slice(g * 512, (g + 1) * 512)
        rc, rs = _rope_tables(self, g)
        A = self.PS()
        for k in range(8):
            self.mm(A[0:64, :], wlv[:, k, 384:448], XT[k][:, gsl], k == 0, k == 7, [wl, XT[k]], [A])
        B = self.PS()
        for k in range(8):
            self.mm(B[0:64, :], wlv[:, k, 448:512], XT[k][:, gsl], k == 0, k == 7, [wl, XT[k]], [B])
        _rope(self, A, B, rc, rs, KPE, KPE[0:64, gsl])
    wf = self.wnext()
    wfv = wf.t[:, 0:64].rearrange("p (k c) -> p k c", c=8)
    self.load("pool", wf, wfv, Win, _kp(Win[:, 1952:1960]))
    ones = P2[7]
    self.sc.op("pool", lambda e: e.memset(ones[:, :], 1.0), [], [ones])
    negfb = self.small2
    self.ts("pool", negfb[0:8, 0:1], self.cols[0:8, 60:61], -1.0, None, ALU.mult, None, [self.cols], [negfb])
    CSPb = [self.T4[0], self.T4[1]]
    FHQb = [self.T4[2], self.T4[3]]

    def cspg(g):
        return CSPb[g // 2], _f32v(CSPb[g // 2])[:, (g % 2) * 512:(g % 2 + 1) * 512]

    def fhqg(g):
        return FHQb[g // 2], _f32v(FHQb[g // 2])[:, (g % 2) * 512:(g % 2 + 1) * 512]
    CSPT = self.cspt
    for g in range(NG):
        gsl = slice(g * 512, (g + 1) * 512)
        ps = self.PS()
        for k in range(8):
            self.mm(ps[0:8, :], wfv[:, k, :], XT[k][:, gsl], k == 0, k == 7, [wf, XT[k]], [ps])
        e_, sp_ = P2[5], P2[6]
        self.act(e_[0:8, :], ps[0:8, :], AF.Exp, [ps, negfb], [e_], bias=negfb[0:8, 0:1], scale=-1.0)
        self.act(sp_[0:8, :], e_[0:8, :], AF.Ln, [e_], [sp_], bias=1.0)
        cb, cap = cspg(g)
        if g == 0:
            init, rds = 0.0, [ones, sp_]
        else:
            pb_, pap = cspg(g - 1)
            init, rds = pap[0:8, 511:512], [ones, sp_, pb_]
        self.sc.op("dve", lambda e, cap=cap, init=init: e.tensor_tensor_scan(
            out=cap[0:8, :], data0=ones[0:8, :], data1=sp_[0:8, :], initial=init, op0=ALU.mult, op1=ALU.add), rds, [cb])
        for tt_ in range(4):
            j = g * 4 + tt_
            pt = self.PS()
            self.tr(pt[:, 0:8], cap[0:8, tt_ * 128:(tt_ + 1) * 128], c["ident"][0:8, 0:8], [cb, c["ident"]], [pt])
            self.cp("dve", CSPT[:, j, :], pt[:, 0:8], [pt], [CSPT])
    self.load_mask("negm_fox")
    OT, DEN = self.acc
    for hp in range(4):
        FQ, FK, FV = sl[16 + 3 * (hp % 2)], sl[17 + 3 * (hp % 2)], sl[18 + 3 * (hp % 2)]
        wq = self.wnext()
        wqv = _wview(wq, 512)
        for i, c0 in enumerate((416, 928, 1440)):
            self.load("pool", wq, wqv[:, :, i * 128:(i + 1) * 128], Win, _kp(Win[:, c0 + hp * 128:c0 + (hp + 1) * 128]))
        for g in range(NG):
            gsl = slice(g * 512, (g + 1) * 512)
            for i, dstb in enumerate((FQ, FK)):
                ps = self.PS()
                for k in range(8):
                    self.mm(ps[:, :], wqv[:, k, i * 128:(i + 1) * 128], XT[k][:, gsl], k == 0, k == 7, [wq, XT[k]], [ps])
                self.evac(dstb[:, gsl], ps[:, :], [ps], [dstb])
        for j in range(NT):
            tsl = slice(j * 128, (j + 1) * 128)
            ps = self.PS()
            for k in range(8):
                self.mm(ps[:, 0:128], XT[k][:, tsl], wqv[:, k, 256:384], k == 0, k == 7, [wq, XT[k]], [ps])
            self.evac(FV[:, tsl], ps[:, 0:128], [ps], [FV])
        for hh in range(2):
            h = 2 * hp + hh
            lo, hi = hh * 64, hh * 64 + 64
            for g in range(NG):
                cb, cap = cspg(g)
                m_ = P2[5]
                self.ts("dve", m_[0:8, :], cap[0:8, :], c["ident"][0:8, h:h + 1], None, ALU.mult, None, [cb, c["ident"]], [m_])
                ps = self.PS()
                self.mm(ps[:, :], ones[0:8, 0:128], m_[0:8, :], True, True, [ones, m_], [ps])
                fb, fap = fhqg(g)
                self.cp("act", fap, ps[:, :], [ps], [fb])
            for G in range(NG):
                gsl = slice(G * 512, (G + 1) * 512)
                fb, fap = fhqg(G)
                nkb = 4 * (G + 1)
                for idx, i in enumerate(range(nkb - 1, -1, -1)):
                    ksl = slice(i * 128, (i + 1) * 128)
                    z = self.PS()
                    self.mm(z[:, :], FK[lo:hi, ksl], FQ[lo:hi, gsl], True, True, [FK, FQ], [z])
                    A_ = P2[idx % 2]
                    self.stt(A_[:, :], z[:, :], sc_fox, fap, ALU.mult, ALU.subtract, [z, fb], [A_])
                    if i >= 4 * G:
                        self.tt("pool", A_[:, :], A_[:, :], self.maskbuf[:, i - 4 * G, :], ALU.add, [A_, self.maskbuf], [A_])
                    Wb = P2[2] if idx % 2 == 0 else P2[8]
                    Wt = _bfv(Wb)[:, 0:512]
                    self.act(Wt, A_[:, :], AF.Exp, [A_, CSPT], [Wb], bias=CSPT[:, i, h:h + 1])
                    self.mm(OT[lo:hi, :], FV[:, i * 128 + lo:i * 128 + hi], Wt, idx == 0, idx == nkb - 1, [FV, Wb], [OT])
                    self.mm(DEN[lo:hi, :], c["onesb"][:, 0:64], Wt, idx == 0, idx == nkb - 1, [c["onesb"], Wb], [DEN])
                _attn_finish(self, OT, DEN, hh, OTF[hp], OTF[hp][lo:hi, gsl])
    self.load_mask("mask_mla")
    wm = self.wnext()
    wq_ = wm.t[:, 0:2048].rearrange("p (k c) -> p k c", c=1024)
    wkv = wm.t[:, 2048:3072]
    self.load("pool", wm, wq_, W["od_w_q_up"], _kp(W["od_w_q_up"][:, :]))
    self.load("pool", wm, wkv, W["od_w_kv_up"], W["od_w_kv_up"][:, :])
    VM = XT[0:4]
    OTM = XT[4:8]
    for j in range(NT):
        ps = self.PS()
        self.mm(ps[:, :], CKVNT[:, j * 128:(j + 1) * 128], wkv[:, 512:1024], True, True, [CKVNT, wm], [ps])
        self.evac(VM[j // 4][:, (j % 4) * 512:(j % 4 + 1) * 512], ps[:, :], [ps], [VM[j // 4]])
    for hp in range(4):
        QN, KN, QP = sl[16 + 3 * (hp % 2)], sl[17 + 3 * (hp % 2)], sl[18 + 3 * (hp % 2)]
        for g in range(NG):
            gsl = slice(g * 512, (g + 1) * 512)
            ps = self.PS()
            for kk in range(2):
                self.mm(ps[:, :], wq_[:, kk, hp * 128:(hp + 1) * 128], CQNT[kk][:, gsl], kk == 0, kk == 1, [wm, CQNT[kk]], [ps])
            self.evac(QN[:, gsl], ps[:, :], [ps], [QN])
            ps = self.PS()
            self.mm(ps[:, :], wkv[:, hp * 128:(hp + 1) * 128], CKVNT[:, gsl], True, True, [wm, CKVNT], [ps])
            self.evac(KN[:, gsl], ps[:, :], [ps], [KN])
            rc, rs = _rope_tables(self, g)
            A = self.PS()
            for kk in range(2):
                self.mm(A[0:64, :], wq_[:, kk, 512 + hp * 64:512 + (hp + 1) * 64], CQNT[kk][:, gsl], kk == 0, kk == 1, [wm, CQNT[kk]], [A])
            B = self.PS()
            for kk in range(2):
                self.mm(B[0:64, :], wq_[:, kk, 768 + hp * 64:768 + (hp + 1) * 64], CQNT[kk][:, gsl], kk == 0, kk == 1, [wm, CQNT[kk]], [B])
            _rope(self, A, B, rc, rs, QP, QP[0:64, gsl])
        for hh in range(2):
            h = 2 * hp + hh
            lo, hi = hh * 64, hh * 64 + 64
            plo, phi = hh * 32, hh * 32 + 32
            for G in range(NG):
                gsl = slice(G * 512, (G + 1) * 512)
                nkb = 4 * (G + 1)
                for idx, i in enumerate(range(nkb - 1, -1, -1)):
                    ksl = slice(i * 128, (i + 1) * 128)
                    z = self.PS()
                    self.mm(z[:, :], KN[lo:hi, ksl], QN[lo:hi, gsl], True, False, [KN, QN], [z])
                    self.mm(z[:, :], KPE[plo:phi, ksl], QP[plo:phi, gsl], False, True, [KPE, QP], [z])
                    Wb = P2[2] if idx % 2 == 0 else P2[8]
                    Wt = _bfv(Wb)[:, 0:512]
                    if i >= 4 * G:
                        Wf = P2[idx % 2]
                        self.act(Wf[:, :], z[:, :], AF.Exp, [z], [Wf], scale=sc_mla)
                        self.tt("pool", Wt, Wf[:, :], self.maskbuf[:, i - 4 * G, :], ALU.mult, [Wf, self.maskbuf], [Wb])
                    else:
                        self.act(Wt, z[:, :], AF.Exp, [z], [Wb], scale=sc_mla)
                    vs = VM[i // 4][:, (i % 4) * 512 + h * 64:(i % 4) * 512 + (h + 1) * 64]
                    self.mm(OT[lo:hi, :], vs, Wt, idx == 0, idx == nkb - 1, [VM[i // 4], Wb], [OT])
                    self.mm(DEN[lo:hi, :], c["onesb"][:, 0:64], Wt, idx == 0, idx == nkb - 1, [c["onesb"], Wb], [DEN])
                _attn_finish(self, OT, DEN, hh, OTM[hp], OTM[hp][lo:hi, gsl])
    wo = [self.wnext(), self.wnext()]
    wov = [_wview(w_, 1024) for w_ in wo]
    Wo = W["od_w_out"]
    for i in range(2):
        self.load("pool", wo[i], wov[i], Wo, _kp(Wo[i * 512:(i + 1) * 512, :]))
    cat = list(OTM) + list(OTF)
    for j in range(NT):
        tsl = slice(j * 128, (j + 1) * 128)
        xr = self.xres[j % 2]
        sb_, sap = src(j)
        self.load("sp", xr, xr[:, :], sb_, sap)
        y = self.tmpA[j % 2]
        for half in range(2):
            hs = slice(half * 512, (half + 1) * 512)
            ps = self.PS()
            for kc in range(8):
                self.mm(ps[:, :], cat[kc][:, tsl], wov[kc // 4][:, kc % 4, hs], kc == 0, kc == 7, [cat[kc], wo[kc // 4]], [ps])
            self.stt(y[:, hs], xr[:, hs], ALPHA, ps[:, :], ALU.mult, ALU.add, [xr, ps], [y])
        self.ln_tile(y, y[:, :], self.row(0), self.row(1))
        db, dap = dst(j)
        self.load("sp", db, dap, y, y[:, :])
        self.transpose_tile(y, y[:, :], j, router_layer=1)


K.odd_mixer = odd_mixer


def _v3(ap, inner):
    return ap.rearrange("p (a b) -> p a b", b=inner)


def _bc(ap2, n):
    return ap2.unsqueeze(2).broadcast_to([ap2.shape[0], ap2.shape[1], n])


def even_mixer(self, src, dst):
    S, NT, NG = self.S, self.NT, self.NG
    W = self.W
    Win = W["ev_w_in"]
    sl = self.slab
    P2 = self.P2
    XT = self.XT
    c = self.c
    sm = self.small
    X = mybir.AxisListType.X
    self.load_rows([0, 1, 8])
    self.load_srow(9, 0, 20)
    self.load_srow(11, 32, 48)
    OTS = sl[12:16]
    self.load_mask("mask_sb")
    OTa = self.acc
    Rf, Rbb = P2[4], P2[5]
    Rb = _bfv(Rbb)[:, 0:512]
    for hp in range(4):
        QT, KT, FV = sl[16 + 3 * (hp % 2)], sl[17 + 3 * (hp % 2)], sl[18 + 3 * (hp % 2)]
        wq = self.wnext()
        wqv = _wview(wq, 512)
        for i, c0 in enumerate((2576, 3088, 3600)):
            self.load("pool", wq, wqv[:, :, i * 128:(i + 1) * 128], Win, _kp(Win[:, c0 + hp * 128:c0 + (hp + 1) * 128]))
        for g in range(NG):
            gsl = slice(g * 512, (g + 1) * 512)
            for i, dstb in enumerate((QT, KT)):
                ps = self.PS()
                for k in range(8):
                    self.mm(ps[:, :], wqv[:, k, i * 128:(i + 1) * 128], XT[k][:, gsl], k == 0, k == 7, [wq, XT[k]], [ps])
                self.evac(dstb[:, gsl], ps[:, :], [ps], [dstb])
        for j in range(NT):
            tsl = slice(j * 128, (j + 1) * 128)
            ps = self.PS()
            for k in range(8):
                self.mm(ps[:, 0:128], XT[k][:, tsl], wqv[:, k, 256:384], k == 0, k == 7, [wq, XT[k]], [ps])
            self.evac(FV[:, tsl], ps[:, 0:128], [ps], [FV])
        for hh in range(2):
            lo, hi = hh * 64, hh * 64 + 64
            for G in range(NG):
                gsl = slice(G * 512, (G + 1) * 512)
                OT = OTa[(2 * hh + G) % 2]
                nkb = 4 * (G + 1)
                for idx, i in enumerate(range(nkb - 1, -1, -1)):
                    ksl = slice(i * 128, (i + 1) * 128)
                    z = self.PS()
                    self.mm(z[:, :], KT[lo:hi, ksl], QT[lo:hi, gsl], True, True, [KT, QT], [z])
                    E = P2[idx % 2]
                    self.act(E[:, :], z[:, :], AF.Exp, [z], [E], scale=0.125)
                    if i >= 4 * G:
                        self.tt("pool", E[:, :], E[:, :], self.maskbuf[:, i - 4 * G, :], ALU.mult, [E, self.maskbuf], [E])
                    Lbb = P2[2 + idx % 2]
                    Lb = _bfv(Lbb)[:, 0:512]
                    self.act(Lb, E[:, :], AF.Ln, [E], [Lbb], bias=1.0)
                    CS = self.PS()
                    self.mm(CS[:, :], c["uincl"][:, :], Lb, True, idx == 0, [c["uincl"], Lbb], [CS])
                    if idx > 0:
                        self.mm(CS[:, :], c["onesb"][:, :], Rb, False, True, [c["onesb"], Rbb], [CS])
                    if idx < nkb - 1:
                        if idx == 0:
                            self.cp("pool", Rf[:, :], Lb, [Lbb], [Rf])
                        else:
                            self.tt("pool", Rf[:, :], Rf[:, :], Lb, ALU.add, [Rf, Lbb], [Rf])
                        self.cp("pool", Rb, Rf[:, :], [Rf], [Rbb])
                    Ecs = P2[6 + idx % 2]
                    self.act(Ecs[:, :], CS[:, :], AF.Exp, [CS], [Ecs], scale=-1.0)
                    Wb = P2[8 + idx % 2]
                    Wt = _bfv(Wb)[:, 0:512]
                    self.tt("dve", Wt, E[:, :], Ecs[:, :], ALU.mult, [E, Ecs], [Wb])
                    self.mm(OT[lo:hi, :], FV[:, i * 128 + lo:i * 128 + hi], Wt, idx == 0, idx == nkb - 1, [FV, Wb], [OT])
                self.cp("act", OTS[hp][lo:hi, gsl], OT[lo:hi, :], [OT], [OTS[hp]])
    ps_save = self.ps
    xtf_save = self.xtf
    self.xtf = [P2[0], P2[3]]
    allps = list(self.ps) + list(self.acc)
    self.ps = allps[0:4]
    YD, YO = allps[4:6], allps[6:8]
    XS_T = list(sl[8:12]) + list(sl[16:20])
    BT, CT = sl[20:22], sl[22:24]
    T4 = self.T4
    dests = XS_T + list(BT) + list(CT)
    for pnl in range(3):
        wp = self.wnext()
        wpv = _wview(wp, 512)
        self.load("pool", wp, wpv, Win, _kp(Win[:, 1024 + pnl * 512:1024 + (pnl + 1) * 512]))
        for q in range(4):
            cc = pnl * 4 + q
            for g in range(NG):
                gsl = slice(g * 512, (g + 1) * 512)
                ps = self.PS()
                for k in range(8):
                    self.mm(ps[:, :], wpv[:, k, q * 128:(q + 1) * 128], XT[k][:, gsl], k == 0, k == 7, [wp, XT[k]], [ps])
                RAWb = T4[g % 2]
                RAW = _f32v(RAWb)
                if g == 0:
                    self.sc.op("pool", lambda e, RAW=RAW: e.memset(RAW[:, 0:3], 0.0), [], [RAWb])
                else:
                    prev = _f32v(T4[(g - 1) % 2])
                    self.cp("pool", RAW[:, 0:3], prev[:, 512:515], [T4[(g - 1) % 2]], [RAWb])
                self.cp("act", RAW[:, 3:515], ps[:, :], [ps], [RAWb])
                ACCb = T4[2 + g % 2]
                ACC = _f32v(ACCb)[:, 0:512]
                self.ts("pool", ACC, RAW[:, 0:512], self.cols[:, cc * 4:cc * 4 + 1], None, ALU.mult, None, [RAWb, self.cols], [ACCb])
                for tap in range(1, 4):
                    self.stt(ACC, RAW[:, tap:tap + 512], self.cols[:, cc * 4 + tap:cc * 4 + tap + 1], ACC, ALU.mult, ALU.add,
                             [RAWb, self.cols, ACCb], [ACCb])
                self.act(dests[cc][:, gsl], ACC, AF.Silu, [ACCb, self.cols], [dests[cc]], bias=self.cols[:, 48 + cc:49 + cc])
    wd = self.wnext()
    wdv = wd.t[:, 0:128].rearrange("p (k c) -> p k c", c=16)
    self.load("pool", wd, wdv, Win, _kp(Win[:, 2560:2576]))
    DT, DA = self.dtb, self.dab
    AROW = self.small2b
    self.act(AROW[:, 0:16], self.srow[:, 48:64], AF.Exp, [self.srow], [AROW])
    self.ts("pool", AROW[:, 0:16], AROW[:, 0:16], -1.0, None, ALU.mult, None, [AROW], [AROW])
    for j in range(NT):
        tsl = slice(j * 128, (j + 1) * 128)
        ps = self.PS()
        for k in range(8):
            self.mm(ps[:, 0:16], XT[k][:, tsl], wdv[:, k, :], k == 0, k == 7, [XT[k], wd], [ps])
        self.tt("dve", sm[:, 0:16], ps[:, 0:16], self.srow[:, 32:48], ALU.add, [ps, self.srow], [sm])
        self.act(sm[:, 0:16], sm[:, 0:16], AF.Exp, [sm], [sm])
        self.act(DT[:, j, :], sm[:, 0:16], AF.Ln, [sm], [DT], bias=1.0)
        self.tt("dve", DA[:, j, :], DT[:, j, :], AROW[:, 0:16], ALU.mult, [DT, AROW], [DA])
    for half in range(2):
        wz = self.wnext()
        wzv = _wview(wz, 512)
        self.load("pool", wz, wzv, Win, _kp(Win[:, half * 512:(half + 1) * 512]))
        for j in range(NT):
            tsl = slice(j * 128, (j + 1) * 128)
            ps = self.PS()
            for k in range(8):
                self.mm(ps[:, :], XT[k][:, tsl], wzv[:, k, :], k == 0, k == 7, [XT[k], wz], [ps])
            szb = self.tmpA[j % 2]
            self.act(szb[:, 0:512], ps[:, :], AF.Silu, [ps], [szb])
            self.load("sp", self.zscr, self.zscr[j * 128:(j + 1) * 128, half * 512:(half + 1) * 512], szb, szb[:, 0:512])
    wo = [self.wnext(), self.wnext(), self.wnext()]
    wov = [_wview(w_, 1024) for w_ in wo]
    Wo = W["ev_w_out"]
    for i in range(3):
        self.load("pool", wo[i], wov[i], Wo, _kp(Wo[i * 512:(i + 1) * 512, :]))
    XSTOKb, Dgb, SEGb, GTBb = T4[0], T4[1], T4[2], T4[3]
    XSTOK, Dg, SEG = _f32v(XSTOKb), _f32v(Dgb), _f32v(SEGb)
    GTB = _bfv(GTBb)
    XSPb, XSPP0b, XSPP1b, YNb = P2[0], P2[1], P2[2], P2[3]
    XSP, YN = _bfv(XSPb), _bfv(YNb)
    XSPP = [_bfv(XSPP0b), _bfv(XSPP1b)]
    XSPPb = [XSPP0b, XSPP1b]
    BTOKb = P2[4]
    BTOK = _bfv(BTOKb)[:, 0:256]
    HT = [P2[5], P2[6]]
    HTbb = P2[7]
    HTb = [_bfv(HTbb)[:, 0:512], _bfv(HTbb)[:, 512:1024]]
    YNTb = P2[8]
    YNT = _bfv(YNTb)
    ssm = self.ssm
    CBM = self.cbm
    ACUM, EA, DTW = ssm[:, 0:16], ssm[:, 16:32], ssm[:, 32:48]
    DEC = [ssm[:, 48:64], ssm[:, 64:80]]
    self.sc.op("pool", lambda e: e.memset(GTB, 0.0), [], [GTBb])
    self.sc.op("pool", lambda e: e.memset(XSPP[0], 0.0), [], [XSPP0b])
    self.sc.op("pool", lambda e: e.memset(XSPP[1], 0.0), [], [XSPP1b])
    for g in range(2):
        self.sc.op("pool", lambda e, g=g: e.memset(HT[g][:, :], 0.0), [], [HT[g]])
    self.sc.op("pool", lambda e: e.memset(_bfv(HTbb), 0.0), [], [HTbb])
    for j in range(NT):
        tsl = slice(j * 128, (j + 1) * 128)
        pb = self.PS()
        pbv = _bfv(pb)
        for cc in range(8):
            self.tr(pbv[:, cc * 128:(cc + 1) * 128], XS_T[cc][:, tsl], c["identb"][:, :], [XS_T[cc], c["identb"]], [pb])
        self.cp("act", XSTOK, pbv, [pb], [XSTOKb])
        pb2 = self.PS()
        pb2v = _bfv(pb2)
        for g in range(2):
            self.tr(pb2v[:, g * 128:(g + 1) * 128], BT[g][:, tsl], c["identb"][:, :], [BT[g], c["identb"]], [pb2])
        self.cp("dve", BTOK, pb2v[:, 0:256], [pb2], [BTOKb])
        ps = self.PS()
        self.mm(ps[:, 0:16], c["tri2"][:, :], DA[:, j, :], True, True, [c["tri2"], DA], [ps])
        self.cp("dve", ACUM, ps[:, 0:16], [ps], [ssm])
        self.act(EA, ACUM, AF.Exp, [ssm], [ssm])
        ps = self.PS()
        self.mm(ps[:, 0:16], c["lastsel"][:, :], ACUM, True, True, [c["lastsel"], ssm], [ps])
        self.tt("dve", DTW, ps[:, 0:16], ACUM, ALU.subtract, [ps, ssm], [ssm])
        self.act(DTW, DTW, AF.Exp, [ssm], [ssm])
        self.tt("dve", DTW, DTW, DT[:, j, :], ALU.mult, [ssm, DT], [ssm])
        for c2 in range(2):
            ps = self.PS()
            self.mm(ps[:, 0:16], c["sellast"][:, c2, :], ACUM, True, True, [c["sellast"], ssm], [ps])
            self.act(DEC[c2], ps[:, 0:16], AF.Exp, [ps], [ssm])
        self.tt("dve", _v3(Dg, 64), _bc(ACUM, 64), c["i64x2"][:, :].unsqueeze(1).broadcast_to([128, 16, 64]), ALU.mult,
                [ssm, c["i64x2"]], [Dgb])
        for hb in range(2):
            p1 = self.PS()
            self.mm(p1[:, :], c["bd"][:, :], Dg[:, hb * 512:(hb + 1) * 512], True, True, [c["bd"], Dgb], [p1])
            self.tt("dve", _v3(SEG[:, hb * 512:(hb + 1) * 512], 64), _v3(p1[:, :], 64), _bc(ssm[:, hb * 8:hb * 8 + 8], 64),
                    ALU.subtract, [p1, ssm], [SEGb])
        self.ts("pool", SEG, SEG, 0.0, None, ALU.min, None, [SEGb], [SEGb])
        self.act(SEG, SEG, AF.Exp, [SEGb], [SEGb])
        ps = self.PS()
        for c2 in range(2):
            csl = slice(j * 128 + c2 * 64, j * 128 + c2 * 64 + 64)
            for g in range(2):
                self.sc.op("pe", lambda e, ps=ps, c2=c2, g=g, csl=csl: e.matmul(
                    ps[c2 * 64:c2 * 64 + 64, g * 64:(g + 1) * 64], BT[g][:, csl], CT[g][:, csl], start=True, stop=True,
                    skip_group_check=True), [BT[g], CT[g]], [ps])
        self.tt("dve", _v3(CBM[:, :], 64), _v3(ps[:, 0:128], 64), c["trimask"][:, :].unsqueeze(1).broadcast_to([128, 2, 64]),
                ALU.mult, [ps, c["trimask"]], [CBM])
        for c2 in range(2):
            lo, hi = c2 * 64, c2 * 64 + 64
            out_ap = GTB[lo:hi, :].rearrange("p (h x) -> p h x", x=128)[:, :, lo:hi].rearrange("p (g r) l -> p g r l", g=2)
            in0 = SEG[lo:hi, :].rearrange("p (g r l) -> p g r l", g=2, r=8)
            in1 = _v3(CBM[lo:hi, :], 64).unsqueeze(2).broadcast_to([64, 2, 8, 64])
            self.tt("dve", out_ap, in0, in1, ALU.mult, [SEGb, CBM], [GTBb])
        self.tt("pool", _v3(XSP, 64), _v3(XSTOK, 64), _bc(DT[:, j, :], 64), ALU.mult, [XSTOKb, DT], [XSPb])
        for c2 in range(2):
            lo, hi = c2 * 64, c2 * 64 + 64
            self.tt("pool", _v3(XSPP[c2][lo:hi, :], 64), _v3(XSTOK[lo:hi, :], 64), _bc(ssm[lo:hi, 32:48], 64), ALU.mult,
                    [XSTOKb, ssm], [XSPPb[c2]])
        for h in range(16):
            self.sc.op("pe", lambda e, h=h: e.matmul(
                YD[h // 8][:, (h % 8) * 64:(h % 8 + 1) * 64], GTB[:, h * 128:(h + 1) * 128], XSP[:, h * 64:(h + 1) * 64],
                start=True, stop=True, skip_group_check=True), [GTBb, XSPb], [YD[h // 8]])
        for c2 in range(2):
            lo, hi = c2 * 64, c2 * 64 + 64
            csl = slice(j * 128 + lo, j * 128 + hi)
            for g in range(2):
                self.sc.op("pe", lambda e, g=g, lo=lo, hi=hi, csl=csl: e.matmul(
                    YO[g][lo:hi, :], CT[g][:, csl], HTb[g], start=True, stop=True, skip_group_check=True),
                    [CT[g], HTbb], [YO[g]])
            for g in range(2):
                st = self.PS()
                self.mm(st[:, :], BTOK[:, g * 128:(g + 1) * 128], XSPP[c2][:, g * 512:(g + 1) * 512], True, True,
                        [BTOKb, XSPPb[c2]], [st])
                self.tt("dve", _v3(HT[g][:, :], 64), _v3(HT[g][:, :], 64), _bc(DEC[c2][:, g * 8:(g + 1) * 8], 64), ALU.mult,
                        [HT[g], ssm], [HT[g]])
                self.tt("dve", HT[g][:, :], HT[g][:, :], st[:, :], ALU.add, [HT[g], st], [HT[g]])
                self.cp("act", HTb[g], HT[g][:, :], [HT[g]], [HTbb])
        Yb = self.tmpA[1]
        SZb = self.tmpA[0]
        self.load("sp", SZb, SZb[:, :], self.zscr, self.zscr[tsl, :])
        for g in range(2):
            hs = slice(g * 512, (g + 1) * 512)
            self.tt("dve", _v3(Yb[:, hs], 64), _v3(YO[g][:, :], 64), _bc(ssm[:, 16 + g * 8:16 + g * 8 + 8], 64), ALU.mult,
                    [YO[g], ssm], [Yb])
            self.tt("dve", Yb[:, hs], Yb[:, hs], YD[g][:, :], ALU.add, [Yb, YD[g]], [Yb])
        self.tt("pool", _v3(Dg, 64), _v3(XSTOK, 64), _bc(self.srow[:, 64:80], 64), ALU.mult, [XSTOKb, self.srow], [Dgb])
        self.tt("pool", Yb[:, :], Yb[:, :], Dg, ALU.add, [Yb, Dgb], [Yb])
        self.tt("pool", Yb[:, :], Yb[:, :], SZb[:, :], ALU.mult, [Yb, SZb], [Yb])
        self.tt("pool", Dg, Yb[:, :], Yb[:, :], ALU.mult, [Yb], [Dgb])
        self.sc.op("dve", lambda e: e.reduce_sum(out=sm[:, 0:1], in_=Dg, axis=X), [Dgb], [sm])
        self.ts("dve", sm[:, 0:1], sm[:, 0:1], 1.0 / 1024, RMS_EPS, ALU.mult, ALU.add, [sm], [sm])
        self.act(sm[:, 0:1], sm[:, 0:1], AF.Sqrt, [sm], [sm])
        self.sc.op("dve", lambda e: e.reciprocal(out=sm[:, 1:2], in_=sm[:, 0:1]), [sm], [sm])
        self.stt(YN, Yb[:, :], sm[:, 1:2], self.rows[:, 2, :], ALU.mult, ALU.mult, [Yb, sm, self.rows], [YNb])
        pb = self.PS()
        pbv = _bfv(pb)
        for cc in range(8):
            self.tr(pbv[:, cc * 128:(cc + 1) * 128], YN[:, cc * 128:(cc + 1) * 128], c["identb"][:, :], [YNb, c["identb"]], [pb])
        self.cp("act", YNT, pbv, [pb], [YNTb])
        xr = self.xres[j % 2]
        sb_, sap = src(j)
        self.load("sp", xr, xr[:, :], sb_, sap)
        for half in range(2):
            hs = slice(half * 512, (half + 1) * 512)
            ps = self.PS()
            for kc in range(12):
                lhsT = YNT[:, kc * 128:(kc + 1) * 128] if kc < 8 else OTS[kc - 8][:, tsl]
                rb_ = YNTb if kc < 8 else OTS[kc - 8]
                self.mm(ps[:, :], lhsT, wov[kc // 4][:, kc % 4, hs], kc == 0, kc == 11, [rb_, wo[kc // 4]], [ps])
            self.stt(SZb[:, hs], xr[:, hs], ALPHA, ps[:, :], ALU.mult, ALU.add, [xr, ps], [SZb])
        self.ln_tile(SZb, SZb[:, :], self.row(0), self.row(1))
        db, dap = dst(j)
        self.load("sp", db, dap, SZb, SZb[:, :])
        self.transpose_tile(SZb, SZb[:, :], j, router_layer=0)
    self.ps = ps_save
    self.xtf = xtf_save


K.even_mixer = even_mixer
```

```python
import math
from contextlib import ExitStack
import numpy as np
import ml_dtypes
import concourse.bass as bass
import concourse.mybir as mybir
from concourse.bass_utils import run_bass_kernel_spmd

F32 = mybir.dt.float32
BF16 = mybir.dt.bfloat16
AF = mybir.ActivationFunctionType
ALU = mybir.AluOpType

D = 1024
NCORES = 8
ALPHA = (2.0 * 2) ** 0.25
LN_EPS = 1e-5
RMS_EPS = 1e-6
EVEN_IN = 4112
ODD_IN = 1960

ENGS = ("pe", "act", "dve", "pool", "sp")


class Buf:
    _n = 0

    def __init__(self, name, t):
        self.name = name
        self.t = t
        self.w = None
        self.r = {}
        self.excl = False
        self.sem = None
        self.semcnt = 0
        Buf._n += 1
        self.id = Buf._n

    def __getitem__(self, k):
        return self.t[k]


class Sched:
    def __init__(self, nc, es):
        self.nc = nc
        self.es = es
        self.ops = {e: [] for e in ENGS}
        self.cnt = {e: 0 for e in ENGS}
        self.known = {e: {} for e in ENGS}
        self.snap = {e: [None] for e in ENGS}
        self.sems = {e: es.enter_context(nc.semaphore("c_" + e)) for e in ENGS}
        self.nsem = len(ENGS)

    def _dma_sem(self, b):
        if b.sem is None:
            b.sem = self.es.enter_context(self.nc.semaphore("d_%d" % b.id))
            self.nsem += 1
        return b.sem

    def _waits(self, eng, reads, writes):
        waits = {}

        def need(ev, same_ok):
            if ev is None:
                return
            k, v = ev
            if k == eng and (same_ok or eng in ("pe", "sp")):
                return
            if v > waits.get(k, 0):
                waits[k] = v
        for b in reads:
            need(b.w, False)
            if b.excl:
                for k, v in b.r.items():
                    need((k, v), True)
        for b in writes:
            need(b.w, False)
            for k, v in b.r.items():
                need((k, v), True)
        kn = self.known[eng]
        out = []
        for k, v in waits.items():
            if kn.get(k, 0) >= v:
                continue
            out.append((k, v))
            kn[k] = v
            if isinstance(k, str):
                sn = self.snap[k][v]
                if sn is not None:
                    for k2, v2 in sn.items():
                        if k2 != eng and kn.get(k2, 0) < v2:
                            kn[k2] = v2
        return out

    def op(self, eng, emit, reads=(), writes=()):
        waits = self._waits(eng, reads, writes)
        self.cnt[eng] += 1
        idx = self.cnt[eng]
        ev = (eng, idx)
        for b in reads:
            b.r[eng] = idx
        for b in writes:
            b.w = ev
            b.r = {}
        self.snap[eng].append({k: v for k, v in self.known[eng].items() if isinstance(k, str)})
        self.ops[eng].append((waits, emit, None))

    def dma(self, q, out_ap, in_ap, dst, src, extra_reads=(), **kw):
        reads = [src] + list(extra_reads)
        waits = self._waits(q, reads, [dst])
        sem = self._dma_sem(dst)
        dst.semcnt += 16
        ev = (("dma", dst.id, sem), dst.semcnt)
        src.r[ev[0]] = ev[1]
        for b in extra_reads:
            b.r[ev[0]] = ev[1]
        dst.w = ev
        dst.r = {}

        def emit(e):
            return e.dma_start(out=out_ap, in_=in_ap, **kw)
        self.ops[q].append((waits, emit, sem))

    def finish_waits(self, eng, bufs):
        waits = self._waits(eng, bufs, [])
        self.ops[eng].append((waits, None, None))

    def emit_all(self):
        nc = self.nc
        handles = {"pe": "tensor", "act": "scalar", "dve": "vector", "pool": "gpsimd", "sp": "sync"}
        with nc.Block() as block:
            for e in ENGS:
                ops = self.ops[e]
                esem = self.sems[e]

                def body(eng, ops=ops, esem=esem):
                    for waits, emit, dsem in ops:
                        for k, v in waits:
                            s = self.sems[k] if isinstance(k, str) else k[2]
                            eng.wait_ge(s, v)
                        if emit is None:
                            continue
                        ins = emit(eng)
                        if dsem is not None:
                            ins.then_inc(dsem, 16)
                        else:
                            ins.then_inc(esem, 1)
                getattr(block, handles[e])(body)


def _consts(S):
    c = {}
    p = np.arange(128)
    c["ident"] = np.eye(128, dtype=np.float32)
    c["identb"] = np.eye(128, dtype=np.float32).astype(ml_dtypes.bfloat16)
    c["onesb"] = np.ones((128, 128), np.float32).astype(ml_dtypes.bfloat16)
    c["uincl"] = (p[:, None] >= p[None, :]).astype(np.float32).astype(ml_dtypes.bfloat16)
    s = p[None, :, None]
    t = np.arange(512)[None, None, :]
    m = np.arange(4)[:, None, None]
    c["mask_sb"] = ((s + 128 * m) < t).astype(np.float32).astype(ml_dtypes.bfloat16)
    c["mask_mla"] = (((s + 128 * m) // 64) <= (t // 64)).astype(np.float32).astype(ml_dtypes.bfloat16)
    c["negm_fox"] = np.where((s + 128 * m) <= t, 0.0, -30000.0).astype(np.float32).astype(ml_dtypes.bfloat16)
    c2 = p // 64
    l = p % 64
    c["tri2"] = ((c2[:, None] == c2[None, :]) & (l[:, None] <= l[None, :])).astype(np.float32)
    c["bd"] = (c2[:, None] == c2[None, :]).astype(np.float32)
    c["lastsel"] = (p[:, None] == (64 * c2[None, :] + 63)).astype(np.float32)
    sl = np.zeros((2, 128, 128), np.float32)
    sl[0, 63, :] = 1.0
    sl[1, 127, :] = 1.0
    c["sellast"] = sl
    c["i64x2"] = (l[:, None] == np.arange(64)[None, :]).astype(np.float32)
    c["trimask"] = (np.arange(64)[None, :] >= l[:, None]).astype(np.float32)
    c["sel8"] = np.repeat(np.eye(8, dtype=np.float32), 128, axis=1)
    ng = np.where(np.arange(64)[None, :] < l[:, None], -30000.0, 0.0).astype(np.float32)
    c["negm_ssd"] = np.tile(ng[:, None, :], (1, 16, 1)).reshape(128, 1024).astype(ml_dtypes.bfloat16)
    inv = 10000.0 ** (-(np.arange(0, 32, 2, dtype=np.float32) / 32.0))
    ang = np.arange(S, dtype=np.float32)[None, :] * inv[:, None]
    cos = np.cos(ang).astype(np.float32)
    sin = np.sin(ang).astype(np.float32)
    cosT = np.concatenate([cos, cos], 0)
    sinT = np.concatenate([-sin, sin], 0)
    c["ropec"] = np.concatenate([cosT, cosT, cosT, cosT], 0).astype(np.float32)
    c["ropes"] = np.concatenate([sinT, sinT, sinT, sinT], 0).astype(np.float32)
    return c


class K:
    def __init__(self, S, NSEQ, layers=(0, 1), parts=("mix", "moe"), taps=()):
        self.S, self.NSEQ = S, NSEQ
        self.NT = S // 128
        self.GW = 512
        self.NG = S // 512
        self.layers, self.parts, self.taps = layers, parts, taps
        self.nc = bass.Bass("TRN2", target_bir_lowering=False)
        self.es = ExitStack()
        self.sc = Sched(self.nc, self.es)
        self.psi = 0
        self.evi = 0
        self.dbg = {}
        self.tap_out = {}

    def dram_in(self, name, shape, dt=F32):
        t = self.nc.dram_tensor(name, list(shape), dt, kind="ExternalInput")
        return Buf(name, t)

    def dram_out(self, name, shape, dt=F32):
        t = self.nc.dram_tensor(name, list(shape), dt, kind="ExternalOutput")
        return Buf(name, t)

    def sb(self, name, shape, dt=F32):
        t = self.es.enter_context(self.nc.sbuf_tensor("s_" + name, list(shape), dt))
        return Buf(name, t)

    def PS(self):
        b = self.ps[self.psi % len(self.ps)]
        self.psi += 1
        return b

    def mm(self, out, lhsT, rhs, start, stop, reads, writes):
        self.sc.op("pe", lambda e: e.matmul(out, lhsT, rhs, start=start, stop=stop), reads, writes)

    def tr(self, out, in_, ident, reads, writes):
        self.sc.op("pe", lambda e: e.transpose(out, in_, ident), reads, writes)

    def act(self, out, in_, func, reads, writes, bias=None, scale=1.0):
        kw = {}
        if bias is not None:
            kw["bias"] = bias
        self.sc.op("act", lambda e: e.activation(out=out, in_=in_, func=func, scale=scale, **kw), reads, writes)

    def tt(self, eng, out, in0, in1, op, reads, writes):
        self.sc.op(eng, lambda e: e.tensor_tensor(out=out, in0=in0, in1=in1, op=op), reads, writes)

    def ts(self, eng, out, in0, s1, s2, op0, op1, reads, writes):
        if op1 is None:
            self.sc.op(eng, lambda e: e.tensor_scalar(out=out, in0=in0, scalar1=s1, scalar2=None, op0=op0), reads, writes)
        else:
            self.sc.op(eng, lambda e: e.tensor_scalar(out=out, in0=in0, scalar1=s1, scalar2=s2, op0=op0, op1=op1), reads, writes)

    def stt(self, out, in0, scalar, in1, op0, op1, reads, writes):
        self.sc.op("dve", lambda e: e.scalar_tensor_tensor(out=out, in0=in0, scalar=scalar, in1=in1, op0=op0, op1=op1), reads, writes)

    def cp(self, eng, out, in_, reads, writes):
        if eng == "act":
            self.sc.op("act", lambda e: e.activation(out=out, in_=in_, func=AF.Copy), reads, writes)
        else:
            self.sc.op(eng, lambda e: e.tensor_copy(out=out, in_=in_), reads, writes)

    def evac(self, out, in_, reads, writes):
        self.evi += 1
        self.cp("act" if self.evi % 2 else "dve", out, in_, reads, writes)

    def load(self, q, dstbuf, out_ap, srcbuf, in_ap, **kw):
        self.sc.dma(q, out_ap, in_ap, dstbuf, srcbuf, **kw)

    def setup(self):
        S, NSEQ, NT = self.S, self.NSEQ, self.NT
        self.x = self.dram_in("x", [NSEQ, S, D])
        self.out = self.dram_out("out", [NSEQ, S, D])
        self.xscr = Buf("xscr", self.nc.dram_tensor("xscr", [S, D], F32, kind="Internal"))
        W = {}
        W["ev_w_in"] = self.dram_in("ev_w_in", [D, EVEN_IN])
        W["ev_w_out"] = self.dram_in("ev_w_out", [1536, D])
        W["od_w_in"] = self.dram_in("od_w_in", [D, ODD_IN + 128])
        W["od_w_q_up"] = self.dram_in("od_w_q_up", [256, 512 + 256 + 256])
        W["od_w_kv_up"] = self.dram_in("od_w_kv_up", [128, 1024])
        W["od_w_out"] = self.dram_in("od_w_out", [D, D])
        W["moe_w_gate"] = self.dram_in("moe_w_gate", [2, 16, D, 256])
        W["moe_w_up"] = self.dram_in("moe_w_up", [2, 16, D, 256])
        W["moe_w_down"] = self.dram_in("moe_w_down", [2, 16, 256, D])
        W["moe_wr"] = self.dram_in("moe_wr", [2, 128, 8, 20])
        W["rows"] = self.dram_in("rows", [NROWS, D])
        W["cols"] = self.dram_in("cols", [128, NCOLS])
        self.W = W
        cs = _consts(S)
        self.cdram = {}
        for k, v in cs.items():
            dt = BF16 if v.dtype == ml_dtypes.bfloat16 else F32
            self.cdram[k] = self.dram_in("c_" + k, v.shape, dt)
        self.slab = [self.sb("slab%d" % i, [128, 2048], BF16) for i in range(28)]
        self.XT = self.slab[0:8]
        self.wslot = [self.sb("wslot%d" % i, [128, 4096], BF16) for i in range(4)]
        self.wi = 0
        self.ps = [Buf("ps%d" % i, self.es.enter_context(self.nc.psum_tensor("ps%d" % i, [128, 512], F32)))
                   for i in range(8)]
        for b in self.ps:
            b.excl = True
        self.acc = self.ps[6:8]
        self.ps = self.ps[0:6]
        c = {}
        for k in ("ident", "identb", "onesb", "uincl", "tri2", "bd", "lastsel", "i64x2", "trimask"):
            v = cs[k]
            c[k] = self.sb("k_" + k, v.shape, BF16 if v.dtype == ml_dtypes.bfloat16 else F32)
            self.load("sp", c[k], c[k][:, :], self.cdram[k], self.cdram[k][:, :])
        c["sellast"] = self.sb("k_sellast", [128, 2, 128])
        for i in range(2):
            self.load("sp", c["sellast"], c["sellast"][:, i, :], self.cdram["sellast"], self.cdram["sellast"][i, :, :])
        self.c = c
        self.rows = self.sb("rows", [128, NROWS_SB, D])
        self.srow = self.sb("srow", [128, 512])
        self.maskbuf = self.sb("maskbuf", [128, 4, 512], BF16)
        self.P2 = [self.sb("p2_%d" % i, [128, 512]) for i in range(8)]
        self.H2 = [self.sb("h2_%d" % i, [128, 512], BF16) for i in range(4)]
        self.cols = self.sb("cols", [128, NCOLS])
        self.load("sp", self.cols, self.cols[:, :], W["cols"], W["cols"][:, :])
        self.wr = self.sb("wr", [128, 2, 8, 20])
        for l in range(2):
            self.load("sp", self.wr, self.wr[:, l, :, :], W["moe_wr"], W["moe_wr"][l, :, :, :])
        self.gate = self.sb("gate", [128, NT, 16])
        self.xres = [self.sb("xres0", [128, D])] * 2
        self.tmpA = [self.sb("tmpA%d" % i, [128, D]) for i in range(2)]
        self.T4 = [Buf("t4_%d" % i, None) for i in range(4)]
        for i in range(4):
            self.T4[i].t = self.slab[24 + i].t
        self.T4 = self.slab[24:28]
        self.small = self.sb("small", [128, 64])
        self.small2 = self.sb("small2", [128, 8])
        self.small2b = self.sb("small2b", [128, 16])
        self.dtb = self.sb("dtb", [128, self.NT, 16])
        self.dab = self.sb("dab", [128, self.NT, 16])
        self.ssm = self.sb("ssm", [128, 80])
        self.cbm = self.sb("cbm", [128, 128])
        self.zscr = Buf("zscr", self.nc.dram_tensor("zscr", [S, D], F32, kind="Internal"))
        self.cspt = self.sb("cspt", [128, self.NT, 8])
        self.xi = 0

    def row(self, r):
        return self.rows[:, r, :]

    def load_srow(self, r, off, n):
        src = self.W["rows"][r:r + 1, 0:n].broadcast_to([128, n])
        self.load("sp", self.srow, self.srow[:, off:off + n], self.W["rows"], src)

    def load_mask(self, name):
        cd = self.cdram[name]
        for m_ in range(4):
            self.load("sp", self.maskbuf, self.maskbuf[:, m_, :], cd, cd[m_, :, :])

    def load_rows(self, idx_list):
        for slot, r in enumerate(idx_list):
            src = self.W["rows"][r:r + 1, :].broadcast_to([128, D])
            self.load("sp", self.rows, self.rows[:, slot, :], self.W["rows"], src)

    def transpose_tile(self, xb, xap, j, router_layer=None):
        c = self.c
        for half in range(2):
            ps = self.PS()
            for kk in range(4):
                k = half * 4 + kk
                self.tr(ps[:, kk * 128:(kk + 1) * 128], xap[:, k * 128:(k + 1) * 128], c["ident"][:, :],
                        [xb, c["ident"]], [ps])
            self.evi += 1
            for kk in range(4):
                k = half * 4 + kk
                self.cp("act" if self.evi % 2 else "dve", self.XT[k][:, j * 128:(j + 1) * 128],
                        ps[:, kk * 128:(kk + 1) * 128], [ps], [self.XT[k]])
            if router_layer is not None and not self.dbg.get('noxtf'):
                xtf = self.xtf[half]
                self.cp("dve", xtf[:, :], ps[:, :], [ps], [xtf])
        if router_layer is not None and not self.dbg.get('norouter'):
            self.router(j, router_layer)

    def router(self, j, l):
        sm = self.small
        ps = self.PS()
        for k in range(8):
            xtf = self.xtf[k // 4]
            self.mm(ps[:, 0:20], xtf[:, (k % 4) * 128:(k % 4 + 1) * 128], self.wr[:, l, k, :], k == 0, k == 7,
                    [xtf, self.wr], [ps])
        lg = sm[:, 0:20]
        rb = self.srow[:, 0:20]
        self.tt("dve", lg, ps[:, 0:20], rb, ALU.add, [ps, self.srow], [sm])
        R, Wr = [sm], [sm]
        self.sc.op("dve", lambda e: e.reduce_max(out=sm[:, 20:21], in_=sm[:, 0:4], axis=mybir.AxisListType.X), R, Wr)
        self.ts("dve", sm[:, 21:25], sm[:, 0:4], sm[:, 20:21], None, ALU.is_equal, None, R, Wr)
        self.ts("dve", sm[:, 25:29], sm[:, 0:4], sm[:, 20:21], None, ALU.subtract, None, R, Wr)
        self.act(sm[:, 25:29], sm[:, 25:29], AF.Exp, R, Wr)
        self.sc.op("dve", lambda e: e.reduce_sum(out=sm[:, 29:30], in_=sm[:, 25:29], axis=mybir.AxisListType.X), R, Wr)
        self.sc.op("dve", lambda e: e.reciprocal(out=sm[:, 30:31], in_=sm[:, 29:30]), R, Wr)
        el = sm[:, 4:20].rearrange("p (g e) -> p g e", e=4)
        ohg_b = sm[:, 21:25].unsqueeze(2).broadcast_to([128, 4, 4])
        prod = sm[:, 32:48].rearrange("p (g e) -> p g e", e=4)
        self.tt("dve", prod, el, ohg_b, ALU.mult, R, Wr)
        prod_t = sm[:, 32:48].rearrange("p (g e) -> p e g", e=4)
        self.sc.op("dve", lambda e: e.reduce_sum(out=sm[:, 48:52], in_=prod_t, axis=mybir.AxisListType.X), R, Wr)
        self.sc.op("dve", lambda e: e.reduce_max(out=sm[:, 52:53], in_=sm[:, 48:52], axis=mybir.AxisListType.X), R, Wr)
        self.ts("dve", sm[:, 53:57], sm[:, 48:52], sm[:, 52:53], None, ALU.is_equal, None, R, Wr)
        self.stt(sm[:, 57:61], sm[:, 53:57], -1e30, sm[:, 48:52], ALU.mult, ALU.add, R, Wr)
        self.sc.op("dve", lambda e: e.reduce_max(out=sm[:, 61:62], in_=sm[:, 57:61], axis=mybir.AxisListType.X), R, Wr)
        self.ts("dve", sm[:, 25:29], sm[:, 57:61], sm[:, 61:62], None, ALU.is_equal, None, R, Wr)
        self.ts("dve", sm[:, 62:63], sm[:, 61:62], sm[:, 52:53], None, ALU.subtract, None, R, Wr)
        self.act(sm[:, 62:63], sm[:, 62:63], AF.Exp, R, Wr)
        self.ts("dve", sm[:, 63:64], sm[:, 62:63], 1.0, None, ALU.add, None, R, Wr)
        self.sc.op("dve", lambda e: e.reciprocal(out=sm[:, 63:64], in_=sm[:, 63:64]), R, Wr)
        self.stt(sm[:, 63:64], sm[:, 63:64], 1.0 / ALPHA, sm[:, 30:31], ALU.mult, ALU.mult, R, Wr)
        self.tt("dve", sm[:, 62:63], sm[:, 62:63], sm[:, 63:64], ALU.mult, R, Wr)
        self.ts("dve", sm[:, 53:57], sm[:, 53:57], sm[:, 63:64], None, ALU.mult, None, R, Wr)
        self.stt(sm[:, 53:57], sm[:, 25:29], sm[:, 62:63], sm[:, 53:57], ALU.mult, ALU.add, R, Wr)
        g4_b = sm[:, 53:57].unsqueeze(1).broadcast_to([128, 4, 4])
        gout = self.gate[:, j, :].rearrange("p (g e) -> p g e", e=4)
        self.tt("dve", gout, ohg_b, g4_b, ALU.mult, R, [self.gate])

    def ln_tile(self, xb, xap, g_row, b_row, eps=LN_EPS):
        sm = self.lnsm
        R, Wr = [sm], [sm]
        for h in range(2):
            self.sc.op("dve", lambda e, h=h: e.bn_stats(out=sm[:, 6 * h:6 * h + 6], in_=xap[:, h * 512:(h + 1) * 512]),
                       [xb], Wr)
        self.sc.op("dve", lambda e: e.bn_aggr(out=sm[:, 12:14], in_=sm[:, 0:12]), R, Wr)
        self.ts("dve", sm[:, 14:15], sm[:, 13:14], eps, None, ALU.add, None, R, Wr)
        self.act(sm[:, 14:15], sm[:, 14:15], AF.Sqrt, R, Wr)
        self.sc.op("dve", lambda e: e.reciprocal(out=sm[:, 15:16], in_=sm[:, 14:15]), R, Wr)
        self.ts("dve", xap, xap, sm[:, 12:13], sm[:, 15:16], ALU.subtract, ALU.mult, [xb, sm], [xb])
        self.tt("pool", xap, xap, g_row, ALU.mult, [xb, self.rows], [xb])
        self.tt("pool", xap, xap, b_row, ALU.add, [xb, self.rows], [xb])

    def wnext(self):
        b = self.wslot[self.wi % len(self.wslot)]
        self.wi += 1
        return b

    def wload_k(self, slot, col0, ncols, wbuf, wap2d, nk=8):
        raise NotImplementedError

    def moe(self, l, src, dst, last):
        S, NT, NG = self.S, self.NT, self.NG
        W = self.W
        XA = [self.slab[8 + j] for j in range(NT)]
        xa = [b.t[:, :].bitcast(F32) for b in XA]
        self.load_rows([2 + 4 * l, 3 + 4 * l])
        for j in range(NT):
            sb_, sap = src(j)
            self.load("sp", XA[j], xa[j], sb_, sap)
        hid = self.hid
        hidv = [h.t[:, :].bitcast(BF16)[:, 0:512] for h in hid]
        for e in range(self.dbg.get('nexp', 16)):
            sa = self.wnext()
            sv = sa.t[:, :].rearrange("p (k c) -> p k c", c=512)
            self.load("pool", sa, sv[:, :, 0:256], W["moe_w_gate"],
                      W["moe_w_gate"][l, e, :, :].rearrange("(k p) f -> p k f", p=128))
            self.load("pool", sa, sv[:, :, 256:512], W["moe_w_up"],
                      W["moe_w_up"][l, e, :, :].rearrange("(k p) f -> p k f", p=128))
            sd = self.wnext()
            dv = sd.t[:, 0:2048].rearrange("p (k c) -> p k c", c=1024)
            self.load("pool", sd, dv, W["moe_w_down"],
                      W["moe_w_down"][l, e, :, :].rearrange("(k p) f -> p k f", p=128))
            for g in range(NG):
                tsl = slice(g * 512, (g + 1) * 512)
                for f in range(2):
                    gps = self.PS()
                    for k in range(8):
                        self.mm(gps[:, :], sv[:, k, f * 128:(f + 1) * 128], self.XT[k][:, tsl], k == 0, k == 7,
                                [sa, self.XT[k]], [gps])
                    ups = self.PS()
                    for k in range(8):
                        self.mm(ups[:, :], sv[:, k, 256 + f * 128:256 + (f + 1) * 128], self.XT[k][:, tsl], k == 0, k == 7,
                                [sa, self.XT[k]], [ups])
                    sg = self.sg[f]
                    self.act(sg[:, :], gps[:, :], AF.Silu, [gps], [sg])
                    self.tt("dve", hidv[f], sg[:, :], ups[:, :], ALU.mult, [sg, ups], [hid[f]])
                for tt_ in range(4):
                    j = g * 4 + tt_
                    for half in range(2):
                        ops = self.PS()
                        for f in range(2):
                            self.mm(ops[:, :], hidv[f][:, tt_ * 128:(tt_ + 1) * 128], dv[:, f, half * 512:(half + 1) * 512],
                                    f == 0, f == 1, [hid[f], sd], [ops])
                        xs_ = xa[j][:, half * 512:(half + 1) * 512]
                        self.stt(xs_, ops[:, :], self.gate[:, j, e:e + 1], xs_, ALU.mult, ALU.add,
                                 [ops, self.gate, XA[j]], [XA[j]])
        for j in range(NT):
            if not self.dbg.get('noln'):
                self.ln_tile(XA[j], xa[j], self.row(0), self.row(1), eps=LN_EPS / (ALPHA * ALPHA))
            db, dap = dst(j)
            self.load("sp", db, dap, XA[j], xa[j])
            if not last:
                self.transpose_tile(XA[j], xa[j], j)

    def prologue(self, seq, router_layer=None):
        for j in range(self.NT):
            xb = self.xres[j % 2]
            self.load("sp", xb, xb[:, :], self.x, self.x[seq, j * 128:(j + 1) * 128, :])
            self.transpose_tile(xb, xb[:, :], j, router_layer)

    def alloc_misc(self):
        self.xtf = self.P2[0:2]
        self.lnsm = self.sb("lnsm", [128, 16])
        self.sg = self.P2[2:4]
        self.hid = self.P2[4:6]

    def run(self):
        self.setup()
        self.alloc_misc()
        NT = self.NT
        for seq in range(self.NSEQ):
            def src_x(j, seq=seq):
                return self.x, self.x[seq, j * 128:(j + 1) * 128, :]

            def src_scr(j):
                return self.xscr, self.xscr[j * 128:(j + 1) * 128, :]

            def dst_out(j, seq=seq):
                return self.out, self.out[seq, j * 128:(j + 1) * 128, :]
            cur = src_x
            first = True
            subl = [(l, p) for l in self.layers for p in self.parts]
            for i, (l, p) in enumerate(subl):
                last = i == len(subl) - 1
                dst = dst_out if last else src_scr
                if p == "moe":
                    if first:
                        self.load_srow(9 + l, 0, 20)
                        self.prologue(seq, router_layer=l)
                    self.moe(l, cur, dst, last)
                else:
                    if first:
                        self.prologue(seq)
                    if l == 0:
                        self.even_mixer(cur, dst)
                    else:
                        self.odd_mixer(cur, dst)
                cur = src_scr
                first = False
        self.sc.finish_waits("sp", [self.out])
        self.sc.emit_all()
        return self.nc


NROWS = 16
NROWS_SB = 3
NCOLS = 64


def _host_inputs(S, inputs):
    f = lambda a: np.ascontiguousarray(np.asarray(a, dtype=np.float32))
    m = {}
    m["ev_w_in"] = f(inputs["ev_w_in"][0])
    m["ev_w_out"] = f(inputs["ev_w_out"][0])
    wi = f(inputs["od_w_in"][0])
    kpe = wi[:, 384:416]
    kpe_sw = np.concatenate([kpe[:, 16:32], kpe[:, 0:16]], 1)
    m["od_w_in"] = np.ascontiguousarray(np.concatenate([wi, kpe, kpe, kpe_sw, kpe_sw], 1))
    wq = f(inputs["od_w_q_up"][0]).reshape(256, 8, 96)
    nope = wq[:, :, :64].reshape(256, 512)
    pe = wq[:, :, 64:]
    pe_sw = np.concatenate([pe[:, :, 16:], pe[:, :, :16]], 2)
    m["od_w_q_up"] = np.ascontiguousarray(np.concatenate([nope, pe.reshape(256, 256), pe_sw.reshape(256, 256)], 1))
    wkv = f(inputs["od_w_kv_up"][0]).reshape(128, 8, 128)
    m["od_w_kv_up"] = np.ascontiguousarray(np.concatenate([wkv[:, :, :64].reshape(128, 512), wkv[:, :, 64:].reshape(128, 512)], 1))
    m["od_w_out"] = f(inputs["od_w_out"][0])
    m["moe_w_gate"] = f(inputs["moe_w_gate"])
    m["moe_w_up"] = f(inputs["moe_w_up"])
    m["moe_w_down"] = f(inputs["moe_w_down"])
    wr = np.concatenate([f(inputs["moe_w_group"]), f(inputs["moe_w_expert"])], 2)
    m["moe_wr"] = np.ascontiguousarray(wr.reshape(2, 8, 128, 20).transpose(0, 2, 1, 3))
    rows = np.zeros((NROWS, D), np.float32)
    for l in range(2):
        rows[4 * l + 0] = inputs["ln1_g"][l]
        rows[4 * l + 1] = inputs["ln1_b"][l]
        rows[4 * l + 2] = inputs["ln2_g"][l]
        rows[4 * l + 3] = inputs["ln2_b"][l]
        rows[9 + l, 0:4] = inputs["moe_b_group"][l]
        rows[9 + l, 4:20] = inputs["moe_b_expert"][l]
    rows[8] = inputs["ev_norm_g"][0]
    rows[11, 0:16] = inputs["ev_dt_bias"][0]
    rows[11, 16:32] = inputs["ev_a_log"][0]
    rows[11, 32:48] = inputs["ev_d_skip"][0]
    rows[12, 0:256] = inputs["od_q_norm_g"][0]
    rows[12, 256:384] = inputs["od_kv_norm_g"][0]
    rows[13, 0:8] = inputs["od_f_bias"][0]
    m["rows"] = rows
    cols = np.zeros((128, NCOLS), np.float32)
    cw = f(inputs["ev_conv_w"][0])
    cols[:, 0:48] = cw.T.reshape(12, 128, 4).transpose(1, 0, 2).reshape(128, 48)
    cols[:, 48:60] = f(inputs["ev_conv_b"][0]).reshape(12, 128).T
    cols[0:8, 60] = f(inputs["od_f_bias"][0])
    m["cols"] = cols
    for k, v in _consts(S).items():
        m["c_" + k] = v
    return m


_CACHE = {}


def kernel(**inputs):
    x = np.ascontiguousarray(np.asarray(inputs["x"], dtype=np.float32))
    B, S, _ = x.shape
    nseq = B // NCORES
    key = (S, nseq)
    if key not in _CACHE:
        _CACHE[key] = K(S, nseq).run()
    nc = _CACHE[key]
    shared = _host_inputs(S, inputs)
    in_maps = []
    for c in range(NCORES):
        m = dict(shared)
        m["x"] = np.ascontiguousarray(x[c * nseq:(c + 1) * nseq])
        in_maps.append(m)
    res = run_bass_kernel_spmd(nc, in_maps, core_ids=list(range(NCORES)))
    return np.concatenate([np.asarray(r["out"]) for r in res.results], axis=0).astype(np.float32)


def _f32v(b):
    return b.t[:, :].bitcast(F32)


def _bfv(b):
    return b.t[:, :].bitcast(BF16)


def _wview(slot, c):
    return slot.t[:, :].rearrange("p (k c) -> p k c", c=c)


def _kp(ap2d):
    return ap2d.rearrange("(k p) c -> p k c", p=128)


def _rope_tables(self, g):
    rc, rs = self.P2[3], self.P2[4]
    cc, cs_ = self.cdram["ropec"], self.cdram["ropes"]
    self.load("sp", rc, rc[0:64, :], cc, cc[0:64, g * 512:(g + 1) * 512])
    self.load("sp", rs, rs[0:64, :], cs_, cs_[0:64, g * 512:(g + 1) * 512])
    return rc, rs


def _rope(self, A, B, rc, rs, dst, dst_ap):
    t1, t2 = self.P2[5], self.P2[6]
    self.tt("dve", t1[0:64, :], A[0:64, :], rc[0:64, :], ALU.mult, [A, rc], [t1])
    self.tt("dve", t2[0:64, :], B[0:64, :], rs[0:64, :], ALU.mult, [B, rs], [t2])
    self.tt("pool", dst_ap, t1[0:64, :], t2[0:64, :], ALU.add, [t1, t2], [dst])


def _attn_finish(self, OT, DEN, hh, dst, dst_ap):
    rec = self.P2[6]
    lo, hi = hh * 64, hh * 64 + 64
    self.sc.op("dve", lambda e: e.reciprocal(out=rec[lo:hi, :], in_=DEN[lo:hi, :]), [DEN], [rec])
    self.tt("dve", dst_ap, OT[lo:hi, :], rec[lo:hi, :], ALU.mult, [OT, rec], [dst])


def odd_mixer(self, src, dst):
    S, NT, NG = self.S, self.NT, self.NG
    W = self.W
    Win = W["od_w_in"]
    sl = self.slab
    P2 = self.P2
    XT = self.XT
    c = self.c
    sc_mla = 1.0 / math.sqrt(96.0)
    sc_fox = 1.0 / 8.0
    self.load_rows([4, 5])
    self.load_srow(10, 0, 20)
    self.load_srow(12, 96, 384)
    CQNT, CKVNT, KPE = sl[8:10], sl[10], sl[11]
    OTF = sl[12:16]
    sm = self.small
    wl = self.wnext()
    wlv = _wview(wl, 512)
    self.load("pool", wl, wlv[:, :, 0:384], Win, _kp(Win[:, 0:384]))
    self.load("pool", wl, wlv[:, :, 384:512], Win, _kp(Win[:, 1960:2088]))
    lat, sq, latn_b = P2[0], P2[1], P2[2]
    latn = _bfv(latn_b)
    for j in range(NT):
        tsl = slice(j * 128, (j + 1) * 128)
        ps = self.PS()
        for k in range(8):
            self.mm(ps[:, 0:384], XT[k][:, tsl], wlv[:, k, 0:384], k == 0, k == 7, [XT[k], wl], [ps])
        self.cp("act", lat[:, 0:384], ps[:, 0:384], [ps], [lat])
        self.tt("pool", sq[:, 0:384], lat[:, 0:384], lat[:, 0:384], ALU.mult, [lat], [sq])
        self.sc.op("dve", lambda e: e.reduce_sum(out=sm[:, 0:1], in_=sq[:, 0:256], axis=mybir.AxisListType.X), [sq], [sm])
        self.sc.op("dve", lambda e: e.reduce_sum(out=sm[:, 1:2], in_=sq[:, 256:384], axis=mybir.AxisListType.X), [sq], [sm])
        self.ts("dve", sm[:, 0:1], sm[:, 0:1], 1.0 / 256, RMS_EPS, ALU.mult, ALU.add, [sm], [sm])
        self.ts("dve", sm[:, 1:2], sm[:, 1:2], 1.0 / 128, RMS_EPS, ALU.mult, ALU.add, [sm], [sm])
        self.act(sm[:, 0:2], sm[:, 0:2], AF.Sqrt, [sm], [sm])
        self.sc.op("dve", lambda e: e.reciprocal(out=sm[:, 2:4], in_=sm[:, 0:2]), [sm], [sm])
        self.stt(latn[:, 0:256], lat[:, 0:256], sm[:, 2:3], self.srow[:, 96:352], ALU.mult, ALU.mult,
                 [lat, sm, self.srow], [latn_b])
        self.stt(latn[:, 256:384], lat[:, 256:384], sm[:, 3:4], self.srow[:, 352:480], ALU.mult, ALU.mult,
                 [lat, sm, self.srow], [latn_b])
        pb = self.PS()
        pbv = _bfv(pb)
        for i in range(3):
            self.tr(pbv[:, i * 128:(i + 1) * 128], latn[:, i * 128:(i + 1) * 128], c["identb"][:, :], [latn_b, c["identb"]], [pb])
        for i, dstb in enumerate((CQNT[0], CQNT[1], CKVNT)):
            self.cp("dve", dstb[:, tsl], pbv[:, i * 128:(i + 1) * 128], [pb], [dstb])
    for g in range(NG):
        gsl = slice(g * 512, (g + 1) * 512)
        rc, rs = _rope_tables(self, g)
        A = self.PS()
        for k in range(8):
            self.mm(A[0:64, :], wlv[:, k, 384:448], XT[k][:, gsl], k == 0, k == 7, [wl, XT[k]], [A])
        B = self.PS()
        for k in range(8):
            self.mm(B[0:64, :], wlv[:, k, 448:512], XT[k][:, gsl], k == 0, k == 7, [wl, XT[k]], [B])
        _rope(self, A, B, rc, rs, KPE, KPE[0:64, gsl])
    wf = self.wnext()
    wfv = wf.t[:, 0:64].rearrange("p (k c) -> p k c", c=8)
    self.load("pool", wf, wfv, Win, _kp(Win[:, 1952:1960]))
    ones = P2[7]
    self.sc.op("pool", lambda e: e.memset(ones[:, :], 1.0), [], [ones])
    negfb = self.small2
    self.ts("pool", negfb[0:8, 0:1], self.cols[0:8, 60:61], -1.0, None, ALU.mult, None, [self.cols], [negfb])
    CSPb = [self.T4[0], self.T4[1]]
    FHQb = [self.T4[2], self.T4[3]]

    def cspg(g):
        return CSPb[g // 2], _f32v(CSPb[g // 2])[:, (g % 2) * 512:(g % 2 + 1) * 512]

    def fhqg(g):
        return FHQb[g // 2], _f32v(FHQb[g // 2])[:, (g % 2) * 512:(g % 2 + 1) * 512]
    CSPT = self.cspt
    for g in range(NG):
        gsl = slice(g * 512, (g + 1) * 512)
        ps = self.PS()
        for k in range(8):
            self.mm(ps[0:8, :], wfv[:, k, :], XT[k][:, gsl], k == 0, k == 7, [wf, XT[k]], [ps])
        e_, sp_ = P2[5], P2[6]
        self.act(e_[0:8, :], ps[0:8, :], AF.Exp, [ps, negfb], [e_], bias=negfb[0:8, 0:1], scale=-1.0)
        self.act(sp_[0:8, :], e_[0:8, :], AF.Ln, [e_], [sp_], bias=1.0)
        cb, cap = cspg(g)
        if g == 0:
            init, rds = 0.0, [ones, sp_]
        else:
            pb_, pap = cspg(g - 1)
            init, rds = pap[0:8, 511:512], [ones, sp_, pb_]
        self.sc.op("dve", lambda e, cap=cap, init=init: e.tensor_tensor_scan(
            out=cap[0:8, :], data0=ones[0:8, :], data1=sp_[0:8, :], initial=init, op0=ALU.mult, op1=ALU.add), rds, [cb])
        for tt_ in range(4):
            j = g * 4 + tt_
            pt = self.PS()
            self.tr(pt[:, 0:8], cap[0:8, tt_ * 128:(tt_ + 1) * 128], c["ident"][0:8, 0:8], [cb, c["ident"]], [pt])
            self.cp("dve", CSPT[:, j, :], pt[:, 0:8], [pt], [CSPT])
    self.load_mask("negm_fox")
    OT, DEN = self.acc
    for hp in range(4):
        q4 = sl[16 + 4 * (hp % 2):20 + 4 * (hp % 2)]
        FQz, FK, FV = q4[0:2], q4[2], q4[3]
        if hp < 2:
            self.sc.op("pool", lambda e, b=FQz[0]: e.memset(b[64:128, :], 0.0), [], [FQz[0]])
            self.sc.op("pool", lambda e, b=FQz[1]: e.memset(b[0:64, :], 0.0), [], [FQz[1]])
        wq = self.wnext()
        wqv = _wview(wq, 512)
        for i, c0 in enumerate((416, 928, 1440)):
            self.load("pool", wq, wqv[:, :, i * 128:(i + 1) * 128], Win, _kp(Win[:, c0 + hp * 128:c0 + (hp + 1) * 128]))
        for g in range(NG):
            gsl = slice(g * 512, (g + 1) * 512)
            for i in range(2):
                ps = self.PS()
                for k in range(8):
                    self.mm(ps[:, :], wqv[:, k, i * 128:(i + 1) * 128], XT[k][:, gsl], k == 0, k == 7, [wq, XT[k]], [ps])
                if i == 0:
                    self.cp("act", FQz[0][0:64, gsl], ps[0:64, :], [ps], [FQz[0]])
                    self.cp("act", FQz[1][64:128, gsl], ps[64:128, :], [ps], [FQz[1]])
                else:
                    self.cp("dve", FK[:, gsl], ps[:, :], [ps], [FK])
        for j in range(NT):
            tsl = slice(j * 128, (j + 1) * 128)
            ps = self.PS()
            for k in range(8):
                self.mm(ps[:, 0:128], XT[k][:, tsl], wqv[:, k, 256:384], k == 0, k == 7, [wq, XT[k]], [ps])
            self.evac(FV[:, tsl], ps[:, 0:128], [ps], [FV])
        FHQs = [[self.T4[2], self.T4[3]], [self.T4[2], self.T4[3]]]
        for hh in range(2):
            h = 2 * hp + hh
            for g in range(NG):
                cb, cap = cspg(g)
                m_ = P2[5]
                self.ts("dve", m_[0:8, :], cap[0:8, :], c["ident"][0:8, h:h + 1], 1.0 / sc_fox, ALU.mult, ALU.mult, [cb, c["ident"]], [m_])
                ps = self.PS()
                self.mm(ps[:, :], ones[0:8, 0:128], m_[0:8, :], True, True, [ones, m_], [ps])
                fb = FHQs[hh][g // 2]
                self.cp("act", _f32v(fb)[:, (g % 2) * 512:(g % 2 + 1) * 512], ps[:, :], [ps], [fb])
            _fox_pipe(self, hp, hh, FQz, FK, FV, FHQs, CSPT, OTF[hp], sc_fox)
    self.load_mask("mask_mla")
    wm = self.wnext()
    wq_ = wm.t[:, 0:2048].rearrange("p (k c) -> p k c", c=1024)
    wkv = wm.t[:, 2048:3072]
    self.load("pool", wm, wq_, W["od_w_q_up"], _kp(W["od_w_q_up"][:, :]))
    self.load("pool", wm, wkv, W["od_w_kv_up"], W["od_w_kv_up"][:, :])
    VM = XT[0:4]
    OTM = XT[4:8]
    for j in range(NT):
        ps = self.PS()
        self.mm(ps[:, :], CKVNT[:, j * 128:(j + 1) * 128], wkv[:, 512:1024], True, True, [CKVNT, wm], [ps])
        self.evac(VM[j // 4][:, (j % 4) * 512:(j % 4 + 1) * 512], ps[:, :], [ps], [VM[j // 4]])
    for hp in range(4):
        QN, KN, QP = sl[16 + 3 * (hp % 2)], sl[17 + 3 * (hp % 2)], sl[18 + 3 * (hp % 2)]
        for g in range(NG):
            gsl = slice(g * 512, (g + 1) * 512)
            ps = self.PS()
            for kk in range(2):
                self.mm(ps[:, :], wq_[:, kk, hp * 128:(hp + 1) * 128], CQNT[kk][:, gsl], kk == 0, kk == 1, [wm, CQNT[kk]], [ps])
            self.evac(QN[:, gsl], ps[:, :], [ps], [QN])
            ps = self.PS()
            self.mm(ps[:, :], wkv[:, hp * 128:(hp + 1) * 128], CKVNT[:, gsl], True, True, [wm, CKVNT], [ps])
            self.evac(KN[:, gsl], ps[:, :], [ps], [KN])
            rc, rs = _rope_tables(self, g)
            A = self.PS()
            for kk in range(2):
                self.mm(A[0:64, :], wq_[:, kk, 512 + hp * 64:512 + (hp + 1) * 64], CQNT[kk][:, gsl], kk == 0, kk == 1, [wm, CQNT[kk]], [A])
            B = self.PS()
            for kk in range(2):
                self.mm(B[0:64, :], wq_[:, kk, 768 + hp * 64:768 + (hp + 1) * 64], CQNT[kk][:, gsl], kk == 0, kk == 1, [wm, CQNT[kk]], [B])
            _rope(self, A, B, rc, rs, QP, QP[0:64, gsl])
        _mla_pipe(self, hp, QN, KN, QP, KPE, VM, OTM[hp], sc_mla)
    wo = [self.wnext(), self.wnext()]
    wov = [_wview(w_, 1024) for w_ in wo]
    Wo = W["od_w_out"]
    for i in range(2):
        self.load("pool", wo[i], wov[i], Wo, _kp(Wo[i * 512:(i + 1) * 512, :]))
    cat = list(OTM) + list(OTF)
    for j in range(NT):
        tsl = slice(j * 128, (j + 1) * 128)
        xr = self.xres[j % 2]
        sb_, sap = src(j)
        self.load("sp", xr, xr[:, :], sb_, sap)
        y = self.tmpA[j % 2]
        for half in range(2):
            hs = slice(half * 512, (half + 1) * 512)
            ps = self.PS()
            for kc in range(8):
                self.mm(ps[:, :], cat[kc][:, tsl], wov[kc // 4][:, kc % 4, hs], kc == 0, kc == 7, [cat[kc], wo[kc // 4]], [ps])
            self.stt(y[:, hs], xr[:, hs], ALPHA, ps[:, :], ALU.mult, ALU.add, [xr, ps], [y])
        self.ln_tile(y, y[:, :], self.row(0), self.row(1))
        db, dap = dst(j)
        self.load("sp", db, dap, y, y[:, :])
        self.transpose_tile(y, y[:, :], j, router_layer=1)


K.odd_mixer = odd_mixer


def _v3(ap, inner):
    return ap.rearrange("p (a b) -> p a b", b=inner)


def _bc(ap2, n):
    return ap2.unsqueeze(2).broadcast_to([ap2.shape[0], ap2.shape[1], n])


def even_mixer(self, src, dst):
    S, NT, NG = self.S, self.NT, self.NG
    W = self.W
    Win = W["ev_w_in"]
    sl = self.slab
    P2 = self.P2
    XT = self.XT
    c = self.c
    sm = self.small
    X = mybir.AxisListType.X
    self.load_rows([0, 1, 8])
    self.load_srow(9, 0, 20)
    self.load_srow(11, 32, 48)
    OTS = sl[12:16]
    self.load_mask("mask_sb")
    OTa = self.acc
    for hp in range(4):
        q4 = sl[16 + 4 * (hp % 2):20 + 4 * (hp % 2)]
        QTz, KT, FV = q4[0:2], q4[2], q4[3]
        if hp < 2:
            self.sc.op("pool", lambda e, b=QTz[0]: e.memset(b[64:128, :], 0.0), [], [QTz[0]])
            self.sc.op("pool", lambda e, b=QTz[1]: e.memset(b[0:64, :], 0.0), [], [QTz[1]])
        wq = self.wnext()
        wqv = _wview(wq, 512)
        for i, c0 in enumerate((2576, 3088, 3600)):
            self.load("pool", wq, wqv[:, :, i * 128:(i + 1) * 128], Win, _kp(Win[:, c0 + hp * 128:c0 + (hp + 1) * 128]))
        for g in range(NG):
            gsl = slice(g * 512, (g + 1) * 512)
            for i in range(2):
                ps = self.PS()
                for k in range(8):
                    self.mm(ps[:, :], wqv[:, k, i * 128:(i + 1) * 128], XT[k][:, gsl], k == 0, k == 7, [wq, XT[k]], [ps])
                if i == 0:
                    self.cp("act", QTz[0][0:64, gsl], ps[0:64, :], [ps], [QTz[0]])
                    self.cp("act", QTz[1][64:128, gsl], ps[64:128, :], [ps], [QTz[1]])
                else:
                    self.cp("dve", KT[:, gsl], ps[:, :], [ps], [KT])
        for j in range(NT):
            tsl = slice(j * 128, (j + 1) * 128)
            ps = self.PS()
            for k in range(8):
                self.mm(ps[:, 0:128], XT[k][:, tsl], wqv[:, k, 256:384], k == 0, k == 7, [wq, XT[k]], [ps])
            self.evac(FV[:, tsl], ps[:, 0:128], [ps], [FV])
        _sb_pipe(self, hp, QTz, KT, FV, OTS[hp])
    ps_save = self.ps
    xtf_save = self.xtf
    self.xtf = [P2[0], P2[3]]
    allps = list(self.ps) + list(self.acc)
    self.ps = allps[0:4]
    YD, YO = allps[4:6], allps[6:8]
    XS_T = list(sl[8:12]) + list(sl[16:20])
    BT, CT = sl[20:22], sl[22:24]
    T4 = self.T4
    dests = XS_T + list(BT) + list(CT)
    for pnl in range(3):
        wp = self.wnext()
        wpv = _wview(wp, 512)
        self.load("pool", wp, wpv, Win, _kp(Win[:, 1024 + pnl * 512:1024 + (pnl + 1) * 512]))
        for q in range(4):
            cc = pnl * 4 + q
            for g in range(NG):
                gsl = slice(g * 512, (g + 1) * 512)
                ps = self.PS()
                for k in range(8):
                    self.mm(ps[:, :], wpv[:, k, q * 128:(q + 1) * 128], XT[k][:, gsl], k == 0, k == 7, [wp, XT[k]], [ps])
                RAWb = T4[g % 2]
                RAW = _bfv(RAWb)
                if g == 0:
                    self.sc.op("pool", lambda e, RAW=RAW: e.memset(RAW[:, 0:3], 0.0), [], [RAWb])
                else:
                    prev = _bfv(T4[(g - 1) % 2])
                    self.cp("pool", RAW[:, 0:3], prev[:, 512:515], [T4[(g - 1) % 2]], [RAWb])
                self.cp("act", RAW[:, 3:515], ps[:, :], [ps], [RAWb])
                DGb = T4[2 + cc % 2]
                DG = _bfv(DGb)
                if g == 0:
                    for tap in range(4):
                        self.ts("dve", DG[:, tap * 128:(tap + 1) * 128], c["identb"][:, :], self.cols[:, cc * 4 + tap:cc * 4 + tap + 1],
                                None, ALU.mult, None, [c["identb"], self.cols], [DGb])
                cps = self.PS()
                for tap in range(4):
                    self.mm(cps[:, :], DG[:, tap * 128:(tap + 1) * 128], RAW[:, tap:tap + 512], tap == 0, tap == 3, [DGb, RAWb], [cps])
                self.act(dests[cc][:, gsl], cps[:, :], AF.Silu, [cps, self.cols], [dests[cc]], bias=self.cols[:, 48 + cc:49 + cc])
    wd = self.wnext()
    wdv = wd.t[:, 0:128].rearrange("p (k c) -> p k c", c=16)
    self.load("pool", wd, wdv, Win, _kp(Win[:, 2560:2576]))
    DT, DA = self.dtb, self.dab
    AROW = self.small2b
    self.act(AROW[:, 0:16], self.srow[:, 48:64], AF.Exp, [self.srow], [AROW])
    self.ts("pool", AROW[:, 0:16], AROW[:, 0:16], -1.0, None, ALU.mult, None, [AROW], [AROW])
    for j in range(NT):
        tsl = slice(j * 128, (j + 1) * 128)
        ps = self.PS()
        for k in range(8):
            self.mm(ps[:, 0:16], XT[k][:, tsl], wdv[:, k, :], k == 0, k == 7, [XT[k], wd], [ps])
        self.tt("dve", sm[:, 0:16], ps[:, 0:16], self.srow[:, 32:48], ALU.add, [ps, self.srow], [sm])
        self.act(sm[:, 0:16], sm[:, 0:16], AF.Exp, [sm], [sm])
        self.act(DT[:, j, :], sm[:, 0:16], AF.Ln, [sm], [DT], bias=1.0)
        self.tt("dve", DA[:, j, :], DT[:, j, :], AROW[:, 0:16], ALU.mult, [DT, AROW], [DA])
    for half in range(2):
        wz = self.wnext()
        wzv = _wview(wz, 512)
        self.load("pool", wz, wzv, Win, _kp(Win[:, half * 512:(half + 1) * 512]))
        for j in range(NT):
            tsl = slice(j * 128, (j + 1) * 128)
            ps = self.PS()
            for k in range(8):
                self.mm(ps[:, :], XT[k][:, tsl], wzv[:, k, :], k == 0, k == 7, [XT[k], wz], [ps])
            szb = self.tmpA[j % 2]
            self.act(szb[:, 0:512], ps[:, :], AF.Silu, [ps], [szb])
            self.load("sp", self.zscr, self.zscr[j * 128:(j + 1) * 128, half * 512:(half + 1) * 512], szb, szb[:, 0:512])
    wo = [self.wnext(), self.wnext(), self.wnext()]
    wov = [_wview(w_, 1024) for w_ in wo]
    Wo = W["ev_w_out"]
    for i in range(3):
        self.load("pool", wo[i], wov[i], Wo, _kp(Wo[i * 512:(i + 1) * 512, :]))
    XSTOKb, Dgb, SEGb, GTBb = T4[0], T4[1], T4[2], T4[3]
    XSTOK, Dg, SEG = _f32v(XSTOKb), _f32v(Dgb), _f32v(SEGb)
    GTB = _bfv(GTBb)
    XSPb, XSPP0b, XSPP1b, YNb = P2[0], P2[1], P2[2], P2[3]
    XSP, YN = _bfv(XSPb), _bfv(YNb)
    XSPP = [_bfv(XSPP0b), _bfv(XSPP1b)]
    XSPPb = [XSPP0b, XSPP1b]
    BTOKb = self.H2[2]
    BTOK = BTOKb[:, 0:256]
    HT = [P2[5], P2[6]]
    HTbb = P2[7]
    HTb = [_bfv(HTbb)[:, 0:512], _bfv(HTbb)[:, 512:1024]]
    YNTh = [self.H2[0], self.H2[1]]
    ssm = self.ssm
    CBM = self.cbm
    ACUM, EA, DTW = ssm[:, 0:16], ssm[:, 16:32], ssm[:, 32:48]
    DEC = [ssm[:, 48:64], ssm[:, 64:80]]
    self.sc.op("pool", lambda e: e.memset(GTB, 0.0), [], [GTBb])
    self.sc.op("pool", lambda e: e.memset(XSPP[0], 0.0), [], [XSPP0b])
    self.sc.op("pool", lambda e: e.memset(XSPP[1], 0.0), [], [XSPP1b])
    for g in range(2):
        self.sc.op("pool", lambda e, g=g: e.memset(HT[g][:, :], 0.0), [], [HT[g]])
    self.sc.op("pool", lambda e: e.memset(_bfv(HTbb), 0.0), [], [HTbb])
    for j in range(NT):
        tsl = slice(j * 128, (j + 1) * 128)
        pb = self.PS()
        pbv = _bfv(pb)
        for cc in range(8):
            self.tr(pbv[:, cc * 128:(cc + 1) * 128], XS_T[cc][:, tsl], c["identb"][:, :], [XS_T[cc], c["identb"]], [pb])
        self.cp("act", XSTOK, pbv, [pb], [XSTOKb])
        pb2 = self.PS()
        pb2v = _bfv(pb2)
        for g in range(2):
            self.tr(pb2v[:, g * 128:(g + 1) * 128], BT[g][:, tsl], c["identb"][:, :], [BT[g], c["identb"]], [pb2])
        self.cp("dve", BTOK, pb2v[:, 0:256], [pb2], [BTOKb])
        ps = self.PS()
        self.mm(ps[:, 0:16], c["tri2"][:, :], DA[:, j, :], True, True, [c["tri2"], DA], [ps])
        self.cp("dve", ACUM, ps[:, 0:16], [ps], [ssm])
        self.act(EA, ACUM, AF.Exp, [ssm], [ssm])
        ps = self.PS()
        self.mm(ps[:, 0:16], c["lastsel"][:, :], ACUM, True, True, [c["lastsel"], ssm], [ps])
        self.tt("dve", DTW, ps[:, 0:16], ACUM, ALU.subtract, [ps, ssm], [ssm])
        self.act(DTW, DTW, AF.Exp, [ssm], [ssm])
        self.tt("dve", DTW, DTW, DT[:, j, :], ALU.mult, [ssm, DT], [ssm])
        for c2 in range(2):
            ps = self.PS()
            self.mm(ps[:, 0:16], c["sellast"][:, c2, :], ACUM, True, True, [c["sellast"], ssm], [ps])
            self.act(DEC[c2], ps[:, 0:16], AF.Exp, [ps], [ssm])
        self.tt("dve", _v3(Dg, 64), _bc(ACUM, 64), c["i64x2"][:, :].unsqueeze(1).broadcast_to([128, 16, 64]), ALU.mult,
                [ssm, c["i64x2"]], [Dgb])
        for hb in range(2):
            p1 = self.PS()
            self.mm(p1[:, :], c["bd"][:, :], Dg[:, hb * 512:(hb + 1) * 512], True, True, [c["bd"], Dgb], [p1])
            self.tt("dve", _v3(SEG[:, hb * 512:(hb + 1) * 512], 64), _v3(p1[:, :], 64), _bc(ssm[:, hb * 8:hb * 8 + 8], 64),
                    ALU.subtract, [p1, ssm], [SEGb])
        self.ts("pool", SEG, SEG, 0.0, None, ALU.min, None, [SEGb], [SEGb])
        self.act(SEG, SEG, AF.Exp, [SEGb], [SEGb])
        ps = self.PS()
        for c2 in range(2):
            csl = slice(j * 128 + c2 * 64, j * 128 + c2 * 64 + 64)
            for g in range(2):
                self.sc.op("pe", lambda e, ps=ps, c2=c2, g=g, csl=csl: e.matmul(
                    ps[c2 * 64:c2 * 64 + 64, g * 64:(g + 1) * 64], BT[g][:, csl], CT[g][:, csl], start=True, stop=True,
                    skip_group_check=True), [BT[g], CT[g]], [ps])
        self.tt("dve", _v3(CBM[:, :], 64), _v3(ps[:, 0:128], 64), c["trimask"][:, :].unsqueeze(1).broadcast_to([128, 2, 64]),
                ALU.mult, [ps, c["trimask"]], [CBM])
        for c2 in range(2):
            lo, hi = c2 * 64, c2 * 64 + 64
            out_ap = GTB[lo:hi, :].rearrange("p (h x) -> p h x", x=128)[:, :, lo:hi].rearrange("p (g r) l -> p g r l", g=2)
            in0 = SEG[lo:hi, :].rearrange("p (g r l) -> p g r l", g=2, r=8)
            in1 = _v3(CBM[lo:hi, :], 64).unsqueeze(2).broadcast_to([64, 2, 8, 64])
            self.tt("dve", out_ap, in0, in1, ALU.mult, [SEGb, CBM], [GTBb])
        self.tt("pool", _v3(XSP, 64), _v3(XSTOK, 64), _bc(DT[:, j, :], 64), ALU.mult, [XSTOKb, DT], [XSPb])
        for c2 in range(2):
            lo, hi = c2 * 64, c2 * 64 + 64
            self.tt("pool", _v3(XSPP[c2][lo:hi, :], 64), _v3(XSTOK[lo:hi, :], 64), _bc(ssm[lo:hi, 32:48], 64), ALU.mult,
                    [XSTOKb, ssm], [XSPPb[c2]])
        for h in range(16):
            self.sc.op("pe", lambda e, h=h: e.matmul(
                YD[h // 8][:, (h % 8) * 64:(h % 8 + 1) * 64], GTB[:, h * 128:(h + 1) * 128], XSP[:, h * 64:(h + 1) * 64],
                start=True, stop=True, skip_group_check=True), [GTBb, XSPb], [YD[h // 8]])
        for c2 in range(2):
            lo, hi = c2 * 64, c2 * 64 + 64
            csl = slice(j * 128 + lo, j * 128 + hi)
            for g in range(2):
                self.sc.op("pe", lambda e, g=g, lo=lo, hi=hi, csl=csl: e.matmul(
                    YO[g][lo:hi, :], CT[g][:, csl], HTb[g], start=True, stop=True, skip_group_check=True),
                    [CT[g], HTbb], [YO[g]])
            for g in range(2):
                st = self.PS()
                self.mm(st[:, :], BTOK[:, g * 128:(g + 1) * 128], XSPP[c2][:, g * 512:(g + 1) * 512], True, True,
                        [BTOKb, XSPPb[c2]], [st])
                self.tt("dve", _v3(HT[g][:, :], 64), _v3(HT[g][:, :], 64), _bc(DEC[c2][:, g * 8:(g + 1) * 8], 64), ALU.mult,
                        [HT[g], ssm], [HT[g]])
                self.tt("dve", HT[g][:, :], HT[g][:, :], st[:, :], ALU.add, [HT[g], st], [HT[g]])
                self.cp("act", HTb[g], HT[g][:, :], [HT[g]], [HTbb])
        Yb = self.tmpA[1]
        SZb = self.tmpA[0]
        self.load("sp", SZb, SZb[:, :], self.zscr, self.zscr[tsl, :])
        for g in range(2):
            hs = slice(g * 512, (g + 1) * 512)
            self.tt("dve", _v3(Yb[:, hs], 64), _v3(YO[g][:, :], 64), _bc(ssm[:, 16 + g * 8:16 + g * 8 + 8], 64), ALU.mult,
                    [YO[g], ssm], [Yb])
            self.tt("dve", Yb[:, hs], Yb[:, hs], YD[g][:, :], ALU.add, [Yb, YD[g]], [Yb])
        self.tt("pool", _v3(Dg, 64), _v3(XSTOK, 64), _bc(self.srow[:, 64:80], 64), ALU.mult, [XSTOKb, self.srow], [Dgb])
        self.tt("pool", Yb[:, :], Yb[:, :], Dg, ALU.add, [Yb, Dgb], [Yb])
        self.tt("pool", Yb[:, :], Yb[:, :], SZb[:, :], ALU.mult, [Yb, SZb], [Yb])
        self.tt("pool", Dg, Yb[:, :], Yb[:, :], ALU.mult, [Yb], [Dgb])
        self.sc.op("dve", lambda e: e.reduce_sum(out=sm[:, 0:1], in_=Dg, axis=X), [Dgb], [sm])
        self.ts("dve", sm[:, 0:1], sm[:, 0:1], 1.0 / 1024, RMS_EPS, ALU.mult, ALU.add, [sm], [sm])
        self.act(sm[:, 0:1], sm[:, 0:1], AF.Sqrt, [sm], [sm])
        self.sc.op("dve", lambda e: e.reciprocal(out=sm[:, 1:2], in_=sm[:, 0:1]), [sm], [sm])
        self.stt(YN, Yb[:, :], sm[:, 1:2], self.rows[:, 2, :], ALU.mult, ALU.mult, [Yb, sm, self.rows], [YNb])
        pb = self.PS()
        pbv = _bfv(pb)
        for cc in range(8):
            self.tr(pbv[:, cc * 128:(cc + 1) * 128], YN[:, cc * 128:(cc + 1) * 128], c["identb"][:, :], [YNb, c["identb"]], [pb])
        self.cp("act", YNTh[0][:, :], pbv[:, 0:512], [pb], [YNTh[0]])
        self.cp("act", YNTh[1][:, :], pbv[:, 512:1024], [pb], [YNTh[1]])
        xr = self.xres[j % 2]
        sb_, sap = src(j)
        self.load("sp", xr, xr[:, :], sb_, sap)
        for half in range(2):
            hs = slice(half * 512, (half + 1) * 512)
            ps = self.PS()
            for kc in range(12):
                lhsT = YNTh[kc // 4][:, (kc % 4) * 128:(kc % 4 + 1) * 128] if kc < 8 else OTS[kc - 8][:, tsl]
                rb_ = YNTh[kc // 4] if kc < 8 else OTS[kc - 8]
                self.mm(ps[:, :], lhsT, wov[kc // 4][:, kc % 4, hs], kc == 0, kc == 11, [rb_, wo[kc // 4]], [ps])
            self.stt(SZb[:, hs], xr[:, hs], ALPHA, ps[:, :], ALU.mult, ALU.add, [xr, ps], [SZb])
        self.ln_tile(SZb, SZb[:, :], self.row(0), self.row(1))
        db, dap = dst(j)
        self.load("sp", db, dap, SZb, SZb[:, :])
        self.transpose_tile(SZb, SZb[:, :], j, router_layer=0)
    self.ps = ps_save
    self.xtf = xtf_save


K.even_mixer = even_mixer


def _pipe(n, stages):
    maxs = max(s for s, _ in stages)
    for t in range(n + maxs):
        for s, fn in stages:
            u = t - s
            if 0 <= u < n:
                fn(u)


def _units(NG, hhs=(0, 1)):
    return [(hh, G, idx, i, 4 * (G + 1)) for hh in hhs for G in range(NG)
            for idx, i in enumerate(range(4 * (G + 1) - 1, -1, -1))]


def _sb_pipe(self, hp, QTz, KT, FV, OTSb):
    P2, H2, c = self.P2, self.H2, self.c
    units = _units(self.NG)
    n = len(units)
    E, Ecs = P2[0:3], P2[3:5]
    Lb, Rb = H2[0:2], H2[2]
    Wb = [H2[3], P2[5]]
    Wt = [H2[3][:, :], _bfv(P2[5])[:, 0:512]]
    zb, cb = {}, {}

    def s_z(u):
        hh, G, idx, i, nkb = units[u]
        lo, hi = hh * 64, hh * 64 + 64
        z = self.PS()
        zb[u] = z
        self.mm(z[:, :], KT[:, i * 128:(i + 1) * 128], QTz[hh][:, G * 512:(G + 1) * 512], True, True, [KT, QTz[hh]], [z])

    def s_E(u):
        hh, G, idx, i, nkb = units[u]
        z = zb.pop(u)
        e = E[u % 3]
        self.act(e[:, :], z[:, :], AF.Exp, [z], [e], scale=0.125)
        if i >= 4 * G:
            self.tt("pool", e[:, :], e[:, :], self.maskbuf[:, i - 4 * G, :], ALU.mult, [e, self.maskbuf], [e])

    def s_Lb(u):
        e, l = E[u % 3], Lb[u % 2]
        self.act(l[:, :], e[:, :], AF.Ln, [e], [l], bias=1.0)

    def s_CS(u):
        hh, G, idx, i, nkb = units[u]
        l = Lb[u % 2]
        CS = self.PS()
        cb[u] = CS
        self.mm(CS[:, :], c["uincl"][:, :], l[:, :], True, idx == 0, [c["uincl"], l], [CS])
        if idx > 0:
            self.mm(CS[:, :], c["onesb"][:, :], Rb[:, :], False, True, [c["onesb"], Rb], [CS])
        if idx < nkb - 1:
            if idx == 0:
                self.cp("pool", Rb[:, :], l[:, :], [l], [Rb])
            else:
                self.tt("pool", Rb[:, :], Rb[:, :], l[:, :], ALU.add, [Rb, l], [Rb])

    def s_Ecs(u):
        CS = cb.pop(u)
        ec = Ecs[u % 2]
        self.act(ec[:, :], CS[:, :], AF.Exp, [CS], [ec], scale=-1.0)
        self.tt("dve", Wt[u % 2], E[u % 3][:, :], ec[:, :], ALU.mult, [E[u % 3], ec], [Wb[u % 2]])

    def s_PV(u):
        hh, G, idx, i, nkb = units[u]
        lo, hi = hh * 64, hh * 64 + 64
        OT = self.acc[(hh * self.NG + G) % 2]
        self.mm(OT[:, :], FV[:, i * 128:(i + 1) * 128], Wt[u % 2], idx == 0, idx == nkb - 1, [FV, Wb[u % 2]], [OT])
        if idx == nkb - 1:
            self.cp("act", OTSb[lo:hi, G * 512:(G + 1) * 512], OT[lo:hi, :], [OT], [OTSb])

    _pipe(n, [(0, s_z), (2, s_Lb), (2, s_CS), (3, s_Ecs), (1, s_E), (4, s_PV)])


def _fox_pipe(self, hp, hh_, FQz, FK, FV, FHQs, CSPT, OTb, scale):
    P2, H2, c = self.P2, self.H2, self.c
    units = _units(self.NG, (hh_,))
    n = len(units)
    A = P2[0:3]
    Wb = H2[0:2]
    OT, DEN = self.acc
    zb = {}

    def s_z(u):
        hh, G, idx, i, nkb = units[u]
        lo, hi = hh * 64, hh * 64 + 64
        z = self.PS()
        zb[u] = z
        self.mm(z[:, :], FK[:, i * 128:(i + 1) * 128], FQz[hh][:, G * 512:(G + 1) * 512], True, True, [FK, FQz[hh]], [z])

    def s_A(u):
        hh, G, idx, i, nkb = units[u]
        z = zb.pop(u)
        a = A[u % 3]
        fb = FHQs[hh][G // 2]
        fap = _f32v(fb)[:, (G % 2) * 512:(G % 2 + 1) * 512]
        self.tt("dve", a[:, :], z[:, :], fap, ALU.subtract, [z, fb], [a])
        if i >= 4 * G:
            self.tt("pool", a[:, :], a[:, :], self.maskbuf[:, i - 4 * G, :], ALU.add, [a, self.maskbuf], [a])

    def s_W(u):
        hh, G, idx, i, nkb = units[u]
        h = 2 * hp + hh
        a, w = A[u % 3], Wb[u % 2]
        self.act(w[:, :], a[:, :], AF.Exp, [a, CSPT], [w], bias=CSPT[:, i, h:h + 1], scale=scale)

    def s_PV(u):
        hh, G, idx, i, nkb = units[u]
        lo, hi = hh * 64, hh * 64 + 64
        w = Wb[u % 2]
        self.mm(OT[:, :], FV[:, i * 128:(i + 1) * 128], w[:, :], idx == 0, idx == nkb - 1, [FV, w], [OT])
        self.mm(DEN[:, :], c["onesb"][:, :], w[:, :], idx == 0, idx == nkb - 1, [c["onesb"], w], [DEN])
        if idx == nkb - 1:
            _attn_finish(self, OT, DEN, hh, OTb, OTb[lo:hi, G * 512:(G + 1) * 512])

    _pipe(n, [(0, s_z), (2, s_W), (1, s_A), (3, s_PV)])


def _mla_pipe(self, hp, QN, KN, QP, KPE, VM, OTb, scale):
    P2, H2, c = self.P2, self.H2, self.c
    units = _units(self.NG)
    n = len(units)
    Wf = P2[0:2]
    Wb = H2[0:2]
    OT, DEN = self.acc
    zb = {}

    def s_z(u):
        hh, G, idx, i, nkb = units[u]
        lo, hi = hh * 64, hh * 64 + 64
        plo, phi = hh * 32, hh * 32 + 32
        ksl, gsl = slice(i * 128, (i + 1) * 128), slice(G * 512, (G + 1) * 512)
        z = self.PS()
        zb[u] = z
        self.mm(z[:, :], KN[lo:hi, ksl], QN[lo:hi, gsl], True, False, [KN, QN], [z])
        self.mm(z[:, :], KPE[plo:phi, ksl], QP[plo:phi, gsl], False, True, [KPE, QP], [z])

    def s_W(u):
        hh, G, idx, i, nkb = units[u]
        z = zb.pop(u)
        w = Wb[u % 2]
        if i >= 4 * G:
            wf = Wf[u % 2]
            self.act(wf[:, :], z[:, :], AF.Exp, [z], [wf], scale=scale)
            self.tt("pool", w[:, :], wf[:, :], self.maskbuf[:, i - 4 * G, :], ALU.mult, [wf, self.maskbuf], [w])
        else:
            self.act(w[:, :], z[:, :], AF.Exp, [z], [w], scale=scale)

    def s_PV(u):
        hh, G, idx, i, nkb = units[u]
        h = 2 * hp + hh
        lo, hi = hh * 64, hh * 64 + 64
        w = Wb[u % 2]
        vs = VM[i // 4][:, (i % 4) * 512 + hp * 128:(i % 4) * 512 + (hp + 1) * 128]
        self.mm(OT[:, :], vs, w[:, :], idx == 0, idx == nkb - 1, [VM[i // 4], w], [OT])
        self.mm(DEN[:, :], c["onesb"][:, :], w[:, :], idx == 0, idx == nkb - 1, [c["onesb"], w], [DEN])
        if idx == nkb - 1:
            _attn_finish(self, OT, DEN, hh, OTb, OTb[lo:hi, G * 512:(G + 1) * 512])

    _pipe(n, [(0, s_z), (1, s_W), (2, s_PV)])
```

```python
import math
from contextlib import ExitStack
import numpy as np
import ml_dtypes
import concourse.bass as bass
import concourse.mybir as mybir
from concourse.bass_utils import run_bass_kernel_spmd

F32 = mybir.dt.float32
BF16 = mybir.dt.bfloat16
AF = mybir.ActivationFunctionType
ALU = mybir.AluOpType

D = 1024
NCORES = 8
ALPHA = (2.0 * 2) ** 0.25
LN_EPS = 1e-5
RMS_EPS = 1e-6
EVEN_IN = 4112
ODD_IN = 1960

ENGS = ("pe", "act", "dve", "pool", "sp")


class Buf:
    _n = 0

    def __init__(self, name, t):
        self.name = name
        self.t = t
        self.w = None
        self.r = {}
        self.excl = False
        self.sem = None
        self.semcnt = 0
        Buf._n += 1
        self.id = Buf._n

    def __getitem__(self, k):
        return self.t[k]


class Sched:
    def __init__(self, nc, es):
        self.nc = nc
        self.es = es
        self.ops = {e: [] for e in ENGS}
        self.cnt = {e: 0 for e in ENGS}
        self.known = {e: {} for e in ENGS}
        self.snap = {e: [None] for e in ENGS}
        self.sems = {e: es.enter_context(nc.semaphore("c_" + e)) for e in ENGS}
        self.nsem = len(ENGS)

    def _dma_sem(self, b):
        if b.sem is None:
            b.sem = self.es.enter_context(self.nc.semaphore("d_%d" % b.id))
            self.nsem += 1
        return b.sem

    def _waits(self, eng, reads, writes):
        waits = {}

        def need(ev, same_ok):
            if ev is None:
                return
            k, v = ev
            if k == eng and (same_ok or eng in ("pe", "sp")):
                return
            if v > waits.get(k, 0):
                waits[k] = v
        for b in reads:
            need(b.w, False)
            if b.excl:
                for k, v in b.r.items():
                    need((k, v), True)
        for b in writes:
            need(b.w, False)
            for k, v in b.r.items():
                need((k, v), False)
        kn = self.known[eng]
        out = []
        for k, v in waits.items():
            if kn.get(k, 0) >= v:
                continue
            out.append((k, v))
            kn[k] = v
            if isinstance(k, str):
                sn = self.snap[k][v]
                if sn is not None:
                    for k2, v2 in sn.items():
                        if k2 != eng and kn.get(k2, 0) < v2:
                            kn[k2] = v2
        return out

    def op(self, eng, emit, reads=(), writes=()):
        waits = self._waits(eng, reads, writes)
        self.cnt[eng] += 1
        idx = self.cnt[eng]
        ev = (eng, idx)
        for b in reads:
            b.r[eng] = idx
        for b in writes:
            b.w = ev
            b.r = {}
        self.snap[eng].append({k: v for k, v in self.known[eng].items() if isinstance(k, str)})
        self.ops[eng].append((waits, emit, None))

    def dma(self, q, out_ap, in_ap, dst, src, extra_reads=(), **kw):
        reads = [src] + list(extra_reads)
        waits = self._waits(q, reads, [dst])
        sem = self._dma_sem(dst)
        dst.semcnt += 16
        ev = (("dma", dst.id, sem), dst.semcnt)
        src.r[ev[0]] = ev[1]
        for b in extra_reads:
            b.r[ev[0]] = ev[1]
        dst.w = ev
        dst.r = {}

        def emit(e):
            return e.dma_start(out=out_ap, in_=in_ap, **kw)
        self.ops[q].append((waits, emit, sem))

    def finish_waits(self, eng, bufs):
        waits = self._waits(eng, bufs, [])
        self.ops[eng].append((waits, None, None))

    def emit_all(self):
        nc = self.nc
        handles = {"pe": "tensor", "act": "scalar", "dve": "vector", "pool": "gpsimd", "sp": "sync"}
        with nc.Block() as block:
            for e in ENGS:
                ops = self.ops[e]
                esem = self.sems[e]

                def body(eng, ops=ops, esem=esem):
                    for waits, emit, dsem in ops:
                        for k, v in waits:
                            s = self.sems[k] if isinstance(k, str) else k[2]
                            eng.wait_ge(s, v)
                        if emit is None:
                            continue
                        ins = emit(eng)
                        if dsem is not None:
                            ins.then_inc(dsem, 16)
                        else:
                            ins.then_inc(esem, 1)
                getattr(block, handles[e])(body)


def _consts(S):
    c = {}
    p = np.arange(128)
    c["ident"] = np.eye(128, dtype=np.float32)
    c["identb"] = np.eye(128, dtype=np.float32).astype(ml_dtypes.bfloat16)
    c["onesb"] = np.ones((128, 128), np.float32).astype(ml_dtypes.bfloat16)
    c["uincl"] = (p[:, None] >= p[None, :]).astype(np.float32).astype(ml_dtypes.bfloat16)
    s = p[None, :, None]
    t = np.arange(512)[None, None, :]
    m = np.arange(4)[:, None, None]
    c["mask_sb"] = ((s + 128 * m) < t).astype(np.float32).astype(ml_dtypes.bfloat16)
    c["mask_mla"] = (((s + 128 * m) // 64) <= (t // 64)).astype(np.float32).astype(ml_dtypes.bfloat16)
    c["negm_fox"] = np.where((s + 128 * m) <= t, 0.0, -30000.0).astype(np.float32).astype(ml_dtypes.bfloat16)
    c2 = p // 64
    l = p % 64
    c["tri2"] = ((c2[:, None] == c2[None, :]) & (l[:, None] <= l[None, :])).astype(np.float32)
    c["bd"] = (c2[:, None] == c2[None, :]).astype(np.float32)
    c["lastsel"] = (p[:, None] == (64 * c2[None, :] + 63)).astype(np.float32)
    sl = np.zeros((2, 128, 128), np.float32)
    sl[0, 63, :] = 1.0
    sl[1, 127, :] = 1.0
    c["sellast"] = sl
    c["i64x2"] = (l[:, None] == np.arange(64)[None, :]).astype(np.float32)
    c["trimask"] = (np.arange(64)[None, :] >= l[:, None]).astype(np.float32)
    c["sel8"] = np.repeat(np.eye(8, dtype=np.float32), 128, axis=1)
    ng = np.where(np.arange(64)[None, :] < l[:, None], -30000.0, 0.0).astype(np.float32)
    c["negm_ssd"] = np.tile(ng[:, None, :], (1, 16, 1)).reshape(128, 1024).astype(ml_dtypes.bfloat16)
    inv = 10000.0 ** (-(np.arange(0, 32, 2, dtype=np.float32) / 32.0))
    ang = np.arange(S, dtype=np.float32)[None, :] * inv[:, None]
    cos = np.cos(ang).astype(np.float32)
    sin = np.sin(ang).astype(np.float32)
    cosT = np.concatenate([cos, cos], 0)
    sinT = np.concatenate([-sin, sin], 0)
    c["ropec"] = np.concatenate([cosT, cosT, cosT, cosT], 0).astype(np.float32)
    c["ropes"] = np.concatenate([sinT, sinT, sinT, sinT], 0).astype(np.float32)
    return c


class K:
    def __init__(self, S, NSEQ, layers=(0, 1), parts=("mix", "moe"), taps=()):
        self.S, self.NSEQ = S, NSEQ
        self.NT = S // 128
        self.GW = 512
        self.NG = S // 512
        self.layers, self.parts, self.taps = layers, parts, taps
        self.nc = bass.Bass("TRN2", target_bir_lowering=False)
        self.es = ExitStack()
        self.sc = Sched(self.nc, self.es)
        self.psi = 0
        self.evi = 0
        self.dbg = {}
        self.tap_out = {}

    def dram_in(self, name, shape, dt=F32):
        t = self.nc.dram_tensor(name, list(shape), dt, kind="ExternalInput")
        return Buf(name, t)

    def dram_out(self, name, shape, dt=F32):
        t = self.nc.dram_tensor(name, list(shape), dt, kind="ExternalOutput")
        return Buf(name, t)

    def sb(self, name, shape, dt=F32):
        t = self.es.enter_context(self.nc.sbuf_tensor("s_" + name, list(shape), dt))
        return Buf(name, t)

    def PS(self):
        b = self.ps[self.psi % len(self.ps)]
        self.psi += 1
        return b

    def mm(self, out, lhsT, rhs, start, stop, reads, writes):
        self.sc.op("pe", lambda e: e.matmul(out, lhsT, rhs, start=start, stop=stop), reads, writes)

    def tr(self, out, in_, ident, reads, writes):
        self.sc.op("pe", lambda e: e.transpose(out, in_, ident), reads, writes)

    def act(self, out, in_, func, reads, writes, bias=None, scale=1.0):
        kw = {}
        if bias is not None:
            kw["bias"] = bias
        self.sc.op("act", lambda e: e.activation(out=out, in_=in_, func=func, scale=scale, **kw), reads, writes)

    def tt(self, eng, out, in0, in1, op, reads, writes):
        self.sc.op(eng, lambda e: e.tensor_tensor(out=out, in0=in0, in1=in1, op=op), reads, writes)

    def ts(self, eng, out, in0, s1, s2, op0, op1, reads, writes):
        if op1 is None:
            self.sc.op(eng, lambda e: e.tensor_scalar(out=out, in0=in0, scalar1=s1, scalar2=None, op0=op0), reads, writes)
        else:
            self.sc.op(eng, lambda e: e.tensor_scalar(out=out, in0=in0, scalar1=s1, scalar2=s2, op0=op0, op1=op1), reads, writes)

    def stt(self, out, in0, scalar, in1, op0, op1, reads, writes):
        self.sc.op("dve", lambda e: e.scalar_tensor_tensor(out=out, in0=in0, scalar=scalar, in1=in1, op0=op0, op1=op1), reads, writes)

    def cp(self, eng, out, in_, reads, writes):
        if eng == "act":
            self.sc.op("act", lambda e: e.activation(out=out, in_=in_, func=AF.Copy), reads, writes)
        else:
            self.sc.op(eng, lambda e: e.tensor_copy(out=out, in_=in_), reads, writes)

    def evac(self, out, in_, reads, writes):
        self.evi += 1
        self.cp("act" if self.evi % 2 else "dve", out, in_, reads, writes)

    def load(self, q, dstbuf, out_ap, srcbuf, in_ap, **kw):
        self.sc.dma(q, out_ap, in_ap, dstbuf, srcbuf, **kw)

    def setup(self):
        S, NSEQ, NT = self.S, self.NSEQ, self.NT
        self.x = self.dram_in("x", [NSEQ, S, D])
        self.out = self.dram_out("out", [NSEQ, S, D])
        self.xscr = Buf("xscr", self.nc.dram_tensor("xscr", [S, D], F32, kind="Internal"))
        W = {}
        W["ev_w_in"] = self.dram_in("ev_w_in", [D, EVEN_IN])
        W["ev_w_out"] = self.dram_in("ev_w_out", [1536, D])
        W["od_w_in"] = self.dram_in("od_w_in", [D, ODD_IN + 128])
        W["od_w_q_up"] = self.dram_in("od_w_q_up", [256, 512 + 256 + 256])
        W["od_w_kv_up"] = self.dram_in("od_w_kv_up", [128, 1024])
        W["od_w_out"] = self.dram_in("od_w_out", [D, D])
        W["moe_w_gate"] = self.dram_in("moe_w_gate", [2, 16, D, 256])
        W["moe_w_up"] = self.dram_in("moe_w_up", [2, 16, D, 256])
        W["moe_w_down"] = self.dram_in("moe_w_down", [2, 16, 256, D])
        W["moe_wr"] = self.dram_in("moe_wr", [2, 128, 8, 20])
        W["rows"] = self.dram_in("rows", [NROWS, D])
        W["cols"] = self.dram_in("cols", [128, NCOLS])
        self.W = W
        cs = _consts(S)
        self.cdram = {}
        for k, v in cs.items():
            dt = BF16 if v.dtype == ml_dtypes.bfloat16 else F32
            self.cdram[k] = self.dram_in("c_" + k, v.shape, dt)
        self.slab = [self.sb("slab%d" % i, [128, 2048], BF16) for i in range(28)]
        self.XT = self.slab[0:8]
        self.wslot = [self.sb("wslot%d" % i, [128, 4096], BF16) for i in range(4)]
        self.wi = 0
        self.ps = [Buf("ps%d" % i, self.es.enter_context(self.nc.psum_tensor("ps%d" % i, [128, 512], F32)))
                   for i in range(8)]
        for b in self.ps:
            b.excl = True
        self.acc = self.ps[6:8]
        self.ps = self.ps[0:6]
        c = {}
        for k in ("ident", "identb", "onesb", "uincl", "tri2", "bd", "lastsel", "i64x2", "trimask"):
            v = cs[k]
            c[k] = self.sb("k_" + k, v.shape, BF16 if v.dtype == ml_dtypes.bfloat16 else F32)
            self.load("sp", c[k], c[k][:, :], self.cdram[k], self.cdram[k][:, :])
        c["sellast"] = self.sb("k_sellast", [128, 2, 128])
        for i in range(2):
            self.load("sp", c["sellast"], c["sellast"][:, i, :], self.cdram["sellast"], self.cdram["sellast"][i, :, :])
        self.c = c
        self.rows = self.sb("rows", [128, NROWS_SB, D])
        self.srow = self.sb("srow", [128, 512])
        self.maskbuf = self.sb("maskbuf", [128, 4, 512], BF16)
        self.P2 = [self.sb("p2_%d" % i, [128, 512]) for i in range(8)]
        self.H2 = [self.sb("h2_%d" % i, [128, 512], BF16) for i in range(4)]
        self.cols = self.sb("cols", [128, NCOLS])
        self.load("sp", self.cols, self.cols[:, :], W["cols"], W["cols"][:, :])
        self.wr = self.sb("wr", [128, 2, 8, 20])
        for l in range(2):
            self.load("sp", self.wr, self.wr[:, l, :, :], W["moe_wr"], W["moe_wr"][l, :, :, :])
        self.gate = self.sb("gate", [128, NT, 16])
        self.xres = [self.sb("xres0", [128, D])] * 2
        self.tmpA = [self.sb("tmpA%d" % i, [128, D]) for i in range(2)]
        self.T4 = [Buf("t4_%d" % i, None) for i in range(4)]
        for i in range(4):
            self.T4[i].t = self.slab[24 + i].t
        self.T4 = self.slab[24:28]
        self.small = self.sb("small", [128, 64])
        self.small2 = self.sb("small2", [128, 8])
        self.small2b = self.sb("small2b", [128, 16])
        self.dtb = self.sb("dtb", [128, self.NT, 16])
        self.dab = self.sb("dab", [128, self.NT, 16])
        self.ssm = self.sb("ssm", [128, 80])
        self.ssm2 = self.sb("ssm2", [128, 80])
        self.cbm = self.sb("cbm", [128, 128])
        self.zscr = Buf("zscr", self.nc.dram_tensor("zscr", [S, D], F32, kind="Internal"))
        self.cspt = self.sb("cspt", [128, self.NT, 8])
        self.xi = 0

    def row(self, r):
        return self.rows[:, r, :]

    def load_srow(self, r, off, n):
        src = self.W["rows"][r:r + 1, 0:n].broadcast_to([128, n])
        self.load("sp", self.srow, self.srow[:, off:off + n], self.W["rows"], src)

    def load_mask(self, name):
        cd = self.cdram[name]
        for m_ in range(4):
            self.load("sp", self.maskbuf, self.maskbuf[:, m_, :], cd, cd[m_, :, :])

    def load_rows(self, idx_list):
        for slot, r in enumerate(idx_list):
            src = self.W["rows"][r:r + 1, :].broadcast_to([128, D])
            self.load("sp", self.rows, self.rows[:, slot, :], self.W["rows"], src)

    def transpose_tile(self, xb, xap, j, router_layer=None):
        c = self.c
        for half in range(2):
            ps = self.PS()
            for kk in range(4):
                k = half * 4 + kk
                self.tr(ps[:, kk * 128:(kk + 1) * 128], xap[:, k * 128:(k + 1) * 128], c["ident"][:, :],
                        [xb, c["ident"]], [ps])
            self.evi += 1
            for kk in range(4):
                k = half * 4 + kk
                self.cp("act" if self.evi % 2 else "dve", self.XT[k][:, j * 128:(j + 1) * 128],
                        ps[:, kk * 128:(kk + 1) * 128], [ps], [self.XT[k]])
            if router_layer is not None and not self.dbg.get('noxtf'):
                xtf = self.xtf[half]
                self.cp("dve", xtf[:, :], ps[:, :], [ps], [xtf])
        if router_layer is not None and not self.dbg.get('norouter'):
            self.router(j, router_layer)

    def router(self, j, l):
        sm = self.small
        ps = self.PS()
        for k in range(8):
            xtf = self.xtf[k // 4]
            self.mm(ps[:, 0:20], xtf[:, (k % 4) * 128:(k % 4 + 1) * 128], self.wr[:, l, k, :], k == 0, k == 7,
                    [xtf, self.wr], [ps])
        lg = sm[:, 0:20]
        rb = self.srow[:, 0:20]
        self.tt("dve", lg, ps[:, 0:20], rb, ALU.add, [ps, self.srow], [sm])
        R, Wr = [sm], [sm]
        self.sc.op("dve", lambda e: e.reduce_max(out=sm[:, 20:21], in_=sm[:, 0:4], axis=mybir.AxisListType.X), R, Wr)
        self.ts("dve", sm[:, 21:25], sm[:, 0:4], sm[:, 20:21], None, ALU.is_equal, None, R, Wr)
        self.ts("dve", sm[:, 25:29], sm[:, 0:4], sm[:, 20:21], None, ALU.subtract, None, R, Wr)
        self.act(sm[:, 25:29], sm[:, 25:29], AF.Exp, R, Wr)
        self.sc.op("dve", lambda e: e.reduce_sum(out=sm[:, 29:30], in_=sm[:, 25:29], axis=mybir.AxisListType.X), R, Wr)
        self.sc.op("dve", lambda e: e.reciprocal(out=sm[:, 30:31], in_=sm[:, 29:30]), R, Wr)
        el = sm[:, 4:20].rearrange("p (g e) -> p g e", e=4)
        ohg_b = sm[:, 21:25].unsqueeze(2).broadcast_to([128, 4, 4])
        prod = sm[:, 32:48].rearrange("p (g e) -> p g e", e=4)
        self.tt("dve", prod, el, ohg_b, ALU.mult, R, Wr)
        prod_t = sm[:, 32:48].rearrange("p (g e) -> p e g", e=4)
        self.sc.op("dve", lambda e: e.reduce_sum(out=sm[:, 48:52], in_=prod_t, axis=mybir.AxisListType.X), R, Wr)
        self.sc.op("dve", lambda e: e.reduce_max(out=sm[:, 52:53], in_=sm[:, 48:52], axis=mybir.AxisListType.X), R, Wr)
        self.ts("dve", sm[:, 53:57], sm[:, 48:52], sm[:, 52:53], None, ALU.is_equal, None, R, Wr)
        self.stt(sm[:, 57:61], sm[:, 53:57], -1e30, sm[:, 48:52], ALU.mult, ALU.add, R, Wr)
        self.sc.op("dve", lambda e: e.reduce_max(out=sm[:, 61:62], in_=sm[:, 57:61], axis=mybir.AxisListType.X), R, Wr)
        self.ts("dve", sm[:, 25:29], sm[:, 57:61], sm[:, 61:62], None, ALU.is_equal, None, R, Wr)
        self.ts("dve", sm[:, 62:63], sm[:, 61:62], sm[:, 52:53], None, ALU.subtract, None, R, Wr)
        self.act(sm[:, 62:63], sm[:, 62:63], AF.Exp, R, Wr)
        self.ts("dve", sm[:, 63:64], sm[:, 62:63], 1.0, None, ALU.add, None, R, Wr)
        self.sc.op("dve", lambda e: e.reciprocal(out=sm[:, 63:64], in_=sm[:, 63:64]), R, Wr)
        self.stt(sm[:, 63:64], sm[:, 63:64], 1.0 / ALPHA, sm[:, 30:31], ALU.mult, ALU.mult, R, Wr)
        self.tt("dve", sm[:, 62:63], sm[:, 62:63], sm[:, 63:64], ALU.mult, R, Wr)
        self.ts("dve", sm[:, 53:57], sm[:, 53:57], sm[:, 63:64], None, ALU.mult, None, R, Wr)
        self.stt(sm[:, 53:57], sm[:, 25:29], sm[:, 62:63], sm[:, 53:57], ALU.mult, ALU.add, R, Wr)
        g4_b = sm[:, 53:57].unsqueeze(1).broadcast_to([128, 4, 4])
        gout = self.gate[:, j, :].rearrange("p (g e) -> p g e", e=4)
        self.tt("dve", gout, ohg_b, g4_b, ALU.mult, R, [self.gate])

    def ln_tile(self, xb, xap, g_row, b_row, eps=LN_EPS):
        sm = self.lnsm
        R, Wr = [sm], [sm]
        for h in range(2):
            self.sc.op("dve", lambda e, h=h: e.bn_stats(out=sm[:, 6 * h:6 * h + 6], in_=xap[:, h * 512:(h + 1) * 512]),
                       [xb], Wr)
        self.sc.op("dve", lambda e: e.bn_aggr(out=sm[:, 12:14], in_=sm[:, 0:12]), R, Wr)
        self.ts("dve", sm[:, 14:15], sm[:, 13:14], eps, None, ALU.add, None, R, Wr)
        self.act(sm[:, 14:15], sm[:, 14:15], AF.Ln, R, Wr)
        self.act(sm[:, 15:16], sm[:, 14:15], AF.Exp, R, Wr, scale=-0.5)
        self.stt(xap, xap, sm[:, 12:13], g_row, ALU.subtract, ALU.mult, [xb, sm, self.rows], [xb])
        self.stt(xap, xap, sm[:, 15:16], b_row, ALU.mult, ALU.add, [xb, sm, self.rows], [xb])

    def wnext(self):
        b = self.wslot[self.wi % len(self.wslot)]
        self.wi += 1
        return b

    def wload_k(self, slot, col0, ncols, wbuf, wap2d, nk=8):
        raise NotImplementedError

    def moe(self, l, src, dst, last):
        S, NT, NG = self.S, self.NT, self.NG
        W = self.W
        XA = [self.slab[8 + j] for j in range(NT)]
        xa = [b.t[:, :].bitcast(F32) for b in XA]
        self.load_rows([2 + 4 * l, 3 + 4 * l])
        for j in range(NT):
            sb_, sap = src(j)
            self.load("sp", XA[j], xa[j], sb_, sap)
        hid = self.hid
        hidv = [h.t[:, :].bitcast(BF16)[:, 0:512] for h in hid]
        for e in range(self.dbg.get('nexp', 16)):
            sa = self.wnext()
            sv = sa.t[:, :].rearrange("p (k c) -> p k c", c=512)
            self.load("pool", sa, sv[:, :, 0:256], W["moe_w_gate"],
                      W["moe_w_gate"][l, e, :, :].rearrange("(k p) f -> p k f", p=128))
            self.load("pool", sa, sv[:, :, 256:512], W["moe_w_up"],
                      W["moe_w_up"][l, e, :, :].rearrange("(k p) f -> p k f", p=128))
            sd = self.wnext()
            dv = sd.t[:, 0:2048].rearrange("p (k c) -> p k c", c=1024)
            self.load("pool", sd, dv, W["moe_w_down"],
                      W["moe_w_down"][l, e, :, :].rearrange("(k p) f -> p k f", p=128))
            for g in range(NG):
                tsl = slice(g * 512, (g + 1) * 512)
                for f in range(2):
                    gps = self.PS()
                    for k in range(8):
                        self.mm(gps[:, :], sv[:, k, f * 128:(f + 1) * 128], self.XT[k][:, tsl], k == 0, k == 7,
                                [sa, self.XT[k]], [gps])
                    ups = self.PS()
                    for k in range(8):
                        self.mm(ups[:, :], sv[:, k, 256 + f * 128:256 + (f + 1) * 128], self.XT[k][:, tsl], k == 0, k == 7,
                                [sa, self.XT[k]], [ups])
                    sg = self.sg[f]
                    self.act(sg[:, :], gps[:, :], AF.Silu, [gps], [sg])
                    self.tt("dve", hidv[f], sg[:, :], ups[:, :], ALU.mult, [sg, ups], [hid[f]])
                for tt_ in range(4):
                    j = g * 4 + tt_
                    for half in range(2):
                        ops = self.PS()
                        for f in range(2):
                            self.mm(ops[:, :], hidv[f][:, tt_ * 128:(tt_ + 1) * 128], dv[:, f, half * 512:(half + 1) * 512],
                                    f == 0, f == 1, [hid[f], sd], [ops])
                        xs_ = xa[j][:, half * 512:(half + 1) * 512]
                        self.stt(xs_, ops[:, :], self.gate[:, j, e:e + 1], xs_, ALU.mult, ALU.add,
                                 [ops, self.gate, XA[j]], [XA[j]])
        for j in range(NT):
            if not self.dbg.get('noln'):
                self.ln_tile(XA[j], xa[j], self.row(0), self.row(1), eps=LN_EPS / (ALPHA * ALPHA))
            db, dap = dst(j)
            self.load("sp", db, dap, XA[j], xa[j])
            if not last:
                self.transpose_tile(XA[j], xa[j], j)

    def prologue(self, seq, router_layer=None):
        for j in range(self.NT):
            xb = self.xres[j % 2]
            self.load("sp", xb, xb[:, :], self.x, self.x[seq, j * 128:(j + 1) * 128, :])
            self.transpose_tile(xb, xb[:, :], j, router_layer)

    def alloc_misc(self):
        self.xtf = self.P2[0:2]
        self.lnsm = self.sb("lnsm", [128, 16])
        self.sg = self.P2[2:4]
        self.hid = self.P2[4:6]

    def run(self):
        self.setup()
        self.alloc_misc()
        NT = self.NT
        for seq in range(self.NSEQ):
            def src_x(j, seq=seq):
                return self.x, self.x[seq, j * 128:(j + 1) * 128, :]

            def src_scr(j):
                return self.xscr, self.xscr[j * 128:(j + 1) * 128, :]

            def dst_out(j, seq=seq):
                return self.out, self.out[seq, j * 128:(j + 1) * 128, :]
            cur = src_x
            first = True
            subl = [(l, p) for l in self.layers for p in self.parts]
            for i, (l, p) in enumerate(subl):
                last = i == len(subl) - 1
                dst = dst_out if last else src_scr
                if p == "moe":
                    if first:
                        self.load_srow(9 + l, 0, 20)
                        self.prologue(seq, router_layer=l)
                    self.moe(l, cur, dst, last)
                else:
                    if first:
                        self.prologue(seq)
                    if l == 0:
                        self.even_mixer(cur, dst)
                    else:
                        self.odd_mixer(cur, dst)
                cur = src_scr
                first = False
        self.sc.finish_waits("sp", [self.out])
        self.sc.emit_all()
        return self.nc


NROWS = 16
NROWS_SB = 3
NCOLS = 64


def _host_inputs(S, inputs):
    f = lambda a: np.ascontiguousarray(np.asarray(a, dtype=np.float32))
    m = {}
    m["ev_w_in"] = f(inputs["ev_w_in"][0])
    m["ev_w_out"] = f(inputs["ev_w_out"][0])
    wi = f(inputs["od_w_in"][0])
    kpe = wi[:, 384:416]
    kpe_sw = np.concatenate([kpe[:, 16:32], kpe[:, 0:16]], 1)
    m["od_w_in"] = np.ascontiguousarray(np.concatenate([wi, kpe, kpe, kpe_sw, kpe_sw], 1))
    wq = f(inputs["od_w_q_up"][0]).reshape(256, 8, 96)
    nope = wq[:, :, :64].reshape(256, 512)
    pe = wq[:, :, 64:]
    pe_sw = np.concatenate([pe[:, :, 16:], pe[:, :, :16]], 2)
    m["od_w_q_up"] = np.ascontiguousarray(np.concatenate([nope, pe.reshape(256, 256), pe_sw.reshape(256, 256)], 1))
    wkv = f(inputs["od_w_kv_up"][0]).reshape(128, 8, 128)
    m["od_w_kv_up"] = np.ascontiguousarray(np.concatenate([wkv[:, :, :64].reshape(128, 512), wkv[:, :, 64:].reshape(128, 512)], 1))
    m["od_w_out"] = f(inputs["od_w_out"][0])
    m["moe_w_gate"] = f(inputs["moe_w_gate"])
    m["moe_w_up"] = f(inputs["moe_w_up"])
    m["moe_w_down"] = f(inputs["moe_w_down"])
    wr = np.concatenate([f(inputs["moe_w_group"]), f(inputs["moe_w_expert"])], 2)
    m["moe_wr"] = np.ascontiguousarray(wr.reshape(2, 8, 128, 20).transpose(0, 2, 1, 3))
    rows = np.zeros((NROWS, D), np.float32)
    for l in range(2):
        rows[4 * l + 0] = inputs["ln1_g"][l]
        rows[4 * l + 1] = inputs["ln1_b"][l]
        rows[4 * l + 2] = inputs["ln2_g"][l]
        rows[4 * l + 3] = inputs["ln2_b"][l]
        rows[9 + l, 0:4] = inputs["moe_b_group"][l]
        rows[9 + l, 4:20] = inputs["moe_b_expert"][l]
    rows[8] = inputs["ev_norm_g"][0]
    rows[11, 0:16] = inputs["ev_dt_bias"][0]
    rows[11, 16:32] = inputs["ev_a_log"][0]
    rows[11, 32:48] = inputs["ev_d_skip"][0]
    rows[12, 0:256] = inputs["od_q_norm_g"][0]
    rows[12, 256:384] = inputs["od_kv_norm_g"][0]
    rows[13, 0:8] = inputs["od_f_bias"][0]
    m["rows"] = rows
    cols = np.zeros((128, NCOLS), np.float32)
    cw = f(inputs["ev_conv_w"][0])
    cols[:, 0:48] = cw.T.reshape(12, 128, 4).transpose(1, 0, 2).reshape(128, 48)
    cols[:, 48:60] = f(inputs["ev_conv_b"][0]).reshape(12, 128).T
    cols[0:8, 60] = f(inputs["od_f_bias"][0])
    m["cols"] = cols
    for k, v in _consts(S).items():
        m["c_" + k] = v
    return m


_CACHE = {}


def kernel(**inputs):
    x = np.ascontiguousarray(np.asarray(inputs["x"], dtype=np.float32))
    B, S, _ = x.shape
    nseq = B // NCORES
    key = (S, nseq)
    if key not in _CACHE:
        _CACHE[key] = K(S, nseq).run()
    nc = _CACHE[key]
    shared = _host_inputs(S, inputs)
    in_maps = []
    for c in range(NCORES):
        m = dict(shared)
        m["x"] = np.ascontiguousarray(x[c * nseq:(c + 1) * nseq])
        in_maps.append(m)
    res = run_bass_kernel_spmd(nc, in_maps, core_ids=list(range(NCORES)))
    return np.concatenate([np.asarray(r["out"]) for r in res.results], axis=0).astype(np.float32)


def _f32v(b):
    return b.t[:, :].bitcast(F32)


def _bfv(b):
    return b.t[:, :].bitcast(BF16)


def _wview(slot, c):
    return slot.t[:, :].rearrange("p (k c) -> p k c", c=c)


def _kp(ap2d):
    return ap2d.rearrange("(k p) c -> p k c", p=128)


def _rope_tables(self, g):
    rc, rs = self.P2[3], self.P2[4]
    cc, cs_ = self.cdram["ropec"], self.cdram["ropes"]
    self.load("sp", rc, rc[:, :], cc, cc[0:128, g * 512:(g + 1) * 512])
    self.load("sp", rs, rs[:, :], cs_, cs_[0:128, g * 512:(g + 1) * 512])
    return rc, rs


def _rope(self, A, B, rc, rs, dst, dst_ap, lo=0, hi=64):
    t1, t2 = self.P2[5], self.P2[6]
    self.tt("dve", t1[lo:hi, :], A[lo:hi, :], rc[lo:hi, :], ALU.mult, [A, rc], [t1])
    self.tt("dve", t2[lo:hi, :], B[lo:hi, :], rs[lo:hi, :], ALU.mult, [B, rs], [t2])
    self.tt("pool", dst_ap, t1[lo:hi, :], t2[lo:hi, :], ALU.add, [t1, t2], [dst])


def _attn_finish(self, OT, DEN, hh, dst, dst_ap):
    rec = self.P2[6]
    lo, hi = hh * 64, hh * 64 + 64
    self.sc.op("dve", lambda e: e.reciprocal(out=rec[lo:hi, :], in_=DEN[lo:hi, :]), [DEN], [rec])
    self.tt("dve", dst_ap, OT[lo:hi, :], rec[lo:hi, :], ALU.mult, [OT, rec], [dst])


def odd_mixer(self, src, dst):
    S, NT, NG = self.S, self.NT, self.NG
    W = self.W
    Win = W["od_w_in"]
    sl = self.slab
    P2 = self.P2
    XT = self.XT
    c = self.c
    sc_mla = 1.0 / math.sqrt(96.0)
    sc_fox = 1.0 / 8.0
    self.load_rows([4, 5])
    self.load_srow(10, 0, 20)
    self.load_srow(12, 96, 384)
    CQNT, CKVNT, KPE = sl[8:10], sl[10], sl[11]
    OTF = sl[12:16]
    sm = self.small
    wl = self.wnext()
    wlv = _wview(wl, 512)
    self.load("pool", wl, wlv[:, :, 0:384], Win, _kp(Win[:, 0:384]))
    self.load("pool", wl, wlv[:, :, 384:512], Win, _kp(Win[:, 1960:2088]))
    lat, sq, latn_b = P2[0], P2[1], P2[2]
    latn = _bfv(latn_b)
    for j in range(NT):
        tsl = slice(j * 128, (j + 1) * 128)
        ps = self.PS()
        for k in range(8):
            self.mm(ps[:, 0:384], XT[k][:, tsl], wlv[:, k, 0:384], k == 0, k == 7, [XT[k], wl], [ps])
        self.cp("act", lat[:, 0:384], ps[:, 0:384], [ps], [lat])
        self.tt("pool", sq[:, 0:384], lat[:, 0:384], lat[:, 0:384], ALU.mult, [lat], [sq])
        self.sc.op("dve", lambda e: e.reduce_sum(out=sm[:, 0:1], in_=sq[:, 0:256], axis=mybir.AxisListType.X), [sq], [sm])
        self.sc.op("dve", lambda e: e.reduce_sum(out=sm[:, 1:2], in_=sq[:, 256:384], axis=mybir.AxisListType.X), [sq], [sm])
        self.ts("dve", sm[:, 0:1], sm[:, 0:1], 1.0 / 256, RMS_EPS, ALU.mult, ALU.add, [sm], [sm])
        self.ts("dve", sm[:, 1:2], sm[:, 1:2], 1.0 / 128, RMS_EPS, ALU.mult, ALU.add, [sm], [sm])
        self.act(sm[:, 0:2], sm[:, 0:2], AF.Ln, [sm], [sm])
        self.act(sm[:, 2:4], sm[:, 0:2], AF.Exp, [sm], [sm], scale=-0.5)
        self.stt(latn[:, 0:256], lat[:, 0:256], sm[:, 2:3], self.srow[:, 96:352], ALU.mult, ALU.mult,
                 [lat, sm, self.srow], [latn_b])
        self.stt(latn[:, 256:384], lat[:, 256:384], sm[:, 3:4], self.srow[:, 352:480], ALU.mult, ALU.mult,
                 [lat, sm, self.srow], [latn_b])
        pb = self.PS()
        pbv = _bfv(pb)
        for i in range(3):
            self.tr(pbv[:, i * 128:(i + 1) * 128], latn[:, i * 128:(i + 1) * 128], c["identb"][:, :], [latn_b, c["identb"]], [pb])
        for i, dstb in enumerate((CQNT[0], CQNT[1], CKVNT)):
            self.cp("dve", dstb[:, tsl], pbv[:, i * 128:(i + 1) * 128], [pb], [dstb])
    for g in range(NG):
        gsl = slice(g * 512, (g + 1) * 512)
        rc, rs = _rope_tables(self, g)
        A = self.PS()
        for k in range(8):
            self.mm(A[64:96, :], wlv[:, k, 384:416], XT[k][:, gsl], k == 0, k == 7, [wl, XT[k]], [A])
        B = self.PS()
        for k in range(8):
            self.mm(B[64:96, :], wlv[:, k, 448:480], XT[k][:, gsl], k == 0, k == 7, [wl, XT[k]], [B])
        _rope(self, A, B, rc, rs, KPE, KPE[64:96, gsl], 64, 96)
    wf = self.wnext()
    wfv = wf.t[:, 0:64].rearrange("p (k c) -> p k c", c=8)
    self.load("pool", wf, wfv, Win, _kp(Win[:, 1952:1960]))
    ones = P2[7]
    self.sc.op("pool", lambda e: e.memset(ones[:, :], 1.0), [], [ones])
    negfb = self.small2
    self.ts("pool", negfb[0:8, 0:1], self.cols[0:8, 60:61], -1.0, None, ALU.mult, None, [self.cols], [negfb])
    CSPb = [self.T4[0], self.T4[1]]
    FHQb = [self.T4[2], self.T4[3]]

    def cspg(g):
        return CSPb[g // 2], _f32v(CSPb[g // 2])[:, (g % 2) * 512:(g % 2 + 1) * 512]

    def fhqg(g):
        return FHQb[g // 2], _f32v(FHQb[g // 2])[:, (g % 2) * 512:(g % 2 + 1) * 512]
    CSPT = self.cspt
    for g in range(NG):
        gsl = slice(g * 512, (g + 1) * 512)
        ps = self.PS()
        for k in range(8):
            self.mm(ps[0:8, :], wfv[:, k, :], XT[k][:, gsl], k == 0, k == 7, [wf, XT[k]], [ps])
        e_, sp_ = P2[5], P2[6]
        self.act(e_[0:8, :], ps[0:8, :], AF.Exp, [ps, negfb], [e_], bias=negfb[0:8, 0:1], scale=-1.0)
        self.act(sp_[0:8, :], e_[0:8, :], AF.Ln, [e_], [sp_], bias=1.0)
        cb, cap = cspg(g)
        if g == 0:
            init, rds = 0.0, [ones, sp_]
        else:
            pb_, pap = cspg(g - 1)
            init, rds = pap[0:8, 511:512], [ones, sp_, pb_]
        self.sc.op("dve", lambda e, cap=cap, init=init: e.tensor_tensor_scan(
            out=cap[0:8, :], data0=ones[0:8, :], data1=sp_[0:8, :], initial=init, op0=ALU.mult, op1=ALU.add), rds, [cb])
        for tt_ in range(4):
            j = g * 4 + tt_
            pt = self.PS()
            self.tr(pt[:, 0:8], cap[0:8, tt_ * 128:(tt_ + 1) * 128], c["ident"][0:8, 0:8], [cb, c["ident"]], [pt])
            self.cp("dve", CSPT[:, j, :], pt[:, 0:8], [pt], [CSPT])
    self.load_mask("negm_fox")
    OT, DEN = self.acc
    for hp in range(4):
        q4 = sl[16 + 4 * (hp % 2):20 + 4 * (hp % 2)]
        FQz, FK, FV = q4[0:2], q4[2], q4[3]
        if hp < 2:
            self.sc.op("pool", lambda e, b=FQz[0]: e.memset(b[64:128, :], 0.0), [], [FQz[0]])
            self.sc.op("pool", lambda e, b=FQz[1]: e.memset(b[0:64, :], 0.0), [], [FQz[1]])
        wq = self.wnext()
        wqv = _wview(wq, 512)
        for i, c0 in enumerate((416, 928, 1440)):
            self.load("pool", wq, wqv[:, :, i * 128:(i + 1) * 128], Win, _kp(Win[:, c0 + hp * 128:c0 + (hp + 1) * 128]))
        for g in range(NG):
            gsl = slice(g * 512, (g + 1) * 512)
            for i in range(2):
                ps = self.PS()
                for k in range(8):
                    self.mm(ps[:, :], wqv[:, k, i * 128:(i + 1) * 128], XT[k][:, gsl], k == 0, k == 7, [wq, XT[k]], [ps])
                if i == 0:
                    self.cp("act", FQz[0][0:64, gsl], ps[0:64, :], [ps], [FQz[0]])
                    self.cp("act", FQz[1][64:128, gsl], ps[64:128, :], [ps], [FQz[1]])
                else:
                    self.cp("dve", FK[:, gsl], ps[:, :], [ps], [FK])
        for j in range(NT):
            tsl = slice(j * 128, (j + 1) * 128)
            ps = self.PS()
            for k in range(8):
                self.mm(ps[:, 0:128], XT[k][:, tsl], wqv[:, k, 256:384], k == 0, k == 7, [wq, XT[k]], [ps])
            self.evac(FV[:, tsl], ps[:, 0:128], [ps], [FV])
        FHQs = [[self.T4[2], self.T4[3]], [self.T4[2], self.T4[3]]]
        for hh in range(2):
            h = 2 * hp + hh
            for g in range(NG):
                cb, cap = cspg(g)
                m_ = P2[5]
                self.ts("dve", m_[0:8, :], cap[0:8, :], c["ident"][0:8, h:h + 1], 1.0 / sc_fox, ALU.mult, ALU.mult, [cb, c["ident"]], [m_])
                ps = self.PS()
                self.mm(ps[:, :], ones[0:8, 0:128], m_[0:8, :], True, True, [ones, m_], [ps])
                fb = FHQs[hh][g // 2]
                self.cp("act", _f32v(fb)[:, (g % 2) * 512:(g % 2 + 1) * 512], ps[:, :], [ps], [fb])
            _fox_pipe(self, hp, hh, FQz, FK, FV, FHQs, CSPT, OTF[hp], sc_fox)
    self.load_mask("mask_mla")
    wm = self.wnext()
    wq_ = wm.t[:, 0:2048].rearrange("p (k c) -> p k c", c=1024)
    wkv = wm.t[:, 2048:3072]
    self.load("pool", wm, wq_, W["od_w_q_up"], _kp(W["od_w_q_up"][:, :]))
    self.load("pool", wm, wkv, W["od_w_kv_up"], W["od_w_kv_up"][:, :])
    VM = XT[0:4]
    OTM = XT[4:8]
    for j in range(NT):
        ps = self.PS()
        self.mm(ps[:, :], CKVNT[:, j * 128:(j + 1) * 128], wkv[:, 512:1024], True, True, [CKVNT, wm], [ps])
        self.evac(VM[j // 4][:, (j % 4) * 512:(j % 4 + 1) * 512], ps[:, :], [ps], [VM[j // 4]])
    for hp in range(4):
        q4 = sl[16 + 4 * (hp % 2):20 + 4 * (hp % 2)]
        QH, KH = q4[0:2], q4[2:4]
        if hp < 2:
            for b in q4:
                self.sc.op("pool", lambda e, b=b: e.memset(b[64:128, :], 0.0), [], [b])
        for hh in range(2):
            h = 2 * hp + hh
            for g in range(NG):
                gsl = slice(g * 512, (g + 1) * 512)
                ps = self.PS()
                for kk in range(2):
                    self.mm(ps[0:64, :], wq_[:, kk, h * 64:(h + 1) * 64], CQNT[kk][:, gsl], kk == 0, kk == 1, [wm, CQNT[kk]], [ps])
                self.evac(QH[hh][0:64, gsl], ps[0:64, :], [ps], [QH[hh]])
                ps = self.PS()
                self.mm(ps[0:64, :], wkv[:, h * 64:(h + 1) * 64], CKVNT[:, gsl], True, True, [wm, CKVNT], [ps])
                self.evac(KH[hh][0:64, gsl], ps[0:64, :], [ps], [KH[hh]])
                rc, rs = _rope_tables(self, g)
                A = self.PS()
                for kk in range(2):
                    self.mm(A[64:96, :], wq_[:, kk, 512 + h * 32:512 + (h + 1) * 32], CQNT[kk][:, gsl], kk == 0, kk == 1, [wm, CQNT[kk]], [A])
                B = self.PS()
                for kk in range(2):
                    self.mm(B[64:96, :], wq_[:, kk, 768 + h * 32:768 + (h + 1) * 32], CQNT[kk][:, gsl], kk == 0, kk == 1, [wm, CQNT[kk]], [B])
                _rope(self, A, B, rc, rs, QH[hh], QH[hh][64:96, gsl], 64, 96)
            self.cp("act", KH[hh][64:96, 0:S], KPE[64:96, 0:S], [KPE], [KH[hh]])
        _mla_pipe(self, hp, QH, KH, VM, OTM[hp], sc_mla)
    wo = [self.wnext(), self.wnext()]
    wov = [_wview(w_, 1024) for w_ in wo]
    Wo = W["od_w_out"]
    for i in range(2):
        self.load("pool", wo[i], wov[i], Wo, _kp(Wo[i * 512:(i + 1) * 512, :]))
    cat = list(OTM) + list(OTF)
    for j in range(NT):
        tsl = slice(j * 128, (j + 1) * 128)
        xr = self.xres[j % 2]
        sb_, sap = src(j)
        self.load("sp", xr, xr[:, :], sb_, sap)
        y = self.tmpA[j % 2]
        for half in range(2):
            hs = slice(half * 512, (half + 1) * 512)
            ps = self.PS()
            for kc in range(8):
                self.mm(ps[:, :], cat[kc][:, tsl], wov[kc // 4][:, kc % 4, hs], kc == 0, kc == 7, [cat[kc], wo[kc // 4]], [ps])
            self.stt(y[:, hs], xr[:, hs], ALPHA, ps[:, :], ALU.mult, ALU.add, [xr, ps], [y])
        self.ln_tile(y, y[:, :], self.row(0), self.row(1))
        db, dap = dst(j)
        self.load("sp", db, dap, y, y[:, :])
        self.transpose_tile(y, y[:, :], j, router_layer=1)


K.odd_mixer = odd_mixer


def _v3(ap, inner):
    return ap.rearrange("p (a b) -> p a b", b=inner)


def _bc(ap2, n):
    return ap2.unsqueeze(2).broadcast_to([ap2.shape[0], ap2.shape[1], n])


def even_mixer(self, src, dst):
    S, NT, NG = self.S, self.NT, self.NG
    W = self.W
    Win = W["ev_w_in"]
    sl = self.slab
    P2 = self.P2
    XT = self.XT
    c = self.c
    sm = self.small
    X = mybir.AxisListType.X
    self.load_rows([0, 1, 8])
    self.load_srow(9, 0, 20)
    self.load_srow(11, 32, 48)
    OTS = sl[12:16]
    self.load_mask("mask_sb")
    OTa = self.acc
    for hp in range(4):
        q4 = sl[16 + 4 * (hp % 2):20 + 4 * (hp % 2)]
        QTz, KT, FV = q4[0:2], q4[2], q4[3]
        if hp < 2:
            self.sc.op("pool", lambda e, b=QTz[0]: e.memset(b[64:128, :], 0.0), [], [QTz[0]])
            self.sc.op("pool", lambda e, b=QTz[1]: e.memset(b[0:64, :], 0.0), [], [QTz[1]])
        wq = self.wnext()
        wqv = _wview(wq, 512)
        for i, c0 in enumerate((2576, 3088, 3600)):
            self.load("pool", wq, wqv[:, :, i * 128:(i + 1) * 128], Win, _kp(Win[:, c0 + hp * 128:c0 + (hp + 1) * 128]))
        for g in range(NG):
            gsl = slice(g * 512, (g + 1) * 512)
            for i in range(2):
                ps = self.PS()
                for k in range(8):
                    self.mm(ps[:, :], wqv[:, k, i * 128:(i + 1) * 128], XT[k][:, gsl], k == 0, k == 7, [wq, XT[k]], [ps])
                if i == 0:
                    self.cp("act", QTz[0][0:64, gsl], ps[0:64, :], [ps], [QTz[0]])
                    self.cp("act", QTz[1][64:128, gsl], ps[64:128, :], [ps], [QTz[1]])
                else:
                    self.cp("dve", KT[:, gsl], ps[:, :], [ps], [KT])
        for j in range(NT):
            tsl = slice(j * 128, (j + 1) * 128)
            ps = self.PS()
            for k in range(8):
                self.mm(ps[:, 0:128], XT[k][:, tsl], wqv[:, k, 256:384], k == 0, k == 7, [wq, XT[k]], [ps])
            self.evac(FV[:, tsl], ps[:, 0:128], [ps], [FV])
        _sb_pipe(self, hp, QTz, KT, FV, OTS[hp])
    ps_save = self.ps
    xtf_save = self.xtf
    self.xtf = [P2[0], P2[3]]
    allps = list(self.ps) + list(self.acc)
    self.ps = allps[0:4]
    YD, YO = allps[4:6], allps[6:8]
    XS_T = list(sl[8:12]) + list(sl[16:20])
    BT, CT = sl[20:22], sl[22:24]
    T4 = self.T4
    dests = XS_T + list(BT) + list(CT)
    for pnl in range(3):
        wp = self.wnext()
        wpv = _wview(wp, 512)
        self.load("pool", wp, wpv, Win, _kp(Win[:, 1024 + pnl * 512:1024 + (pnl + 1) * 512]))
        for q in range(4):
            cc = pnl * 4 + q
            for g in range(NG):
                gsl = slice(g * 512, (g + 1) * 512)
                ps = self.PS()
                for k in range(8):
                    self.mm(ps[:, :], wpv[:, k, q * 128:(q + 1) * 128], XT[k][:, gsl], k == 0, k == 7, [wp, XT[k]], [ps])
                RAWb = T4[g % 2]
                RAW = _bfv(RAWb)
                if g == 0:
                    self.sc.op("pool", lambda e, RAW=RAW: e.memset(RAW[:, 0:3], 0.0), [], [RAWb])
                else:
                    prev = _bfv(T4[(g - 1) % 2])
                    self.cp("pool", RAW[:, 0:3], prev[:, 512:515], [T4[(g - 1) % 2]], [RAWb])
                self.cp("act", RAW[:, 3:515], ps[:, :], [ps], [RAWb])
                DGb = T4[2 + cc % 2]
                DG = _bfv(DGb)
                if g == 0:
                    for tap in range(4):
                        self.ts("dve", DG[:, tap * 128:(tap + 1) * 128], c["identb"][:, :], self.cols[:, cc * 4 + tap:cc * 4 + tap + 1],
                                None, ALU.mult, None, [c["identb"], self.cols], [DGb])
                cps = self.PS()
                for tap in range(4):
                    self.mm(cps[:, :], DG[:, tap * 128:(tap + 1) * 128], RAW[:, tap:tap + 512], tap == 0, tap == 3, [DGb, RAWb], [cps])
                self.act(dests[cc][:, gsl], cps[:, :], AF.Silu, [cps, self.cols], [dests[cc]], bias=self.cols[:, 48 + cc:49 + cc])
    wd = self.wnext()
    wdv = wd.t[:, 0:128].rearrange("p (k c) -> p k c", c=16)
    self.load("pool", wd, wdv, Win, _kp(Win[:, 2560:2576]))
    DT, DA = self.dtb, self.dab
    AROW = self.small2b
    self.act(AROW[:, 0:16], self.srow[:, 48:64], AF.Exp, [self.srow], [AROW])
    self.ts("pool", AROW[:, 0:16], AROW[:, 0:16], -1.0, None, ALU.mult, None, [AROW], [AROW])
    for j in range(NT):
        tsl = slice(j * 128, (j + 1) * 128)
        ps = self.PS()
        for k in range(8):
            self.mm(ps[:, 0:16], XT[k][:, tsl], wdv[:, k, :], k == 0, k == 7, [XT[k], wd], [ps])
        self.tt("dve", sm[:, 0:16], ps[:, 0:16], self.srow[:, 32:48], ALU.add, [ps, self.srow], [sm])
        self.act(sm[:, 0:16], sm[:, 0:16], AF.Exp, [sm], [sm])
        self.act(DT[:, j, :], sm[:, 0:16], AF.Ln, [sm], [DT], bias=1.0)
        self.tt("dve", DA[:, j, :], DT[:, j, :], AROW[:, 0:16], ALU.mult, [DT, AROW], [DA])
    for half in range(2):
        wz = self.wnext()
        wzv = _wview(wz, 512)
        self.load("pool", wz, wzv, Win, _kp(Win[:, half * 512:(half + 1) * 512]))
        for j in range(NT):
            tsl = slice(j * 128, (j + 1) * 128)
            ps = self.PS()
            for k in range(8):
                self.mm(ps[:, :], XT[k][:, tsl], wzv[:, k, :], k == 0, k == 7, [XT[k], wz], [ps])
            szb = self.tmpA[j % 2]
            self.act(szb[:, 0:512], ps[:, :], AF.Silu, [ps], [szb])
            self.load("sp", self.zscr, self.zscr[j * 128:(j + 1) * 128, half * 512:(half + 1) * 512], szb, szb[:, 0:512])
    wo = [self.wnext(), self.wnext(), self.wnext()]
    wov = [_wview(w_, 1024) for w_ in wo]
    Wo = W["ev_w_out"]
    for i in range(3):
        self.load("pool", wo[i], wov[i], Wo, _kp(Wo[i * 512:(i + 1) * 512, :]))
    XSTOKb, SEGb, GTBb = T4[0], T4[1], T4[3]
    Dgb = SEGb
    XSTOK, SEG = _f32v(XSTOKb), _f32v(SEGb)
    Dg = SEG
    GTB = _bfv(GTBb)
    XSPb, YNb = P2[0], P2[3]
    XSP, YN = _bfv(XSPb), _bfv(YNb)
    XSPPb = [P2[1], P2[2]]
    XSPP = [_bfv(b) for b in XSPPb]
    BTOKb = [self.H2[2], self.H2[3]]
    HT = [P2[5], P2[6]]
    HTbb = P2[7]
    HTb = [_bfv(HTbb)[:, 0:512], _bfv(HTbb)[:, 512:1024]]
    YNTh = [self.H2[0], self.H2[1]]
    TMPb = P2[4]
    self.xtf = [P2[4], P2[3]]
    Y0b = [self.tmpA[1], T4[2]]
    Y0 = [self.tmpA[1][:, :], _f32v(T4[2])]
    ssms = [self.ssm, self.ssm2]
    CBM = self.cbm
    self.sc.op("pool", lambda e: e.memset(GTB, 0.0), [], [GTBb])
    for g in range(2):
        self.sc.op("pool", lambda e, g=g: e.memset(HT[g][:, :], 0.0), [], [HT[g]])
    self.sc.op("pool", lambda e: e.memset(_bfv(HTbb), 0.0), [], [HTbb])

    def stage_a(j):
        tsl = slice(j * 128, (j + 1) * 128)
        ssm = ssms[j % 2]
        ACUM, EA, DTW = ssm[:, 0:16], ssm[:, 16:32], ssm[:, 32:48]
        DEC = [ssm[:, 48:64], ssm[:, 64:80]]
        BTOK = BTOKb[j % 2]
        pb = self.PS()
        pbv = _bfv(pb)
        for cc in range(8):
            self.tr(pbv[:, cc * 128:(cc + 1) * 128], XS_T[cc][:, tsl], c["identb"][:, :], [XS_T[cc], c["identb"]], [pb])
        self.cp("act", XSTOK, pbv, [pb], [XSTOKb])
        pb2 = self.PS()
        pb2v = _bfv(pb2)
        for g in range(2):
            self.tr(pb2v[:, g * 128:(g + 1) * 128], BT[g][:, tsl], c["identb"][:, :], [BT[g], c["identb"]], [pb2])
        self.cp("dve", BTOK[:, 0:256], pb2v[:, 0:256], [pb2], [BTOK])
        ps = self.PS()
        self.mm(ps[:, 0:16], c["tri2"][:, :], DA[:, j, :], True, True, [c["tri2"], DA], [ps])
        self.cp("dve", ACUM, ps[:, 0:16], [ps], [ssm])
        self.act(EA, ACUM, AF.Exp, [ssm], [ssm])
        ps = self.PS()
        self.mm(ps[:, 0:16], c["lastsel"][:, :], ACUM, True, True, [c["lastsel"], ssm], [ps])
        self.tt("dve", DTW, ps[:, 0:16], ACUM, ALU.subtract, [ps, ssm], [ssm])
        self.act(DTW, DTW, AF.Exp, [ssm], [ssm])
        self.tt("dve", DTW, DTW, DT[:, j, :], ALU.mult, [ssm, DT], [ssm])
        for c2 in range(2):
            ps = self.PS()
            self.mm(ps[:, 0:16], c["sellast"][:, c2, :], ACUM, True, True, [c["sellast"], ssm], [ps])
            self.act(DEC[c2], ps[:, 0:16], AF.Exp, [ps], [ssm])
        self.tt("dve", _v3(Dg, 64), _bc(ACUM, 64), c["i64x2"][:, :].unsqueeze(1).broadcast_to([128, 16, 64]), ALU.mult,
                [ssm, c["i64x2"]], [Dgb])
        p1s = []
        for hb in range(2):
            p1 = self.PS()
            p1s.append(p1)
            self.mm(p1[:, :], c["bd"][:, :], Dg[:, hb * 512:(hb + 1) * 512], True, True, [c["bd"], Dgb], [p1])
        for hb in range(2):
            p1 = p1s[hb]
            self.tt("dve", _v3(SEG[:, hb * 512:(hb + 1) * 512], 64), _v3(p1[:, :], 64), _bc(ssm[:, hb * 8:hb * 8 + 8], 64),
                    ALU.subtract, [p1, ssm], [SEGb])
        self.ts("pool", SEG, SEG, 0.0, None, ALU.min, None, [SEGb], [SEGb])
        self.act(SEG, SEG, AF.Exp, [SEGb], [SEGb])
        ps = self.PS()
        for c2 in range(2):
            csl = slice(j * 128 + c2 * 64, j * 128 + c2 * 64 + 64)
            for g in range(2):
                self.sc.op("pe", lambda e, ps=ps, c2=c2, g=g, csl=csl: e.matmul(
                    ps[c2 * 64:c2 * 64 + 64, g * 64:(g + 1) * 64], BT[g][:, csl], CT[g][:, csl], start=True, stop=True,
                    skip_group_check=True), [BT[g], CT[g]], [ps])
        self.tt("dve", _v3(CBM[:, :], 64), _v3(ps[:, 0:128], 64), c["trimask"][:, :].unsqueeze(1).broadcast_to([128, 2, 64]),
                ALU.mult, [ps, c["trimask"]], [CBM])
        for c2 in range(2):
            lo, hi = c2 * 64, c2 * 64 + 64
            out_ap = GTB[lo:hi, :].rearrange("p (h x) -> p h x", x=128)[:, :, lo:hi].rearrange("p (g r) l -> p g r l", g=2)
            in0 = SEG[lo:hi, :].rearrange("p (g r l) -> p g r l", g=2, r=8)
            in1 = _v3(CBM[lo:hi, :], 64).unsqueeze(2).broadcast_to([64, 2, 8, 64])
            self.tt("dve", out_ap, in0, in1, ALU.mult, [SEGb, CBM], [GTBb])
        self.tt("pool", _v3(XSP, 64), _v3(XSTOK, 64), _bc(DT[:, j, :], 64), ALU.mult, [XSTOKb, DT], [XSPb])
        self.tt("pool", _v3(XSPP[j % 2], 64), _v3(XSTOK, 64), _bc(DTW, 64), ALU.mult, [XSTOKb, ssm], [XSPPb[j % 2]])
        for h in range(16):
            self.sc.op("pe", lambda e, h=h: e.matmul(
                YD[h // 8][:, (h % 8) * 64:(h % 8 + 1) * 64], GTB[:, h * 128:(h + 1) * 128], XSP[:, h * 64:(h + 1) * 64],
                start=True, stop=True, skip_group_check=True), [GTBb, XSPb], [YD[h // 8]])
        self.tt("pool", _v3(SEG, 64), _v3(XSTOK, 64), _bc(self.srow[:, 64:80], 64), ALU.mult, [XSTOKb, self.srow], [SEGb])
        for g in range(2):
            hs = slice(g * 512, (g + 1) * 512)
            self.tt("dve", Y0[j % 2][:, hs], SEG[:, hs], YD[g][:, :], ALU.add, [SEGb, YD[g]], [Y0b[j % 2]])

    def stage_b(j):
        tsl = slice(j * 128, (j + 1) * 128)
        ssm = ssms[j % 2]
        DEC = [ssm[:, 48:64], ssm[:, 64:80]]
        BTOK = BTOKb[j % 2]
        Yb, Yap = Y0b[j % 2], Y0[j % 2]
        for c2 in range(2):
            lo, hi = c2 * 64, c2 * 64 + 64
            csl = slice(j * 128 + lo, j * 128 + hi)
            for g in range(2):
                self.sc.op("pe", lambda e, g=g, lo=lo, hi=hi, csl=csl: e.matmul(
                    YO[g][lo:hi, :], CT[g][:, csl], HTb[g], start=True, stop=True, skip_group_check=True),
                    [CT[g], HTbb], [YO[g]])
            for g in range(2):
                st = self.PS()
                self.mm(st[:, :], BTOK[lo:hi, g * 128:(g + 1) * 128], XSPP[j % 2][lo:hi, g * 512:(g + 1) * 512], True, True,
                        [BTOK, XSPPb[j % 2]], [st])
                self.tt("dve", _v3(HT[g][:, :], 64), _v3(HT[g][:, :], 64), _bc(DEC[c2][:, g * 8:(g + 1) * 8], 64), ALU.mult,
                        [HT[g], ssm], [HT[g]])
                self.tt("dve", HT[g][:, :], HT[g][:, :], st[:, :], ALU.add, [HT[g], st], [HT[g]])
                self.cp("act", HTb[g], HT[g][:, :], [HT[g]], [HTbb])
        SZb = self.tmpA[0]
        self.load("sp", SZb, SZb[:, :], self.zscr, self.zscr[tsl, :])
        for g in range(2):
            hs = slice(g * 512, (g + 1) * 512)
            self.tt("dve", _v3(TMPb[:, :], 64), _v3(YO[g][:, :], 64), _bc(ssm[:, 16 + g * 8:16 + g * 8 + 8], 64), ALU.mult,
                    [YO[g], ssm], [TMPb])
            self.tt("pool", Yap[:, hs], Yap[:, hs], TMPb[:, :], ALU.add, [Yb, TMPb], [Yb])
        self.tt("pool", Yap, Yap, SZb[:, :], ALU.mult, [Yb, SZb], [Yb])
        self.tt("pool", SZb[:, :], Yap, Yap, ALU.mult, [Yb], [SZb])
        self.sc.op("dve", lambda e: e.reduce_sum(out=sm[:, 0:1], in_=SZb[:, :], axis=X), [SZb], [sm])
        self.ts("dve", sm[:, 0:1], sm[:, 0:1], 1.0 / 1024, RMS_EPS, ALU.mult, ALU.add, [sm], [sm])
        self.act(sm[:, 0:1], sm[:, 0:1], AF.Ln, [sm], [sm])
        self.act(sm[:, 1:2], sm[:, 0:1], AF.Exp, [sm], [sm], scale=-0.5)
        self.stt(YN, Yap, sm[:, 1:2], self.rows[:, 2, :], ALU.mult, ALU.mult, [Yb, sm, self.rows], [YNb])
        pb = self.PS()
        pbv = _bfv(pb)
        for cc in range(8):
            self.tr(pbv[:, cc * 128:(cc + 1) * 128], YN[:, cc * 128:(cc + 1) * 128], c["identb"][:, :], [YNb, c["identb"]], [pb])
        self.cp("act", YNTh[0][:, :], pbv[:, 0:512], [pb], [YNTh[0]])
        self.cp("act", YNTh[1][:, :], pbv[:, 512:1024], [pb], [YNTh[1]])
        xr = self.xres[j % 2]
        sb_, sap = src(j)
        self.load("sp", xr, xr[:, :], sb_, sap)
        for half in range(2):
            hs = slice(half * 512, (half + 1) * 512)
            ps = self.PS()
            for kc in range(12):
                lhsT = YNTh[kc // 4][:, (kc % 4) * 128:(kc % 4 + 1) * 128] if kc < 8 else OTS[kc - 8][:, tsl]
                rb_ = YNTh[kc // 4] if kc < 8 else OTS[kc - 8]
                self.mm(ps[:, :], lhsT, wov[kc // 4][:, kc % 4, hs], kc == 0, kc == 11, [rb_, wo[kc // 4]], [ps])
            self.stt(SZb[:, hs], xr[:, hs], ALPHA, ps[:, :], ALU.mult, ALU.add, [xr, ps], [SZb])
        self.ln_tile(SZb, SZb[:, :], self.row(0), self.row(1))
        db, dap = dst(j)
        self.load("sp", db, dap, SZb, SZb[:, :])
        self.transpose_tile(SZb, SZb[:, :], j, router_layer=0)

    stage_a(0)
    for j in range(NT):
        if j + 1 < NT:
            stage_a(j + 1)
        stage_b(j)
    self.ps = ps_save
    self.xtf = xtf_save


K.even_mixer = even_mixer


def _pipe(n, stages):
    maxs = max(s for s, _ in stages)
    for t in range(n + maxs):
        for s, fn in stages:
            u = t - s
            if 0 <= u < n:
                fn(u)


def _units(NG, hhs=(0, 1)):
    return [(hh, G, idx, i, 4 * (G + 1)) for hh in hhs for G in range(NG)
            for idx, i in enumerate(range(4 * (G + 1) - 1, -1, -1))]


def _sb_pipe(self, hp, QTz, KT, FV, OTSb):
    P2, H2, c = self.P2, self.H2, self.c
    units = _units(self.NG)
    n = len(units)
    E, Ecs = P2[0:3], P2[3:5]
    Lb, Rb = H2[0:2], H2[2]
    Wb = [H2[3], P2[5]]
    Wt = [H2[3][:, :], _bfv(P2[5])[:, 0:512]]
    zb, cb = {}, {}

    def s_z(u):
        hh, G, idx, i, nkb = units[u]
        lo, hi = hh * 64, hh * 64 + 64
        z = self.PS()
        zb[u] = z
        self.mm(z[:, :], KT[:, i * 128:(i + 1) * 128], QTz[hh][:, G * 512:(G + 1) * 512], True, True, [KT, QTz[hh]], [z])

    def s_E(u):
        hh, G, idx, i, nkb = units[u]
        z = zb.pop(u)
        e = E[u % 3]
        self.act(e[:, :], z[:, :], AF.Exp, [z], [e], scale=0.125)
        if i >= 4 * G:
            self.tt("pool", e[:, :], e[:, :], self.maskbuf[:, i - 4 * G, :], ALU.mult, [e, self.maskbuf], [e])

    def s_Lb(u):
        e, l = E[u % 3], Lb[u % 2]
        self.act(l[:, :], e[:, :], AF.Ln, [e], [l], bias=1.0)

    def s_CS(u):
        hh, G, idx, i, nkb = units[u]
        l = Lb[u % 2]
        CS = self.PS()
        cb[u] = CS
        self.mm(CS[:, :], c["uincl"][:, :], l[:, :], True, idx == 0, [c["uincl"], l], [CS])
        if idx > 0:
            self.mm(CS[:, :], c["onesb"][:, :], Rb[:, :], False, True, [c["onesb"], Rb], [CS])
        if idx < nkb - 1:
            if idx == 0:
                self.cp("pool", Rb[:, :], l[:, :], [l], [Rb])
            else:
                self.tt("pool", Rb[:, :], Rb[:, :], l[:, :], ALU.add, [Rb, l], [Rb])

    def s_Ecs(u):
        CS = cb.pop(u)
        ec = Ecs[u % 2]
        self.act(ec[:, :], CS[:, :], AF.Exp, [CS], [ec], scale=-1.0)
        self.tt("dve", Wt[u % 2], E[u % 3][:, :], ec[:, :], ALU.mult, [E[u % 3], ec], [Wb[u % 2]])

    def s_PV(u):
        hh, G, idx, i, nkb = units[u]
        lo, hi = hh * 64, hh * 64 + 64
        OT = self.acc[(hh * self.NG + G) % 2]
        self.mm(OT[:, :], FV[:, i * 128:(i + 1) * 128], Wt[u % 2], idx == 0, idx == nkb - 1, [FV, Wb[u % 2]], [OT])
        if idx == nkb - 1:
            self.cp("act", OTSb[lo:hi, G * 512:(G + 1) * 512], OT[lo:hi, :], [OT], [OTSb])

    _pipe(n, [(0, s_z), (2, s_Lb), (2, s_CS), (3, s_Ecs), (1, s_E), (4, s_PV)])


def _fox_pipe(self, hp, hh_, FQz, FK, FV, FHQs, CSPT, OTb, scale):
    P2, H2, c = self.P2, self.H2, self.c
    units = _units(self.NG, (hh_,))
    n = len(units)
    A = P2[0:3]
    Wb = H2[0:2]
    OT, DEN = self.acc
    zb = {}

    def s_z(u):
        hh, G, idx, i, nkb = units[u]
        lo, hi = hh * 64, hh * 64 + 64
        z = self.PS()
        zb[u] = z
        self.mm(z[:, :], FK[:, i * 128:(i + 1) * 128], FQz[hh][:, G * 512:(G + 1) * 512], True, True, [FK, FQz[hh]], [z])

    def s_A(u):
        hh, G, idx, i, nkb = units[u]
        z = zb.pop(u)
        a = A[u % 3]
        fb = FHQs[hh][G // 2]
        fap = _f32v(fb)[:, (G % 2) * 512:(G % 2 + 1) * 512]
        self.tt("dve", a[:, :], z[:, :], fap, ALU.subtract, [z, fb], [a])
        if i >= 4 * G:
            self.tt("pool", a[:, :], a[:, :], self.maskbuf[:, i - 4 * G, :], ALU.add, [a, self.maskbuf], [a])

    def s_W(u):
        hh, G, idx, i, nkb = units[u]
        h = 2 * hp + hh
        a, w = A[u % 3], Wb[u % 2]
        self.act(w[:, :], a[:, :], AF.Exp, [a, CSPT], [w], bias=CSPT[:, i, h:h + 1], scale=scale)

    def s_PV(u):
        hh, G, idx, i, nkb = units[u]
        lo, hi = hh * 64, hh * 64 + 64
        w = Wb[u % 2]
        self.mm(OT[:, :], FV[:, i * 128:(i + 1) * 128], w[:, :], idx == 0, idx == nkb - 1, [FV, w], [OT])
        self.mm(DEN[:, :], c["onesb"][:, :], w[:, :], idx == 0, idx == nkb - 1, [c["onesb"], w], [DEN])
        if idx == nkb - 1:
            _attn_finish(self, OT, DEN, hh, OTb, OTb[lo:hi, G * 512:(G + 1) * 512])

    _pipe(n, [(0, s_z), (2, s_W), (1, s_A), (3, s_PV)])


def _mla_pipe(self, hp, QH, KH, VM, OTb, scale):
    P2, H2, c = self.P2, self.H2, self.c
    units = _units(self.NG)
    n = len(units)
    Wf = P2[0:2]
    Wb = H2[0:2]
    OT, DEN = self.acc
    zb = {}

    def s_z(u):
        hh, G, idx, i, nkb = units[u]
        lo, hi = hh * 64, hh * 64 + 64
        plo, phi = hh * 32, hh * 32 + 32
        ksl, gsl = slice(i * 128, (i + 1) * 128), slice(G * 512, (G + 1) * 512)
        z = self.PS()
        zb[u] = z
        self.mm(z[:, :], KH[hh][:, ksl], QH[hh][:, gsl], True, True, [KH[hh], QH[hh]], [z])

    def s_W(u):
        hh, G, idx, i, nkb = units[u]
        z = zb.pop(u)
        w = Wb[u % 2]
        if i >= 4 * G:
            wf = Wf[u % 2]
            self.act(wf[:, :], z[:, :], AF.Exp, [z], [wf], scale=scale)
            self.tt("pool", w[:, :], wf[:, :], self.maskbuf[:, i - 4 * G, :], ALU.mult, [wf, self.maskbuf], [w])
        else:
            self.act(w[:, :], z[:, :], AF.Exp, [z], [w], scale=scale)

    def s_PV(u):
        hh, G, idx, i, nkb = units[u]
        h = 2 * hp + hh
        lo, hi = hh * 64, hh * 64 + 64
        w = Wb[u % 2]
        vs = VM[i // 4][:, (i % 4) * 512 + hp * 128:(i % 4) * 512 + (hp + 1) * 128]
        self.mm(OT[:, :], vs, w[:, :], idx == 0, idx == nkb - 1, [VM[i // 4], w], [OT])
        self.mm(DEN[:, :], c["onesb"][:, :], w[:, :], idx == 0, idx == nkb - 1, [c["onesb"], w], [DEN])
        if idx == nkb - 1:
            _attn_finish(self, OT, DEN, hh, OTb, OTb[lo:hi, G * 512:(G + 1) * 512])

    _pipe(n, [(0, s_z), (1, s_W), (2, s_PV)])
```

```python
import math
from contextlib import ExitStack
import numpy as np
import ml_dtypes
import concourse.bass as bass
import concourse.mybir as mybir
from concourse.bass_utils import run_bass_kernel_spmd

F32 = mybir.dt.float32
BF16 = mybir.dt.bfloat16
AF = mybir.ActivationFunctionType
ALU = mybir.AluOpType

D = 1024
NCORES = 8
ALPHA = (2.0 * 2) ** 0.25
LN_EPS = 1e-5
RMS_EPS = 1e-6
EVEN_IN = 4112
ODD_IN = 1960

ENGS = ("pe", "act", "dve", "pool", "sp")


class Buf:
    _n = 0

    def __init__(self, name, t):
        self.name = name
        self.t = t
        self.w = None
        self.r = {}
        self.excl = False
        self.sem = None
        self.semcnt = 0
        Buf._n += 1
        self.id = Buf._n

    def __getitem__(self, k):
        return self.t[k]


class Sched:
    def __init__(self, nc, es):
        self.nc = nc
        self.es = es
        self.ops = {e: [] for e in ENGS}
        self.cnt = {e: 0 for e in ENGS}
        self.known = {e: {} for e in ENGS}
        self.snap = {e: [None] for e in ENGS}
        self.rec = None
        self.sems = {e: es.enter_context(nc.semaphore("c_" + e)) for e in ENGS}
        self.nsem = len(ENGS)

    def _dma_sem(self, b):
        if b.sem is None:
            b.sem = self.es.enter_context(self.nc.semaphore("d_%d" % b.id))
            self.nsem += 1
        return b.sem

    def _waits(self, eng, reads, writes):
        waits = {}

        def need(ev, same_ok):
            if ev is None:
                return
            k, v = ev
            if k == eng and (same_ok or eng in ("pe", "sp")):
                return
            if v > waits.get(k, 0):
                waits[k] = v
        for b in reads:
            need(b.w, False)
            if b.excl:
                for k, v in b.r.items():
                    need((k, v), True)
        for b in writes:
            need(b.w, False)
            for k, v in b.r.items():
                need((k, v), False)
        kn = self.known[eng]
        out = []
        for k, v in waits.items():
            if kn.get(k, 0) >= v:
                continue
            out.append((k, v))
            kn[k] = v
            if isinstance(k, str):
                sn = self.snap[k][v]
                if sn is not None:
                    for k2, v2 in sn.items():
                        if k2 != eng and kn.get(k2, 0) < v2:
                            kn[k2] = v2
        return out

    def op(self, eng, emit, reads=(), writes=()):
        if self.rec is not None:
            self.rec.append(("op", (eng, emit, tuple(reads), tuple(writes)), {}))
            return
        waits = self._waits(eng, reads, writes)
        self.cnt[eng] += 1
        idx = self.cnt[eng]
        ev = (eng, idx)
        for b in reads:
            b.r[eng] = idx
        for b in writes:
            b.w = ev
            b.r = {}
        self.snap[eng].append({k: v for k, v in self.known[eng].items() if isinstance(k, str)})
        self.ops[eng].append((waits, emit, None))

    def dma(self, q, out_ap, in_ap, dst, src, extra_reads=(), **kw):
        if self.rec is not None:
            self.rec.append(("dma", (q, out_ap, in_ap, dst, src, extra_reads), kw))
            return
        reads = [src] + list(extra_reads)
        waits = self._waits(q, reads, [dst])
        sem = self._dma_sem(dst)
        dst.semcnt += 16
        ev = (("dma", dst.id, sem), dst.semcnt)
        src.r[ev[0]] = ev[1]
        for b in extra_reads:
            b.r[ev[0]] = ev[1]
        dst.w = ev
        dst.r = {}

        def emit(e):
            return e.dma_start(out=out_ap, in_=in_ap, **kw)
        self.ops[q].append((waits, emit, sem))

    def record(self, fn):
        assert self.rec is None
        self.rec = []
        fn()
        lst, self.rec = self.rec, None
        return lst

    def replay(self, la, lb=()):
        ia = ib = 0
        na, nb = len(la), len(lb)
        while ia < na or ib < nb:
            if ib >= nb or (ia < na and ia * nb <= ib * na):
                kind, a, k = la[ia]
                ia += 1
            else:
                kind, a, k = lb[ib]
                ib += 1
            if kind == "op":
                self.op(*a)
            else:
                self.dma(*a, **k)

    def finish_waits(self, eng, bufs):
        waits = self._waits(eng, bufs, [])
        self.ops[eng].append((waits, None, None))

    def emit_all(self):
        nc = self.nc
        handles = {"pe": "tensor", "act": "scalar", "dve": "vector", "pool": "gpsimd", "sp": "sync"}
        with nc.Block() as block:
            for e in ENGS:
                ops = self.ops[e]
                esem = self.sems[e]

                def body(eng, ops=ops, esem=esem):
                    for waits, emit, dsem in ops:
                        for k, v in waits:
                            s = self.sems[k] if isinstance(k, str) else k[2]
                            eng.wait_ge(s, v)
                        if emit is None:
                            continue
                        ins = emit(eng)
                        if dsem is not None:
                            ins.then_inc(dsem, 16)
                        else:
                            ins.then_inc(esem, 1)
                getattr(block, handles[e])(body)


def _consts(S):
    c = {}
    p = np.arange(128)
    c["ident"] = np.eye(128, dtype=np.float32)
    c["identb"] = np.eye(128, dtype=np.float32).astype(ml_dtypes.bfloat16)
    c["onesb"] = np.ones((128, 128), np.float32).astype(ml_dtypes.bfloat16)
    c["uincl"] = (p[:, None] >= p[None, :]).astype(np.float32).astype(ml_dtypes.bfloat16)
    s = p[None, :, None]
    t = np.arange(512)[None, None, :]
    m = np.arange(4)[:, None, None]
    c["mask_sb"] = ((s + 128 * m) < t).astype(np.float32).astype(ml_dtypes.bfloat16)
    c["mask_mla"] = (((s + 128 * m) // 64) <= (t // 64)).astype(np.float32).astype(ml_dtypes.bfloat16)
    c["negm_fox"] = np.where((s + 128 * m) <= t, 0.0, -30000.0).astype(np.float32).astype(ml_dtypes.bfloat16)
    c2 = p // 64
    l = p % 64
    c["tri2"] = ((c2[:, None] == c2[None, :]) & (l[:, None] <= l[None, :])).astype(np.float32)
    c["bd"] = (c2[:, None] == c2[None, :]).astype(np.float32)
    c["lastsel"] = (p[:, None] == (64 * c2[None, :] + 63)).astype(np.float32)
    sl = np.zeros((2, 128, 128), np.float32)
    sl[0, 63, :] = 1.0
    sl[1, 127, :] = 1.0
    c["sellast"] = sl
    c["i64x2"] = (l[:, None] == np.arange(64)[None, :]).astype(np.float32)
    c["trimask"] = (np.arange(64)[None, :] >= l[:, None]).astype(np.float32)
    c["sel8"] = np.repeat(np.eye(8, dtype=np.float32), 128, axis=1)
    ng = np.where(np.arange(64)[None, :] < l[:, None], -30000.0, 0.0).astype(np.float32)
    c["negm_ssd"] = np.tile(ng[:, None, :], (1, 16, 1)).reshape(128, 1024).astype(ml_dtypes.bfloat16)
    inv = 10000.0 ** (-(np.arange(0, 32, 2, dtype=np.float32) / 32.0))
    ang = np.arange(S, dtype=np.float32)[None, :] * inv[:, None]
    cos = np.cos(ang).astype(np.float32)
    sin = np.sin(ang).astype(np.float32)
    cosT = np.concatenate([cos, cos], 0)
    sinT = np.concatenate([-sin, sin], 0)
    c["ropec"] = np.concatenate([cosT, cosT, cosT, cosT], 0).astype(np.float32)
    c["ropes"] = np.concatenate([sinT, sinT, sinT, sinT], 0).astype(np.float32)
    return c


class K:
    def __init__(self, S, NSEQ, layers=(0, 1), parts=("mix", "moe"), taps=()):
        self.S, self.NSEQ = S, NSEQ
        self.NT = S // 128
        self.GW = 512
        self.NG = S // 512
        self.layers, self.parts, self.taps = layers, parts, taps
        self.nc = bass.Bass("TRN2", target_bir_lowering=False)
        self.es = ExitStack()
        self.sc = Sched(self.nc, self.es)
        self.psi = 0
        self.evi = 0
        self.dbg = {}
        self.tap_out = {}

    def dram_in(self, name, shape, dt=F32):
        t = self.nc.dram_tensor(name, list(shape), dt, kind="ExternalInput")
        return Buf(name, t)

    def dram_out(self, name, shape, dt=F32):
        t = self.nc.dram_tensor(name, list(shape), dt, kind="ExternalOutput")
        return Buf(name, t)

    def sb(self, name, shape, dt=F32):
        t = self.es.enter_context(self.nc.sbuf_tensor("s_" + name, list(shape), dt))
        return Buf(name, t)

    def PS(self):
        b = self.ps[self.psi % len(self.ps)]
        self.psi += 1
        return b

    def mm(self, out, lhsT, rhs, start, stop, reads, writes):
        self.sc.op("pe", lambda e: e.matmul(out, lhsT, rhs, start=start, stop=stop), reads, writes)

    def tr(self, out, in_, ident, reads, writes):
        self.sc.op("pe", lambda e: e.transpose(out, in_, ident), reads, writes)

    def act(self, out, in_, func, reads, writes, bias=None, scale=1.0):
        kw = {}
        if bias is not None:
            kw["bias"] = bias
        self.sc.op("act", lambda e: e.activation(out=out, in_=in_, func=func, scale=scale, **kw), reads, writes)

    def tt(self, eng, out, in0, in1, op, reads, writes):
        self.sc.op(eng, lambda e: e.tensor_tensor(out=out, in0=in0, in1=in1, op=op), reads, writes)

    def ts(self, eng, out, in0, s1, s2, op0, op1, reads, writes):
        if op1 is None:
            self.sc.op(eng, lambda e: e.tensor_scalar(out=out, in0=in0, scalar1=s1, scalar2=None, op0=op0), reads, writes)
        else:
            self.sc.op(eng, lambda e: e.tensor_scalar(out=out, in0=in0, scalar1=s1, scalar2=s2, op0=op0, op1=op1), reads, writes)

    def stt(self, out, in0, scalar, in1, op0, op1, reads, writes):
        self.sc.op("dve", lambda e: e.scalar_tensor_tensor(out=out, in0=in0, scalar=scalar, in1=in1, op0=op0, op1=op1), reads, writes)

    def cp(self, eng, out, in_, reads, writes):
        if eng == "act":
            self.sc.op("act", lambda e: e.activation(out=out, in_=in_, func=AF.Copy), reads, writes)
        else:
            self.sc.op(eng, lambda e: e.tensor_copy(out=out, in_=in_), reads, writes)

    def evac(self, out, in_, reads, writes):
        self.evi += 1
        self.cp("act" if self.evi % 2 else "dve", out, in_, reads, writes)

    def load(self, q, dstbuf, out_ap, srcbuf, in_ap, **kw):
        self.sc.dma(q, out_ap, in_ap, dstbuf, srcbuf, **kw)

    def setup(self):
        S, NSEQ, NT = self.S, self.NSEQ, self.NT
        self.x = self.dram_in("x", [NSEQ, S, D])
        self.out = self.dram_out("out", [NSEQ, S, D])
        self.xscr = Buf("xscr", self.nc.dram_tensor("xscr", [S, D], F32, kind="Internal"))
        W = {}
        W["ev_w_in"] = self.dram_in("ev_w_in", [D, EVEN_IN])
        W["ev_w_out"] = self.dram_in("ev_w_out", [1536, D])
        W["od_w_in"] = self.dram_in("od_w_in", [D, ODD_IN + 128])
        W["od_w_q_up"] = self.dram_in("od_w_q_up", [256, 512 + 256 + 256])
        W["od_w_kv_up"] = self.dram_in("od_w_kv_up", [128, 1024])
        W["od_w_out"] = self.dram_in("od_w_out", [D, D])
        W["moe_w_gate"] = self.dram_in("moe_w_gate", [2, 16, D, 256])
        W["moe_w_up"] = self.dram_in("moe_w_up", [2, 16, D, 256])
        W["moe_w_down"] = self.dram_in("moe_w_down", [2, 16, 256, D])
        W["moe_wr"] = self.dram_in("moe_wr", [2, 128, 8, 20])
        W["rows"] = self.dram_in("rows", [NROWS, D])
        W["cols"] = self.dram_in("cols", [128, NCOLS])
        self.W = W
        cs = _consts(S)
        self.cdram = {}
        for k, v in cs.items():
            dt = BF16 if v.dtype == ml_dtypes.bfloat16 else F32
            self.cdram[k] = self.dram_in("c_" + k, v.shape, dt)
        self.slab = [self.sb("slab%d" % i, [128, 2048], BF16) for i in range(28)]
        self.XT = self.slab[0:8]
        self.wslot = [self.sb("wslot%d" % i, [128, 4096], BF16) for i in range(4)]
        self.wi = 0
        self.ps = [Buf("ps%d" % i, self.es.enter_context(self.nc.psum_tensor("ps%d" % i, [128, 512], F32)))
                   for i in range(8)]
        for b in self.ps:
            b.excl = True
        self.acc = self.ps[6:8]
        self.ps = self.ps[0:6]
        c = {}
        for k in ("ident", "identb", "onesb", "uincl", "tri2", "bd", "lastsel", "i64x2", "trimask"):
            v = cs[k]
            c[k] = self.sb("k_" + k, v.shape, BF16 if v.dtype == ml_dtypes.bfloat16 else F32)
            self.load("sp", c[k], c[k][:, :], self.cdram[k], self.cdram[k][:, :])
        c["sellast"] = self.sb("k_sellast", [128, 2, 128])
        for i in range(2):
            self.load("sp", c["sellast"], c["sellast"][:, i, :], self.cdram["sellast"], self.cdram["sellast"][i, :, :])
        self.c = c
        self.rows = self.sb("rows", [128, NROWS_SB, D])
        self.srow = self.sb("srow", [128, 512])
        self.maskbuf = self.sb("maskbuf", [128, 4, 512], BF16)
        self.P2 = [self.sb("p2_%d" % i, [128, 512]) for i in range(8)]
        self.H2 = [self.sb("h2_%d" % i, [128, 512], BF16) for i in range(4)]
        self.cols = self.sb("cols", [128, NCOLS])
        self.load("sp", self.cols, self.cols[:, :], W["cols"], W["cols"][:, :])
        self.wr = self.sb("wr", [128, 2, 8, 20])
        for l in range(2):
            self.load("sp", self.wr, self.wr[:, l, :, :], W["moe_wr"], W["moe_wr"][l, :, :, :])
        self.gate = self.sb("gate", [128, NT, 16])
        self.xres = [self.sb("xres0", [128, D])] * 2
        self.tmpA = [self.sb("tmpA%d" % i, [128, D]) for i in range(2)]
        self.T4 = [Buf("t4_%d" % i, None) for i in range(4)]
        for i in range(4):
            self.T4[i].t = self.slab[24 + i].t
        self.T4 = self.slab[24:28]
        self.small = self.sb("small", [128, 64])
        self.small2 = self.sb("small2", [128, 8])
        self.small2b = self.sb("small2b", [128, 16])
        self.dtb = self.sb("dtb", [128, self.NT, 16])
        self.dab = self.sb("dab", [128, self.NT, 16])
        self.ssm = self.sb("ssm", [128, 80])
        self.ssm2 = self.sb("ssm2", [128, 80])
        self.cbm = self.sb("cbm", [128, 128])
        self.zscr = Buf("zscr", self.nc.dram_tensor("zscr", [S, D], F32, kind="Internal"))
        self.cspt = self.sb("cspt", [128, self.NT, 8])
        self.xi = 0

    def row(self, r):
        return self.rows[:, r, :]

    def load_srow(self, r, off, n):
        src = self.W["rows"][r:r + 1, 0:n].broadcast_to([128, n])
        self.load("sp", self.srow, self.srow[:, off:off + n], self.W["rows"], src)

    def load_mask(self, name):
        cd = self.cdram[name]
        for m_ in range(4):
            self.load("sp", self.maskbuf, self.maskbuf[:, m_, :], cd, cd[m_, :, :])

    def load_rows(self, idx_list):
        for slot, r in enumerate(idx_list):
            src = self.W["rows"][r:r + 1, :].broadcast_to([128, D])
            self.load("sp", self.rows, self.rows[:, slot, :], self.W["rows"], src)

    def transpose_tile(self, xb, xap, j, router_layer=None):
        c = self.c
        for half in range(2):
            ps = self.PS()
            for kk in range(4):
                k = half * 4 + kk
                self.tr(ps[:, kk * 128:(kk + 1) * 128], xap[:, k * 128:(k + 1) * 128], c["ident"][:, :],
                        [xb, c["ident"]], [ps])
            self.evi += 1
            for kk in range(4):
                k = half * 4 + kk
                self.cp("act" if self.evi % 2 else "dve", self.XT[k][:, j * 128:(j + 1) * 128],
                        ps[:, kk * 128:(kk + 1) * 128], [ps], [self.XT[k]])
            if router_layer is not None and not self.dbg.get('noxtf'):
                xtf = self.xtf[half]
                self.cp("dve", xtf[:, :], ps[:, :], [ps], [xtf])
        if router_layer is not None and not self.dbg.get('norouter'):
            self.router(j, router_layer)

    def router(self, j, l):
        sm = self.small
        ps = self.PS()
        for k in range(8):
            xtf = self.xtf[k // 4]
            self.mm(ps[:, 0:20], xtf[:, (k % 4) * 128:(k % 4 + 1) * 128], self.wr[:, l, k, :], k == 0, k == 7,
                    [xtf, self.wr], [ps])
        lg = sm[:, 0:20]
        rb = self.srow[:, 0:20]
        self.tt("dve", lg, ps[:, 0:20], rb, ALU.add, [ps, self.srow], [sm])
        R, Wr = [sm], [sm]
        self.sc.op("dve", lambda e: e.reduce_max(out=sm[:, 20:21], in_=sm[:, 0:4], axis=mybir.AxisListType.X), R, Wr)
        self.ts("dve", sm[:, 21:25], sm[:, 0:4], sm[:, 20:21], None, ALU.is_equal, None, R, Wr)
        self.ts("dve", sm[:, 25:29], sm[:, 0:4], sm[:, 20:21], None, ALU.subtract, None, R, Wr)
        self.act(sm[:, 25:29], sm[:, 25:29], AF.Exp, R, Wr)
        self.sc.op("dve", lambda e: e.reduce_sum(out=sm[:, 29:30], in_=sm[:, 25:29], axis=mybir.AxisListType.X), R, Wr)
        self.sc.op("dve", lambda e: e.reciprocal(out=sm[:, 30:31], in_=sm[:, 29:30]), R, Wr)
        el = sm[:, 4:20].rearrange("p (g e) -> p g e", e=4)
        ohg_b = sm[:, 21:25].unsqueeze(2).broadcast_to([128, 4, 4])
        prod = sm[:, 32:48].rearrange("p (g e) -> p g e", e=4)
        self.tt("dve", prod, el, ohg_b, ALU.mult, R, Wr)
        prod_t = sm[:, 32:48].rearrange("p (g e) -> p e g", e=4)
        self.sc.op("dve", lambda e: e.reduce_sum(out=sm[:, 48:52], in_=prod_t, axis=mybir.AxisListType.X), R, Wr)
        self.sc.op("dve", lambda e: e.reduce_max(out=sm[:, 52:53], in_=sm[:, 48:52], axis=mybir.AxisListType.X), R, Wr)
        self.ts("dve", sm[:, 53:57], sm[:, 48:52], sm[:, 52:53], None, ALU.is_equal, None, R, Wr)
        self.stt(sm[:, 57:61], sm[:, 53:57], -1e30, sm[:, 48:52], ALU.mult, ALU.add, R, Wr)
        self.sc.op("dve", lambda e: e.reduce_max(out=sm[:, 61:62], in_=sm[:, 57:61], axis=mybir.AxisListType.X), R, Wr)
        self.ts("dve", sm[:, 25:29], sm[:, 57:61], sm[:, 61:62], None, ALU.is_equal, None, R, Wr)
        self.ts("dve", sm[:, 62:63], sm[:, 61:62], sm[:, 52:53], None, ALU.subtract, None, R, Wr)
        self.act(sm[:, 62:63], sm[:, 62:63], AF.Exp, R, Wr)
        self.ts("dve", sm[:, 63:64], sm[:, 62:63], 1.0, None, ALU.add, None, R, Wr)
        self.sc.op("dve", lambda e: e.reciprocal(out=sm[:, 63:64], in_=sm[:, 63:64]), R, Wr)
        self.stt(sm[:, 63:64], sm[:, 63:64], 1.0 / ALPHA, sm[:, 30:31], ALU.mult, ALU.mult, R, Wr)
        self.tt("dve", sm[:, 62:63], sm[:, 62:63], sm[:, 63:64], ALU.mult, R, Wr)
        self.ts("dve", sm[:, 53:57], sm[:, 53:57], sm[:, 63:64], None, ALU.mult, None, R, Wr)
        self.stt(sm[:, 53:57], sm[:, 25:29], sm[:, 62:63], sm[:, 53:57], ALU.mult, ALU.add, R, Wr)
        g4_b = sm[:, 53:57].unsqueeze(1).broadcast_to([128, 4, 4])
        gout = self.gate[:, j, :].rearrange("p (g e) -> p g e", e=4)
        self.tt("dve", gout, ohg_b, g4_b, ALU.mult, R, [self.gate])

    def ln_tile(self, xb, xap, g_row, b_row, eps=LN_EPS):
        sm = self.lnsm
        R, Wr = [sm], [sm]
        for h in range(2):
            self.sc.op("dve", lambda e, h=h: e.bn_stats(out=sm[:, 6 * h:6 * h + 6], in_=xap[:, h * 512:(h + 1) * 512]),
                       [xb], Wr)
        self.sc.op("dve", lambda e: e.bn_aggr(out=sm[:, 12:14], in_=sm[:, 0:12]), R, Wr)
        self.ts("dve", sm[:, 14:15], sm[:, 13:14], eps, None, ALU.add, None, R, Wr)
        self.act(sm[:, 14:15], sm[:, 14:15], AF.Ln, R, Wr)
        self.act(sm[:, 15:16], sm[:, 14:15], AF.Exp, R, Wr, scale=-0.5)
        self.stt(xap, xap, sm[:, 12:13], g_row, ALU.subtract, ALU.mult, [xb, sm, self.rows], [xb])
        self.stt(xap, xap, sm[:, 15:16], b_row, ALU.mult, ALU.add, [xb, sm, self.rows], [xb])

    def wnext(self):
        b = self.wslot[self.wi % len(self.wslot)]
        self.wi += 1
        return b

    def wload_k(self, slot, col0, ncols, wbuf, wap2d, nk=8):
        raise NotImplementedError

    def moe(self, l, src, dst, last):
        S, NT, NG = self.S, self.NT, self.NG
        W = self.W
        XA = [self.slab[8 + j] for j in range(NT)]
        xa = [b.t[:, :].bitcast(F32) for b in XA]
        self.load_rows([2 + 4 * l, 3 + 4 * l])
        for j in range(NT):
            sb_, sap = src(j)
            self.load("sp", XA[j], xa[j], sb_, sap)
        hid = self.hid
        hidv = [h.t[:, :].bitcast(BF16)[:, 0:512] for h in hid]
        for e in range(self.dbg.get('nexp', 16)):
            sa = self.wnext()
            sv = sa.t[:, :].rearrange("p (k c) -> p k c", c=512)
            self.load("pool", sa, sv[:, :, 0:256], W["moe_w_gate"],
                      W["moe_w_gate"][l, e, :, :].rearrange("(k p) f -> p k f", p=128))
            self.load("pool", sa, sv[:, :, 256:512], W["moe_w_up"],
                      W["moe_w_up"][l, e, :, :].rearrange("(k p) f -> p k f", p=128))
            sd = self.wnext()
            dv = sd.t[:, 0:2048].rearrange("p (k c) -> p k c", c=1024)
            self.load("pool", sd, dv, W["moe_w_down"],
                      W["moe_w_down"][l, e, :, :].rearrange("(k p) f -> p k f", p=128))
            for g in range(NG):
                tsl = slice(g * 512, (g + 1) * 512)
                for f in range(2):
                    gps = self.PS()
                    for k in range(8):
                        self.mm(gps[:, :], sv[:, k, f * 128:(f + 1) * 128], self.XT[k][:, tsl], k == 0, k == 7,
                                [sa, self.XT[k]], [gps])
                    ups = self.PS()
                    for k in range(8):
                        self.mm(ups[:, :], sv[:, k, 256 + f * 128:256 + (f + 1) * 128], self.XT[k][:, tsl], k == 0, k == 7,
                                [sa, self.XT[k]], [ups])
                    sg = self.sg[f]
                    self.act(sg[:, :], gps[:, :], AF.Silu, [gps], [sg])
                    self.tt("dve", hidv[f], sg[:, :], ups[:, :], ALU.mult, [sg, ups], [hid[f]])
                for tt_ in range(4):
                    j = g * 4 + tt_
                    for half in range(2):
                        ops = self.PS()
                        for f in range(2):
                            self.mm(ops[:, :], hidv[f][:, tt_ * 128:(tt_ + 1) * 128], dv[:, f, half * 512:(half + 1) * 512],
                                    f == 0, f == 1, [hid[f], sd], [ops])
                        xs_ = xa[j][:, half * 512:(half + 1) * 512]
                        self.stt(xs_, ops[:, :], self.gate[:, j, e:e + 1], xs_, ALU.mult, ALU.add,
                                 [ops, self.gate, XA[j]], [XA[j]])
        for j in range(NT):
            if not self.dbg.get('noln'):
                self.ln_tile(XA[j], xa[j], self.row(0), self.row(1), eps=LN_EPS / (ALPHA * ALPHA))
            db, dap = dst(j)
            self.load("sp", db, dap, XA[j], xa[j])
            if not last:
                self.transpose_tile(XA[j], xa[j], j)

    def prologue(self, seq, router_layer=None):
        for j in range(self.NT):
            xb = self.xres[j % 2]
            self.load("sp", xb, xb[:, :], self.x, self.x[seq, j * 128:(j + 1) * 128, :])
            self.transpose_tile(xb, xb[:, :], j, router_layer)

    def alloc_misc(self):
        self.xtf = self.P2[0:2]
        self.lnsm = self.sb("lnsm", [128, 16])
        self.sg = self.P2[2:4]
        self.hid = self.P2[4:6]

    def run(self):
        self.setup()
        self.alloc_misc()
        NT = self.NT
        for seq in range(self.NSEQ):
            def src_x(j, seq=seq):
                return self.x, self.x[seq, j * 128:(j + 1) * 128, :]

            def src_scr(j):
                return self.xscr, self.xscr[j * 128:(j + 1) * 128, :]

            def dst_out(j, seq=seq):
                return self.out, self.out[seq, j * 128:(j + 1) * 128, :]
            cur = src_x
            first = True
            subl = [(l, p) for l in self.layers for p in self.parts]
            for i, (l, p) in enumerate(subl):
                last = i == len(subl) - 1
                dst = dst_out if last else src_scr
                if p == "moe":
                    if first:
                        self.load_srow(9 + l, 0, 20)
                        self.prologue(seq, router_layer=l)
                    self.moe(l, cur, dst, last)
                else:
                    if first:
                        self.prologue(seq)
                    if l == 0:
                        self.even_mixer(cur, dst)
                    else:
                        self.odd_mixer(cur, dst)
                cur = src_scr
                first = False
        self.sc.finish_waits("sp", [self.out])
        self.sc.emit_all()
        return self.nc


NROWS = 16
NROWS_SB = 3
NCOLS = 64


def _host_inputs(S, inputs):
    f = lambda a: np.ascontiguousarray(np.asarray(a, dtype=np.float32))
    m = {}
    m["ev_w_in"] = f(inputs["ev_w_in"][0])
    m["ev_w_out"] = f(inputs["ev_w_out"][0])
    wi = f(inputs["od_w_in"][0])
    kpe = wi[:, 384:416]
    kpe_sw = np.concatenate([kpe[:, 16:32], kpe[:, 0:16]], 1)
    m["od_w_in"] = np.ascontiguousarray(np.concatenate([wi, kpe, kpe, kpe_sw, kpe_sw], 1))
    wq = f(inputs["od_w_q_up"][0]).reshape(256, 8, 96)
    nope = wq[:, :, :64].reshape(256, 512)
    pe = wq[:, :, 64:]
    pe_sw = np.concatenate([pe[:, :, 16:], pe[:, :, :16]], 2)
    m["od_w_q_up"] = np.ascontiguousarray(np.concatenate([nope, pe.reshape(256, 256), pe_sw.reshape(256, 256)], 1))
    wkv = f(inputs["od_w_kv_up"][0]).reshape(128, 8, 128)
    m["od_w_kv_up"] = np.ascontiguousarray(np.concatenate([wkv[:, :, :64].reshape(128, 512), wkv[:, :, 64:].reshape(128, 512)], 1))
    m["od_w_out"] = f(inputs["od_w_out"][0])
    m["moe_w_gate"] = f(inputs["moe_w_gate"])
    m["moe_w_up"] = f(inputs["moe_w_up"])
    m["moe_w_down"] = f(inputs["moe_w_down"])
    wr = np.concatenate([f(inputs["moe_w_group"]), f(inputs["moe_w_expert"])], 2)
    m["moe_wr"] = np.ascontiguousarray(wr.reshape(2, 8, 128, 20).transpose(0, 2, 1, 3))
    rows = np.zeros((NROWS, D), np.float32)
    for l in range(2):
        rows[4 * l + 0] = inputs["ln1_g"][l]
        rows[4 * l + 1] = inputs["ln1_b"][l]
        rows[4 * l + 2] = inputs["ln2_g"][l]
        rows[4 * l + 3] = inputs["ln2_b"][l]
        rows[9 + l, 0:4] = inputs["moe_b_group"][l]
        rows[9 + l, 4:20] = inputs["moe_b_expert"][l]
    rows[8] = inputs["ev_norm_g"][0]
    rows[11, 0:16] = inputs["ev_dt_bias"][0]
    rows[11, 16:32] = inputs["ev_a_log"][0]
    rows[11, 32:48] = inputs["ev_d_skip"][0]
    rows[12, 0:256] = inputs["od_q_norm_g"][0]
    rows[12, 256:384] = inputs["od_kv_norm_g"][0]
    rows[13, 0:8] = inputs["od_f_bias"][0]
    m["rows"] = rows
    cols = np.zeros((128, NCOLS), np.float32)
    cw = f(inputs["ev_conv_w"][0])
    cols[:, 0:48] = cw.T.reshape(12, 128, 4).transpose(1, 0, 2).reshape(128, 48)
    cols[:, 48:60] = f(inputs["ev_conv_b"][0]).reshape(12, 128).T
    cols[0:8, 60] = f(inputs["od_f_bias"][0])
    m["cols"] = cols
    for k, v in _consts(S).items():
        m["c_" + k] = v
    return m


_CACHE = {}


def kernel(**inputs):
    x = np.ascontiguousarray(np.asarray(inputs["x"], dtype=np.float32))
    B, S, _ = x.shape
    nseq = B // NCORES
    key = (S, nseq)
    if key not in _CACHE:
        _CACHE[key] = K(S, nseq).run()
    nc = _CACHE[key]
    shared = _host_inputs(S, inputs)
    in_maps = []
    for c in range(NCORES):
        m = dict(shared)
        m["x"] = np.ascontiguousarray(x[c * nseq:(c + 1) * nseq])
        in_maps.append(m)
    res = run_bass_kernel_spmd(nc, in_maps, core_ids=list(range(NCORES)))
    return np.concatenate([np.asarray(r["out"]) for r in res.results], axis=0).astype(np.float32)


def _f32v(b):
    return b.t[:, :].bitcast(F32)


def _bfv(b):
    return b.t[:, :].bitcast(BF16)


def _wview(slot, c):
    return slot.t[:, :].rearrange("p (k c) -> p k c", c=c)


def _kp(ap2d):
    return ap2d.rearrange("(k p) c -> p k c", p=128)


def _rope_tables(self, g):
    rc, rs = self.P2[3], self.P2[4]
    cc, cs_ = self.cdram["ropec"], self.cdram["ropes"]
    self.load("sp", rc, rc[:, :], cc, cc[0:128, g * 512:(g + 1) * 512])
    self.load("sp", rs, rs[:, :], cs_, cs_[0:128, g * 512:(g + 1) * 512])
    return rc, rs


def _rope(self, A, B, rc, rs, dst, dst_ap, lo=0, hi=64):
    t1, t2 = self.P2[5], self.P2[6]
    self.tt("dve", t1[lo:hi, :], A[lo:hi, :], rc[lo:hi, :], ALU.mult, [A, rc], [t1])
    self.tt("dve", t2[lo:hi, :], B[lo:hi, :], rs[lo:hi, :], ALU.mult, [B, rs], [t2])
    self.tt("pool", dst_ap, t1[lo:hi, :], t2[lo:hi, :], ALU.add, [t1, t2], [dst])


def _attn_finish(self, OT, DEN, hh, dst, dst_ap):
    rec = self.P2[6]
    lo, hi = hh * 64, hh * 64 + 64
    self.sc.op("dve", lambda e: e.reciprocal(out=rec[lo:hi, :], in_=DEN[lo:hi, :]), [DEN], [rec])
    self.tt("dve", dst_ap, OT[lo:hi, :], rec[lo:hi, :], ALU.mult, [OT, rec], [dst])


def odd_mixer(self, src, dst):
    S, NT, NG = self.S, self.NT, self.NG
    W = self.W
    Win = W["od_w_in"]
    sl = self.slab
    P2 = self.P2
    XT = self.XT
    c = self.c
    sc_mla = 1.0 / math.sqrt(96.0)
    sc_fox = 1.0 / 8.0
    self.load_rows([4, 5])
    self.load_srow(10, 0, 20)
    self.load_srow(12, 96, 384)
    CQNT, CKVNT, KPE = sl[8:10], sl[10], sl[11]
    OTF = sl[12:16]
    sm = self.small
    wl = self.wnext()
    wlv = _wview(wl, 512)
    self.load("pool", wl, wlv[:, :, 0:384], Win, _kp(Win[:, 0:384]))
    self.load("pool", wl, wlv[:, :, 384:512], Win, _kp(Win[:, 1960:2088]))
    lat, sq, latn_b = P2[0], P2[1], P2[2]
    latn = _bfv(latn_b)
    for j in range(NT):
        tsl = slice(j * 128, (j + 1) * 128)
        ps = self.PS()
        for k in range(8):
            self.mm(ps[:, 0:384], XT[k][:, tsl], wlv[:, k, 0:384], k == 0, k == 7, [XT[k], wl], [ps])
        self.cp("act", lat[:, 0:384], ps[:, 0:384], [ps], [lat])
        self.tt("pool", sq[:, 0:384], lat[:, 0:384], lat[:, 0:384], ALU.mult, [lat], [sq])
        self.sc.op("dve", lambda e: e.reduce_sum(out=sm[:, 0:1], in_=sq[:, 0:256], axis=mybir.AxisListType.X), [sq], [sm])
        self.sc.op("dve", lambda e: e.reduce_sum(out=sm[:, 1:2], in_=sq[:, 256:384], axis=mybir.AxisListType.X), [sq], [sm])
        self.ts("dve", sm[:, 0:1], sm[:, 0:1], 1.0 / 256, RMS_EPS, ALU.mult, ALU.add, [sm], [sm])
        self.ts("dve", sm[:, 1:2], sm[:, 1:2], 1.0 / 128, RMS_EPS, ALU.mult, ALU.add, [sm], [sm])
        self.act(sm[:, 0:2], sm[:, 0:2], AF.Ln, [sm], [sm])
        self.act(sm[:, 2:4], sm[:, 0:2], AF.Exp, [sm], [sm], scale=-0.5)
        self.stt(latn[:, 0:256], lat[:, 0:256], sm[:, 2:3], self.srow[:, 96:352], ALU.mult, ALU.mult,
                 [lat, sm, self.srow], [latn_b])
        self.stt(latn[:, 256:384], lat[:, 256:384], sm[:, 3:4], self.srow[:, 352:480], ALU.mult, ALU.mult,
                 [lat, sm, self.srow], [latn_b])
        pb = self.PS()
        pbv = _bfv(pb)
        for i in range(3):
            self.tr(pbv[:, i * 128:(i + 1) * 128], latn[:, i * 128:(i + 1) * 128], c["identb"][:, :], [latn_b, c["identb"]], [pb])
        for i, dstb in enumerate((CQNT[0], CQNT[1], CKVNT)):
            self.cp("dve", dstb[:, tsl], pbv[:, i * 128:(i + 1) * 128], [pb], [dstb])
    for g in range(NG):
        gsl = slice(g * 512, (g + 1) * 512)
        rc, rs = _rope_tables(self, g)
        A = self.PS()
        for k in range(8):
            self.mm(A[64:96, :], wlv[:, k, 384:416], XT[k][:, gsl], k == 0, k == 7, [wl, XT[k]], [A])
        B = self.PS()
        for k in range(8):
            self.mm(B[64:96, :], wlv[:, k, 448:480], XT[k][:, gsl], k == 0, k == 7, [wl, XT[k]], [B])
        _rope(self, A, B, rc, rs, KPE, KPE[64:96, gsl], 64, 96)
    wf = self.wnext()
    wfv = wf.t[:, 0:64].rearrange("p (k c) -> p k c", c=8)
    self.load("pool", wf, wfv, Win, _kp(Win[:, 1952:1960]))
    ones = P2[7]
    self.sc.op("pool", lambda e: e.memset(ones[:, :], 1.0), [], [ones])
    negfb = self.small2
    self.ts("pool", negfb[0:8, 0:1], self.cols[0:8, 60:61], -1.0, None, ALU.mult, None, [self.cols], [negfb])
    CSPb = [self.T4[0], self.T4[1]]
    FHQb = [self.T4[2], self.T4[3]]

    def cspg(g):
        return CSPb[g // 2], _f32v(CSPb[g // 2])[:, (g % 2) * 512:(g % 2 + 1) * 512]

    def fhqg(g):
        return FHQb[g // 2], _f32v(FHQb[g // 2])[:, (g % 2) * 512:(g % 2 + 1) * 512]
    CSPT = self.cspt
    for g in range(NG):
        gsl = slice(g * 512, (g + 1) * 512)
        ps = self.PS()
        for k in range(8):
            self.mm(ps[0:8, :], wfv[:, k, :], XT[k][:, gsl], k == 0, k == 7, [wf, XT[k]], [ps])
        e_, sp_ = P2[5], P2[6]
        self.act(e_[0:8, :], ps[0:8, :], AF.Exp, [ps, negfb], [e_], bias=negfb[0:8, 0:1], scale=-1.0)
        self.act(sp_[0:8, :], e_[0:8, :], AF.Ln, [e_], [sp_], bias=1.0)
        cb, cap = cspg(g)
        if g == 0:
            init, rds = 0.0, [ones, sp_]
        else:
            pb_, pap = cspg(g - 1)
            init, rds = pap[0:8, 511:512], [ones, sp_, pb_]
        self.sc.op("dve", lambda e, cap=cap, init=init: e.tensor_tensor_scan(
            out=cap[0:8, :], data0=ones[0:8, :], data1=sp_[0:8, :], initial=init, op0=ALU.mult, op1=ALU.add), rds, [cb])
        for tt_ in range(4):
            j = g * 4 + tt_
            pt = self.PS()
            self.tr(pt[:, 0:8], cap[0:8, tt_ * 128:(tt_ + 1) * 128], c["ident"][0:8, 0:8], [cb, c["ident"]], [pt])
            self.cp("dve", CSPT[:, j, :], pt[:, 0:8], [pt], [CSPT])
    self.load_mask("negm_fox")
    OT, DEN = self.acc
    for hp in range(4):
        q4 = sl[16 + 4 * (hp % 2):20 + 4 * (hp % 2)]
        FQz, FK, FV = q4[0:2], q4[2], q4[3]
        if hp < 2:
            self.sc.op("pool", lambda e, b=FQz[0]: e.memset(b[64:128, :], 0.0), [], [FQz[0]])
            self.sc.op("pool", lambda e, b=FQz[1]: e.memset(b[0:64, :], 0.0), [], [FQz[1]])
        wq = self.wnext()
        wqv = _wview(wq, 512)
        for i, c0 in enumerate((416, 928, 1440)):
            self.load("pool", wq, wqv[:, :, i * 128:(i + 1) * 128], Win, _kp(Win[:, c0 + hp * 128:c0 + (hp + 1) * 128]))
        for g in range(NG):
            gsl = slice(g * 512, (g + 1) * 512)
            for i in range(2):
                ps = self.PS()
                for k in range(8):
                    self.mm(ps[:, :], wqv[:, k, i * 128:(i + 1) * 128], XT[k][:, gsl], k == 0, k == 7, [wq, XT[k]], [ps])
                if i == 0:
                    self.cp("act", FQz[0][0:64, gsl], ps[0:64, :], [ps], [FQz[0]])
                    self.cp("act", FQz[1][64:128, gsl], ps[64:128, :], [ps], [FQz[1]])
                else:
                    self.cp("dve", FK[:, gsl], ps[:, :], [ps], [FK])
        for j in range(NT):
            tsl = slice(j * 128, (j + 1) * 128)
            ps = self.PS()
            for k in range(8):
                self.mm(ps[:, 0:128], XT[k][:, tsl], wqv[:, k, 256:384], k == 0, k == 7, [wq, XT[k]], [ps])
            self.evac(FV[:, tsl], ps[:, 0:128], [ps], [FV])
        FHQs = [[self.T4[2], self.T4[3]], [self.T4[2], self.T4[3]]]
        for hh in range(2):
            h = 2 * hp + hh
            for g in range(NG):
                cb, cap = cspg(g)
                m_ = P2[5]
                self.ts("dve", m_[0:8, :], cap[0:8, :], c["ident"][0:8, h:h + 1], 1.0 / sc_fox, ALU.mult, ALU.mult, [cb, c["ident"]], [m_])
                ps = self.PS()
                self.mm(ps[:, :], ones[0:8, 0:128], m_[0:8, :], True, True, [ones, m_], [ps])
                fb = FHQs[hh][g // 2]
                self.cp("act", _f32v(fb)[:, (g % 2) * 512:(g % 2 + 1) * 512], ps[:, :], [ps], [fb])
            _fox_pipe(self, hp, hh, FQz, FK, FV, FHQs, CSPT, OTF[hp], sc_fox)
    self.load_mask("mask_mla")
    wm = self.wnext()
    wq_ = wm.t[:, 0:2048].rearrange("p (k c) -> p k c", c=1024)
    wkv = wm.t[:, 2048:3072]
    self.load("pool", wm, wq_, W["od_w_q_up"], _kp(W["od_w_q_up"][:, :]))
    self.load("pool", wm, wkv, W["od_w_kv_up"], W["od_w_kv_up"][:, :])
    VM = XT[0:4]
    OTM = XT[4:8]
    for j in range(NT):
        ps = self.PS()
        self.mm(ps[:, :], CKVNT[:, j * 128:(j + 1) * 128], wkv[:, 512:1024], True, True, [CKVNT, wm], [ps])
        self.evac(VM[j // 4][:, (j % 4) * 512:(j % 4 + 1) * 512], ps[:, :], [ps], [VM[j // 4]])
    for hp in range(4):
        q4 = sl[16 + 4 * (hp % 2):20 + 4 * (hp % 2)]
        QH, KH = q4[0:2], q4[2:4]
        if hp < 2:
            for b in q4:
                self.sc.op("pool", lambda e, b=b: e.memset(b[64:128, :], 0.0), [], [b])
        for hh in range(2):
            h = 2 * hp + hh
            for g in range(NG):
                gsl = slice(g * 512, (g + 1) * 512)
                ps = self.PS()
                for kk in range(2):
                    self.mm(ps[0:64, :], wq_[:, kk, h * 64:(h + 1) * 64], CQNT[kk][:, gsl], kk == 0, kk == 1, [wm, CQNT[kk]], [ps])
                self.evac(QH[hh][0:64, gsl], ps[0:64, :], [ps], [QH[hh]])
                ps = self.PS()
                self.mm(ps[0:64, :], wkv[:, h * 64:(h + 1) * 64], CKVNT[:, gsl], True, True, [wm, CKVNT], [ps])
                self.evac(KH[hh][0:64, gsl], ps[0:64, :], [ps], [KH[hh]])
                rc, rs = _rope_tables(self, g)
                A = self.PS()
                for kk in range(2):
                    self.mm(A[64:96, :], wq_[:, kk, 512 + h * 32:512 + (h + 1) * 32], CQNT[kk][:, gsl], kk == 0, kk == 1, [wm, CQNT[kk]], [A])
                B = self.PS()
                for kk in range(2):
                    self.mm(B[64:96, :], wq_[:, kk, 768 + h * 32:768 + (h + 1) * 32], CQNT[kk][:, gsl], kk == 0, kk == 1, [wm, CQNT[kk]], [B])
                _rope(self, A, B, rc, rs, QH[hh], QH[hh][64:96, gsl], 64, 96)
            self.cp("act", KH[hh][64:96, 0:S], KPE[64:96, 0:S], [KPE], [KH[hh]])
        _mla_pipe(self, hp, QH, KH, VM, OTM[hp], sc_mla)
    wo = [self.wnext(), self.wnext()]
    wov = [_wview(w_, 1024) for w_ in wo]
    Wo = W["od_w_out"]
    for i in range(2):
        self.load("pool", wo[i], wov[i], Wo, _kp(Wo[i * 512:(i + 1) * 512, :]))
    cat = list(OTM) + list(OTF)
    for j in range(NT):
        tsl = slice(j * 128, (j + 1) * 128)
        xr = self.xres[j % 2]
        sb_, sap = src(j)
        self.load("sp", xr, xr[:, :], sb_, sap)
        y = self.tmpA[j % 2]
        for half in range(2):
            hs = slice(half * 512, (half + 1) * 512)
            ps = self.PS()
            for kc in range(8):
                self.mm(ps[:, :], cat[kc][:, tsl], wov[kc // 4][:, kc % 4, hs], kc == 0, kc == 7, [cat[kc], wo[kc // 4]], [ps])
            self.stt(y[:, hs], xr[:, hs], ALPHA, ps[:, :], ALU.mult, ALU.add, [xr, ps], [y])
        self.ln_tile(y, y[:, :], self.row(0), self.row(1))
        db, dap = dst(j)
        self.load("sp", db, dap, y, y[:, :])
        self.transpose_tile(y, y[:, :], j, router_layer=1)


K.odd_mixer = odd_mixer


def _v3(ap, inner):
    return ap.rearrange("p (a b) -> p a b", b=inner)


def _bc(ap2, n):
    return ap2.unsqueeze(2).broadcast_to([ap2.shape[0], ap2.shape[1], n])


def even_mixer(self, src, dst):
    S, NT, NG = self.S, self.NT, self.NG
    W = self.W
    Win = W["ev_w_in"]
    sl = self.slab
    P2 = self.P2
    XT = self.XT
    c = self.c
    sm = self.small
    X = mybir.AxisListType.X
    self.load_rows([0, 1, 8])
    self.load_srow(9, 0, 20)
    self.load_srow(11, 32, 48)
    OTS = sl[12:16]
    self.load_mask("mask_sb")
    OTa = self.acc
    for hp in range(4):
        q4 = sl[16 + 4 * (hp % 2):20 + 4 * (hp % 2)]
        QTz, KT, FV = q4[0:2], q4[2], q4[3]
        if hp < 2:
            self.sc.op("pool", lambda e, b=QTz[0]: e.memset(b[64:128, :], 0.0), [], [QTz[0]])
            self.sc.op("pool", lambda e, b=QTz[1]: e.memset(b[0:64, :], 0.0), [], [QTz[1]])
        wq = self.wnext()
        wqv = _wview(wq, 512)
        for i, c0 in enumerate((2576, 3088, 3600)):
            self.load("pool", wq, wqv[:, :, i * 128:(i + 1) * 128], Win, _kp(Win[:, c0 + hp * 128:c0 + (hp + 1) * 128]))
        for g in range(NG):
            gsl = slice(g * 512, (g + 1) * 512)
            for i in range(2):
                ps = self.PS()
                for k in range(8):
                    self.mm(ps[:, :], wqv[:, k, i * 128:(i + 1) * 128], XT[k][:, gsl], k == 0, k == 7, [wq, XT[k]], [ps])
                if i == 0:
                    self.cp("act", QTz[0][0:64, gsl], ps[0:64, :], [ps], [QTz[0]])
                    self.cp("act", QTz[1][64:128, gsl], ps[64:128, :], [ps], [QTz[1]])
                else:
                    self.cp("dve", KT[:, gsl], ps[:, :], [ps], [KT])
        for j in range(NT):
            tsl = slice(j * 128, (j + 1) * 128)
            ps = self.PS()
            for k in range(8):
                self.mm(ps[:, 0:128], XT[k][:, tsl], wqv[:, k, 256:384], k == 0, k == 7, [wq, XT[k]], [ps])
            self.evac(FV[:, tsl], ps[:, 0:128], [ps], [FV])
        _sb_pipe(self, hp, QTz, KT, FV, OTS[hp])
    ps_save = self.ps
    xtf_save = self.xtf
    self.xtf = [P2[0], P2[3]]
    allps = list(self.ps) + list(self.acc)
    self.ps = allps[0:4]
    YD, YO = allps[4:6], allps[6:8]
    XS_T = list(sl[8:12]) + list(sl[16:20])
    BT, CT = sl[20:22], sl[22:24]
    T4 = self.T4
    dests = XS_T + list(BT) + list(CT)
    for pnl in range(3):
        wp = self.wnext()
        wpv = _wview(wp, 512)
        self.load("pool", wp, wpv, Win, _kp(Win[:, 1024 + pnl * 512:1024 + (pnl + 1) * 512]))
        for q in range(4):
            cc = pnl * 4 + q
            for g in range(NG):
                gsl = slice(g * 512, (g + 1) * 512)
                ps = self.PS()
                for k in range(8):
                    self.mm(ps[:, :], wpv[:, k, q * 128:(q + 1) * 128], XT[k][:, gsl], k == 0, k == 7, [wp, XT[k]], [ps])
                RAWb = T4[g % 2]
                RAW = _bfv(RAWb)
                if g == 0:
                    self.sc.op("pool", lambda e, RAW=RAW: e.memset(RAW[:, 0:3], 0.0), [], [RAWb])
                else:
                    prev = _bfv(T4[(g - 1) % 2])
                    self.cp("pool", RAW[:, 0:3], prev[:, 512:515], [T4[(g - 1) % 2]], [RAWb])
                self.cp("act", RAW[:, 3:515], ps[:, :], [ps], [RAWb])
                DGb = T4[2 + cc % 2]
                DG = _bfv(DGb)
                if g == 0:
                    for tap in range(4):
                        self.ts("dve", DG[:, tap * 128:(tap + 1) * 128], c["identb"][:, :], self.cols[:, cc * 4 + tap:cc * 4 + tap + 1],
                                None, ALU.mult, None, [c["identb"], self.cols], [DGb])
                cps = self.PS()
                for tap in range(4):
                    self.mm(cps[:, :], DG[:, tap * 128:(tap + 1) * 128], RAW[:, tap:tap + 512], tap == 0, tap == 3, [DGb, RAWb], [cps])
                self.act(dests[cc][:, gsl], cps[:, :], AF.Silu, [cps, self.cols], [dests[cc]], bias=self.cols[:, 48 + cc:49 + cc])
    wd = self.wnext()
    wdv = wd.t[:, 0:128].rearrange("p (k c) -> p k c", c=16)
    self.load("pool", wd, wdv, Win, _kp(Win[:, 2560:2576]))
    DT, DA = self.dtb, self.dab
    AROW = self.small2b
    self.act(AROW[:, 0:16], self.srow[:, 48:64], AF.Exp, [self.srow], [AROW])
    self.ts("pool", AROW[:, 0:16], AROW[:, 0:16], -1.0, None, ALU.mult, None, [AROW], [AROW])
    for j in range(NT):
        tsl = slice(j * 128, (j + 1) * 128)
        ps = self.PS()
        for k in range(8):
            self.mm(ps[:, 0:16], XT[k][:, tsl], wdv[:, k, :], k == 0, k == 7, [XT[k], wd], [ps])
        self.tt("dve", sm[:, 0:16], ps[:, 0:16], self.srow[:, 32:48], ALU.add, [ps, self.srow], [sm])
        self.act(sm[:, 0:16], sm[:, 0:16], AF.Exp, [sm], [sm])
        self.act(DT[:, j, :], sm[:, 0:16], AF.Ln, [sm], [DT], bias=1.0)
        self.tt("dve", DA[:, j, :], DT[:, j, :], AROW[:, 0:16], ALU.mult, [DT, AROW], [DA])
    for half in range(2):
        wz = self.wnext()
        wzv = _wview(wz, 512)
        self.load("pool", wz, wzv, Win, _kp(Win[:, half * 512:(half + 1) * 512]))
        for j in range(NT):
            tsl = slice(j * 128, (j + 1) * 128)
            ps = self.PS()
            for k in range(8):
                self.mm(ps[:, :], XT[k][:, tsl], wzv[:, k, :], k == 0, k == 7, [XT[k], wz], [ps])
            szb = self.tmpA[j % 2]
            self.act(szb[:, 0:512], ps[:, :], AF.Silu, [ps], [szb])
            self.load("sp", self.zscr, self.zscr[j * 128:(j + 1) * 128, half * 512:(half + 1) * 512], szb, szb[:, 0:512])
    wo = [self.wnext(), self.wnext(), self.wnext()]
    wov = [_wview(w_, 1024) for w_ in wo]
    Wo = W["ev_w_out"]
    for i in range(3):
        self.load("pool", wo[i], wov[i], Wo, _kp(Wo[i * 512:(i + 1) * 512, :]))
    XSTOKb, SEGb, GTBb = T4[0], T4[1], T4[3]
    Dgb = SEGb
    XSTOK, SEG = _f32v(XSTOKb), _f32v(SEGb)
    Dg = SEG
    GTB = _bfv(GTBb)
    XSPb, YNb = P2[0], P2[3]
    XSP, YN = _bfv(XSPb), _bfv(YNb)
    XSPPb = [P2[1], P2[2]]
    XSPP = [_bfv(b) for b in XSPPb]
    BTOKb = [self.H2[2], self.H2[3]]
    HT = [P2[5], P2[6]]
    HTbb = P2[7]
    HTb = [_bfv(HTbb)[:, 0:512], _bfv(HTbb)[:, 512:1024]]
    YNTh = [self.H2[0], self.H2[1]]
    TMPb = P2[4]
    self.xtf = [P2[4], P2[3]]
    Y0b = [self.tmpA[1], T4[2]]
    Y0 = [self.tmpA[1][:, :], _f32v(T4[2])]
    ssms = [self.ssm, self.ssm2]
    CBM = self.cbm
    self.sc.op("pool", lambda e: e.memset(GTB, 0.0), [], [GTBb])
    for g in range(2):
        self.sc.op("pool", lambda e, g=g: e.memset(HT[g][:, :], 0.0), [], [HT[g]])
    self.sc.op("pool", lambda e: e.memset(_bfv(HTbb), 0.0), [], [HTbb])

    def stage_a(j):
        tsl = slice(j * 128, (j + 1) * 128)
        ssm = ssms[j % 2]
        ACUM, EA, DTW = ssm[:, 0:16], ssm[:, 16:32], ssm[:, 32:48]
        DEC = [ssm[:, 48:64], ssm[:, 64:80]]
        BTOK = BTOKb[j % 2]
        pb = self.PS()
        pbv = _bfv(pb)
        for cc in range(8):
            self.tr(pbv[:, cc * 128:(cc + 1) * 128], XS_T[cc][:, tsl], c["identb"][:, :], [XS_T[cc], c["identb"]], [pb])
        self.cp("act", XSTOK, pbv, [pb], [XSTOKb])
        pb2 = self.PS()
        pb2v = _bfv(pb2)
        for g in range(2):
            self.tr(pb2v[:, g * 128:(g + 1) * 128], BT[g][:, tsl], c["identb"][:, :], [BT[g], c["identb"]], [pb2])
        self.cp("dve", BTOK[:, 0:256], pb2v[:, 0:256], [pb2], [BTOK])
        ps = self.PS()
        self.mm(ps[:, 0:16], c["tri2"][:, :], DA[:, j, :], True, True, [c["tri2"], DA], [ps])
        self.cp("dve", ACUM, ps[:, 0:16], [ps], [ssm])
        self.act(EA, ACUM, AF.Exp, [ssm], [ssm])
        ps = self.PS()
        self.mm(ps[:, 0:16], c["lastsel"][:, :], ACUM, True, True, [c["lastsel"], ssm], [ps])
        self.tt("dve", DTW, ps[:, 0:16], ACUM, ALU.subtract, [ps, ssm], [ssm])
        self.act(DTW, DTW, AF.Exp, [ssm], [ssm])
        self.tt("dve", DTW, DTW, DT[:, j, :], ALU.mult, [ssm, DT], [ssm])
        for c2 in range(2):
            ps = self.PS()
            self.mm(ps[:, 0:16], c["sellast"][:, c2, :], ACUM, True, True, [c["sellast"], ssm], [ps])
            self.act(DEC[c2], ps[:, 0:16], AF.Exp, [ps], [ssm])
        self.tt("dve", _v3(Dg, 64), _bc(ACUM, 64), c["i64x2"][:, :].unsqueeze(1).broadcast_to([128, 16, 64]), ALU.mult,
                [ssm, c["i64x2"]], [Dgb])
        p1s = []
        for hb in range(2):
            p1 = self.PS()
            p1s.append(p1)
            self.mm(p1[:, :], c["bd"][:, :], Dg[:, hb * 512:(hb + 1) * 512], True, True, [c["bd"], Dgb], [p1])
        for hb in range(2):
            p1 = p1s[hb]
            self.tt("dve", _v3(SEG[:, hb * 512:(hb + 1) * 512], 64), _v3(p1[:, :], 64), _bc(ssm[:, hb * 8:hb * 8 + 8], 64),
                    ALU.subtract, [p1, ssm], [SEGb])
        self.ts("pool", SEG, SEG, 0.0, None, ALU.min, None, [SEGb], [SEGb])
        self.act(SEG, SEG, AF.Exp, [SEGb], [SEGb])
        ps = self.PS()
        for c2 in range(2):
            csl = slice(j * 128 + c2 * 64, j * 128 + c2 * 64 + 64)
            for g in range(2):
                self.sc.op("pe", lambda e, ps=ps, c2=c2, g=g, csl=csl: e.matmul(
                    ps[c2 * 64:c2 * 64 + 64, g * 64:(g + 1) * 64], BT[g][:, csl], CT[g][:, csl], start=True, stop=True,
                    skip_group_check=True), [BT[g], CT[g]], [ps])
        self.tt("dve", _v3(CBM[:, :], 64), _v3(ps[:, 0:128], 64), c["trimask"][:, :].unsqueeze(1).broadcast_to([128, 2, 64]),
                ALU.mult, [ps, c["trimask"]], [CBM])
        for c2 in range(2):
            lo, hi = c2 * 64, c2 * 64 + 64
            out_ap = GTB[lo:hi, :].rearrange("p (h x) -> p h x", x=128)[:, :, lo:hi].rearrange("p (g r) l -> p g r l", g=2)
            in0 = SEG[lo:hi, :].rearrange("p (g r l) -> p g r l", g=2, r=8)
            in1 = _v3(CBM[lo:hi, :], 64).unsqueeze(2).broadcast_to([64, 2, 8, 64])
            self.tt("dve", out_ap, in0, in1, ALU.mult, [SEGb, CBM], [GTBb])
        self.tt("pool", _v3(XSP, 64), _v3(XSTOK, 64), _bc(DT[:, j, :], 64), ALU.mult, [XSTOKb, DT], [XSPb])
        self.tt("pool", _v3(XSPP[j % 2], 64), _v3(XSTOK, 64), _bc(DTW, 64), ALU.mult, [XSTOKb, ssm], [XSPPb[j % 2]])
        for h in range(16):
            self.sc.op("pe", lambda e, h=h: e.matmul(
                YD[h // 8][:, (h % 8) * 64:(h % 8 + 1) * 64], GTB[:, h * 128:(h + 1) * 128], XSP[:, h * 64:(h + 1) * 64],
                start=True, stop=True, skip_group_check=True), [GTBb, XSPb], [YD[h // 8]])
        self.tt("pool", _v3(SEG, 64), _v3(XSTOK, 64), _bc(self.srow[:, 64:80], 64), ALU.mult, [XSTOKb, self.srow], [SEGb])
        for g in range(2):
            hs = slice(g * 512, (g + 1) * 512)
            self.tt("dve", Y0[j % 2][:, hs], SEG[:, hs], YD[g][:, :], ALU.add, [SEGb, YD[g]], [Y0b[j % 2]])

    def stage_b(j):
        tsl = slice(j * 128, (j + 1) * 128)
        ssm = ssms[j % 2]
        DEC = [ssm[:, 48:64], ssm[:, 64:80]]
        BTOK = BTOKb[j % 2]
        Yb, Yap = Y0b[j % 2], Y0[j % 2]
        for c2 in range(2):
            lo, hi = c2 * 64, c2 * 64 + 64
            csl = slice(j * 128 + lo, j * 128 + hi)
            for g in range(2):
                self.sc.op("pe", lambda e, g=g, lo=lo, hi=hi, csl=csl: e.matmul(
                    YO[g][lo:hi, :], CT[g][:, csl], HTb[g], start=True, stop=True, skip_group_check=True),
                    [CT[g], HTbb], [YO[g]])
            for g in range(2):
                st = self.PS()
                self.mm(st[:, :], BTOK[lo:hi, g * 128:(g + 1) * 128], XSPP[j % 2][lo:hi, g * 512:(g + 1) * 512], True, True,
                        [BTOK, XSPPb[j % 2]], [st])
                self.tt("dve", _v3(HT[g][:, :], 64), _v3(HT[g][:, :], 64), _bc(DEC[c2][:, g * 8:(g + 1) * 8], 64), ALU.mult,
                        [HT[g], ssm], [HT[g]])
                self.tt("dve", HT[g][:, :], HT[g][:, :], st[:, :], ALU.add, [HT[g], st], [HT[g]])
                self.cp("act", HTb[g], HT[g][:, :], [HT[g]], [HTbb])
        SZb = self.tmpA[0]
        self.load("sp", SZb, SZb[:, :], self.zscr, self.zscr[tsl, :])
        for g in range(2):
            hs = slice(g * 512, (g + 1) * 512)
            self.tt("dve", _v3(TMPb[:, :], 64), _v3(YO[g][:, :], 64), _bc(ssm[:, 16 + g * 8:16 + g * 8 + 8], 64), ALU.mult,
                    [YO[g], ssm], [TMPb])
            self.tt("pool", Yap[:, hs], Yap[:, hs], TMPb[:, :], ALU.add, [Yb, TMPb], [Yb])
        self.tt("pool", Yap, Yap, SZb[:, :], ALU.mult, [Yb, SZb], [Yb])
        self.tt("pool", SZb[:, :], Yap, Yap, ALU.mult, [Yb], [SZb])
        self.sc.op("dve", lambda e: e.reduce_sum(out=sm[:, 0:1], in_=SZb[:, :], axis=X), [SZb], [sm])
        self.ts("dve", sm[:, 0:1], sm[:, 0:1], 1.0 / 1024, RMS_EPS, ALU.mult, ALU.add, [sm], [sm])
        self.act(sm[:, 0:1], sm[:, 0:1], AF.Ln, [sm], [sm])
        self.act(sm[:, 1:2], sm[:, 0:1], AF.Exp, [sm], [sm], scale=-0.5)
        self.stt(YN, Yap, sm[:, 1:2], self.rows[:, 2, :], ALU.mult, ALU.mult, [Yb, sm, self.rows], [YNb])
        pb = self.PS()
        pbv = _bfv(pb)
        for cc in range(8):
            self.tr(pbv[:, cc * 128:(cc + 1) * 128], YN[:, cc * 128:(cc + 1) * 128], c["identb"][:, :], [YNb, c["identb"]], [pb])
        self.cp("act", YNTh[0][:, :], pbv[:, 0:512], [pb], [YNTh[0]])
        self.cp("act", YNTh[1][:, :], pbv[:, 512:1024], [pb], [YNTh[1]])
        xr = self.xres[j % 2]
        sb_, sap = src(j)
        self.load("sp", xr, xr[:, :], sb_, sap)
        for half in range(2):
            hs = slice(half * 512, (half + 1) * 512)
            ps = self.PS()
            for kc in range(12):
                lhsT = YNTh[kc // 4][:, (kc % 4) * 128:(kc % 4 + 1) * 128] if kc < 8 else OTS[kc - 8][:, tsl]
                rb_ = YNTh[kc // 4] if kc < 8 else OTS[kc - 8]
                self.mm(ps[:, :], lhsT, wov[kc // 4][:, kc % 4, hs], kc == 0, kc == 11, [rb_, wo[kc // 4]], [ps])
            self.stt(SZb[:, hs], xr[:, hs], ALPHA, ps[:, :], ALU.mult, ALU.add, [xr, ps], [SZb])
        self.ln_tile(SZb, SZb[:, :], self.row(0), self.row(1))
        db, dap = dst(j)
        self.load("sp", db, dap, SZb, SZb[:, :])
        self.transpose_tile(SZb, SZb[:, :], j, router_layer=0)

    psA, psB = allps[0:2], allps[2:4]

    def rec_stage(fn, j, banks):
        self.ps = banks
        return self.sc.record(lambda: fn(j))
    self.sc.replay(rec_stage(stage_a, 0, psA))
    for j in range(NT):
        lb = rec_stage(stage_b, j, psB)
        la = rec_stage(stage_a, j + 1, psA) if j + 1 < NT else []
        self.sc.replay(la, lb)
    self.ps = ps_save
    self.xtf = xtf_save


K.even_mixer = even_mixer


def _pipe(n, stages):
    maxs = max(s for s, _ in stages)
    for t in range(n + maxs):
        for s, fn in stages:
            u = t - s
            if 0 <= u < n:
                fn(u)


def _units(NG, hhs=(0, 1)):
    return [(hh, G, idx, i, 4 * (G + 1)) for hh in hhs for G in range(NG)
            for idx, i in enumerate(range(4 * (G + 1) - 1, -1, -1))]


def _sb_pipe(self, hp, QTz, KT, FV, OTSb):
    P2, H2, c = self.P2, self.H2, self.c
    units = _units(self.NG)
    n = len(units)
    E, Ecs = P2[0:3], P2[3:5]
    Lb, Rb = H2[0:2], H2[2]
    Wb = [H2[3], P2[5]]
    Wt = [H2[3][:, :], _bfv(P2[5])[:, 0:512]]
    zb, cb = {}, {}

    def s_z(u):
        hh, G, idx, i, nkb = units[u]
        lo, hi = hh * 64, hh * 64 + 64
        z = self.PS()
        zb[u] = z
        self.mm(z[:, :], KT[:, i * 128:(i + 1) * 128], QTz[hh][:, G * 512:(G + 1) * 512], True, True, [KT, QTz[hh]], [z])

    def s_E(u):
        hh, G, idx, i, nkb = units[u]
        z = zb.pop(u)
        e = E[u % 3]
        self.act(e[:, :], z[:, :], AF.Exp, [z], [e], scale=0.125)
        if i >= 4 * G:
            self.tt("dve", e[:, :], e[:, :], self.maskbuf[:, i - 4 * G, :], ALU.mult, [e, self.maskbuf], [e])

    def s_Lb(u):
        e, l = E[u % 3], Lb[u % 2]
        self.act(l[:, :], e[:, :], AF.Ln, [e], [l], bias=1.0)

    def s_CS(u):
        hh, G, idx, i, nkb = units[u]
        l = Lb[u % 2]
        CS = self.PS()
        cb[u] = CS
        self.mm(CS[:, :], c["uincl"][:, :], l[:, :], True, idx == 0, [c["uincl"], l], [CS])
        if idx > 0:
            self.mm(CS[:, :], c["onesb"][:, :], Rb[:, :], False, True, [c["onesb"], Rb], [CS])
        if idx < nkb - 1:
            if idx == 0:
                self.cp("dve", Rb[:, :], l[:, :], [l], [Rb])
            else:
                self.tt("pool", Rb[:, :], Rb[:, :], l[:, :], ALU.add, [Rb, l], [Rb])

    def s_Ecs(u):
        CS = cb.pop(u)
        ec = Ecs[u % 2]
        self.act(ec[:, :], CS[:, :], AF.Exp, [CS], [ec], scale=-1.0)
        self.tt("dve", Wt[u % 2], E[u % 3][:, :], ec[:, :], ALU.mult, [E[u % 3], ec], [Wb[u % 2]])

    def s_PV(u):
        hh, G, idx, i, nkb = units[u]
        lo, hi = hh * 64, hh * 64 + 64
        OT = self.acc[(hh * self.NG + G) % 2]
        self.mm(OT[:, :], FV[:, i * 128:(i + 1) * 128], Wt[u % 2], idx == 0, idx == nkb - 1, [FV, Wb[u % 2]], [OT])
        if idx == nkb - 1:
            self.cp("act", OTSb[lo:hi, G * 512:(G + 1) * 512], OT[lo:hi, :], [OT], [OTSb])

    _pipe(n, [(0, s_z), (2, s_Lb), (2, s_CS), (3, s_Ecs), (1, s_E), (4, s_PV)])


def _fox_pipe(self, hp, hh_, FQz, FK, FV, FHQs, CSPT, OTb, scale):
    P2, H2, c = self.P2, self.H2, self.c
    units = _units(self.NG, (hh_,))
    n = len(units)
    A = P2[0:3]
    Wb = H2[0:2]
    OT, DEN = self.acc
    zb = {}

    def s_z(u):
        hh, G, idx, i, nkb = units[u]
        lo, hi = hh * 64, hh * 64 + 64
        z = self.PS()
        zb[u] = z
        self.mm(z[:, :], FK[:, i * 128:(i + 1) * 128], FQz[hh][:, G * 512:(G + 1) * 512], True, True, [FK, FQz[hh]], [z])

    def s_A(u):
        hh, G, idx, i, nkb = units[u]
        z = zb.pop(u)
        a = A[u % 3]
        fb = FHQs[hh][G // 2]
        fap = _f32v(fb)[:, (G % 2) * 512:(G % 2 + 1) * 512]
        self.tt("dve", a[:, :], z[:, :], fap, ALU.subtract, [z, fb], [a])
        if i >= 4 * G:
            self.tt("pool", a[:, :], a[:, :], self.maskbuf[:, i - 4 * G, :], ALU.add, [a, self.maskbuf], [a])

    def s_W(u):
        hh, G, idx, i, nkb = units[u]
        h = 2 * hp + hh
        a, w = A[u % 3], Wb[u % 2]
        self.act(w[:, :], a[:, :], AF.Exp, [a, CSPT], [w], bias=CSPT[:, i, h:h + 1], scale=scale)

    def s_PV(u):
        hh, G, idx, i, nkb = units[u]
        lo, hi = hh * 64, hh * 64 + 64
        w = Wb[u % 2]
        self.mm(OT[:, :], FV[:, i * 128:(i + 1) * 128], w[:, :], idx == 0, idx == nkb - 1, [FV, w], [OT])
        self.mm(DEN[:, :], c["onesb"][:, :], w[:, :], idx == 0, idx == nkb - 1, [c["onesb"], w], [DEN])
        if idx == nkb - 1:
            _attn_finish(self, OT, DEN, hh, OTb, OTb[lo:hi, G * 512:(G + 1) * 512])

    _pipe(n, [(0, s_z), (2, s_W), (1, s_A), (3, s_PV)])


def _mla_pipe(self, hp, QH, KH, VM, OTb, scale):
    P2, H2, c = self.P2, self.H2, self.c
    units = _units(self.NG)
    n = len(units)
    Wf = P2[0:2]
    Wb = H2[0:2]
    OT, DEN = self.acc
    zb = {}

    def s_z(u):
        hh, G, idx, i, nkb = units[u]
        lo, hi = hh * 64, hh * 64 + 64
        plo, phi = hh * 32, hh * 32 + 32
        ksl, gsl = slice(i * 128, (i + 1) * 128), slice(G * 512, (G + 1) * 512)
        z = self.PS()
        zb[u] = z
        self.mm(z[:, :], KH[hh][:, ksl], QH[hh][:, gsl], True, True, [KH[hh], QH[hh]], [z])

    def s_W(u):
        hh, G, idx, i, nkb = units[u]
        z = zb.pop(u)
        w = Wb[u % 2]
        if i >= 4 * G:
            wf = Wf[u % 2]
            self.act(wf[:, :], z[:, :], AF.Exp, [z], [wf], scale=scale)
            self.tt("pool", w[:, :], wf[:, :], self.maskbuf[:, i - 4 * G, :], ALU.mult, [wf, self.maskbuf], [w])
        else:
            self.act(w[:, :], z[:, :], AF.Exp, [z], [w], scale=scale)

    def s_PV(u):
        hh, G, idx, i, nkb = units[u]
        h = 2 * hp + hh
        lo, hi = hh * 64, hh * 64 + 64
        w = Wb[u % 2]
        vs = VM[i // 4][:, (i % 4) * 512 + hp * 128:(i % 4) * 512 + (hp + 1) * 128]
        self.mm(OT[:, :], vs, w[:, :], idx == 0, idx == nkb - 1, [VM[i // 4], w], [OT])
        self.mm(DEN[:, :], c["onesb"][:, :], w[:, :], idx == 0, idx == nkb - 1, [c["onesb"], w], [DEN])
        if idx == nkb - 1:
            _attn_finish(self, OT, DEN, hh, OTb, OTb[lo:hi, G * 512:(G + 1) * 512])

    _pipe(n, [(0, s_z), (1, s_W), (2, s_PV)])
```

```python
import math
from contextlib import ExitStack
import numpy as np
import ml_dtypes
import concourse.bass as bass
import concourse.mybir as mybir
from concourse.bass_utils import run_bass_kernel_spmd

F32 = mybir.dt.float32
BF16 = mybir.dt.bfloat16
AF = mybir.ActivationFunctionType
ALU = mybir.AluOpType

D = 1024
NCORES = 8
ALPHA = (2.0 * 2) ** 0.25
LN_EPS = 1e-5
RMS_EPS = 1e-6
EVEN_IN = 4112
ODD_IN = 1960

ENGS = ("pe", "act", "dve", "pool", "sp")


class Buf:
    _n = 0

    def __init__(self, name, t):
        self.name = name
        self.t = t
        self.w = None
        self.r = {}
        self.excl = False
        self.sem = None
        self.semcnt = 0
        Buf._n += 1
        self.id = Buf._n

    def __getitem__(self, k):
        return self.t[k]


class Sched:
    def __init__(self, nc, es):
        self.nc = nc
        self.es = es
        self.ops = {e: [] for e in ENGS}
        self.cnt = {e: 0 for e in ENGS}
        self.known = {e: {} for e in ENGS}
        self.snap = {e: [None] for e in ENGS}
        self.rec = None
        self.sems = {e: es.enter_context(nc.semaphore("c_" + e)) for e in ENGS}
        self.nsem = len(ENGS)

    def _dma_sem(self, b):
        if b.sem is None:
            b.sem = self.es.enter_context(self.nc.semaphore("d_%d" % b.id))
            self.nsem += 1
        return b.sem

    def _waits(self, eng, reads, writes):
        waits = {}

        def need(ev, same_ok):
            if ev is None:
                return
            k, v = ev
            if k == eng and (same_ok or eng in ("pe", "sp")):
                return
            if v > waits.get(k, 0):
                waits[k] = v
        for b in reads:
            need(b.w, False)
            if b.excl:
                for k, v in b.r.items():
                    need((k, v), True)
        for b in writes:
            need(b.w, False)
            for k, v in b.r.items():
                need((k, v), False)
        kn = self.known[eng]
        out = []
        for k, v in waits.items():
            if kn.get(k, 0) >= v:
                continue
            out.append((k, v))
            kn[k] = v
            if isinstance(k, str):
                sn = self.snap[k][v]
                if sn is not None:
                    for k2, v2 in sn.items():
                        if k2 != eng and kn.get(k2, 0) < v2:
                            kn[k2] = v2
        return out

    def op(self, eng, emit, reads=(), writes=()):
        if self.rec is not None:
            self.rec.append(("op", (eng, emit, tuple(reads), tuple(writes)), {}))
            return
        waits = self._waits(eng, reads, writes)
        self.cnt[eng] += 1
        idx = self.cnt[eng]
        ev = (eng, idx)
        for b in reads:
            b.r[eng] = idx
        for b in writes:
            b.w = ev
            b.r = {}
        self.snap[eng].append({k: v for k, v in self.known[eng].items() if isinstance(k, str)})
        self.ops[eng].append((waits, emit, None))

    def dma(self, q, out_ap, in_ap, dst, src, extra_reads=(), **kw):
        if self.rec is not None:
            self.rec.append(("dma", (q, out_ap, in_ap, dst, src, extra_reads), kw))
            return
        reads = [src] + list(extra_reads)
        waits = self._waits(q, reads, [dst])
        sem = self._dma_sem(dst)
        dst.semcnt += 16
        ev = (("dma", dst.id, sem), dst.semcnt)
        src.r[ev[0]] = ev[1]
        for b in extra_reads:
            b.r[ev[0]] = ev[1]
        dst.w = ev
        dst.r = {}

        def emit(e):
            return e.dma_start(out=out_ap, in_=in_ap, **kw)
        self.ops[q].append((waits, emit, sem))

    def record(self, fn):
        assert self.rec is None
        self.rec = []
        fn()
        lst, self.rec = self.rec, None
        return lst

    def replay(self, la, lb=()):
        ia = ib = 0
        na, nb = len(la), len(lb)
        while ia < na or ib < nb:
            if ib >= nb or (ia < na and ia * nb <= ib * na):
                kind, a, k = la[ia]
                ia += 1
            else:
                kind, a, k = lb[ib]
                ib += 1
            if kind == "op":
                self.op(*a)
            else:
                self.dma(*a, **k)

    def finish_waits(self, eng, bufs):
        waits = self._waits(eng, bufs, [])
        self.ops[eng].append((waits, None, None))

    def emit_all(self):
        nc = self.nc
        handles = {"pe": "tensor", "act": "scalar", "dve": "vector", "pool": "gpsimd", "sp": "sync"}
        with nc.Block() as block:
            for e in ENGS:
                ops = self.ops[e]
                esem = self.sems[e]

                def body(eng, ops=ops, esem=esem):
                    for waits, emit, dsem in ops:
                        for k, v in waits:
                            s = self.sems[k] if isinstance(k, str) else k[2]
                            eng.wait_ge(s, v)
                        if emit is None:
                            continue
                        ins = emit(eng)
                        if dsem is not None:
                            ins.then_inc(dsem, 16)
                        else:
                            ins.then_inc(esem, 1)
                getattr(block, handles[e])(body)


def _consts(S):
    c = {}
    p = np.arange(128)
    c["ident"] = np.eye(128, dtype=np.float32)
    c["identb"] = np.eye(128, dtype=np.float32).astype(ml_dtypes.bfloat16)
    c["onesb"] = np.ones((128, 128), np.float32).astype(ml_dtypes.bfloat16)
    c["uincl"] = (p[:, None] >= p[None, :]).astype(np.float32).astype(ml_dtypes.bfloat16)
    s = p[None, :, None]
    t = np.arange(512)[None, None, :]
    m = np.arange(4)[:, None, None]
    c["mask_sb"] = ((s + 128 * m) < t).astype(np.float32).astype(ml_dtypes.bfloat16)
    c["mask_mla"] = (((s + 128 * m) // 64) <= (t // 64)).astype(np.float32).astype(ml_dtypes.bfloat16)
    c["negm_fox"] = np.where((s + 128 * m) <= t, 0.0, -30000.0).astype(np.float32).astype(ml_dtypes.bfloat16)
    c2 = p // 64
    l = p % 64
    c["tri2"] = ((c2[:, None] == c2[None, :]) & (l[:, None] <= l[None, :])).astype(np.float32)
    c["bd"] = (c2[:, None] == c2[None, :]).astype(np.float32)
    c["lastsel"] = (p[:, None] == (64 * c2[None, :] + 63)).astype(np.float32)
    sl = np.zeros((2, 128, 128), np.float32)
    sl[0, 63, :] = 1.0
    sl[1, 127, :] = 1.0
    c["sellast"] = sl
    c["i64x2"] = (l[:, None] == np.arange(64)[None, :]).astype(np.float32)
    c["trimask"] = (np.arange(64)[None, :] >= l[:, None]).astype(np.float32)
    c["sel8"] = np.repeat(np.eye(8, dtype=np.float32), 128, axis=1)
    ng = np.where(np.arange(64)[None, :] < l[:, None], -30000.0, 0.0).astype(np.float32)
    c["negm_ssd"] = np.tile(ng[:, None, :], (1, 16, 1)).reshape(128, 1024).astype(ml_dtypes.bfloat16)
    inv = 10000.0 ** (-(np.arange(0, 32, 2, dtype=np.float32) / 32.0))
    ang = np.arange(S, dtype=np.float32)[None, :] * inv[:, None]
    cos = np.cos(ang).astype(np.float32)
    sin = np.sin(ang).astype(np.float32)
    cosT = np.concatenate([cos, cos], 0)
    sinT = np.concatenate([-sin, sin], 0)
    c["ropec"] = np.concatenate([cosT, cosT, cosT, cosT], 0).astype(np.float32)
    c["ropes"] = np.concatenate([sinT, sinT, sinT, sinT], 0).astype(np.float32)
    return c


class K:
    def __init__(self, S, NSEQ, layers=(0, 1), parts=("mix", "moe"), taps=()):
        self.S, self.NSEQ = S, NSEQ
        self.NT = S // 128
        self.GW = 512
        self.NG = S // 512
        self.layers, self.parts, self.taps = layers, parts, taps
        self.nc = bass.Bass("TRN2", target_bir_lowering=False)
        self.es = ExitStack()
        self.sc = Sched(self.nc, self.es)
        self.psi = 0
        self.evi = 0
        self.dbg = {}
        self.tap_out = {}

    def dram_in(self, name, shape, dt=F32):
        t = self.nc.dram_tensor(name, list(shape), dt, kind="ExternalInput")
        return Buf(name, t)

    def dram_out(self, name, shape, dt=F32):
        t = self.nc.dram_tensor(name, list(shape), dt, kind="ExternalOutput")
        return Buf(name, t)

    def sb(self, name, shape, dt=F32):
        t = self.es.enter_context(self.nc.sbuf_tensor("s_" + name, list(shape), dt))
        return Buf(name, t)

    def PS(self):
        b = self.ps[self.psi % len(self.ps)]
        self.psi += 1
        return b

    def mm(self, out, lhsT, rhs, start, stop, reads, writes):
        self.sc.op("pe", lambda e: e.matmul(out, lhsT, rhs, start=start, stop=stop), reads, writes)

    def tr(self, out, in_, ident, reads, writes):
        self.sc.op("pe", lambda e: e.transpose(out, in_, ident), reads, writes)

    def act(self, out, in_, func, reads, writes, bias=None, scale=1.0):
        kw = {}
        if bias is not None:
            kw["bias"] = bias
        self.sc.op("act", lambda e: e.activation(out=out, in_=in_, func=func, scale=scale, **kw), reads, writes)

    def tt(self, eng, out, in0, in1, op, reads, writes):
        self.sc.op(eng, lambda e: e.tensor_tensor(out=out, in0=in0, in1=in1, op=op), reads, writes)

    def ts(self, eng, out, in0, s1, s2, op0, op1, reads, writes):
        if op1 is None:
            self.sc.op(eng, lambda e: e.tensor_scalar(out=out, in0=in0, scalar1=s1, scalar2=None, op0=op0), reads, writes)
        else:
            self.sc.op(eng, lambda e: e.tensor_scalar(out=out, in0=in0, scalar1=s1, scalar2=s2, op0=op0, op1=op1), reads, writes)

    def stt(self, out, in0, scalar, in1, op0, op1, reads, writes):
        self.sc.op("dve", lambda e: e.scalar_tensor_tensor(out=out, in0=in0, scalar=scalar, in1=in1, op0=op0, op1=op1), reads, writes)

    def cp(self, eng, out, in_, reads, writes):
        if eng == "act":
            self.sc.op("act", lambda e: e.activation(out=out, in_=in_, func=AF.Copy), reads, writes)
        else:
            self.sc.op(eng, lambda e: e.tensor_copy(out=out, in_=in_), reads, writes)

    def evac(self, out, in_, reads, writes):
        self.evi += 1
        self.cp("act" if self.evi % 2 else "dve", out, in_, reads, writes)

    def load(self, q, dstbuf, out_ap, srcbuf, in_ap, **kw):
        self.sc.dma(q, out_ap, in_ap, dstbuf, srcbuf, **kw)

    def setup(self):
        S, NSEQ, NT = self.S, self.NSEQ, self.NT
        self.x = self.dram_in("x", [NSEQ, S, D])
        self.out = self.dram_out("out", [NSEQ, S, D])
        self.xscr = Buf("xscr", self.nc.dram_tensor("xscr", [S, D], F32, kind="Internal"))
        W = {}
        W["ev_w_in"] = self.dram_in("ev_w_in", [D, EVEN_IN])
        W["ev_w_out"] = self.dram_in("ev_w_out", [1536, D])
        W["od_w_in"] = self.dram_in("od_w_in", [D, ODD_IN + 128])
        W["od_w_q_up"] = self.dram_in("od_w_q_up", [256, 512 + 256 + 256])
        W["od_w_kv_up"] = self.dram_in("od_w_kv_up", [128, 1024])
        W["od_w_out"] = self.dram_in("od_w_out", [D, D])
        W["moe_w_gate"] = self.dram_in("moe_w_gate", [2, 16, D, 256])
        W["moe_w_up"] = self.dram_in("moe_w_up", [2, 16, D, 256])
        W["moe_w_down"] = self.dram_in("moe_w_down", [2, 16, 256, D])
        W["moe_wr"] = self.dram_in("moe_wr", [2, 128, 8, 20])
        W["rows"] = self.dram_in("rows", [NROWS, D])
        W["cols"] = self.dram_in("cols", [128, NCOLS])
        self.W = W
        cs = _consts(S)
        self.cdram = {}
        for k, v in cs.items():
            dt = BF16 if v.dtype == ml_dtypes.bfloat16 else F32
            self.cdram[k] = self.dram_in("c_" + k, v.shape, dt)
        self.slab = [self.sb("slab%d" % i, [128, 2048], BF16) for i in range(28)]
        self.XT = self.slab[0:8]
        self.wslot = [self.sb("wslot%d" % i, [128, 4096], BF16) for i in range(4)]
        self.wi = 0
        self.ps = [Buf("ps%d" % i, self.es.enter_context(self.nc.psum_tensor("ps%d" % i, [128, 512], F32)))
                   for i in range(8)]
        for b in self.ps:
            b.excl = True
        self.acc = self.ps[6:8]
        self.ps = self.ps[0:6]
        c = {}
        for k in ("ident", "identb", "onesb", "uincl", "tri2", "bd", "lastsel", "i64x2", "trimask"):
            v = cs[k]
            c[k] = self.sb("k_" + k, v.shape, BF16 if v.dtype == ml_dtypes.bfloat16 else F32)
            self.load("sp", c[k], c[k][:, :], self.cdram[k], self.cdram[k][:, :])
        c["sellast"] = self.sb("k_sellast", [128, 2, 128])
        for i in range(2):
            self.load("sp", c["sellast"], c["sellast"][:, i, :], self.cdram["sellast"], self.cdram["sellast"][i, :, :])
        self.c = c
        self.rows = self.sb("rows", [128, NROWS_SB, D])
        self.srow = self.sb("srow", [128, 512])
        self.maskbuf = self.sb("maskbuf", [128, 4, 512], BF16)
        self.P2 = [self.sb("p2_%d" % i, [128, 512]) for i in range(8)]
        self.H2 = [self.sb("h2_%d" % i, [128, 512], BF16) for i in range(4)]
        self.cols = self.sb("cols", [128, NCOLS])
        self.load("sp", self.cols, self.cols[:, :], W["cols"], W["cols"][:, :])
        self.wr = self.sb("wr", [128, 2, 8, 20])
        for l in range(2):
            self.load("sp", self.wr, self.wr[:, l, :, :], W["moe_wr"], W["moe_wr"][l, :, :, :])
        self.gate = self.sb("gate", [128, NT, 16])
        self.xres = [self.sb("xres0", [128, D])] * 2
        self.tmpA = [self.sb("tmpA%d" % i, [128, D]) for i in range(2)]
        self.T4 = [Buf("t4_%d" % i, None) for i in range(4)]
        for i in range(4):
            self.T4[i].t = self.slab[24 + i].t
        self.T4 = self.slab[24:28]
        self.small = self.sb("small", [128, 64])
        self.small2 = self.sb("small2", [128, 8])
        self.small2b = self.sb("small2b", [128, 16])
        self.dtb = self.sb("dtb", [128, self.NT, 16])
        self.dab = self.sb("dab", [128, self.NT, 16])
        self.ssm = self.sb("ssm", [128, 80])
        self.ssm2 = self.sb("ssm2", [128, 80])
        self.cbm = self.sb("cbm", [128, 128])
        self.zscr = Buf("zscr", self.nc.dram_tensor("zscr", [S, D], F32, kind="Internal"))
        self.cspt = self.sb("cspt", [128, self.NT, 8])
        self.xi = 0

    def row(self, r):
        return self.rows[:, r, :]

    def load_srow(self, r, off, n):
        src = self.W["rows"][r:r + 1, 0:n].broadcast_to([128, n])
        self.load("sp", self.srow, self.srow[:, off:off + n], self.W["rows"], src)

    def load_mask(self, name):
        cd = self.cdram[name]
        for m_ in range(4):
            self.load("sp", self.maskbuf, self.maskbuf[:, m_, :], cd, cd[m_, :, :])

    def load_rows(self, idx_list):
        for slot, r in enumerate(idx_list):
            src = self.W["rows"][r:r + 1, :].broadcast_to([128, D])
            self.load("sp", self.rows, self.rows[:, slot, :], self.W["rows"], src)

    def transpose_tile(self, xb, xap, j, router_layer=None):
        c = self.c
        for half in range(2):
            ps = self.PS()
            for kk in range(4):
                k = half * 4 + kk
                self.tr(ps[:, kk * 128:(kk + 1) * 128], xap[:, k * 128:(k + 1) * 128], c["ident"][:, :],
                        [xb, c["ident"]], [ps])
            self.evi += 1
            for kk in range(4):
                k = half * 4 + kk
                self.cp("act" if self.evi % 2 else "dve", self.XT[k][:, j * 128:(j + 1) * 128],
                        ps[:, kk * 128:(kk + 1) * 128], [ps], [self.XT[k]])
            if router_layer is not None and not self.dbg.get('noxtf'):
                xtf = self.xtf[half]
                self.cp("dve", xtf[:, :], ps[:, :], [ps], [xtf])
        if router_layer is not None and not self.dbg.get('norouter'):
            self.router(j, router_layer)

    def router(self, j, l):
        sm = self.small
        ps = self.PS()
        for k in range(8):
            xtf = self.xtf[k // 4]
            self.mm(ps[:, 0:20], xtf[:, (k % 4) * 128:(k % 4 + 1) * 128], self.wr[:, l, k, :], k == 0, k == 7,
                    [xtf, self.wr], [ps])
        lg = sm[:, 0:20]
        rb = self.srow[:, 0:20]
        self.tt("dve", lg, ps[:, 0:20], rb, ALU.add, [ps, self.srow], [sm])
        R, Wr = [sm], [sm]
        self.sc.op("dve", lambda e: e.reduce_max(out=sm[:, 20:21], in_=sm[:, 0:4], axis=mybir.AxisListType.X), R, Wr)
        self.ts("dve", sm[:, 21:25], sm[:, 0:4], sm[:, 20:21], None, ALU.is_equal, None, R, Wr)
        self.ts("dve", sm[:, 25:29], sm[:, 0:4], sm[:, 20:21], None, ALU.subtract, None, R, Wr)
        self.act(sm[:, 25:29], sm[:, 25:29], AF.Exp, R, Wr)
        self.sc.op("dve", lambda e: e.reduce_sum(out=sm[:, 29:30], in_=sm[:, 25:29], axis=mybir.AxisListType.X), R, Wr)
        self.sc.op("dve", lambda e: e.reciprocal(out=sm[:, 30:31], in_=sm[:, 29:30]), R, Wr)
        el = sm[:, 4:20].rearrange("p (g e) -> p g e", e=4)
        ohg_b = sm[:, 21:25].unsqueeze(2).broadcast_to([128, 4, 4])
        prod = sm[:, 32:48].rearrange("p (g e) -> p g e", e=4)
        self.tt("dve", prod, el, ohg_b, ALU.mult, R, Wr)
        prod_t = sm[:, 32:48].rearrange("p (g e) -> p e g", e=4)
        self.sc.op("dve", lambda e: e.reduce_sum(out=sm[:, 48:52], in_=prod_t, axis=mybir.AxisListType.X), R, Wr)
        self.sc.op("dve", lambda e: e.reduce_max(out=sm[:, 52:53], in_=sm[:, 48:52], axis=mybir.AxisListType.X), R, Wr)
        self.ts("dve", sm[:, 53:57], sm[:, 48:52], sm[:, 52:53], None, ALU.is_equal, None, R, Wr)
        self.stt(sm[:, 57:61], sm[:, 53:57], -1e30, sm[:, 48:52], ALU.mult, ALU.add, R, Wr)
        self.sc.op("dve", lambda e: e.reduce_max(out=sm[:, 61:62], in_=sm[:, 57:61], axis=mybir.AxisListType.X), R, Wr)
        self.ts("dve", sm[:, 25:29], sm[:, 57:61], sm[:, 61:62], None, ALU.is_equal, None, R, Wr)
        self.ts("dve", sm[:, 62:63], sm[:, 61:62], sm[:, 52:53], None, ALU.subtract, None, R, Wr)
        self.act(sm[:, 62:63], sm[:, 62:63], AF.Exp, R, Wr)
        self.ts("dve", sm[:, 63:64], sm[:, 62:63], 1.0, None, ALU.add, None, R, Wr)
        self.sc.op("dve", lambda e: e.reciprocal(out=sm[:, 63:64], in_=sm[:, 63:64]), R, Wr)
        self.stt(sm[:, 63:64], sm[:, 63:64], 1.0 / ALPHA, sm[:, 30:31], ALU.mult, ALU.mult, R, Wr)
        self.tt("dve", sm[:, 62:63], sm[:, 62:63], sm[:, 63:64], ALU.mult, R, Wr)
        self.ts("dve", sm[:, 53:57], sm[:, 53:57], sm[:, 63:64], None, ALU.mult, None, R, Wr)
        self.stt(sm[:, 53:57], sm[:, 25:29], sm[:, 62:63], sm[:, 53:57], ALU.mult, ALU.add, R, Wr)
        g4_b = sm[:, 53:57].unsqueeze(1).broadcast_to([128, 4, 4])
        gout = self.gate[:, j, :].rearrange("p (g e) -> p g e", e=4)
        self.tt("dve", gout, ohg_b, g4_b, ALU.mult, R, [self.gate])

    def ln_tile(self, xb, xap, g_row, b_row, eps=LN_EPS):
        sm = self.lnsm
        R, Wr = [sm], [sm]
        for h in range(2):
            self.sc.op("dve", lambda e, h=h: e.bn_stats(out=sm[:, 6 * h:6 * h + 6], in_=xap[:, h * 512:(h + 1) * 512]),
                       [xb], Wr)
        self.sc.op("dve", lambda e: e.bn_aggr(out=sm[:, 12:14], in_=sm[:, 0:12]), R, Wr)
        self.ts("dve", sm[:, 14:15], sm[:, 13:14], eps, None, ALU.add, None, R, Wr)
        self.act(sm[:, 14:15], sm[:, 14:15], AF.Ln, R, Wr)
        self.act(sm[:, 15:16], sm[:, 14:15], AF.Exp, R, Wr, scale=-0.5)
        self.stt(xap, xap, sm[:, 12:13], g_row, ALU.subtract, ALU.mult, [xb, sm, self.rows], [xb])
        self.stt(xap, xap, sm[:, 15:16], b_row, ALU.mult, ALU.add, [xb, sm, self.rows], [xb])

    def wnext(self):
        b = self.wslot[self.wi % len(self.wslot)]
        self.wi += 1
        return b

    def wload_k(self, slot, col0, ncols, wbuf, wap2d, nk=8):
        raise NotImplementedError

    def moe(self, l, src, dst, last):
        S, NT, NG = self.S, self.NT, self.NG
        W = self.W
        XA = [self.slab[8 + j] for j in range(NT)]
        xa = [b.t[:, :].bitcast(F32) for b in XA]
        self.load_rows([2 + 4 * l, 3 + 4 * l])
        for j in range(NT):
            sb_, sap = src(j)
            self.load("sp", XA[j], xa[j], sb_, sap)
        hid = self.hid
        hidv = [h.t[:, :].bitcast(BF16)[:, 0:512] for h in hid]
        for e in range(self.dbg.get('nexp', 16)):
            sa = self.wnext()
            sv = sa.t[:, :].rearrange("p (k c) -> p k c", c=512)
            self.load("pool", sa, sv[:, :, 0:256], W["moe_w_gate"],
                      W["moe_w_gate"][l, e, :, :].rearrange("(k p) f -> p k f", p=128))
            self.load("pool", sa, sv[:, :, 256:512], W["moe_w_up"],
                      W["moe_w_up"][l, e, :, :].rearrange("(k p) f -> p k f", p=128))
            sd = self.wnext()
            dv = sd.t[:, 0:2048].rearrange("p (k c) -> p k c", c=1024)
            self.load("pool", sd, dv, W["moe_w_down"],
                      W["moe_w_down"][l, e, :, :].rearrange("(k p) f -> p k f", p=128))
            for g in range(NG):
                tsl = slice(g * 512, (g + 1) * 512)
                for f in range(2):
                    gps = self.PS()
                    for k in range(8):
                        self.mm(gps[:, :], sv[:, k, f * 128:(f + 1) * 128], self.XT[k][:, tsl], k == 0, k == 7,
                                [sa, self.XT[k]], [gps])
                    ups = self.PS()
                    for k in range(8):
                        self.mm(ups[:, :], sv[:, k, 256 + f * 128:256 + (f + 1) * 128], self.XT[k][:, tsl], k == 0, k == 7,
                                [sa, self.XT[k]], [ups])
                    sg = self.sg[f]
                    self.act(sg[:, :], gps[:, :], AF.Silu, [gps], [sg])
                    self.tt("dve", hidv[f], sg[:, :], ups[:, :], ALU.mult, [sg, ups], [hid[f]])
                for tt_ in range(4):
                    j = g * 4 + tt_
                    for half in range(2):
                        ops = self.PS()
                        for f in range(2):
                            self.mm(ops[:, :], hidv[f][:, tt_ * 128:(tt_ + 1) * 128], dv[:, f, half * 512:(half + 1) * 512],
                                    f == 0, f == 1, [hid[f], sd], [ops])
                        xs_ = xa[j][:, half * 512:(half + 1) * 512]
                        self.stt(xs_, ops[:, :], self.gate[:, j, e:e + 1], xs_, ALU.mult, ALU.add,
                                 [ops, self.gate, XA[j]], [XA[j]])
        for j in range(NT):
            if not self.dbg.get('noln'):
                self.ln_tile(XA[j], xa[j], self.row(0), self.row(1), eps=LN_EPS / (ALPHA * ALPHA))
            db, dap = dst(j)
            self.load("sp", db, dap, XA[j], xa[j])
            if not last:
                self.transpose_tile(XA[j], xa[j], j)

    def prologue(self, seq, router_layer=None):
        for j in range(self.NT):
            xb = self.xres[j % 2]
            self.load("sp", xb, xb[:, :], self.x, self.x[seq, j * 128:(j + 1) * 128, :])
            self.transpose_tile(xb, xb[:, :], j, router_layer)

    def alloc_misc(self):
        self.xtf = self.P2[0:2]
        self.lnsm = self.sb("lnsm", [128, 16])
        self.sg = self.P2[2:4]
        self.hid = self.P2[4:6]

    def run(self):
        self.setup()
        self.alloc_misc()
        NT = self.NT
        for seq in range(self.NSEQ):
            def src_x(j, seq=seq):
                return self.x, self.x[seq, j * 128:(j + 1) * 128, :]

            def src_scr(j):
                return self.xscr, self.xscr[j * 128:(j + 1) * 128, :]

            def dst_out(j, seq=seq):
                return self.out, self.out[seq, j * 128:(j + 1) * 128, :]
            cur = src_x
            first = True
            subl = [(l, p) for l in self.layers for p in self.parts]
            for i, (l, p) in enumerate(subl):
                last = i == len(subl) - 1
                dst = dst_out if last else src_scr
                if p == "moe":
                    if first:
                        self.load_srow(9 + l, 0, 20)
                        self.prologue(seq, router_layer=l)
                    self.moe(l, cur, dst, last)
                else:
                    if first:
                        self.prologue(seq)
                    if l == 0:
                        self.even_mixer(cur, dst)
                    else:
                        self.odd_mixer(cur, dst)
                cur = src_scr
                first = False
        self.sc.finish_waits("sp", [self.out])
        self.sc.emit_all()
        return self.nc


NROWS = 16
NROWS_SB = 3
NCOLS = 64


def _host_inputs(S, inputs):
    f = lambda a: np.ascontiguousarray(np.asarray(a, dtype=np.float32))
    m = {}
    m["ev_w_in"] = f(inputs["ev_w_in"][0])
    m["ev_w_out"] = f(inputs["ev_w_out"][0])
    wi = f(inputs["od_w_in"][0])
    kpe = wi[:, 384:416]
    kpe_sw = np.concatenate([kpe[:, 16:32], kpe[:, 0:16]], 1)
    m["od_w_in"] = np.ascontiguousarray(np.concatenate([wi, kpe, kpe, kpe_sw, kpe_sw], 1))
    wq = f(inputs["od_w_q_up"][0]).reshape(256, 8, 96)
    nope = wq[:, :, :64].reshape(256, 512)
    pe = wq[:, :, 64:]
    pe_sw = np.concatenate([pe[:, :, 16:], pe[:, :, :16]], 2)
    m["od_w_q_up"] = np.ascontiguousarray(np.concatenate([nope, pe.reshape(256, 256), pe_sw.reshape(256, 256)], 1))
    wkv = f(inputs["od_w_kv_up"][0]).reshape(128, 8, 128)
    m["od_w_kv_up"] = np.ascontiguousarray(np.concatenate([wkv[:, :, :64].reshape(128, 512), wkv[:, :, 64:].reshape(128, 512)], 1))
    m["od_w_out"] = f(inputs["od_w_out"][0])
    m["moe_w_gate"] = f(inputs["moe_w_gate"])
    m["moe_w_up"] = f(inputs["moe_w_up"])
    m["moe_w_down"] = f(inputs["moe_w_down"])
    wr = np.concatenate([f(inputs["moe_w_group"]), f(inputs["moe_w_expert"])], 2)
    m["moe_wr"] = np.ascontiguousarray(wr.reshape(2, 8, 128, 20).transpose(0, 2, 1, 3))
    rows = np.zeros((NROWS, D), np.float32)
    for l in range(2):
        rows[4 * l + 0] = inputs["ln1_g"][l]
        rows[4 * l + 1] = inputs["ln1_b"][l]
        rows[4 * l + 2] = inputs["ln2_g"][l]
        rows[4 * l + 3] = inputs["ln2_b"][l]
        rows[9 + l, 0:4] = inputs["moe_b_group"][l]
        rows[9 + l, 4:20] = inputs["moe_b_expert"][l]
    rows[8] = inputs["ev_norm_g"][0]
    rows[11, 0:16] = inputs["ev_dt_bias"][0]
    rows[11, 16:32] = inputs["ev_a_log"][0]
    rows[11, 32:48] = inputs["ev_d_skip"][0]
    rows[12, 0:256] = inputs["od_q_norm_g"][0]
    rows[12, 256:384] = inputs["od_kv_norm_g"][0]
    rows[13, 0:8] = inputs["od_f_bias"][0]
    m["rows"] = rows
    cols = np.zeros((128, NCOLS), np.float32)
    cw = f(inputs["ev_conv_w"][0])
    cols[:, 0:48] = cw.T.reshape(12, 128, 4).transpose(1, 0, 2).reshape(128, 48)
    cols[:, 48:60] = f(inputs["ev_conv_b"][0]).reshape(12, 128).T
    cols[0:8, 60] = f(inputs["od_f_bias"][0])
    m["cols"] = cols
    for k, v in _consts(S).items():
        m["c_" + k] = v
    return m


_CACHE = {}


def kernel(**inputs):
    x = np.ascontiguousarray(np.asarray(inputs["x"], dtype=np.float32))
    B, S, _ = x.shape
    nseq = B // NCORES
    key = (S, nseq)
    if key not in _CACHE:
        _CACHE[key] = K(S, nseq).run()
    nc = _CACHE[key]
    shared = _host_inputs(S, inputs)
    in_maps = []
    for c in range(NCORES):
        m = dict(shared)
        m["x"] = np.ascontiguousarray(x[c * nseq:(c + 1) * nseq])
        in_maps.append(m)
    res = run_bass_kernel_spmd(nc, in_maps, core_ids=list(range(NCORES)))
    return np.concatenate([np.asarray(r["out"]) for r in res.results], axis=0).astype(np.float32)


def _f32v(b):
    return b.t[:, :].bitcast(F32)


def _bfv(b):
    return b.t[:, :].bitcast(BF16)


def _wview(slot, c):
    return slot.t[:, :].rearrange("p (k c) -> p k c", c=c)


def _kp(ap2d):
    return ap2d.rearrange("(k p) c -> p k c", p=128)


def _rope_tables(self, g):
    rc, rs = self.P2[3], self.P2[4]
    cc, cs_ = self.cdram["ropec"], self.cdram["ropes"]
    self.load("sp", rc, rc[:, :], cc, cc[0:128, g * 512:(g + 1) * 512])
    self.load("sp", rs, rs[:, :], cs_, cs_[0:128, g * 512:(g + 1) * 512])
    return rc, rs


def _rope(self, A, B, rc, rs, dst, dst_ap, lo=0, hi=64):
    t1, t2 = self.P2[5], self.P2[6]
    self.tt("dve", t1[lo:hi, :], A[lo:hi, :], rc[lo:hi, :], ALU.mult, [A, rc], [t1])
    self.tt("dve", t2[lo:hi, :], B[lo:hi, :], rs[lo:hi, :], ALU.mult, [B, rs], [t2])
    self.tt("pool", dst_ap, t1[lo:hi, :], t2[lo:hi, :], ALU.add, [t1, t2], [dst])


def _attn_finish(self, OT, DEN, hh, dst, dst_ap):
    rec = self.P2[6]
    lo, hi = hh * 64, hh * 64 + 64
    self.act(rec[lo:hi, :], DEN[lo:hi, :], AF.Ln, [DEN], [rec])
    self.act(rec[lo:hi, :], rec[lo:hi, :], AF.Exp, [rec], [rec], scale=-1.0)
    self.tt("dve", dst_ap, OT[lo:hi, :], rec[lo:hi, :], ALU.mult, [OT, rec], [dst])


def _replay_hp(self, recs):
    self.sc.replay(recs[0][0])
    for hp in range(len(recs)):
        if hp + 1 < len(recs):
            self.sc.replay(recs[hp + 1][0])
        self.sc.replay(recs[hp][1])


def odd_mixer(self, src, dst):
    S, NT, NG = self.S, self.NT, self.NG
    W = self.W
    Win = W["od_w_in"]
    sl = self.slab
    P2 = self.P2
    XT = self.XT
    c = self.c
    sc_mla = 1.0 / math.sqrt(96.0)
    sc_fox = 1.0 / 8.0
    self.load_rows([4, 5])
    self.load_srow(10, 0, 20)
    self.load_srow(12, 96, 384)
    CQNT, CKVNT, KPE = sl[8:10], sl[10], sl[11]
    OTF = sl[12:16]
    sm = self.small
    wl = self.wnext()
    wlv = _wview(wl, 512)
    self.load("pool", wl, wlv[:, :, 0:384], Win, _kp(Win[:, 0:384]))
    self.load("pool", wl, wlv[:, :, 384:512], Win, _kp(Win[:, 1960:2088]))
    lat, sq, latn_b = P2[0], P2[1], P2[2]
    latn = _bfv(latn_b)
    for j in range(NT):
        tsl = slice(j * 128, (j + 1) * 128)
        ps = self.PS()
        for k in range(8):
            self.mm(ps[:, 0:384], XT[k][:, tsl], wlv[:, k, 0:384], k == 0, k == 7, [XT[k], wl], [ps])
        self.cp("act", lat[:, 0:384], ps[:, 0:384], [ps], [lat])
        self.tt("pool", sq[:, 0:384], lat[:, 0:384], lat[:, 0:384], ALU.mult, [lat], [sq])
        self.sc.op("dve", lambda e: e.reduce_sum(out=sm[:, 0:1], in_=sq[:, 0:256], axis=mybir.AxisListType.X), [sq], [sm])
        self.sc.op("dve", lambda e: e.reduce_sum(out=sm[:, 1:2], in_=sq[:, 256:384], axis=mybir.AxisListType.X), [sq], [sm])
        self.ts("dve", sm[:, 0:1], sm[:, 0:1], 1.0 / 256, RMS_EPS, ALU.mult, ALU.add, [sm], [sm])
        self.ts("dve", sm[:, 1:2], sm[:, 1:2], 1.0 / 128, RMS_EPS, ALU.mult, ALU.add, [sm], [sm])
        self.act(sm[:, 0:2], sm[:, 0:2], AF.Ln, [sm], [sm])
        self.act(sm[:, 2:4], sm[:, 0:2], AF.Exp, [sm], [sm], scale=-0.5)
        self.stt(latn[:, 0:256], lat[:, 0:256], sm[:, 2:3], self.srow[:, 96:352], ALU.mult, ALU.mult,
                 [lat, sm, self.srow], [latn_b])
        self.stt(latn[:, 256:384], lat[:, 256:384], sm[:, 3:4], self.srow[:, 352:480], ALU.mult, ALU.mult,
                 [lat, sm, self.srow], [latn_b])
        pb = self.PS()
        pbv = _bfv(pb)
        for i in range(3):
            self.tr(pbv[:, i * 128:(i + 1) * 128], latn[:, i * 128:(i + 1) * 128], c["identb"][:, :], [latn_b, c["identb"]], [pb])
        for i, dstb in enumerate((CQNT[0], CQNT[1], CKVNT)):
            self.cp("dve", dstb[:, tsl], pbv[:, i * 128:(i + 1) * 128], [pb], [dstb])
    for g in range(NG):
        gsl = slice(g * 512, (g + 1) * 512)
        rc, rs = _rope_tables(self, g)
        A = self.PS()
        for k in range(8):
            self.mm(A[64:96, :], wlv[:, k, 384:416], XT[k][:, gsl], k == 0, k == 7, [wl, XT[k]], [A])
        B = self.PS()
        for k in range(8):
            self.mm(B[64:96, :], wlv[:, k, 448:480], XT[k][:, gsl], k == 0, k == 7, [wl, XT[k]], [B])
        _rope(self, A, B, rc, rs, KPE, KPE[64:96, gsl], 64, 96)
    wf = self.wnext()
    wfv = wf.t[:, 0:64].rearrange("p (k c) -> p k c", c=8)
    self.load("pool", wf, wfv, Win, _kp(Win[:, 1952:1960]))
    ones = P2[7]
    self.sc.op("pool", lambda e: e.memset(ones[:, :], 1.0), [], [ones])
    negfb = self.small2
    self.ts("pool", negfb[0:8, 0:1], self.cols[0:8, 60:61], -1.0, None, ALU.mult, None, [self.cols], [negfb])
    CSPb = [self.T4[0], self.T4[1]]
    FHQb = [self.T4[2], self.T4[3]]

    def cspg(g):
        return CSPb[g // 2], _f32v(CSPb[g // 2])[:, (g % 2) * 512:(g % 2 + 1) * 512]

    def fhqg(g):
        return FHQb[g // 2], _f32v(FHQb[g // 2])[:, (g % 2) * 512:(g % 2 + 1) * 512]
    CSPT = self.cspt
    for g in range(NG):
        gsl = slice(g * 512, (g + 1) * 512)
        ps = self.PS()
        for k in range(8):
            self.mm(ps[0:8, :], wfv[:, k, :], XT[k][:, gsl], k == 0, k == 7, [wf, XT[k]], [ps])
        e_, sp_ = P2[5], P2[6]
        self.act(e_[0:8, :], ps[0:8, :], AF.Exp, [ps, negfb], [e_], bias=negfb[0:8, 0:1], scale=-1.0)
        self.act(sp_[0:8, :], e_[0:8, :], AF.Ln, [e_], [sp_], bias=1.0)
        cb, cap = cspg(g)
        if g == 0:
            init, rds = 0.0, [ones, sp_]
        else:
            pb_, pap = cspg(g - 1)
            init, rds = pap[0:8, 511:512], [ones, sp_, pb_]
        self.sc.op("dve", lambda e, cap=cap, init=init: e.tensor_tensor_scan(
            out=cap[0:8, :], data0=ones[0:8, :], data1=sp_[0:8, :], initial=init, op0=ALU.mult, op1=ALU.add), rds, [cb])
        for tt_ in range(4):
            j = g * 4 + tt_
            pt = self.PS()
            self.tr(pt[:, 0:8], cap[0:8, tt_ * 128:(tt_ + 1) * 128], c["ident"][0:8, 0:8], [cb, c["ident"]], [pt])
            self.cp("dve", CSPT[:, j, :], pt[:, 0:8], [pt], [CSPT])
    self.load_mask("negm_fox")
    OT, DEN = self.acc
    _recs = []
    for hp in range(4):
        self.sc.rec = []
        q4 = sl[16 + 4 * (hp % 2):20 + 4 * (hp % 2)]
        FQz, FK, FV = q4[0:2], q4[2], q4[3]
        if hp < 2:
            self.sc.op("pool", lambda e, b=FQz[0]: e.memset(b[64:128, :], 0.0), [], [FQz[0]])
            self.sc.op("pool", lambda e, b=FQz[1]: e.memset(b[0:64, :], 0.0), [], [FQz[1]])
        wq = self.wnext()
        wqv = _wview(wq, 512)
        for i, c0 in enumerate((416, 928, 1440)):
            self.load("pool", wq, wqv[:, :, i * 128:(i + 1) * 128], Win, _kp(Win[:, c0 + hp * 128:c0 + (hp + 1) * 128]))
        for g in range(NG):
            gsl = slice(g * 512, (g + 1) * 512)
            for i in range(2):
                ps = self.PS()
                for k in range(8):
                    self.mm(ps[:, :], wqv[:, k, i * 128:(i + 1) * 128], XT[k][:, gsl], k == 0, k == 7, [wq, XT[k]], [ps])
                if i == 0:
                    self.cp("act", FQz[0][0:64, gsl], ps[0:64, :], [ps], [FQz[0]])
                    self.cp("act", FQz[1][64:128, gsl], ps[64:128, :], [ps], [FQz[1]])
                else:
                    self.cp("dve", FK[:, gsl], ps[:, :], [ps], [FK])
        for j in range(NT):
            tsl = slice(j * 128, (j + 1) * 128)
            ps = self.PS()
            for k in range(8):
                self.mm(ps[:, 0:128], XT[k][:, tsl], wqv[:, k, 256:384], k == 0, k == 7, [wq, XT[k]], [ps])
            self.evac(FV[:, tsl], ps[:, 0:128], [ps], [FV])
        _split = len(self.sc.rec)
        FHQs = [[self.T4[2], self.T4[3]], [self.T4[2], self.T4[3]]]
        for hh in range(2):
            h = 2 * hp + hh
            for g in range(NG):
                cb, cap = cspg(g)
                m_ = P2[5]
                self.ts("dve", m_[0:8, :], cap[0:8, :], c["ident"][0:8, h:h + 1], 1.0 / sc_fox, ALU.mult, ALU.mult, [cb, c["ident"]], [m_])
                ps = self.PS()
                self.mm(ps[:, :], ones[0:8, 0:128], m_[0:8, :], True, True, [ones, m_], [ps])
                fb = FHQs[hh][g // 2]
                self.cp("act", _f32v(fb)[:, (g % 2) * 512:(g % 2 + 1) * 512], ps[:, :], [ps], [fb])
            _fox_pipe(self, hp, hh, FQz, FK, FV, FHQs, CSPT, OTF[hp], sc_fox)
        _lst, self.sc.rec = self.sc.rec, None
        _recs.append((_lst[:_split], _lst[_split:]))
    _replay_hp(self, _recs)
    self.load_mask("mask_mla")
    wm = self.wnext()
    wq_ = wm.t[:, 0:2048].rearrange("p (k c) -> p k c", c=1024)
    wkv = wm.t[:, 2048:3072]
    self.load("pool", wm, wq_, W["od_w_q_up"], _kp(W["od_w_q_up"][:, :]))
    self.load("pool", wm, wkv, W["od_w_kv_up"], W["od_w_kv_up"][:, :])
    VM = XT[0:4]
    OTM = XT[4:8]
    for j in range(NT):
        ps = self.PS()
        self.mm(ps[:, :], CKVNT[:, j * 128:(j + 1) * 128], wkv[:, 512:1024], True, True, [CKVNT, wm], [ps])
        self.evac(VM[j // 4][:, (j % 4) * 512:(j % 4 + 1) * 512], ps[:, :], [ps], [VM[j // 4]])
    _recs = []
    for hp in range(4):
        self.sc.rec = []
        q4 = sl[16 + 4 * (hp % 2):20 + 4 * (hp % 2)]
        QH, KH = q4[0:2], q4[2:4]
        if hp < 2:
            for b in q4:
                self.sc.op("pool", lambda e, b=b: e.memset(b[64:128, :], 0.0), [], [b])
        for hh in range(2):
            h = 2 * hp + hh
            for g in range(NG):
                gsl = slice(g * 512, (g + 1) * 512)
                ps = self.PS()
                for kk in range(2):
                    self.mm(ps[0:64, :], wq_[:, kk, h * 64:(h + 1) * 64], CQNT[kk][:, gsl], kk == 0, kk == 1, [wm, CQNT[kk]], [ps])
                self.evac(QH[hh][0:64, gsl], ps[0:64, :], [ps], [QH[hh]])
                ps = self.PS()
                self.mm(ps[0:64, :], wkv[:, h * 64:(h + 1) * 64], CKVNT[:, gsl], True, True, [wm, CKVNT], [ps])
                self.evac(KH[hh][0:64, gsl], ps[0:64, :], [ps], [KH[hh]])
                rc, rs = _rope_tables(self, g)
                A = self.PS()
                for kk in range(2):
                    self.mm(A[64:96, :], wq_[:, kk, 512 + h * 32:512 + (h + 1) * 32], CQNT[kk][:, gsl], kk == 0, kk == 1, [wm, CQNT[kk]], [A])
                B = self.PS()
                for kk in range(2):
                    self.mm(B[64:96, :], wq_[:, kk, 768 + h * 32:768 + (h + 1) * 32], CQNT[kk][:, gsl], kk == 0, kk == 1, [wm, CQNT[kk]], [B])
                _rope(self, A, B, rc, rs, QH[hh], QH[hh][64:96, gsl], 64, 96)
            self.cp("act", KH[hh][64:96, 0:S], KPE[64:96, 0:S], [KPE], [KH[hh]])
        _split = len(self.sc.rec)
        _mla_pipe(self, hp, QH, KH, VM, OTM[hp], sc_mla)
        _lst, self.sc.rec = self.sc.rec, None
        _recs.append((_lst[:_split], _lst[_split:]))
    _replay_hp(self, _recs)
    wo = [self.wnext(), self.wnext()]
    wov = [_wview(w_, 1024) for w_ in wo]
    Wo = W["od_w_out"]
    for i in range(2):
        self.load("pool", wo[i], wov[i], Wo, _kp(Wo[i * 512:(i + 1) * 512, :]))
    cat = list(OTM) + list(OTF)
    for j in range(NT):
        tsl = slice(j * 128, (j + 1) * 128)
        xr = self.xres[j % 2]
        sb_, sap = src(j)
        self.load("sp", xr, xr[:, :], sb_, sap)
        y = self.tmpA[j % 2]
        for half in range(2):
            hs = slice(half * 512, (half + 1) * 512)
            ps = self.PS()
            for kc in range(8):
                self.mm(ps[:, :], cat[kc][:, tsl], wov[kc // 4][:, kc % 4, hs], kc == 0, kc == 7, [cat[kc], wo[kc // 4]], [ps])
            self.stt(y[:, hs], xr[:, hs], ALPHA, ps[:, :], ALU.mult, ALU.add, [xr, ps], [y])
        self.ln_tile(y, y[:, :], self.row(0), self.row(1))
        db, dap = dst(j)
        self.load("sp", db, dap, y, y[:, :])
        self.transpose_tile(y, y[:, :], j, router_layer=1)


K.odd_mixer = odd_mixer


def _v3(ap, inner):
    return ap.rearrange("p (a b) -> p a b", b=inner)


def _bc(ap2, n):
    return ap2.unsqueeze(2).broadcast_to([ap2.shape[0], ap2.shape[1], n])


def even_mixer(self, src, dst):
    S, NT, NG = self.S, self.NT, self.NG
    W = self.W
    Win = W["ev_w_in"]
    sl = self.slab
    P2 = self.P2
    XT = self.XT
    c = self.c
    sm = self.small
    X = mybir.AxisListType.X
    self.load_rows([0, 1, 8])
    self.load_srow(9, 0, 20)
    self.load_srow(11, 32, 48)
    OTS = sl[12:16]
    self.load_mask("mask_sb")
    OTa = self.acc
    _recs = []
    for hp in range(4):
        self.sc.rec = []
        q4 = sl[16 + 4 * (hp % 2):20 + 4 * (hp % 2)]
        QTz, KT, FV = q4[0:2], q4[2], q4[3]
        if hp < 2:
            self.sc.op("pool", lambda e, b=QTz[0]: e.memset(b[64:128, :], 0.0), [], [QTz[0]])
            self.sc.op("pool", lambda e, b=QTz[1]: e.memset(b[0:64, :], 0.0), [], [QTz[1]])
        wq = self.wnext()
        wqv = _wview(wq, 512)
        for i, c0 in enumerate((2576, 3088, 3600)):
            self.load("pool", wq, wqv[:, :, i * 128:(i + 1) * 128], Win, _kp(Win[:, c0 + hp * 128:c0 + (hp + 1) * 128]))
        for g in range(NG):
            gsl = slice(g * 512, (g + 1) * 512)
            for i in range(2):
                ps = self.PS()
                for k in range(8):
                    self.mm(ps[:, :], wqv[:, k, i * 128:(i + 1) * 128], XT[k][:, gsl], k == 0, k == 7, [wq, XT[k]], [ps])
                if i == 0:
                    self.cp("act", QTz[0][0:64, gsl], ps[0:64, :], [ps], [QTz[0]])
                    self.cp("act", QTz[1][64:128, gsl], ps[64:128, :], [ps], [QTz[1]])
                else:
                    self.cp("dve", KT[:, gsl], ps[:, :], [ps], [KT])
        for j in range(NT):
            tsl = slice(j * 128, (j + 1) * 128)
            ps = self.PS()
            for k in range(8):
                self.mm(ps[:, 0:128], XT[k][:, tsl], wqv[:, k, 256:384], k == 0, k == 7, [wq, XT[k]], [ps])
            self.evac(FV[:, tsl], ps[:, 0:128], [ps], [FV])
        _split = len(self.sc.rec)
        _sb_pipe(self, hp, QTz, KT, FV, OTS[hp])
        _lst, self.sc.rec = self.sc.rec, None
        _recs.append((_lst[:_split], _lst[_split:]))
    _replay_hp(self, _recs)
    ps_save = self.ps
    xtf_save = self.xtf
    self.xtf = [P2[0], P2[3]]
    allps = list(self.ps) + list(self.acc)
    self.ps = allps[0:4]
    YD, YO = allps[4:6], allps[6:8]
    XS_T = list(sl[8:12]) + list(sl[16:20])
    BT, CT = sl[20:22], sl[22:24]
    T4 = self.T4
    dests = XS_T + list(BT) + list(CT)
    for pnl in range(3):
        wp = self.wnext()
        wpv = _wview(wp, 512)
        self.load("pool", wp, wpv, Win, _kp(Win[:, 1024 + pnl * 512:1024 + (pnl + 1) * 512]))
        for q in range(4):
            cc = pnl * 4 + q
            for g in range(NG):
                gsl = slice(g * 512, (g + 1) * 512)
                ps = self.PS()
                for k in range(8):
                    self.mm(ps[:, :], wpv[:, k, q * 128:(q + 1) * 128], XT[k][:, gsl], k == 0, k == 7, [wp, XT[k]], [ps])
                RAWb = T4[g % 2]
                RAW = _bfv(RAWb)
                if g == 0:
                    self.sc.op("pool", lambda e, RAW=RAW: e.memset(RAW[:, 0:3], 0.0), [], [RAWb])
                else:
                    prev = _bfv(T4[(g - 1) % 2])
                    self.cp("pool", RAW[:, 0:3], prev[:, 512:515], [T4[(g - 1) % 2]], [RAWb])
                self.cp("act", RAW[:, 3:515], ps[:, :], [ps], [RAWb])
                DGb = T4[2 + cc % 2]
                DG = _bfv(DGb)
                if g == 0:
                    for tap in range(4):
                        self.ts("dve", DG[:, tap * 128:(tap + 1) * 128], c["identb"][:, :], self.cols[:, cc * 4 + tap:cc * 4 + tap + 1],
                                None, ALU.mult, None, [c["identb"], self.cols], [DGb])
                cps = self.PS()
                for tap in range(4):
                    self.mm(cps[:, :], DG[:, tap * 128:(tap + 1) * 128], RAW[:, tap:tap + 512], tap == 0, tap == 3, [DGb, RAWb], [cps])
                self.act(dests[cc][:, gsl], cps[:, :], AF.Silu, [cps, self.cols], [dests[cc]], bias=self.cols[:, 48 + cc:49 + cc])
    wd = self.wnext()
    wdv = wd.t[:, 0:128].rearrange("p (k c) -> p k c", c=16)
    self.load("pool", wd, wdv, Win, _kp(Win[:, 2560:2576]))
    DT, DA = self.dtb, self.dab
    AROW = self.small2b
    self.act(AROW[:, 0:16], self.srow[:, 48:64], AF.Exp, [self.srow], [AROW])
    self.ts("pool", AROW[:, 0:16], AROW[:, 0:16], -1.0, None, ALU.mult, None, [AROW], [AROW])
    for j in range(NT):
        tsl = slice(j * 128, (j + 1) * 128)
        ps = self.PS()
        for k in range(8):
            self.mm(ps[:, 0:16], XT[k][:, tsl], wdv[:, k, :], k == 0, k == 7, [XT[k], wd], [ps])
        self.tt("dve", sm[:, 0:16], ps[:, 0:16], self.srow[:, 32:48], ALU.add, [ps, self.srow], [sm])
        self.act(sm[:, 0:16], sm[:, 0:16], AF.Exp, [sm], [sm])
        self.act(DT[:, j, :], sm[:, 0:16], AF.Ln, [sm], [DT], bias=1.0)
        self.tt("dve", DA[:, j, :], DT[:, j, :], AROW[:, 0:16], ALU.mult, [DT, AROW], [DA])
    for half in range(2):
        wz = self.wnext()
        wzv = _wview(wz, 512)
        self.load("pool", wz, wzv, Win, _kp(Win[:, half * 512:(half + 1) * 512]))
        for j in range(NT):
            tsl = slice(j * 128, (j + 1) * 128)
            ps = self.PS()
            for k in range(8):
                self.mm(ps[:, :], XT[k][:, tsl], wzv[:, k, :], k == 0, k == 7, [XT[k], wz], [ps])
            szb = self.tmpA[j % 2]
            self.act(szb[:, 0:512], ps[:, :], AF.Silu, [ps], [szb])
            self.load("sp", self.zscr, self.zscr[j * 128:(j + 1) * 128, half * 512:(half + 1) * 512], szb, szb[:, 0:512])
    wo = [self.wnext(), self.wnext(), self.wnext()]
    wov = [_wview(w_, 1024) for w_ in wo]
    Wo = W["ev_w_out"]
    for i in range(3):
        self.load("pool", wo[i], wov[i], Wo, _kp(Wo[i * 512:(i + 1) * 512, :]))
    XSTOKb, SEGb, GTBb = T4[0], T4[1], T4[3]
    Dgb = SEGb
    XSTOK, SEG = _f32v(XSTOKb), _f32v(SEGb)
    Dg = SEG
    GTB = _bfv(GTBb)
    XSPb, YNb = P2[0], P2[3]
    XSP, YN = _bfv(XSPb), _bfv(YNb)
    XSPPb = [P2[1], P2[2]]
    XSPP = [_bfv(b) for b in XSPPb]
    BTOKb = [self.H2[2], self.H2[3]]
    HT = [P2[5], P2[6]]
    HTbb = P2[7]
    HTb = [_bfv(HTbb)[:, 0:512], _bfv(HTbb)[:, 512:1024]]
    YNTh = [self.H2[0], self.H2[1]]
    TMPb = P2[4]
    self.xtf = [P2[4], P2[3]]
    Y0b = [self.tmpA[1], T4[2]]
    Y0 = [self.tmpA[1][:, :], _f32v(T4[2])]
    ssms = [self.ssm, self.ssm2]
    CBM = self.cbm
    self.sc.op("pool", lambda e: e.memset(GTB, 0.0), [], [GTBb])
    for g in range(2):
        self.sc.op("pool", lambda e, g=g: e.memset(HT[g][:, :], 0.0), [], [HT[g]])
    self.sc.op("pool", lambda e: e.memset(_bfv(HTbb), 0.0), [], [HTbb])

    def stage_a(j):
        tsl = slice(j * 128, (j + 1) * 128)
        ssm = ssms[j % 2]
        ACUM, EA, DTW = ssm[:, 0:16], ssm[:, 16:32], ssm[:, 32:48]
        DEC = [ssm[:, 48:64], ssm[:, 64:80]]
        BTOK = BTOKb[j % 2]
        pb = self.PS()
        pbv = _bfv(pb)
        for cc in range(8):
            self.tr(pbv[:, cc * 128:(cc + 1) * 128], XS_T[cc][:, tsl], c["identb"][:, :], [XS_T[cc], c["identb"]], [pb])
        self.cp("act", XSTOK, pbv, [pb], [XSTOKb])
        pb2 = self.PS()
        pb2v = _bfv(pb2)
        for g in range(2):
            self.tr(pb2v[:, g * 128:(g + 1) * 128], BT[g][:, tsl], c["identb"][:, :], [BT[g], c["identb"]], [pb2])
        self.cp("dve", BTOK[:, 0:256], pb2v[:, 0:256], [pb2], [BTOK])
        ps = self.PS()
        self.mm(ps[:, 0:16], c["tri2"][:, :], DA[:, j, :], True, True, [c["tri2"], DA], [ps])
        self.cp("dve", ACUM, ps[:, 0:16], [ps], [ssm])
        self.act(EA, ACUM, AF.Exp, [ssm], [ssm])
        ps = self.PS()
        self.mm(ps[:, 0:16], c["lastsel"][:, :], ACUM, True, True, [c["lastsel"], ssm], [ps])
        self.tt("dve", DTW, ps[:, 0:16], ACUM, ALU.subtract, [ps, ssm], [ssm])
        self.act(DTW, DTW, AF.Exp, [ssm], [ssm])
        self.tt("dve", DTW, DTW, DT[:, j, :], ALU.mult, [ssm, DT], [ssm])
        for c2 in range(2):
            ps = self.PS()
            self.mm(ps[:, 0:16], c["sellast"][:, c2, :], ACUM, True, True, [c["sellast"], ssm], [ps])
            self.act(DEC[c2], ps[:, 0:16], AF.Exp, [ps], [ssm])
        self.tt("dve", _v3(Dg, 64), _bc(ACUM, 64), c["i64x2"][:, :].unsqueeze(1).broadcast_to([128, 16, 64]), ALU.mult,
                [ssm, c["i64x2"]], [Dgb])
        p1s = []
        for hb in range(2):
            p1 = self.PS()
            p1s.append(p1)
            self.mm(p1[:, :], c["bd"][:, :], Dg[:, hb * 512:(hb + 1) * 512], True, True, [c["bd"], Dgb], [p1])
        for hb in range(2):
            p1 = p1s[hb]
            self.tt("dve", _v3(SEG[:, hb * 512:(hb + 1) * 512], 64), _v3(p1[:, :], 64), _bc(ssm[:, hb * 8:hb * 8 + 8], 64),
                    ALU.subtract, [p1, ssm], [SEGb])
        self.ts("pool", SEG, SEG, 0.0, None, ALU.min, None, [SEGb], [SEGb])
        self.act(SEG, SEG, AF.Exp, [SEGb], [SEGb])
        ps = self.PS()
        for c2 in range(2):
            csl = slice(j * 128 + c2 * 64, j * 128 + c2 * 64 + 64)
            for g in range(2):
                self.sc.op("pe", lambda e, ps=ps, c2=c2, g=g, csl=csl: e.matmul(
                    ps[c2 * 64:c2 * 64 + 64, g * 64:(g + 1) * 64], BT[g][:, csl], CT[g][:, csl], start=True, stop=True,
                    skip_group_check=True), [BT[g], CT[g]], [ps])
        self.tt("dve", _v3(CBM[:, :], 64), _v3(ps[:, 0:128], 64), c["trimask"][:, :].unsqueeze(1).broadcast_to([128, 2, 64]),
                ALU.mult, [ps, c["trimask"]], [CBM])
        for c2 in range(2):
            lo, hi = c2 * 64, c2 * 64 + 64
            out_ap = GTB[lo:hi, :].rearrange("p (h x) -> p h x", x=128)[:, :, lo:hi].rearrange("p (g r) l -> p g r l", g=2)
            in0 = SEG[lo:hi, :].rearrange("p (g r l) -> p g r l", g=2, r=8)
            in1 = _v3(CBM[lo:hi, :], 64).unsqueeze(2).broadcast_to([64, 2, 8, 64])
            self.tt("dve", out_ap, in0, in1, ALU.mult, [SEGb, CBM], [GTBb])
        self.tt("pool", _v3(XSP, 64), _v3(XSTOK, 64), _bc(DT[:, j, :], 64), ALU.mult, [XSTOKb, DT], [XSPb])
        self.tt("pool", _v3(XSPP[j % 2], 64), _v3(XSTOK, 64), _bc(DTW, 64), ALU.mult, [XSTOKb, ssm], [XSPPb[j % 2]])
        for h in range(16):
            self.sc.op("pe", lambda e, h=h: e.matmul(
                YD[h // 8][:, (h % 8) * 64:(h % 8 + 1) * 64], GTB[:, h * 128:(h + 1) * 128], XSP[:, h * 64:(h + 1) * 64],
                start=True, stop=True, skip_group_check=True), [GTBb, XSPb], [YD[h // 8]])
        self.tt("pool", _v3(SEG, 64), _v3(XSTOK, 64), _bc(self.srow[:, 64:80], 64), ALU.mult, [XSTOKb, self.srow], [SEGb])
        for g in range(2):
            hs = slice(g * 512, (g + 1) * 512)
            self.tt("dve", Y0[j % 2][:, hs], SEG[:, hs], YD[g][:, :], ALU.add, [SEGb, YD[g]], [Y0b[j % 2]])

    def stage_b(j):
        tsl = slice(j * 128, (j + 1) * 128)
        ssm = ssms[j % 2]
        DEC = [ssm[:, 48:64], ssm[:, 64:80]]
        BTOK = BTOKb[j % 2]
        Yb, Yap = Y0b[j % 2], Y0[j % 2]
        for c2 in range(2):
            lo, hi = c2 * 64, c2 * 64 + 64
            csl = slice(j * 128 + lo, j * 128 + hi)
            for g in range(2):
                self.sc.op("pe", lambda e, g=g, lo=lo, hi=hi, csl=csl: e.matmul(
                    YO[g][lo:hi, :], CT[g][:, csl], HTb[g], start=True, stop=True, skip_group_check=True),
                    [CT[g], HTbb], [YO[g]])
            for g in range(2):
                st = self.PS()
                self.mm(st[:, :], BTOK[lo:hi, g * 128:(g + 1) * 128], XSPP[j % 2][lo:hi, g * 512:(g + 1) * 512], True, True,
                        [BTOK, XSPPb[j % 2]], [st])
                self.tt("dve", _v3(HT[g][:, :], 64), _v3(HT[g][:, :], 64), _bc(DEC[c2][:, g * 8:(g + 1) * 8], 64), ALU.mult,
                        [HT[g], ssm], [HT[g]])
                self.tt("dve", HT[g][:, :], HT[g][:, :], st[:, :], ALU.add, [HT[g], st], [HT[g]])
                self.cp("act", HTb[g], HT[g][:, :], [HT[g]], [HTbb])
        SZb = self.tmpA[0]
        self.load("sp", SZb, SZb[:, :], self.zscr, self.zscr[tsl, :])
        for g in range(2):
            hs = slice(g * 512, (g + 1) * 512)
            self.tt("dve", _v3(TMPb[:, :], 64), _v3(YO[g][:, :], 64), _bc(ssm[:, 16 + g * 8:16 + g * 8 + 8], 64), ALU.mult,
                    [YO[g], ssm], [TMPb])
            self.tt("pool", Yap[:, hs], Yap[:, hs], TMPb[:, :], ALU.add, [Yb, TMPb], [Yb])
        self.tt("pool", Yap, Yap, SZb[:, :], ALU.mult, [Yb, SZb], [Yb])
        self.tt("pool", SZb[:, :], Yap, Yap, ALU.mult, [Yb], [SZb])
        self.sc.op("dve", lambda e: e.reduce_sum(out=sm[:, 0:1], in_=SZb[:, :], axis=X), [SZb], [sm])
        self.ts("dve", sm[:, 0:1], sm[:, 0:1], 1.0 / 1024, RMS_EPS, ALU.mult, ALU.add, [sm], [sm])
        self.act(sm[:, 0:1], sm[:, 0:1], AF.Ln, [sm], [sm])
        self.act(sm[:, 1:2], sm[:, 0:1], AF.Exp, [sm], [sm], scale=-0.5)
        self.stt(YN, Yap, sm[:, 1:2], self.rows[:, 2, :], ALU.mult, ALU.mult, [Yb, sm, self.rows], [YNb])
        pb = self.PS()
        pbv = _bfv(pb)
        for cc in range(8):
            self.tr(pbv[:, cc * 128:(cc + 1) * 128], YN[:, cc * 128:(cc + 1) * 128], c["identb"][:, :], [YNb, c["identb"]], [pb])
        self.cp("act", YNTh[0][:, :], pbv[:, 0:512], [pb], [YNTh[0]])
        self.cp("act", YNTh[1][:, :], pbv[:, 512:1024], [pb], [YNTh[1]])
        xr = self.xres[j % 2]
        sb_, sap = src(j)
        self.load("sp", xr, xr[:, :], sb_, sap)
        for half in range(2):
            hs = slice(half * 512, (half + 1) * 512)
            ps = self.PS()
            for kc in range(12):
                lhsT = YNTh[kc // 4][:, (kc % 4) * 128:(kc % 4 + 1) * 128] if kc < 8 else OTS[kc - 8][:, tsl]
                rb_ = YNTh[kc // 4] if kc < 8 else OTS[kc - 8]
                self.mm(ps[:, :], lhsT, wov[kc // 4][:, kc % 4, hs], kc == 0, kc == 11, [rb_, wo[kc // 4]], [ps])
            self.stt(SZb[:, hs], xr[:, hs], ALPHA, ps[:, :], ALU.mult, ALU.add, [xr, ps], [SZb])
        self.ln_tile(SZb, SZb[:, :], self.row(0), self.row(1))
        db, dap = dst(j)
        self.load("sp", db, dap, SZb, SZb[:, :])
        self.transpose_tile(SZb, SZb[:, :], j, router_layer=0)

    psA, psB = allps[0:2], allps[2:4]

    def rec_stage(fn, j, banks):
        self.ps = banks
        return self.sc.record(lambda: fn(j))
    self.sc.replay(rec_stage(stage_a, 0, psA))
    for j in range(NT):
        lb = rec_stage(stage_b, j, psB)
        la = rec_stage(stage_a, j + 1, psA) if j + 1 < NT else []
        self.sc.replay(la, lb)
    self.ps = ps_save
    self.xtf = xtf_save


K.even_mixer = even_mixer


def _pipe(n, stages):
    maxs = max(s for s, _ in stages)
    for t in range(n + maxs):
        for s, fn in stages:
            u = t - s
            if 0 <= u < n:
                fn(u)


def _units(NG, hhs=(0, 1)):
    return [(hh, G, idx, i, 4 * (G + 1)) for hh in hhs for G in range(NG)
            for idx, i in enumerate(range(4 * (G + 1) - 1, -1, -1))]


def _sb_pipe(self, hp, QTz, KT, FV, OTSb):
    P2, H2, c = self.P2, self.H2, self.c
    units = _units(self.NG)
    n = len(units)
    E, Ecs = P2[0:3], P2[3:5]
    Lb, Rb = H2[0:2], H2[2]
    Wb = [H2[3], P2[5]]
    Wt = [H2[3][:, :], _bfv(P2[5])[:, 0:512]]
    zb, cb = {}, {}

    def s_z(u):
        hh, G, idx, i, nkb = units[u]
        lo, hi = hh * 64, hh * 64 + 64
        z = self.PS()
        zb[u] = z
        self.mm(z[:, :], KT[:, i * 128:(i + 1) * 128], QTz[hh][:, G * 512:(G + 1) * 512], True, True, [KT, QTz[hh]], [z])

    def s_E(u):
        hh, G, idx, i, nkb = units[u]
        z = zb.pop(u)
        e = E[u % 3]
        self.act(e[:, :], z[:, :], AF.Exp, [z], [e], scale=0.125)
        if i >= 4 * G:
            self.tt("dve", e[:, :], e[:, :], self.maskbuf[:, i - 4 * G, :], ALU.mult, [e, self.maskbuf], [e])

    def s_Lb(u):
        e, l = E[u % 3], Lb[u % 2]
        self.act(l[:, :], e[:, :], AF.Ln, [e], [l], bias=1.0)

    def s_CS(u):
        hh, G, idx, i, nkb = units[u]
        l = Lb[u % 2]
        CS = self.PS()
        cb[u] = CS
        self.mm(CS[:, :], c["uincl"][:, :], l[:, :], True, idx == 0, [c["uincl"], l], [CS])
        if idx > 0:
            self.mm(CS[:, :], c["onesb"][:, :], Rb[:, :], False, True, [c["onesb"], Rb], [CS])
        if idx < nkb - 1:
            if idx == 0:
                self.cp("dve", Rb[:, :], l[:, :], [l], [Rb])
            else:
                self.tt("pool", Rb[:, :], Rb[:, :], l[:, :], ALU.add, [Rb, l], [Rb])

    def s_Ecs(u):
        CS = cb.pop(u)
        ec = Ecs[u % 2]
        self.act(ec[:, :], CS[:, :], AF.Exp, [CS], [ec], scale=-1.0)
        self.tt("dve", Wt[u % 2], E[u % 3][:, :], ec[:, :], ALU.mult, [E[u % 3], ec], [Wb[u % 2]])

    def s_PV(u):
        hh, G, idx, i, nkb = units[u]
        lo, hi = hh * 64, hh * 64 + 64
        OT = self.acc[(hh * self.NG + G) % 2]
        self.mm(OT[:, :], FV[:, i * 128:(i + 1) * 128], Wt[u % 2], idx == 0, idx == nkb - 1, [FV, Wb[u % 2]], [OT])
        if idx == nkb - 1:
            self.cp("act", OTSb[lo:hi, G * 512:(G + 1) * 512], OT[lo:hi, :], [OT], [OTSb])

    _pipe(n, [(0, s_z), (2, s_Lb), (2, s_CS), (3, s_Ecs), (1, s_E), (4, s_PV)])


def _fox_pipe(self, hp, hh_, FQz, FK, FV, FHQs, CSPT, OTb, scale):
    P2, H2, c = self.P2, self.H2, self.c
    units = _units(self.NG, (hh_,))
    n = len(units)
    A = P2[0:3]
    Wb = H2[0:2]
    OT, DEN = self.acc
    zb = {}

    def s_z(u):
        hh, G, idx, i, nkb = units[u]
        lo, hi = hh * 64, hh * 64 + 64
        z = self.PS()
        zb[u] = z
        self.mm(z[:, :], FK[:, i * 128:(i + 1) * 128], FQz[hh][:, G * 512:(G + 1) * 512], True, True, [FK, FQz[hh]], [z])

    def s_A(u):
        hh, G, idx, i, nkb = units[u]
        z = zb.pop(u)
        a = A[u % 3]
        fb = FHQs[hh][G // 2]
        fap = _f32v(fb)[:, (G % 2) * 512:(G % 2 + 1) * 512]
        self.tt("dve", a[:, :], z[:, :], fap, ALU.subtract, [z, fb], [a])
        if i >= 4 * G:
            self.tt("pool", a[:, :], a[:, :], self.maskbuf[:, i - 4 * G, :], ALU.add, [a, self.maskbuf], [a])

    def s_W(u):
        hh, G, idx, i, nkb = units[u]
        h = 2 * hp + hh
        a, w = A[u % 3], Wb[u % 2]
        self.act(w[:, :], a[:, :], AF.Exp, [a, CSPT], [w], bias=CSPT[:, i, h:h + 1], scale=scale)

    def s_PV(u):
        hh, G, idx, i, nkb = units[u]
        lo, hi = hh * 64, hh * 64 + 64
        w = Wb[u % 2]
        self.mm(OT[:, :], FV[:, i * 128:(i + 1) * 128], w[:, :], idx == 0, idx == nkb - 1, [FV, w], [OT])
        self.mm(DEN[:, :], c["onesb"][:, :], w[:, :], idx == 0, idx == nkb - 1, [c["onesb"], w], [DEN])
        if idx == nkb - 1:
            _attn_finish(self, OT, DEN, hh, OTb, OTb[lo:hi, G * 512:(G + 1) * 512])

    _pipe(n, [(0, s_z), (2, s_W), (1, s_A), (3, s_PV)])


def _mla_pipe(self, hp, QH, KH, VM, OTb, scale):
    P2, H2, c = self.P2, self.H2, self.c
    units = _units(self.NG)
    n = len(units)
    Wf = P2[0:2]
    Wb = H2[0:2]
    OT, DEN = self.acc
    zb = {}

    def s_z(u):
        hh, G, idx, i, nkb = units[u]
        lo, hi = hh * 64, hh * 64 + 64
        plo, phi = hh * 32, hh * 32 + 32
        ksl, gsl = slice(i * 128, (i + 1) * 128), slice(G * 512, (G + 1) * 512)
        z = self.PS()
        zb[u] = z
        self.mm(z[:, :], KH[hh][:, ksl], QH[hh][:, gsl], True, True, [KH[hh], QH[hh]], [z])

    def s_W(u):
        hh, G, idx, i, nkb = units[u]
        z = zb.pop(u)
        w = Wb[u % 2]
        if i >= 4 * G:
            wf = Wf[u % 2]
            self.act(wf[:, :], z[:, :], AF.Exp, [z], [wf], scale=scale)
            self.tt("dve", w[:, :], wf[:, :], self.maskbuf[:, i - 4 * G, :], ALU.mult, [wf, self.maskbuf], [w])
        else:
            self.act(w[:, :], z[:, :], AF.Exp, [z], [w], scale=scale)

    def s_PV(u):
        hh, G, idx, i, nkb = units[u]
        h = 2 * hp + hh
        lo, hi = hh * 64, hh * 64 + 64
        w = Wb[u % 2]
        vs = VM[i // 4][:, (i % 4) * 512 + hp * 128:(i % 4) * 512 + (hp + 1) * 128]
        self.mm(OT[:, :], vs, w[:, :], idx == 0, idx == nkb - 1, [VM[i // 4], w], [OT])
        self.mm(DEN[:, :], c["onesb"][:, :], w[:, :], idx == 0, idx == nkb - 1, [c["onesb"], w], [DEN])
        if idx == nkb - 1:
            _attn_finish(self, OT, DEN, hh, OTb, OTb[lo:hi, G * 512:(G + 1) * 512])

    _pipe(n, [(0, s_z), (1, s_W), (2, s_PV)])
```

```python
import math
from contextlib import ExitStack
import numpy as np
import ml_dtypes
import concourse.bass as bass
import concourse.mybir as mybir
from concourse.bass_utils import run_bass_kernel_spmd

F32 = mybir.dt.float32
BF16 = mybir.dt.bfloat16
AF = mybir.ActivationFunctionType
ALU = mybir.AluOpType

D = 1024
NCORES = 8
ALPHA = (2.0 * 2) ** 0.25
LN_EPS = 1e-5
RMS_EPS = 1e-6
EVEN_IN = 4112
ODD_IN = 1960

ENGS = ("pe", "act", "dve", "pool", "sp")


class Buf:
    _n = 0

    def __init__(self, name, t):
        self.name = name
        self.t = t
        self.w = None
        self.r = {}
        self.excl = False
        self.sem = None
        self.semcnt = 0
        Buf._n += 1
        self.id = Buf._n

    def __getitem__(self, k):
        return self.t[k]


class Sched:
    def __init__(self, nc, es):
        self.nc = nc
        self.es = es
        self.ops = {e: [] for e in ENGS}
        self.cnt = {e: 0 for e in ENGS}
        self.known = {e: {} for e in ENGS}
        self.snap = {e: [None] for e in ENGS}
        self.rec = None
        self.sems = {e: es.enter_context(nc.semaphore("c_" + e)) for e in ENGS}
        self.nsem = len(ENGS)

    def _dma_sem(self, b):
        if b.sem is None:
            b.sem = self.es.enter_context(self.nc.semaphore("d_%d" % b.id))
            self.nsem += 1
        return b.sem

    def _waits(self, eng, reads, writes):
        waits = {}

        def need(ev, same_ok):
            if ev is None:
                return
            k, v = ev
            if k == eng and (same_ok or eng in ("pe", "sp")):
                return
            if v > waits.get(k, 0):
                waits[k] = v
        for b in reads:
            need(b.w, False)
            if b.excl:
                for k, v in b.r.items():
                    need((k, v), True)
        for b in writes:
            need(b.w, False)
            for k, v in b.r.items():
                need((k, v), False)
        kn = self.known[eng]
        out = []
        for k, v in waits.items():
            if kn.get(k, 0) >= v:
                continue
            out.append((k, v))
            kn[k] = v
            if isinstance(k, str):
                sn = self.snap[k][v]
                if sn is not None:
                    for k2, v2 in sn.items():
                        if k2 != eng and kn.get(k2, 0) < v2:
                            kn[k2] = v2
        return out

    def op(self, eng, emit, reads=(), writes=()):
        if self.rec is not None:
            self.rec.append(("op", (eng, emit, tuple(reads), tuple(writes)), {}))
            return
        waits = self._waits(eng, reads, writes)
        self.cnt[eng] += 1
        idx = self.cnt[eng]
        ev = (eng, idx)
        for b in reads:
            b.r[eng] = idx
        for b in writes:
            b.w = ev
            b.r = {}
        self.snap[eng].append({k: v for k, v in self.known[eng].items() if isinstance(k, str)})
        self.ops[eng].append((waits, emit, None))

    def dma(self, q, out_ap, in_ap, dst, src, extra_reads=(), **kw):
        if self.rec is not None:
            self.rec.append(("dma", (q, out_ap, in_ap, dst, src, extra_reads), kw))
            return
        reads = [src] + list(extra_reads)
        waits = self._waits(q, reads, [dst])
        sem = self._dma_sem(dst)
        dst.semcnt += 16
        ev = (("dma", dst.id, sem), dst.semcnt)
        src.r[ev[0]] = ev[1]
        for b in extra_reads:
            b.r[ev[0]] = ev[1]
        dst.w = ev
        dst.r = {}

        def emit(e):
            return e.dma_start(out=out_ap, in_=in_ap, **kw)
        self.ops[q].append((waits, emit, sem))

    def record(self, fn):
        assert self.rec is None
        self.rec = []
        fn()
        lst, self.rec = self.rec, None
        return lst

    def replay(self, la, lb=()):
        ia = ib = 0
        na, nb = len(la), len(lb)
        while ia < na or ib < nb:
            if ib >= nb or (ia < na and ia * nb <= ib * na):
                kind, a, k = la[ia]
                ia += 1
            else:
                kind, a, k = lb[ib]
                ib += 1
            if kind == "op":
                self.op(*a)
            else:
                self.dma(*a, **k)

    def finish_waits(self, eng, bufs):
        waits = self._waits(eng, bufs, [])
        self.ops[eng].append((waits, None, None))

    def emit_all(self):
        nc = self.nc
        handles = {"pe": "tensor", "act": "scalar", "dve": "vector", "pool": "gpsimd", "sp": "sync"}
        with nc.Block() as block:
            for e in ENGS:
                ops = self.ops[e]
                esem = self.sems[e]

                def body(eng, ops=ops, esem=esem):
                    for waits, emit, dsem in ops:
                        for k, v in waits:
                            s = self.sems[k] if isinstance(k, str) else k[2]
                            eng.wait_ge(s, v)
                        if emit is None:
                            continue
                        ins = emit(eng)
                        if dsem is not None:
                            ins.then_inc(dsem, 16)
                        else:
                            ins.then_inc(esem, 1)
                getattr(block, handles[e])(body)


def _consts(S):
    c = {}
    p = np.arange(128)
    c["ident"] = np.eye(128, dtype=np.float32)
    c["identb"] = np.eye(128, dtype=np.float32).astype(ml_dtypes.bfloat16)
    c["onesb"] = np.ones((128, 128), np.float32).astype(ml_dtypes.bfloat16)
    c["uincl"] = (p[:, None] >= p[None, :]).astype(np.float32).astype(ml_dtypes.bfloat16)
    s = p[None, :, None]
    t = np.arange(512)[None, None, :]
    m = np.arange(4)[:, None, None]
    c["mask_sb"] = ((s + 128 * m) < t).astype(np.float32).astype(ml_dtypes.bfloat16)
    c["mask_mla"] = (((s + 128 * m) // 64) <= (t // 64)).astype(np.float32).astype(ml_dtypes.bfloat16)
    c["negm_fox"] = np.where((s + 128 * m) <= t, 0.0, -30000.0).astype(np.float32).astype(ml_dtypes.bfloat16)
    c2 = p // 64
    l = p % 64
    c["tri2"] = ((c2[:, None] == c2[None, :]) & (l[:, None] <= l[None, :])).astype(np.float32)
    c["bd"] = (c2[:, None] == c2[None, :]).astype(np.float32)
    c["lastsel"] = (p[:, None] == (64 * c2[None, :] + 63)).astype(np.float32)
    sl = np.zeros((2, 128, 128), np.float32)
    sl[0, 63, :] = 1.0
    sl[1, 127, :] = 1.0
    c["sellast"] = sl
    c["i64x2"] = (l[:, None] == np.arange(64)[None, :]).astype(np.float32)
    c["trimask"] = (np.arange(64)[None, :] >= l[:, None]).astype(np.float32)
    c["sel8"] = np.repeat(np.eye(8, dtype=np.float32), 128, axis=1)
    ng = np.where(np.arange(64)[None, :] < l[:, None], -30000.0, 0.0).astype(np.float32)
    c["negm_ssd"] = np.tile(ng[:, None, :], (1, 16, 1)).reshape(128, 1024).astype(ml_dtypes.bfloat16)
    inv = 10000.0 ** (-(np.arange(0, 32, 2, dtype=np.float32) / 32.0))
    ang = np.arange(S, dtype=np.float32)[None, :] * inv[:, None]
    cos = np.cos(ang).astype(np.float32)
    sin = np.sin(ang).astype(np.float32)
    cosT = np.concatenate([cos, cos], 0)
    sinT = np.concatenate([-sin, sin], 0)
    c["ropec"] = np.concatenate([cosT, cosT, cosT, cosT], 0).astype(np.float32)
    c["ropes"] = np.concatenate([sinT, sinT, sinT, sinT], 0).astype(np.float32)
    return c


class K:
    def __init__(self, S, NSEQ, layers=(0, 1), parts=("mix", "moe"), taps=()):
        self.S, self.NSEQ = S, NSEQ
        self.NT = S // 128
        self.GW = 512
        self.NG = S // 512
        self.layers, self.parts, self.taps = layers, parts, taps
        self.nc = bass.Bass("TRN2", target_bir_lowering=False)
        self.es = ExitStack()
        self.sc = Sched(self.nc, self.es)
        self.psi = 0
        self.evi = 0
        self.dbg = {}
        self.tap_out = {}

    def dram_in(self, name, shape, dt=F32):
        t = self.nc.dram_tensor(name, list(shape), dt, kind="ExternalInput")
        return Buf(name, t)

    def dram_out(self, name, shape, dt=F32):
        t = self.nc.dram_tensor(name, list(shape), dt, kind="ExternalOutput")
        return Buf(name, t)

    def sb(self, name, shape, dt=F32):
        t = self.es.enter_context(self.nc.sbuf_tensor("s_" + name, list(shape), dt))
        return Buf(name, t)

    def PS(self):
        b = self.ps[self.psi % len(self.ps)]
        self.psi += 1
        return b

    def mm(self, out, lhsT, rhs, start, stop, reads, writes):
        self.sc.op("pe", lambda e: e.matmul(out, lhsT, rhs, start=start, stop=stop), reads, writes)

    def tr(self, out, in_, ident, reads, writes):
        self.sc.op("pe", lambda e: e.transpose(out, in_, ident), reads, writes)

    def act(self, out, in_, func, reads, writes, bias=None, scale=1.0):
        kw = {}
        if bias is not None:
            kw["bias"] = bias
        self.sc.op("act", lambda e: e.activation(out=out, in_=in_, func=func, scale=scale, **kw), reads, writes)

    def tt(self, eng, out, in0, in1, op, reads, writes):
        self.sc.op(eng, lambda e: e.tensor_tensor(out=out, in0=in0, in1=in1, op=op), reads, writes)

    def ts(self, eng, out, in0, s1, s2, op0, op1, reads, writes):
        if op1 is None:
            self.sc.op(eng, lambda e: e.tensor_scalar(out=out, in0=in0, scalar1=s1, scalar2=None, op0=op0), reads, writes)
        else:
            self.sc.op(eng, lambda e: e.tensor_scalar(out=out, in0=in0, scalar1=s1, scalar2=s2, op0=op0, op1=op1), reads, writes)

    def stt(self, out, in0, scalar, in1, op0, op1, reads, writes):
        self.sc.op("dve", lambda e: e.scalar_tensor_tensor(out=out, in0=in0, scalar=scalar, in1=in1, op0=op0, op1=op1), reads, writes)

    def cp(self, eng, out, in_, reads, writes):
        if eng == "act":
            self.sc.op("act", lambda e: e.activation(out=out, in_=in_, func=AF.Copy), reads, writes)
        else:
            self.sc.op(eng, lambda e: e.tensor_copy(out=out, in_=in_), reads, writes)

    def evac(self, out, in_, reads, writes):
        self.evi += 1
        self.cp("act" if self.evi % 2 else "dve", out, in_, reads, writes)

    def load(self, q, dstbuf, out_ap, srcbuf, in_ap, **kw):
        self.sc.dma(q, out_ap, in_ap, dstbuf, srcbuf, **kw)

    def setup(self):
        S, NSEQ, NT = self.S, self.NSEQ, self.NT
        self.x = self.dram_in("x", [NSEQ, S, D])
        self.out = self.dram_out("out", [NSEQ, S, D])
        self.xscr = Buf("xscr", self.nc.dram_tensor("xscr", [S, D], F32, kind="Internal"))
        W = {}
        W["ev_w_in"] = self.dram_in("ev_w_in", [D, EVEN_IN])
        W["ev_w_out"] = self.dram_in("ev_w_out", [1536, D])
        W["od_w_in"] = self.dram_in("od_w_in", [D, ODD_IN + 128])
        W["od_w_q_up"] = self.dram_in("od_w_q_up", [256, 512 + 256 + 256])
        W["od_w_kv_up"] = self.dram_in("od_w_kv_up", [128, 1024])
        W["od_w_out"] = self.dram_in("od_w_out", [D, D])
        W["moe_w_gate"] = self.dram_in("moe_w_gate", [2, 16, D, 256])
        W["moe_w_up"] = self.dram_in("moe_w_up", [2, 16, D, 256])
        W["moe_w_down"] = self.dram_in("moe_w_down", [2, 16, 256, D])
        W["moe_wr"] = self.dram_in("moe_wr", [2, 128, 8, 20])
        W["rows"] = self.dram_in("rows", [NROWS, D])
        W["cols"] = self.dram_in("cols", [128, NCOLS])
        self.W = W
        cs = _consts(S)
        self.cdram = {}
        for k, v in cs.items():
            dt = BF16 if v.dtype == ml_dtypes.bfloat16 else F32
            self.cdram[k] = self.dram_in("c_" + k, v.shape, dt)
        self.slab = [self.sb("slab%d" % i, [128, 2048], BF16) for i in range(28)]
        self.XT = self.slab[0:8]
        self.wslot = [self.sb("wslot%d" % i, [128, 4096], BF16) for i in range(4)]
        self.wi = 0
        self.ps = [Buf("ps%d" % i, self.es.enter_context(self.nc.psum_tensor("ps%d" % i, [128, 512], F32)))
                   for i in range(8)]
        for b in self.ps:
            b.excl = True
        self.acc = self.ps[6:8]
        self.ps = self.ps[0:6]
        c = {}
        for k in ("ident", "identb", "onesb", "uincl", "tri2", "bd", "lastsel", "i64x2", "trimask"):
            v = cs[k]
            c[k] = self.sb("k_" + k, v.shape, BF16 if v.dtype == ml_dtypes.bfloat16 else F32)
            self.load("sp", c[k], c[k][:, :], self.cdram[k], self.cdram[k][:, :])
        c["sellast"] = self.sb("k_sellast", [128, 2, 128])
        for i in range(2):
            self.load("sp", c["sellast"], c["sellast"][:, i, :], self.cdram["sellast"], self.cdram["sellast"][i, :, :])
        self.c = c
        self.rows = self.sb("rows", [128, NROWS_SB, D])
        self.srow = self.sb("srow", [128, 512])
        self.maskbuf = self.sb("maskbuf", [128, 4, 512], BF16)
        self.P2 = [self.sb("p2_%d" % i, [128, 512]) for i in range(8)]
        self.H2 = [self.sb("h2_%d" % i, [128, 512], BF16) for i in range(4)]
        self.cols = self.sb("cols", [128, NCOLS])
        self.load("sp", self.cols, self.cols[:, :], W["cols"], W["cols"][:, :])
        self.wr = self.sb("wr", [128, 2, 8, 20])
        for l in range(2):
            self.load("sp", self.wr, self.wr[:, l, :, :], W["moe_wr"], W["moe_wr"][l, :, :, :])
        self.gate = self.sb("gate", [128, NT, 16])
        self.xres = [self.sb("xres0", [128, D])] * 2
        self.tmpA = [self.sb("tmpA%d" % i, [128, D]) for i in range(2)]
        self.T4 = [Buf("t4_%d" % i, None) for i in range(4)]
        for i in range(4):
            self.T4[i].t = self.slab[24 + i].t
        self.T4 = self.slab[24:28]
        self.small = self.sb("small", [128, 64])
        self.small2 = self.sb("small2", [128, 8])
        self.small2b = self.sb("small2b", [128, 16])
        self.dtb = self.sb("dtb", [128, self.NT, 16])
        self.dab = self.sb("dab", [128, self.NT, 16])
        self.ssm = self.sb("ssm", [128, 80])
        self.ssm2 = self.sb("ssm2", [128, 80])
        self.cbm = self.sb("cbm", [128, 128])
        self.zscr = Buf("zscr", self.nc.dram_tensor("zscr", [S, D], F32, kind="Internal"))
        self.cspt = self.sb("cspt", [128, self.NT, 8])
        self.xi = 0

    def row(self, r):
        return self.rows[:, r, :]

    def load_srow(self, r, off, n):
        src = self.W["rows"][r:r + 1, 0:n].broadcast_to([128, n])
        self.load("sp", self.srow, self.srow[:, off:off + n], self.W["rows"], src)

    def load_mask(self, name):
        cd = self.cdram[name]
        for m_ in range(4):
            self.load("sp", self.maskbuf, self.maskbuf[:, m_, :], cd, cd[m_, :, :])

    def load_rows(self, idx_list):
        for slot, r in enumerate(idx_list):
            src = self.W["rows"][r:r + 1, :].broadcast_to([128, D])
            self.load("sp", self.rows, self.rows[:, slot, :], self.W["rows"], src)

    def transpose_tile(self, xb, xap, j, router_layer=None):
        c = self.c
        for half in range(2):
            ps = self.PS()
            for kk in range(4):
                k = half * 4 + kk
                self.tr(ps[:, kk * 128:(kk + 1) * 128], xap[:, k * 128:(k + 1) * 128], c["ident"][:, :],
                        [xb, c["ident"]], [ps])
            self.evi += 1
            for kk in range(4):
                k = half * 4 + kk
                self.cp("act" if self.evi % 2 else "dve", self.XT[k][:, j * 128:(j + 1) * 128],
                        ps[:, kk * 128:(kk + 1) * 128], [ps], [self.XT[k]])
            if router_layer is not None and not self.dbg.get('noxtf'):
                xtf = self.xtf[half]
                self.cp("dve", xtf[:, :], ps[:, :], [ps], [xtf])
        if router_layer is not None and not self.dbg.get('norouter'):
            self.router(j, router_layer)

    def router(self, j, l):
        sm = self.small
        ps = self.PS()
        for k in range(8):
            xtf = self.xtf[k // 4]
            self.mm(ps[:, 0:20], xtf[:, (k % 4) * 128:(k % 4 + 1) * 128], self.wr[:, l, k, :], k == 0, k == 7,
                    [xtf, self.wr], [ps])
        lg = sm[:, 0:20]
        rb = self.srow[:, 0:20]
        self.tt("dve", lg, ps[:, 0:20], rb, ALU.add, [ps, self.srow], [sm])
        R, Wr = [sm], [sm]
        self.sc.op("dve", lambda e: e.reduce_max(out=sm[:, 20:21], in_=sm[:, 0:4], axis=mybir.AxisListType.X), R, Wr)
        self.ts("dve", sm[:, 21:25], sm[:, 0:4], sm[:, 20:21], None, ALU.is_equal, None, R, Wr)
        self.ts("dve", sm[:, 25:29], sm[:, 0:4], sm[:, 20:21], None, ALU.subtract, None, R, Wr)
        self.act(sm[:, 25:29], sm[:, 25:29], AF.Exp, R, Wr)
        self.sc.op("dve", lambda e: e.reduce_sum(out=sm[:, 29:30], in_=sm[:, 25:29], axis=mybir.AxisListType.X), R, Wr)
        self.sc.op("dve", lambda e: e.reciprocal(out=sm[:, 30:31], in_=sm[:, 29:30]), R, Wr)
        el = sm[:, 4:20].rearrange("p (g e) -> p g e", e=4)
        ohg_b = sm[:, 21:25].unsqueeze(2).broadcast_to([128, 4, 4])
        prod = sm[:, 32:48].rearrange("p (g e) -> p g e", e=4)
        self.tt("dve", prod, el, ohg_b, ALU.mult, R, Wr)
        prod_t = sm[:, 32:48].rearrange("p (g e) -> p e g", e=4)
        self.sc.op("dve", lambda e: e.reduce_sum(out=sm[:, 48:52], in_=prod_t, axis=mybir.AxisListType.X), R, Wr)
        self.sc.op("dve", lambda e: e.reduce_max(out=sm[:, 52:53], in_=sm[:, 48:52], axis=mybir.AxisListType.X), R, Wr)
        self.ts("dve", sm[:, 53:57], sm[:, 48:52], sm[:, 52:53], None, ALU.is_equal, None, R, Wr)
        self.stt(sm[:, 57:61], sm[:, 53:57], -1e30, sm[:, 48:52], ALU.mult, ALU.add, R, Wr)
        self.sc.op("dve", lambda e: e.reduce_max(out=sm[:, 61:62], in_=sm[:, 57:61], axis=mybir.AxisListType.X), R, Wr)
        self.ts("dve", sm[:, 25:29], sm[:, 57:61], sm[:, 61:62], None, ALU.is_equal, None, R, Wr)
        self.ts("dve", sm[:, 62:63], sm[:, 61:62], sm[:, 52:53], None, ALU.subtract, None, R, Wr)
        self.act(sm[:, 62:63], sm[:, 62:63], AF.Exp, R, Wr)
        self.ts("dve", sm[:, 63:64], sm[:, 62:63], 1.0, None, ALU.add, None, R, Wr)
        self.sc.op("dve", lambda e: e.reciprocal(out=sm[:, 63:64], in_=sm[:, 63:64]), R, Wr)
        self.stt(sm[:, 63:64], sm[:, 63:64], 1.0 / ALPHA, sm[:, 30:31], ALU.mult, ALU.mult, R, Wr)
        self.tt("dve", sm[:, 62:63], sm[:, 62:63], sm[:, 63:64], ALU.mult, R, Wr)
        self.ts("dve", sm[:, 53:57], sm[:, 53:57], sm[:, 63:64], None, ALU.mult, None, R, Wr)
        self.stt(sm[:, 53:57], sm[:, 25:29], sm[:, 62:63], sm[:, 53:57], ALU.mult, ALU.add, R, Wr)
        g4_b = sm[:, 53:57].unsqueeze(1).broadcast_to([128, 4, 4])
        gout = self.gate[:, j, :].rearrange("p (g e) -> p g e", e=4)
        self.tt("dve", gout, ohg_b, g4_b, ALU.mult, R, [self.gate])

    def ln_tile(self, xb, xap, g_row, b_row, eps=LN_EPS):
        sm = self.lnsm
        R, Wr = [sm], [sm]
        for h in range(2):
            self.sc.op("dve", lambda e, h=h: e.bn_stats(out=sm[:, 6 * h:6 * h + 6], in_=xap[:, h * 512:(h + 1) * 512]),
                       [xb], Wr)
        self.sc.op("dve", lambda e: e.bn_aggr(out=sm[:, 12:14], in_=sm[:, 0:12]), R, Wr)
        self.ts("dve", sm[:, 14:15], sm[:, 13:14], eps, None, ALU.add, None, R, Wr)
        self.act(sm[:, 14:15], sm[:, 14:15], AF.Ln, R, Wr)
        self.act(sm[:, 15:16], sm[:, 14:15], AF.Exp, R, Wr, scale=-0.5)
        self.stt(xap, xap, sm[:, 12:13], g_row, ALU.subtract, ALU.mult, [xb, sm, self.rows], [xb])
        self.stt(xap, xap, sm[:, 15:16], b_row, ALU.mult, ALU.add, [xb, sm, self.rows], [xb])

    def wnext(self):
        b = self.wslot[self.wi % len(self.wslot)]
        self.wi += 1
        return b

    def wload_k(self, slot, col0, ncols, wbuf, wap2d, nk=8):
        raise NotImplementedError

    def moe(self, l, src, dst, last):
        S, NT, NG = self.S, self.NT, self.NG
        W = self.W
        XA = [self.slab[8 + j] for j in range(NT)]
        xa = [b.t[:, :].bitcast(F32) for b in XA]
        self.load_rows([2 + 4 * l, 3 + 4 * l])
        for j in range(NT):
            sb_, sap = src(j)
            self.load("sp", XA[j], xa[j], sb_, sap)
        hid4 = self.P2[4:8]
        hidv4 = [h.t[:, :].bitcast(BF16)[:, 0:512] for h in hid4]
        self.sc.rec = []
        _marks = []
        _s0 = 0
        _u = 0
        for e in range(self.dbg.get('nexp', 16)):
            sa = self.wnext()
            sv = sa.t[:, :].rearrange("p (k c) -> p k c", c=512)
            self.load("pool", sa, sv[:, :, 0:256], W["moe_w_gate"],
                      W["moe_w_gate"][l, e, :, :].rearrange("(k p) f -> p k f", p=128))
            self.load("pool", sa, sv[:, :, 256:512], W["moe_w_up"],
                      W["moe_w_up"][l, e, :, :].rearrange("(k p) f -> p k f", p=128))
            sd = self.wnext()
            dv = sd.t[:, 0:2048].rearrange("p (k c) -> p k c", c=1024)
            self.load("pool", sd, dv, W["moe_w_down"],
                      W["moe_w_down"][l, e, :, :].rearrange("(k p) f -> p k f", p=128))
            for g in range(NG):
                tsl = slice(g * 512, (g + 1) * 512)
                hid = hid4[2 * (_u % 2):2 * (_u % 2) + 2]
                hidv = hidv4[2 * (_u % 2):2 * (_u % 2) + 2]
                _u += 1
                for f in range(2):
                    gps = self.PS()
                    for k in range(8):
                        self.mm(gps[:, :], sv[:, k, f * 128:(f + 1) * 128], self.XT[k][:, tsl], k == 0, k == 7,
                                [sa, self.XT[k]], [gps])
                    ups = self.PS()
                    for k in range(8):
                        self.mm(ups[:, :], sv[:, k, 256 + f * 128:256 + (f + 1) * 128], self.XT[k][:, tsl], k == 0, k == 7,
                                [sa, self.XT[k]], [ups])
                    sg = self.sg[f]
                    self.act(sg[:, :], gps[:, :], AF.Silu, [gps], [sg])
                    self.tt("dve", hidv[f], sg[:, :], ups[:, :], ALU.mult, [sg, ups], [hid[f]])
                _mid = len(self.sc.rec)
                for tt_ in range(4):
                    j = g * 4 + tt_
                    for half in range(2):
                        ops = self.PS()
                        for f in range(2):
                            self.mm(ops[:, :], hidv[f][:, tt_ * 128:(tt_ + 1) * 128], dv[:, f, half * 512:(half + 1) * 512],
                                    f == 0, f == 1, [hid[f], sd], [ops])
                        xs_ = xa[j][:, half * 512:(half + 1) * 512]
                        self.stt(xs_, ops[:, :], self.gate[:, j, e:e + 1], xs_, ALU.mult, ALU.add,
                                 [ops, self.gate, XA[j]], [XA[j]])
                _marks.append((_s0, _mid, len(self.sc.rec)))
                _s0 = len(self.sc.rec)
        _lst, self.sc.rec = self.sc.rec, None
        if _marks:
            self.sc.replay(_lst[_marks[0][0]:_marks[0][1]])
            for _i in range(len(_marks)):
                if _i + 1 < len(_marks):
                    self.sc.replay(_lst[_marks[_i + 1][0]:_marks[_i + 1][1]])
                self.sc.replay(_lst[_marks[_i][1]:_marks[_i][2]])
        for j in range(NT):
            if not self.dbg.get('noln'):
                self.ln_tile(XA[j], xa[j], self.row(0), self.row(1), eps=LN_EPS / (ALPHA * ALPHA))
            db, dap = dst(j)
            self.load("sp", db, dap, XA[j], xa[j])
            if not last:
                self.transpose_tile(XA[j], xa[j], j)

    def prologue(self, seq, router_layer=None):
        for j in range(self.NT):
            xb = self.xres[j % 2]
            self.load("sp", xb, xb[:, :], self.x, self.x[seq, j * 128:(j + 1) * 128, :])
            self.transpose_tile(xb, xb[:, :], j, router_layer)

    def alloc_misc(self):
        self.xtf = self.P2[0:2]
        self.lnsm = self.sb("lnsm", [128, 16])
        self.sg = self.P2[2:4]
        self.hid = self.P2[4:6]

    def run(self):
        self.setup()
        self.alloc_misc()
        NT = self.NT
        for seq in range(self.NSEQ):
            def src_x(j, seq=seq):
                return self.x, self.x[seq, j * 128:(j + 1) * 128, :]

            def src_scr(j):
                return self.xscr, self.xscr[j * 128:(j + 1) * 128, :]

            def dst_out(j, seq=seq):
                return self.out, self.out[seq, j * 128:(j + 1) * 128, :]
            cur = src_x
            first = True
            subl = [(l, p) for l in self.layers for p in self.parts]
            for i, (l, p) in enumerate(subl):
                last = i == len(subl) - 1
                dst = dst_out if last else src_scr
                if p == "moe":
                    if first:
                        self.load_srow(9 + l, 0, 20)
                        self.prologue(seq, router_layer=l)
                    self.moe(l, cur, dst, last)
                else:
                    if first:
                        self.prologue(seq)
                    if l == 0:
                        self.even_mixer(cur, dst)
                    else:
                        self.odd_mixer(cur, dst)
                cur = src_scr
                first = False
        self.sc.finish_waits("sp", [self.out])
        self.sc.emit_all()
        return self.nc


NROWS = 16
NROWS_SB = 3
NCOLS = 64


def _host_inputs(S, inputs):
    f = lambda a: np.ascontiguousarray(np.asarray(a, dtype=np.float32))
    m = {}
    m["ev_w_in"] = f(inputs["ev_w_in"][0])
    m["ev_w_out"] = f(inputs["ev_w_out"][0])
    wi = f(inputs["od_w_in"][0])
    kpe = wi[:, 384:416]
    kpe_sw = np.concatenate([kpe[:, 16:32], kpe[:, 0:16]], 1)
    m["od_w_in"] = np.ascontiguousarray(np.concatenate([wi, kpe, kpe, kpe_sw, kpe_sw], 1))
    wq = f(inputs["od_w_q_up"][0]).reshape(256, 8, 96)
    nope = wq[:, :, :64].reshape(256, 512)
    pe = wq[:, :, 64:]
    pe_sw = np.concatenate([pe[:, :, 16:], pe[:, :, :16]], 2)
    m["od_w_q_up"] = np.ascontiguousarray(np.concatenate([nope, pe.reshape(256, 256), pe_sw.reshape(256, 256)], 1))
    wkv = f(inputs["od_w_kv_up"][0]).reshape(128, 8, 128)
    m["od_w_kv_up"] = np.ascontiguousarray(np.concatenate([wkv[:, :, :64].reshape(128, 512), wkv[:, :, 64:].reshape(128, 512)], 1))
    m["od_w_out"] = f(inputs["od_w_out"][0])
    m["moe_w_gate"] = f(inputs["moe_w_gate"])
    m["moe_w_up"] = f(inputs["moe_w_up"])
    m["moe_w_down"] = f(inputs["moe_w_down"])
    wr = np.concatenate([f(inputs["moe_w_group"]), f(inputs["moe_w_expert"])], 2)
    m["moe_wr"] = np.ascontiguousarray(wr.reshape(2, 8, 128, 20).transpose(0, 2, 1, 3))
    rows = np.zeros((NROWS, D), np.float32)
    for l in range(2):
        rows[4 * l + 0] = inputs["ln1_g"][l]
        rows[4 * l + 1] = inputs["ln1_b"][l]
        rows[4 * l + 2] = inputs["ln2_g"][l]
        rows[4 * l + 3] = inputs["ln2_b"][l]
        rows[9 + l, 0:4] = inputs["moe_b_group"][l]
        rows[9 + l, 4:20] = inputs["moe_b_expert"][l]
    rows[8] = inputs["ev_norm_g"][0]
    rows[11, 0:16] = inputs["ev_dt_bias"][0]
    rows[11, 16:32] = inputs["ev_a_log"][0]
    rows[11, 32:48] = inputs["ev_d_skip"][0]
    rows[12, 0:256] = inputs["od_q_norm_g"][0]
    rows[12, 256:384] = inputs["od_kv_norm_g"][0]
    rows[13, 0:8] = inputs["od_f_bias"][0]
    m["rows"] = rows
    cols = np.zeros((128, NCOLS), np.float32)
    cw = f(inputs["ev_conv_w"][0])
    cols[:, 0:48] = cw.T.reshape(12, 128, 4).transpose(1, 0, 2).reshape(128, 48)
    cols[:, 48:60] = f(inputs["ev_conv_b"][0]).reshape(12, 128).T
    cols[0:8, 60] = f(inputs["od_f_bias"][0])
    m["cols"] = cols
    for k, v in _consts(S).items():
        m["c_" + k] = v
    return m


_CACHE = {}


def kernel(**inputs):
    x = np.ascontiguousarray(np.asarray(inputs["x"], dtype=np.float32))
    B, S, _ = x.shape
    nseq = B // NCORES
    key = (S, nseq)
    if key not in _CACHE:
        _CACHE[key] = K(S, nseq).run()
    nc = _CACHE[key]
    shared = _host_inputs(S, inputs)
    in_maps = []
    for c in range(NCORES):
        m = dict(shared)
        m["x"] = np.ascontiguousarray(x[c * nseq:(c + 1) * nseq])
        in_maps.append(m)
    res = run_bass_kernel_spmd(nc, in_maps, core_ids=list(range(NCORES)))
    return np.concatenate([np.asarray(r["out"]) for r in res.results], axis=0).astype(np.float32)


def _f32v(b):
    return b.t[:, :].bitcast(F32)


def _bfv(b):
    return b.t[:, :].bitcast(BF16)


def _wview(slot, c):
    return slot.t[:, :].rearrange("p (k c) -> p k c", c=c)


def _kp(ap2d):
    return ap2d.rearrange("(k p) c -> p k c", p=128)


def _rope_tables(self, g):
    rc, rs = self.P2[3], self.P2[4]
    cc, cs_ = self.cdram["ropec"], self.cdram["ropes"]
    self.load("sp", rc, rc[:, :], cc, cc[0:128, g * 512:(g + 1) * 512])
    self.load("sp", rs, rs[:, :], cs_, cs_[0:128, g * 512:(g + 1) * 512])
    return rc, rs


def _rope(self, A, B, rc, rs, dst, dst_ap, lo=0, hi=64):
    t1, t2 = self.P2[5], self.P2[6]
    self.tt("dve", t1[lo:hi, :], A[lo:hi, :], rc[lo:hi, :], ALU.mult, [A, rc], [t1])
    self.tt("dve", t2[lo:hi, :], B[lo:hi, :], rs[lo:hi, :], ALU.mult, [B, rs], [t2])
    self.tt("pool", dst_ap, t1[lo:hi, :], t2[lo:hi, :], ALU.add, [t1, t2], [dst])


def _attn_finish(self, OT, DEN, hh, dst, dst_ap):
    rec = self.P2[6]
    lo, hi = hh * 64, hh * 64 + 64
    self.act(rec[lo:hi, :], DEN[lo:hi, :], AF.Ln, [DEN], [rec])
    self.act(rec[lo:hi, :], rec[lo:hi, :], AF.Exp, [rec], [rec], scale=-1.0)
    self.tt("dve", dst_ap, OT[lo:hi, :], rec[lo:hi, :], ALU.mult, [OT, rec], [dst])


def _replay_hp(self, recs):
    self.sc.replay(recs[0][0])
    for hp in range(len(recs)):
        if hp + 1 < len(recs):
            self.sc.replay(recs[hp + 1][0])
        self.sc.replay(recs[hp][1])


def odd_mixer(self, src, dst):
    S, NT, NG = self.S, self.NT, self.NG
    W = self.W
    Win = W["od_w_in"]
    sl = self.slab
    P2 = self.P2
    XT = self.XT
    c = self.c
    sc_mla = 1.0 / math.sqrt(96.0)
    sc_fox = 1.0 / 8.0
    self.load_rows([4, 5])
    self.load_srow(10, 0, 20)
    self.load_srow(12, 96, 384)
    CQNT, CKVNT, KPE = sl[8:10], sl[10], sl[11]
    OTF = sl[12:16]
    sm = self.small
    wl = self.wnext()
    wlv = _wview(wl, 512)
    self.load("pool", wl, wlv[:, :, 0:384], Win, _kp(Win[:, 0:384]))
    self.load("pool", wl, wlv[:, :, 384:512], Win, _kp(Win[:, 1960:2088]))
    lat, sq, latn_b = P2[0], P2[1], P2[2]
    latn = _bfv(latn_b)
    for j in range(NT):
        tsl = slice(j * 128, (j + 1) * 128)
        ps = self.PS()
        for k in range(8):
            self.mm(ps[:, 0:384], XT[k][:, tsl], wlv[:, k, 0:384], k == 0, k == 7, [XT[k], wl], [ps])
        self.cp("act", lat[:, 0:384], ps[:, 0:384], [ps], [lat])
        self.tt("pool", sq[:, 0:384], lat[:, 0:384], lat[:, 0:384], ALU.mult, [lat], [sq])
        self.sc.op("dve", lambda e: e.reduce_sum(out=sm[:, 0:1], in_=sq[:, 0:256], axis=mybir.AxisListType.X), [sq], [sm])
        self.sc.op("dve", lambda e: e.reduce_sum(out=sm[:, 1:2], in_=sq[:, 256:384], axis=mybir.AxisListType.X), [sq], [sm])
        self.ts("dve", sm[:, 0:1], sm[:, 0:1], 1.0 / 256, RMS_EPS, ALU.mult, ALU.add, [sm], [sm])
        self.ts("dve", sm[:, 1:2], sm[:, 1:2], 1.0 / 128, RMS_EPS, ALU.mult, ALU.add, [sm], [sm])
        self.act(sm[:, 0:2], sm[:, 0:2], AF.Ln, [sm], [sm])
        self.act(sm[:, 2:4], sm[:, 0:2], AF.Exp, [sm], [sm], scale=-0.5)
        self.stt(latn[:, 0:256], lat[:, 0:256], sm[:, 2:3], self.srow[:, 96:352], ALU.mult, ALU.mult,
                 [lat, sm, self.srow], [latn_b])
        self.stt(latn[:, 256:384], lat[:, 256:384], sm[:, 3:4], self.srow[:, 352:480], ALU.mult, ALU.mult,
                 [lat, sm, self.srow], [latn_b])
        pb = self.PS()
        pbv = _bfv(pb)
        for i in range(3):
            self.tr(pbv[:, i * 128:(i + 1) * 128], latn[:, i * 128:(i + 1) * 128], c["identb"][:, :], [latn_b, c["identb"]], [pb])
        for i, dstb in enumerate((CQNT[0], CQNT[1], CKVNT)):
            self.cp("dve", dstb[:, tsl], pbv[:, i * 128:(i + 1) * 128], [pb], [dstb])
    for g in range(NG):
        gsl = slice(g * 512, (g + 1) * 512)
        rc, rs = _rope_tables(self, g)
        A = self.PS()
        for k in range(8):
            self.mm(A[64:96, :], wlv[:, k, 384:416], XT[k][:, gsl], k == 0, k == 7, [wl, XT[k]], [A])
        B = self.PS()
        for k in range(8):
            self.mm(B[64:96, :], wlv[:, k, 448:480], XT[k][:, gsl], k == 0, k == 7, [wl, XT[k]], [B])
        _rope(self, A, B, rc, rs, KPE, KPE[64:96, gsl], 64, 96)
    wf = self.wnext()
    wfv = wf.t[:, 0:64].rearrange("p (k c) -> p k c", c=8)
    self.load("pool", wf, wfv, Win, _kp(Win[:, 1952:1960]))
    ones = P2[7]
    self.sc.op("pool", lambda e: e.memset(ones[:, :], 1.0), [], [ones])
    negfb = self.small2
    self.ts("pool", negfb[0:8, 0:1], self.cols[0:8, 60:61], -1.0, None, ALU.mult, None, [self.cols], [negfb])
    CSPb = [self.T4[0], self.T4[1]]
    FHQb = [self.T4[2], self.T4[3]]

    def cspg(g):
        return CSPb[g // 2], _f32v(CSPb[g // 2])[:, (g % 2) * 512:(g % 2 + 1) * 512]

    def fhqg(g):
        return FHQb[g // 2], _f32v(FHQb[g // 2])[:, (g % 2) * 512:(g % 2 + 1) * 512]
    CSPT = self.cspt
    for g in range(NG):
        gsl = slice(g * 512, (g + 1) * 512)
        ps = self.PS()
        for k in range(8):
            self.mm(ps[0:8, :], wfv[:, k, :], XT[k][:, gsl], k == 0, k == 7, [wf, XT[k]], [ps])
        e_, sp_ = P2[5], P2[6]
        self.act(e_[0:8, :], ps[0:8, :], AF.Exp, [ps, negfb], [e_], bias=negfb[0:8, 0:1], scale=-1.0)
        self.act(sp_[0:8, :], e_[0:8, :], AF.Ln, [e_], [sp_], bias=1.0)
        cb, cap = cspg(g)
        if g == 0:
            init, rds = 0.0, [ones, sp_]
        else:
            pb_, pap = cspg(g - 1)
            init, rds = pap[0:8, 511:512], [ones, sp_, pb_]
        self.sc.op("dve", lambda e, cap=cap, init=init: e.tensor_tensor_scan(
            out=cap[0:8, :], data0=ones[0:8, :], data1=sp_[0:8, :], initial=init, op0=ALU.mult, op1=ALU.add), rds, [cb])
        for tt_ in range(4):
            j = g * 4 + tt_
            pt = self.PS()
            self.tr(pt[:, 0:8], cap[0:8, tt_ * 128:(tt_ + 1) * 128], c["ident"][0:8, 0:8], [cb, c["ident"]], [pt])
            self.cp("dve", CSPT[:, j, :], pt[:, 0:8], [pt], [CSPT])
    self.load_mask("negm_fox")
    OT, DEN = self.acc
    _recs = []
    for hp in range(4):
        self.sc.rec = []
        q4 = sl[16 + 4 * (hp % 2):20 + 4 * (hp % 2)]
        FQz, FK, FV = q4[0:2], q4[2], q4[3]
        if hp < 2:
            self.sc.op("pool", lambda e, b=FQz[0]: e.memset(b[64:128, :], 0.0), [], [FQz[0]])
            self.sc.op("pool", lambda e, b=FQz[1]: e.memset(b[0:64, :], 0.0), [], [FQz[1]])
        wq = self.wnext()
        wqv = _wview(wq, 512)
        for i, c0 in enumerate((416, 928, 1440)):
            self.load("pool", wq, wqv[:, :, i * 128:(i + 1) * 128], Win, _kp(Win[:, c0 + hp * 128:c0 + (hp + 1) * 128]))
        for g in range(NG):
            gsl = slice(g * 512, (g + 1) * 512)
            for i in range(2):
                ps = self.PS()
                for k in range(8):
                    self.mm(ps[:, :], wqv[:, k, i * 128:(i + 1) * 128], XT[k][:, gsl], k == 0, k == 7, [wq, XT[k]], [ps])
                if i == 0:
                    self.cp("act", FQz[0][0:64, gsl], ps[0:64, :], [ps], [FQz[0]])
                    self.cp("act", FQz[1][64:128, gsl], ps[64:128, :], [ps], [FQz[1]])
                else:
                    self.cp("dve", FK[:, gsl], ps[:, :], [ps], [FK])
        for j in range(NT):
            tsl = slice(j * 128, (j + 1) * 128)
            ps = self.PS()
            for k in range(8):
                self.mm(ps[:, 0:128], XT[k][:, tsl], wqv[:, k, 256:384], k == 0, k == 7, [wq, XT[k]], [ps])
            self.evac(FV[:, tsl], ps[:, 0:128], [ps], [FV])
        _split = len(self.sc.rec)
        FHQs = [[self.T4[2], self.T4[3]], [self.T4[2], self.T4[3]]]
        for hh in range(2):
            h = 2 * hp + hh
            for g in range(NG):
                cb, cap = cspg(g)
                m_ = P2[5]
                self.ts("dve", m_[0:8, :], cap[0:8, :], c["ident"][0:8, h:h + 1], 1.0 / sc_fox, ALU.mult, ALU.mult, [cb, c["ident"]], [m_])
                ps = self.PS()
                self.mm(ps[:, :], ones[0:8, 0:128], m_[0:8, :], True, True, [ones, m_], [ps])
                fb = FHQs[hh][g // 2]
                self.cp("act", _f32v(fb)[:, (g % 2) * 512:(g % 2 + 1) * 512], ps[:, :], [ps], [fb])
            _fox_pipe(self, hp, hh, FQz, FK, FV, FHQs, CSPT, OTF[hp], sc_fox)
        _lst, self.sc.rec = self.sc.rec, None
        _recs.append((_lst[:_split], _lst[_split:]))
    _replay_hp(self, _recs)
    self.load_mask("mask_mla")
    wm = self.wnext()
    wq_ = wm.t[:, 0:2048].rearrange("p (k c) -> p k c", c=1024)
    wkv = wm.t[:, 2048:3072]
    self.load("pool", wm, wq_, W["od_w_q_up"], _kp(W["od_w_q_up"][:, :]))
    self.load("pool", wm, wkv, W["od_w_kv_up"], W["od_w_kv_up"][:, :])
    VM = XT[0:4]
    OTM = XT[4:8]
    for j in range(NT):
        ps = self.PS()
        self.mm(ps[:, :], CKVNT[:, j * 128:(j + 1) * 128], wkv[:, 512:1024], True, True, [CKVNT, wm], [ps])
        self.evac(VM[j // 4][:, (j % 4) * 512:(j % 4 + 1) * 512], ps[:, :], [ps], [VM[j // 4]])
    _recs = []
    for hp in range(4):
        self.sc.rec = []
        q4 = sl[16 + 4 * (hp % 2):20 + 4 * (hp % 2)]
        QH, KH = q4[0:2], q4[2:4]
        if hp < 2:
            for b in q4:
                self.sc.op("pool", lambda e, b=b: e.memset(b[64:128, :], 0.0), [], [b])
        for hh in range(2):
            h = 2 * hp + hh
            for g in range(NG):
                gsl = slice(g * 512, (g + 1) * 512)
                ps = self.PS()
                for kk in range(2):
                    self.mm(ps[0:64, :], wq_[:, kk, h * 64:(h + 1) * 64], CQNT[kk][:, gsl], kk == 0, kk == 1, [wm, CQNT[kk]], [ps])
                self.evac(QH[hh][0:64, gsl], ps[0:64, :], [ps], [QH[hh]])
                ps = self.PS()
                self.mm(ps[0:64, :], wkv[:, h * 64:(h + 1) * 64], CKVNT[:, gsl], True, True, [wm, CKVNT], [ps])
                self.evac(KH[hh][0:64, gsl], ps[0:64, :], [ps], [KH[hh]])
                rc, rs = _rope_tables(self, g)
                A = self.PS()
                for kk in range(2):
                    self.mm(A[64:96, :], wq_[:, kk, 512 + h * 32:512 + (h + 1) * 32], CQNT[kk][:, gsl], kk == 0, kk == 1, [wm, CQNT[kk]], [A])
                B = self.PS()
                for kk in range(2):
                    self.mm(B[64:96, :], wq_[:, kk, 768 + h * 32:768 + (h + 1) * 32], CQNT[kk][:, gsl], kk == 0, kk == 1, [wm, CQNT[kk]], [B])
                _rope(self, A, B, rc, rs, QH[hh], QH[hh][64:96, gsl], 64, 96)
            self.cp("act", KH[hh][64:96, 0:S], KPE[64:96, 0:S], [KPE], [KH[hh]])
        _split = len(self.sc.rec)
        _mla_pipe(self, hp, QH, KH, VM, OTM[hp], sc_mla)
        _lst, self.sc.rec = self.sc.rec, None
        _recs.append((_lst[:_split], _lst[_split:]))
    _replay_hp(self, _recs)
    wo = [self.wnext(), self.wnext()]
    wov = [_wview(w_, 1024) for w_ in wo]
    Wo = W["od_w_out"]
    for i in range(2):
        self.load("pool", wo[i], wov[i], Wo, _kp(Wo[i * 512:(i + 1) * 512, :]))
    cat = list(OTM) + list(OTF)
    for j in range(NT):
        tsl = slice(j * 128, (j + 1) * 128)
        xr = self.xres[j % 2]
        sb_, sap = src(j)
        self.load("sp", xr, xr[:, :], sb_, sap)
        y = self.tmpA[j % 2]
        for half in range(2):
            hs = slice(half * 512, (half + 1) * 512)
            ps = self.PS()
            for kc in range(8):
                self.mm(ps[:, :], cat[kc][:, tsl], wov[kc // 4][:, kc % 4, hs], kc == 0, kc == 7, [cat[kc], wo[kc // 4]], [ps])
            self.stt(y[:, hs], xr[:, hs], ALPHA, ps[:, :], ALU.mult, ALU.add, [xr, ps], [y])
        self.ln_tile(y, y[:, :], self.row(0), self.row(1))
        db, dap = dst(j)
        self.load("sp", db, dap, y, y[:, :])
        self.transpose_tile(y, y[:, :], j, router_layer=1)


K.odd_mixer = odd_mixer


def _v3(ap, inner):
    return ap.rearrange("p (a b) -> p a b", b=inner)


def _bc(ap2, n):
    return ap2.unsqueeze(2).broadcast_to([ap2.shape[0], ap2.shape[1], n])


def even_mixer(self, src, dst):
    S, NT, NG = self.S, self.NT, self.NG
    W = self.W
    Win = W["ev_w_in"]
    sl = self.slab
    P2 = self.P2
    XT = self.XT
    c = self.c
    sm = self.small
    X = mybir.AxisListType.X
    self.load_rows([0, 1, 8])
    self.load_srow(9, 0, 20)
    self.load_srow(11, 32, 48)
    OTS = sl[12:16]
    self.load_mask("mask_sb")
    OTa = self.acc
    _recs = []
    for hp in range(4):
        self.sc.rec = []
        q4 = sl[16 + 4 * (hp % 2):20 + 4 * (hp % 2)]
        QTz, KT, FV = q4[0:2], q4[2], q4[3]
        if hp < 2:
            self.sc.op("pool", lambda e, b=QTz[0]: e.memset(b[64:128, :], 0.0), [], [QTz[0]])
            self.sc.op("pool", lambda e, b=QTz[1]: e.memset(b[0:64, :], 0.0), [], [QTz[1]])
        wq = self.wnext()
        wqv = _wview(wq, 512)
        for i, c0 in enumerate((2576, 3088, 3600)):
            self.load("pool", wq, wqv[:, :, i * 128:(i + 1) * 128], Win, _kp(Win[:, c0 + hp * 128:c0 + (hp + 1) * 128]))
        for g in range(NG):
            gsl = slice(g * 512, (g + 1) * 512)
            for i in range(2):
                ps = self.PS()
                for k in range(8):
                    self.mm(ps[:, :], wqv[:, k, i * 128:(i + 1) * 128], XT[k][:, gsl], k == 0, k == 7, [wq, XT[k]], [ps])
                if i == 0:
                    self.cp("act", QTz[0][0:64, gsl], ps[0:64, :], [ps], [QTz[0]])
                    self.cp("act", QTz[1][64:128, gsl], ps[64:128, :], [ps], [QTz[1]])
                else:
                    self.cp("dve", KT[:, gsl], ps[:, :], [ps], [KT])
        for j in range(NT):
            tsl = slice(j * 128, (j + 1) * 128)
            ps = self.PS()
            for k in range(8):
                self.mm(ps[:, 0:128], XT[k][:, tsl], wqv[:, k, 256:384], k == 0, k == 7, [wq, XT[k]], [ps])
            self.evac(FV[:, tsl], ps[:, 0:128], [ps], [FV])
        _split = len(self.sc.rec)
        _sb_pipe(self, hp, QTz, KT, FV, OTS[hp])
        _lst, self.sc.rec = self.sc.rec, None
        _recs.append((_lst[:_split], _lst[_split:]))
    _replay_hp(self, _recs)
    ps_save = self.ps
    xtf_save = self.xtf
    self.xtf = [P2[0], P2[3]]
    allps = list(self.ps) + list(self.acc)
    self.ps = allps[0:4]
    YD, YO = allps[4:6], allps[6:8]
    XS_T = list(sl[8:12]) + list(sl[16:20])
    BT, CT = sl[20:22], sl[22:24]
    T4 = self.T4
    dests = XS_T + list(BT) + list(CT)
    self.sc.rec = []
    _marks = []
    _s0 = 0
    _uc = 0
    for pnl in range(3):
        wp = self.wnext()
        wpv = _wview(wp, 512)
        self.load("pool", wp, wpv, Win, _kp(Win[:, 1024 + pnl * 512:1024 + (pnl + 1) * 512]))
        for q in range(4):
            cc = pnl * 4 + q
            for g in range(NG):
                gsl = slice(g * 512, (g + 1) * 512)
                ps = self.PS()
                for k in range(8):
                    self.mm(ps[:, :], wpv[:, k, q * 128:(q + 1) * 128], XT[k][:, gsl], k == 0, k == 7, [wp, XT[k]], [ps])
                RAWb = T4[_uc % 2]
                RAW = _bfv(RAWb)
                if g == 0:
                    self.sc.op("pool", lambda e, RAW=RAW: e.memset(RAW[:, 0:3], 0.0), [], [RAWb])
                else:
                    prev = _bfv(T4[(_uc - 1) % 2])
                    self.cp("pool", RAW[:, 0:3], prev[:, 512:515], [T4[(_uc - 1) % 2]], [RAWb])
                _uc += 1
                self.cp("act", RAW[:, 3:515], ps[:, :], [ps], [RAWb])
                DGb = T4[2 + cc % 2]
                DG = _bfv(DGb)
                if g == 0:
                    for tap in range(4):
                        self.ts("dve", DG[:, tap * 128:(tap + 1) * 128], c["identb"][:, :], self.cols[:, cc * 4 + tap:cc * 4 + tap + 1],
                                None, ALU.mult, None, [c["identb"], self.cols], [DGb])
                _mid = len(self.sc.rec)
                cps = self.PS()
                for tap in range(4):
                    self.mm(cps[:, :], DG[:, tap * 128:(tap + 1) * 128], RAW[:, tap:tap + 512], tap == 0, tap == 3, [DGb, RAWb], [cps])
                self.act(dests[cc][:, gsl], cps[:, :], AF.Silu, [cps, self.cols], [dests[cc]], bias=self.cols[:, 48 + cc:49 + cc])
                _marks.append((_s0, _mid, len(self.sc.rec)))
                _s0 = len(self.sc.rec)
    _lst, self.sc.rec = self.sc.rec, None
    self.sc.replay(_lst[_marks[0][0]:_marks[0][1]])
    for _u in range(len(_marks)):
        if _u + 1 < len(_marks):
            self.sc.replay(_lst[_marks[_u + 1][0]:_marks[_u + 1][1]])
        self.sc.replay(_lst[_marks[_u][1]:_marks[_u][2]])
    wd = self.wnext()
    wdv = wd.t[:, 0:128].rearrange("p (k c) -> p k c", c=16)
    self.load("pool", wd, wdv, Win, _kp(Win[:, 2560:2576]))
    DT, DA = self.dtb, self.dab
    AROW = self.small2b
    self.act(AROW[:, 0:16], self.srow[:, 48:64], AF.Exp, [self.srow], [AROW])
    self.ts("pool", AROW[:, 0:16], AROW[:, 0:16], -1.0, None, ALU.mult, None, [AROW], [AROW])
    for j in range(NT):
        tsl = slice(j * 128, (j + 1) * 128)
        ps = self.PS()
        for k in range(8):
            self.mm(ps[:, 0:16], XT[k][:, tsl], wdv[:, k, :], k == 0, k == 7, [XT[k], wd], [ps])
        self.tt("dve", sm[:, 0:16], ps[:, 0:16], self.srow[:, 32:48], ALU.add, [ps, self.srow], [sm])
        self.act(sm[:, 0:16], sm[:, 0:16], AF.Exp, [sm], [sm])
        self.act(DT[:, j, :], sm[:, 0:16], AF.Ln, [sm], [DT], bias=1.0)
        self.tt("dve", DA[:, j, :], DT[:, j, :], AROW[:, 0:16], ALU.mult, [DT, AROW], [DA])
    for half in range(2):
        wz = self.wnext()
        wzv = _wview(wz, 512)
        self.load("pool", wz, wzv, Win, _kp(Win[:, half * 512:(half + 1) * 512]))
        for j in range(NT):
            tsl = slice(j * 128, (j + 1) * 128)
            ps = self.PS()
            for k in range(8):
                self.mm(ps[:, :], XT[k][:, tsl], wzv[:, k, :], k == 0, k == 7, [XT[k], wz], [ps])
            szb = self.tmpA[j % 2]
            self.act(szb[:, 0:512], ps[:, :], AF.Silu, [ps], [szb])
            self.load("sp", self.zscr, self.zscr[j * 128:(j + 1) * 128, half * 512:(half + 1) * 512], szb, szb[:, 0:512])
    wo = [self.wnext(), self.wnext(), self.wnext()]
    wov = [_wview(w_, 1024) for w_ in wo]
    Wo = W["ev_w_out"]
    for i in range(3):
        self.load("pool", wo[i], wov[i], Wo, _kp(Wo[i * 512:(i + 1) * 512, :]))
    XSTOKb, SEGb, GTBb = T4[0], T4[1], T4[3]
    Dgb = SEGb
    XSTOK, SEG = _f32v(XSTOKb), _f32v(SEGb)
    Dg = SEG
    GTB = _bfv(GTBb)
    XSPb, YNb = P2[0], P2[3]
    XSP, YN = _bfv(XSPb), _bfv(YNb)
    XSPPb = [P2[1], P2[2]]
    XSPP = [_bfv(b) for b in XSPPb]
    BTOKb = [self.H2[2], self.H2[3]]
    HT = [P2[5], P2[6]]
    HTbb = P2[7]
    HTb = [_bfv(HTbb)[:, 0:512], _bfv(HTbb)[:, 512:1024]]
    YNTh = [self.H2[0], self.H2[1]]
    TMPb = P2[4]
    self.xtf = [P2[4], P2[3]]
    Y0b = [self.tmpA[1], T4[2]]
    Y0 = [self.tmpA[1][:, :], _f32v(T4[2])]
    ssms = [self.ssm, self.ssm2]
    CBM = self.cbm
    self.sc.op("pool", lambda e: e.memset(GTB, 0.0), [], [GTBb])
    for g in range(2):
        self.sc.op("pool", lambda e, g=g: e.memset(HT[g][:, :], 0.0), [], [HT[g]])
    self.sc.op("pool", lambda e: e.memset(_bfv(HTbb), 0.0), [], [HTbb])

    def stage_a(j):
        tsl = slice(j * 128, (j + 1) * 128)
        ssm = ssms[j % 2]
        ACUM, EA, DTW = ssm[:, 0:16], ssm[:, 16:32], ssm[:, 32:48]
        DEC = [ssm[:, 48:64], ssm[:, 64:80]]
        BTOK = BTOKb[j % 2]
        pb = self.PS()
        pbv = _bfv(pb)
        for cc in range(8):
            self.tr(pbv[:, cc * 128:(cc + 1) * 128], XS_T[cc][:, tsl], c["identb"][:, :], [XS_T[cc], c["identb"]], [pb])
        self.cp("act", XSTOK, pbv, [pb], [XSTOKb])
        pb2 = self.PS()
        pb2v = _bfv(pb2)
        for g in range(2):
            self.tr(pb2v[:, g * 128:(g + 1) * 128], BT[g][:, tsl], c["identb"][:, :], [BT[g], c["identb"]], [pb2])
        self.cp("dve", BTOK[:, 0:256], pb2v[:, 0:256], [pb2], [BTOK])
        ps = self.PS()
        self.mm(ps[:, 0:16], c["tri2"][:, :], DA[:, j, :], True, True, [c["tri2"], DA], [ps])
        self.cp("dve", ACUM, ps[:, 0:16], [ps], [ssm])
        self.act(EA, ACUM, AF.Exp, [ssm], [ssm])
        ps = self.PS()
        self.mm(ps[:, 0:16], c["lastsel"][:, :], ACUM, True, True, [c["lastsel"], ssm], [ps])
        self.tt("dve", DTW, ps[:, 0:16], ACUM, ALU.subtract, [ps, ssm], [ssm])
        self.act(DTW, DTW, AF.Exp, [ssm], [ssm])
        self.tt("dve", DTW, DTW, DT[:, j, :], ALU.mult, [ssm, DT], [ssm])
        for c2 in range(2):
            ps = self.PS()
            self.mm(ps[:, 0:16], c["sellast"][:, c2, :], ACUM, True, True, [c["sellast"], ssm], [ps])
            self.act(DEC[c2], ps[:, 0:16], AF.Exp, [ps], [ssm])
        self.tt("dve", _v3(Dg, 64), _bc(ACUM, 64), c["i64x2"][:, :].unsqueeze(1).broadcast_to([128, 16, 64]), ALU.mult,
                [ssm, c["i64x2"]], [Dgb])
        p1s = []
        for hb in range(2):
            p1 = self.PS()
            p1s.append(p1)
            self.mm(p1[:, :], c["bd"][:, :], Dg[:, hb * 512:(hb + 1) * 512], True, True, [c["bd"], Dgb], [p1])
        for hb in range(2):
            p1 = p1s[hb]
            self.tt("dve", _v3(SEG[:, hb * 512:(hb + 1) * 512], 64), _v3(p1[:, :], 64), _bc(ssm[:, hb * 8:hb * 8 + 8], 64),
                    ALU.subtract, [p1, ssm], [SEGb])
        self.ts("pool", SEG, SEG, 0.0, None, ALU.min, None, [SEGb], [SEGb])
        self.act(SEG, SEG, AF.Exp, [SEGb], [SEGb])
        ps = self.PS()
        for c2 in range(2):
            csl = slice(j * 128 + c2 * 64, j * 128 + c2 * 64 + 64)
            for g in range(2):
                self.sc.op("pe", lambda e, ps=ps, c2=c2, g=g, csl=csl: e.matmul(
                    ps[c2 * 64:c2 * 64 + 64, g * 64:(g + 1) * 64], BT[g][:, csl], CT[g][:, csl], start=True, stop=True,
                    skip_group_check=True), [BT[g], CT[g]], [ps])
        self.tt("dve", _v3(CBM[:, :], 64), _v3(ps[:, 0:128], 64), c["trimask"][:, :].unsqueeze(1).broadcast_to([128, 2, 64]),
                ALU.mult, [ps, c["trimask"]], [CBM])
        for c2 in range(2):
            lo, hi = c2 * 64, c2 * 64 + 64
            out_ap = GTB[lo:hi, :].rearrange("p (h x) -> p h x", x=128)[:, :, lo:hi].rearrange("p (g r) l -> p g r l", g=2)
            in0 = SEG[lo:hi, :].rearrange("p (g r l) -> p g r l", g=2, r=8)
            in1 = _v3(CBM[lo:hi, :], 64).unsqueeze(2).broadcast_to([64, 2, 8, 64])
            self.tt("dve", out_ap, in0, in1, ALU.mult, [SEGb, CBM], [GTBb])
        self.tt("pool", _v3(XSP, 64), _v3(XSTOK, 64), _bc(DT[:, j, :], 64), ALU.mult, [XSTOKb, DT], [XSPb])
        self.tt("pool", _v3(XSPP[j % 2], 64), _v3(XSTOK, 64), _bc(DTW, 64), ALU.mult, [XSTOKb, ssm], [XSPPb[j % 2]])
        for h in range(16):
            self.sc.op("pe", lambda e, h=h: e.matmul(
                YD[h // 8][:, (h % 8) * 64:(h % 8 + 1) * 64], GTB[:, h * 128:(h + 1) * 128], XSP[:, h * 64:(h + 1) * 64],
                start=True, stop=True, skip_group_check=True), [GTBb, XSPb], [YD[h // 8]])
        self.tt("pool", _v3(SEG, 64), _v3(XSTOK, 64), _bc(self.srow[:, 64:80], 64), ALU.mult, [XSTOKb, self.srow], [SEGb])
        for g in range(2):
            hs = slice(g * 512, (g + 1) * 512)
            self.tt("dve", Y0[j % 2][:, hs], SEG[:, hs], YD[g][:, :], ALU.add, [SEGb, YD[g]], [Y0b[j % 2]])

    def stage_b(j):
        tsl = slice(j * 128, (j + 1) * 128)
        ssm = ssms[j % 2]
        DEC = [ssm[:, 48:64], ssm[:, 64:80]]
        BTOK = BTOKb[j % 2]
        Yb, Yap = Y0b[j % 2], Y0[j % 2]
        for c2 in range(2):
            lo, hi = c2 * 64, c2 * 64 + 64
            csl = slice(j * 128 + lo, j * 128 + hi)
            for g in range(2):
                self.sc.op("pe", lambda e, g=g, lo=lo, hi=hi, csl=csl: e.matmul(
                    YO[g][lo:hi, :], CT[g][:, csl], HTb[g], start=True, stop=True, skip_group_check=True),
                    [CT[g], HTbb], [YO[g]])
            for g in range(2):
                st = self.PS()
                self.mm(st[:, :], BTOK[lo:hi, g * 128:(g + 1) * 128], XSPP[j % 2][lo:hi, g * 512:(g + 1) * 512], True, True,
                        [BTOK, XSPPb[j % 2]], [st])
                self.tt("dve", _v3(HT[g][:, :], 64), _v3(HT[g][:, :], 64), _bc(DEC[c2][:, g * 8:(g + 1) * 8], 64), ALU.mult,
                        [HT[g], ssm], [HT[g]])
                self.tt("dve", HT[g][:, :], HT[g][:, :], st[:, :], ALU.add, [HT[g], st], [HT[g]])
                self.cp("act", HTb[g], HT[g][:, :], [HT[g]], [HTbb])
        SZb = self.tmpA[0]
        self.load("sp", SZb, SZb[:, :], self.zscr, self.zscr[tsl, :])
        for g in range(2):
            hs = slice(g * 512, (g + 1) * 512)
            self.tt("dve", _v3(TMPb[:, :], 64), _v3(YO[g][:, :], 64), _bc(ssm[:, 16 + g * 8:16 + g * 8 + 8], 64), ALU.mult,
                    [YO[g], ssm], [TMPb])
            self.tt("pool", Yap[:, hs], Yap[:, hs], TMPb[:, :], ALU.add, [Yb, TMPb], [Yb])
        self.tt("pool", Yap, Yap, SZb[:, :], ALU.mult, [Yb, SZb], [Yb])
        self.tt("pool", SZb[:, :], Yap, Yap, ALU.mult, [Yb], [SZb])
        self.sc.op("dve", lambda e: e.reduce_sum(out=sm[:, 0:1], in_=SZb[:, :], axis=X), [SZb], [sm])
        self.ts("dve", sm[:, 0:1], sm[:, 0:1], 1.0 / 1024, RMS_EPS, ALU.mult, ALU.add, [sm], [sm])
        self.act(sm[:, 0:1], sm[:, 0:1], AF.Ln, [sm], [sm])
        self.act(sm[:, 1:2], sm[:, 0:1], AF.Exp, [sm], [sm], scale=-0.5)
        self.stt(YN, Yap, sm[:, 1:2], self.rows[:, 2, :], ALU.mult, ALU.mult, [Yb, sm, self.rows], [YNb])
        pb = self.PS()
        pbv = _bfv(pb)
        for cc in range(8):
            self.tr(pbv[:, cc * 128:(cc + 1) * 128], YN[:, cc * 128:(cc + 1) * 128], c["identb"][:, :], [YNb, c["identb"]], [pb])
        self.cp("act", YNTh[0][:, :], pbv[:, 0:512], [pb], [YNTh[0]])
        self.cp("act", YNTh[1][:, :], pbv[:, 512:1024], [pb], [YNTh[1]])
        xr = self.xres[j % 2]
        sb_, sap = src(j)
        self.load("sp", xr, xr[:, :], sb_, sap)
        for half in range(2):
            hs = slice(half * 512, (half + 1) * 512)
            ps = self.PS()
            for kc in range(12):
                lhsT = YNTh[kc // 4][:, (kc % 4) * 128:(kc % 4 + 1) * 128] if kc < 8 else OTS[kc - 8][:, tsl]
                rb_ = YNTh[kc // 4] if kc < 8 else OTS[kc - 8]
                self.mm(ps[:, :], lhsT, wov[kc // 4][:, kc % 4, hs], kc == 0, kc == 11, [rb_, wo[kc // 4]], [ps])
            self.stt(SZb[:, hs], xr[:, hs], ALPHA, ps[:, :], ALU.mult, ALU.add, [xr, ps], [SZb])
        self.ln_tile(SZb, SZb[:, :], self.row(0), self.row(1))
        db, dap = dst(j)
        self.load("sp", db, dap, SZb, SZb[:, :])
        self.transpose_tile(SZb, SZb[:, :], j, router_layer=0)

    psA, psB = allps[0:2], allps[2:4]

    def rec_stage(fn, j, banks):
        self.ps = banks
        return self.sc.record(lambda: fn(j))
    self.sc.replay(rec_stage(stage_a, 0, psA))
    for j in range(NT):
        lb = rec_stage(stage_b, j, psB)
        la = rec_stage(stage_a, j + 1, psA) if j + 1 < NT else []
        self.sc.replay(la, lb)
    self.ps = ps_save
    self.xtf = xtf_save


K.even_mixer = even_mixer


def _pipe(n, stages):
    maxs = max(s for s, _ in stages)
    for t in range(n + maxs):
        for s, fn in stages:
            u = t - s
            if 0 <= u < n:
                fn(u)


def _units(NG, hhs=(0, 1)):
    return [(hh, G, idx, i, 4 * (G + 1)) for hh in hhs for G in range(NG)
            for idx, i in enumerate(range(4 * (G + 1) - 1, -1, -1))]


def _sb_pipe(self, hp, QTz, KT, FV, OTSb):
    P2, H2, c = self.P2, self.H2, self.c
    units = _units(self.NG)
    n = len(units)
    E, Ecs = P2[0:3], P2[3:5]
    Lb, Rb = H2[0:2], H2[2]
    Wb = [H2[3], P2[5]]
    Wt = [H2[3][:, :], _bfv(P2[5])[:, 0:512]]
    zb, cb = {}, {}

    def s_z(u):
        hh, G, idx, i, nkb = units[u]
        lo, hi = hh * 64, hh * 64 + 64
        z = self.PS()
        zb[u] = z
        self.mm(z[:, :], KT[:, i * 128:(i + 1) * 128], QTz[hh][:, G * 512:(G + 1) * 512], True, True, [KT, QTz[hh]], [z])

    def s_E(u):
        hh, G, idx, i, nkb = units[u]
        z = zb.pop(u)
        e = E[u % 3]
        self.act(e[:, :], z[:, :], AF.Exp, [z], [e], scale=0.125)
        if i >= 4 * G:
            self.tt("dve", e[:, :], e[:, :], self.maskbuf[:, i - 4 * G, :], ALU.mult, [e, self.maskbuf], [e])

    def s_Lb(u):
        e, l = E[u % 3], Lb[u % 2]
        self.act(l[:, :], e[:, :], AF.Ln, [e], [l], bias=1.0)

    def s_CS(u):
        hh, G, idx, i, nkb = units[u]
        l = Lb[u % 2]
        CS = self.PS()
        cb[u] = CS
        self.mm(CS[:, :], c["uincl"][:, :], l[:, :], True, idx == 0, [c["uincl"], l], [CS])
        if idx > 0:
            self.mm(CS[:, :], c["onesb"][:, :], Rb[:, :], False, True, [c["onesb"], Rb], [CS])
        if idx < nkb - 1:
            if idx == 0:
                self.cp("dve", Rb[:, :], l[:, :], [l], [Rb])
            else:
                self.tt("pool", Rb[:, :], Rb[:, :], l[:, :], ALU.add, [Rb, l], [Rb])

    def s_Ecs(u):
        CS = cb.pop(u)
        ec = Ecs[u % 2]
        self.act(ec[:, :], CS[:, :], AF.Exp, [CS], [ec], scale=-1.0)
        self.tt("dve", Wt[u % 2], E[u % 3][:, :], ec[:, :], ALU.mult, [E[u % 3], ec], [Wb[u % 2]])

    def s_PV(u):
        hh, G, idx, i, nkb = units[u]
        lo, hi = hh * 64, hh * 64 + 64
        OT = self.acc[(hh * self.NG + G) % 2]
        self.mm(OT[:, :], FV[:, i * 128:(i + 1) * 128], Wt[u % 2], idx == 0, idx == nkb - 1, [FV, Wb[u % 2]], [OT])
        if idx == nkb - 1:
            self.cp("act", OTSb[lo:hi, G * 512:(G + 1) * 512], OT[lo:hi, :], [OT], [OTSb])

    _pipe(n, [(0, s_z), (2, s_Lb), (2, s_CS), (3, s_Ecs), (1, s_E), (4, s_PV)])


def _fox_pipe(self, hp, hh_, FQz, FK, FV, FHQs, CSPT, OTb, scale):
    P2, H2, c = self.P2, self.H2, self.c
    units = _units(self.NG, (hh_,))
    n = len(units)
    A = P2[0:3]
    Wb = H2[0:2]
    OT, DEN = self.acc
    zb = {}

    def s_z(u):
        hh, G, idx, i, nkb = units[u]
        lo, hi = hh * 64, hh * 64 + 64
        z = self.PS()
        zb[u] = z
        self.mm(z[:, :], FK[:, i * 128:(i + 1) * 128], FQz[hh][:, G * 512:(G + 1) * 512], True, True, [FK, FQz[hh]], [z])

    def s_A(u):
        hh, G, idx, i, nkb = units[u]
        z = zb.pop(u)
        a = A[u % 3]
        fb = FHQs[hh][G // 2]
        fap = _f32v(fb)[:, (G % 2) * 512:(G % 2 + 1) * 512]
        self.tt("dve", a[:, :], z[:, :], fap, ALU.subtract, [z, fb], [a])
        if i >= 4 * G:
            self.tt("pool", a[:, :], a[:, :], self.maskbuf[:, i - 4 * G, :], ALU.add, [a, self.maskbuf], [a])

    def s_W(u):
        hh, G, idx, i, nkb = units[u]
        h = 2 * hp + hh
        a, w = A[u % 3], Wb[u % 2]
        self.act(w[:, :], a[:, :], AF.Exp, [a, CSPT], [w], bias=CSPT[:, i, h:h + 1], scale=scale)

    def s_PV(u):
        hh, G, idx, i, nkb = units[u]
        lo, hi = hh * 64, hh * 64 + 64
        w = Wb[u % 2]
        self.mm(OT[:, :], FV[:, i * 128:(i + 1) * 128], w[:, :], idx == 0, idx == nkb - 1, [FV, w], [OT])
        self.mm(DEN[:, :], c["onesb"][:, :], w[:, :], idx == 0, idx == nkb - 1, [c["onesb"], w], [DEN])
        if idx == nkb - 1:
            _attn_finish(self, OT, DEN, hh, OTb, OTb[lo:hi, G * 512:(G + 1) * 512])

    _pipe(n, [(0, s_z), (2, s_W), (1, s_A), (3, s_PV)])


def _mla_pipe(self, hp, QH, KH, VM, OTb, scale):
    P2, H2, c = self.P2, self.H2, self.c
    units = _units(self.NG)
    n = len(units)
    Wf = P2[0:2]
    Wb = H2[0:2]
    OT, DEN = self.acc
    zb = {}

    def s_z(u):
        hh, G, idx, i, nkb = units[u]
        lo, hi = hh * 64, hh * 64 + 64
        plo, phi = hh * 32, hh * 32 + 32
        ksl, gsl = slice(i * 128, (i + 1) * 128), slice(G * 512, (G + 1) * 512)
        z = self.PS()
        zb[u] = z
        self.mm(z[:, :], KH[hh][:, ksl], QH[hh][:, gsl], True, True, [KH[hh], QH[hh]], [z])

    def s_W(u):
        hh, G, idx, i, nkb = units[u]
        z = zb.pop(u)
        w = Wb[u % 2]
        if i >= 4 * G:
            wf = Wf[u % 2]
            self.act(wf[:, :], z[:, :], AF.Exp, [z], [wf], scale=scale)
            self.tt("dve", w[:, :], wf[:, :], self.maskbuf[:, i - 4 * G, :], ALU.mult, [wf, self.maskbuf], [w])
        else:
            self.act(w[:, :], z[:, :], AF.Exp, [z], [w], scale=scale)

    def s_PV(u):
        hh, G, idx, i, nkb = units[u]
        h = 2 * hp + hh
        lo, hi = hh * 64, hh * 64 + 64
        w = Wb[u % 2]
        vs = VM[i // 4][:, (i % 4) * 512 + hp * 128:(i % 4) * 512 + (hp + 1) * 128]
        self.mm(OT[:, :], vs, w[:, :], idx == 0, idx == nkb - 1, [VM[i // 4], w], [OT])
        self.mm(DEN[:, :], c["onesb"][:, :], w[:, :], idx == 0, idx == nkb - 1, [c["onesb"], w], [DEN])
        if idx == nkb - 1:
            _attn_finish(self, OT, DEN, hh, OTb, OTb[lo:hi, G * 512:(G + 1) * 512])

    _pipe(n, [(0, s_z), (1, s_W), (2, s_PV)])
```

```python
import math
from contextlib import ExitStack
import numpy as np
import ml_dtypes
import concourse.bass as bass
import concourse.mybir as mybir
from concourse.bass_utils import run_bass_kernel_spmd

F32 = mybir.dt.float32
BF16 = mybir.dt.bfloat16
AF = mybir.ActivationFunctionType
ALU = mybir.AluOpType

D = 1024
NCORES = 8
ALPHA = (2.0 * 2) ** 0.25
LN_EPS = 1e-5
RMS_EPS = 1e-6
EVEN_IN = 4112
ODD_IN = 1960

ENGS = ("pe", "act", "dve", "pool", "sp")


class Buf:
    _n = 0

    def __init__(self, name, t):
        self.name = name
        self.t = t
        self.w = None
        self.r = {}
        self.excl = False
        self.sem = None
        self.semcnt = 0
        Buf._n += 1
        self.id = Buf._n

    def __getitem__(self, k):
        return self.t[k]


class Sched:
    def __init__(self, nc, es):
        self.nc = nc
        self.es = es
        self.ops = {e: [] for e in ENGS}
        self.cnt = {e: 0 for e in ENGS}
        self.known = {e: {} for e in ENGS}
        self.snap = {e: [None] for e in ENGS}
        self.rec = None
        self.sems = {e: es.enter_context(nc.semaphore("c_" + e)) for e in ENGS}
        self.nsem = len(ENGS)

    def _dma_sem(self, b):
        if b.sem is None:
            b.sem = self.es.enter_context(self.nc.semaphore("d_%d" % b.id))
            self.nsem += 1
        return b.sem

    def _waits(self, eng, reads, writes):
        waits = {}

        def need(ev, same_ok):
            if ev is None:
                return
            k, v = ev
            if k == eng and (same_ok or eng in ("pe", "sp")):
                return
            if v > waits.get(k, 0):
                waits[k] = v
        for b in reads:
            need(b.w, False)
            if b.excl:
                for k, v in b.r.items():
                    need((k, v), True)
        for b in writes:
            need(b.w, False)
            for k, v in b.r.items():
                need((k, v), False)
        kn = self.known[eng]
        out = []
        for k, v in waits.items():
            if kn.get(k, 0) >= v:
                continue
            out.append((k, v))
            kn[k] = v
            if isinstance(k, str):
                sn = self.snap[k][v]
                if sn is not None:
                    for k2, v2 in sn.items():
                        if k2 != eng and kn.get(k2, 0) < v2:
                            kn[k2] = v2
        return out

    def op(self, eng, emit, reads=(), writes=()):
        if self.rec is not None:
            self.rec.append(("op", (eng, emit, tuple(reads), tuple(writes)), {}))
            return
        waits = self._waits(eng, reads, writes)
        self.cnt[eng] += 1
        idx = self.cnt[eng]
        ev = (eng, idx)
        for b in reads:
            b.r[eng] = idx
        for b in writes:
            b.w = ev
            b.r = {}
        self.snap[eng].append({k: v for k, v in self.known[eng].items() if isinstance(k, str)})
        self.ops[eng].append((waits, emit, None))

    def dma(self, q, out_ap, in_ap, dst, src, extra_reads=(), **kw):
        if self.rec is not None:
            self.rec.append(("dma", (q, out_ap, in_ap, dst, src, extra_reads), kw))
            return
        reads = [src] + list(extra_reads)
        waits = self._waits(q, reads, [dst])
        sem = self._dma_sem(dst)
        dst.semcnt += 16
        ev = (("dma", dst.id, sem), dst.semcnt)
        src.r[ev[0]] = ev[1]
        for b in extra_reads:
            b.r[ev[0]] = ev[1]
        dst.w = ev
        dst.r = {}

        def emit(e):
            return e.dma_start(out=out_ap, in_=in_ap, **kw)
        self.ops[q].append((waits, emit, sem))

    def record(self, fn):
        assert self.rec is None
        self.rec = []
        fn()
        lst, self.rec = self.rec, None
        return lst

    def replay(self, la, lb=()):
        ia = ib = 0
        na, nb = len(la), len(lb)
        while ia < na or ib < nb:
            if ib >= nb or (ia < na and ia * nb <= ib * na):
                kind, a, k = la[ia]
                ia += 1
            else:
                kind, a, k = lb[ib]
                ib += 1
            if kind == "op":
                self.op(*a)
            else:
                self.dma(*a, **k)

    def finish_waits(self, eng, bufs):
        waits = self._waits(eng, bufs, [])
        self.ops[eng].append((waits, None, None))

    def emit_all(self):
        nc = self.nc
        handles = {"pe": "tensor", "act": "scalar", "dve": "vector", "pool": "gpsimd", "sp": "sync"}
        with nc.Block() as block:
            for e in ENGS:
                ops = self.ops[e]
                esem = self.sems[e]

                def body(eng, ops=ops, esem=esem):
                    for waits, emit, dsem in ops:
                        for k, v in waits:
                            s = self.sems[k] if isinstance(k, str) else k[2]
                            eng.wait_ge(s, v)
                        if emit is None:
                            continue
                        ins = emit(eng)
                        if dsem is not None:
                            ins.then_inc(dsem, 16)
                        else:
                            ins.then_inc(esem, 1)
                getattr(block, handles[e])(body)


def _consts(S):
    c = {}
    p = np.arange(128)
    c["ident"] = np.eye(128, dtype=np.float32)
    c["identb"] = np.eye(128, dtype=np.float32).astype(ml_dtypes.bfloat16)
    c["onesb"] = np.ones((128, 128), np.float32).astype(ml_dtypes.bfloat16)
    c["uincl"] = (p[:, None] >= p[None, :]).astype(np.float32).astype(ml_dtypes.bfloat16)
    s = p[None, :, None]
    t = np.arange(512)[None, None, :]
    m = np.arange(4)[:, None, None]
    c["mask_sb"] = ((s + 128 * m) < t).astype(np.float32).astype(ml_dtypes.bfloat16)
    c["mask_mla"] = (((s + 128 * m) // 64) <= (t // 64)).astype(np.float32).astype(ml_dtypes.bfloat16)
    c["negm_fox"] = np.where((s + 128 * m) <= t, 0.0, -30000.0).astype(np.float32).astype(ml_dtypes.bfloat16)
    c2 = p // 64
    l = p % 64
    c["tri2"] = ((c2[:, None] == c2[None, :]) & (l[:, None] <= l[None, :])).astype(np.float32)
    c["bd"] = (c2[:, None] == c2[None, :]).astype(np.float32)
    c["lastsel"] = (p[:, None] == (64 * c2[None, :] + 63)).astype(np.float32)
    sl = np.zeros((2, 128, 128), np.float32)
    sl[0, 63, :] = 1.0
    sl[1, 127, :] = 1.0
    c["sellast"] = sl
    c["i64x2"] = (l[:, None] == np.arange(64)[None, :]).astype(np.float32)
    c["trimask"] = (np.arange(64)[None, :] >= l[:, None]).astype(np.float32)
    c["sel8"] = np.repeat(np.eye(8, dtype=np.float32), 128, axis=1)
    ng = np.where(np.arange(64)[None, :] < l[:, None], -30000.0, 0.0).astype(np.float32)
    c["negm_ssd"] = np.tile(ng[:, None, :], (1, 16, 1)).reshape(128, 1024).astype(ml_dtypes.bfloat16)
    inv = 10000.0 ** (-(np.arange(0, 32, 2, dtype=np.float32) / 32.0))
    ang = np.arange(S, dtype=np.float32)[None, :] * inv[:, None]
    cos = np.cos(ang).astype(np.float32)
    sin = np.sin(ang).astype(np.float32)
    cosT = np.concatenate([cos, cos], 0)
    sinT = np.concatenate([-sin, sin], 0)
    c["ropec"] = np.concatenate([cosT, cosT, cosT, cosT], 0).astype(np.float32)
    c["ropes"] = np.concatenate([sinT, sinT, sinT, sinT], 0).astype(np.float32)
    return c


class K:
    def __init__(self, S, NSEQ, layers=(0, 1), parts=("mix", "moe"), taps=()):
        self.S, self.NSEQ = S, NSEQ
        self.NT = S // 128
        self.GW = 512
        self.NG = S // 512
        self.layers, self.parts, self.taps = layers, parts, taps
        self.nc = bass.Bass("TRN2", target_bir_lowering=False)
        self.es = ExitStack()
        self.sc = Sched(self.nc, self.es)
        self.psi = 0
        self.evi = 0
        self.dbg = {}
        self.tap_out = {}

    def dram_in(self, name, shape, dt=F32):
        t = self.nc.dram_tensor(name, list(shape), dt, kind="ExternalInput")
        return Buf(name, t)

    def dram_out(self, name, shape, dt=F32):
        t = self.nc.dram_tensor(name, list(shape), dt, kind="ExternalOutput")
        return Buf(name, t)

    def sb(self, name, shape, dt=F32):
        t = self.es.enter_context(self.nc.sbuf_tensor("s_" + name, list(shape), dt))
        return Buf(name, t)

    def PS(self):
        b = self.ps[self.psi % len(self.ps)]
        self.psi += 1
        return b

    def mm(self, out, lhsT, rhs, start, stop, reads, writes):
        self.sc.op("pe", lambda e: e.matmul(out, lhsT, rhs, start=start, stop=stop), reads, writes)

    def tr(self, out, in_, ident, reads, writes):
        self.sc.op("pe", lambda e: e.transpose(out, in_, ident), reads, writes)

    def act(self, out, in_, func, reads, writes, bias=None, scale=1.0):
        kw = {}
        if bias is not None:
            kw["bias"] = bias
        self.sc.op("act", lambda e: e.activation(out=out, in_=in_, func=func, scale=scale, **kw), reads, writes)

    def tt(self, eng, out, in0, in1, op, reads, writes):
        self.sc.op(eng, lambda e: e.tensor_tensor(out=out, in0=in0, in1=in1, op=op), reads, writes)

    def ts(self, eng, out, in0, s1, s2, op0, op1, reads, writes):
        if op1 is None:
            self.sc.op(eng, lambda e: e.tensor_scalar(out=out, in0=in0, scalar1=s1, scalar2=None, op0=op0), reads, writes)
        else:
            self.sc.op(eng, lambda e: e.tensor_scalar(out=out, in0=in0, scalar1=s1, scalar2=s2, op0=op0, op1=op1), reads, writes)

    def stt(self, out, in0, scalar, in1, op0, op1, reads, writes):
        self.sc.op("dve", lambda e: e.scalar_tensor_tensor(out=out, in0=in0, scalar=scalar, in1=in1, op0=op0, op1=op1), reads, writes)

    def cp(self, eng, out, in_, reads, writes):
        if eng == "act":
            self.sc.op("act", lambda e: e.activation(out=out, in_=in_, func=AF.Copy), reads, writes)
        else:
            self.sc.op(eng, lambda e: e.tensor_copy(out=out, in_=in_), reads, writes)

    def evac(self, out, in_, reads, writes):
        self.evi += 1
        self.cp("act" if self.evi % 2 else "dve", out, in_, reads, writes)

    def load(self, q, dstbuf, out_ap, srcbuf, in_ap, **kw):
        self.sc.dma(q, out_ap, in_ap, dstbuf, srcbuf, **kw)

    def setup(self):
        S, NSEQ, NT = self.S, self.NSEQ, self.NT
        self.x = self.dram_in("x", [NSEQ, S, D])
        self.out = self.dram_out("out", [NSEQ, S, D])
        self.xscr = Buf("xscr", self.nc.dram_tensor("xscr", [S, D], F32, kind="Internal"))
        W = {}
        W["ev_w_in"] = self.dram_in("ev_w_in", [D, EVEN_IN])
        W["ev_w_out"] = self.dram_in("ev_w_out", [1536, D])
        W["od_w_in"] = self.dram_in("od_w_in", [D, ODD_IN + 128])
        W["od_w_q_up"] = self.dram_in("od_w_q_up", [256, 512 + 256 + 256])
        W["od_w_kv_up"] = self.dram_in("od_w_kv_up", [128, 1024])
        W["od_w_out"] = self.dram_in("od_w_out", [D, D])
        W["moe_w_gate"] = self.dram_in("moe_w_gate", [2, 16, D, 256])
        W["moe_w_up"] = self.dram_in("moe_w_up", [2, 16, D, 256])
        W["moe_w_down"] = self.dram_in("moe_w_down", [2, 16, 256, D])
        W["moe_wr"] = self.dram_in("moe_wr", [2, 128, 8, 20])
        W["rows"] = self.dram_in("rows", [NROWS, D])
        W["cols"] = self.dram_in("cols", [128, NCOLS])
        self.W = W
        cs = _consts(S)
        self.cdram = {}
        for k, v in cs.items():
            dt = BF16 if v.dtype == ml_dtypes.bfloat16 else F32
            self.cdram[k] = self.dram_in("c_" + k, v.shape, dt)
        self.slab = [self.sb("slab%d" % i, [128, 2048], BF16) for i in range(28)]
        self.XT = self.slab[0:8]
        self.wslot = [self.sb("wslot%d" % i, [128, 4096], BF16) for i in range(4)]
        self.wi = 0
        self.ps = [Buf("ps%d" % i, self.es.enter_context(self.nc.psum_tensor("ps%d" % i, [128, 512], F32)))
                   for i in range(8)]
        for b in self.ps:
            b.excl = True
        self.acc = self.ps[6:8]
        self.ps = self.ps[0:6]
        c = {}
        for k in ("ident", "identb", "onesb", "uincl", "tri2", "bd", "lastsel", "i64x2", "trimask"):
            v = cs[k]
            c[k] = self.sb("k_" + k, v.shape, BF16 if v.dtype == ml_dtypes.bfloat16 else F32)
            self.load("sp", c[k], c[k][:, :], self.cdram[k], self.cdram[k][:, :])
        c["sellast"] = self.sb("k_sellast", [128, 2, 128])
        for i in range(2):
            self.load("sp", c["sellast"], c["sellast"][:, i, :], self.cdram["sellast"], self.cdram["sellast"][i, :, :])
        self.c = c
        self.rows = self.sb("rows", [128, NROWS_SB, D])
        self.srow = self.sb("srow", [128, 512])
        self.maskbuf = self.sb("maskbuf", [128, 4, 512], BF16)
        self.P2 = [self.sb("p2_%d" % i, [128, 512]) for i in range(8)]
        self.H2 = [self.sb("h2_%d" % i, [128, 512], BF16) for i in range(4)]
        self.cols = self.sb("cols", [128, NCOLS])
        self.load("sp", self.cols, self.cols[:, :], W["cols"], W["cols"][:, :])
        self.wr = self.sb("wr", [128, 2, 8, 20])
        for l in range(2):
            self.load("sp", self.wr, self.wr[:, l, :, :], W["moe_wr"], W["moe_wr"][l, :, :, :])
        self.gate = self.sb("gate", [128, NT, 16])
        self.xres = [self.sb("xres0", [128, D])] * 2
        self.tmpA = [self.sb("tmpA%d" % i, [128, D]) for i in range(2)]
        self.T4 = [Buf("t4_%d" % i, None) for i in range(4)]
        for i in range(4):
            self.T4[i].t = self.slab[24 + i].t
        self.T4 = self.slab[24:28]
        self.small = self.sb("small", [128, 64])
        self.small2 = self.sb("small2", [128, 8])
        self.small2b = self.sb("small2b", [128, 16])
        self.dtb = self.sb("dtb", [128, self.NT, 16])
        self.dab = self.sb("dab", [128, self.NT, 16])
        self.ssm = self.sb("ssm", [128, 80])
        self.ssm2 = self.sb("ssm2", [128, 80])
        self.cbm = self.sb("cbm", [128, 128])
        self.zscr = Buf("zscr", self.nc.dram_tensor("zscr", [S, D], F32, kind="Internal"))
        self.cspt = self.sb("cspt", [128, self.NT, 8])
        self.xi = 0

    def row(self, r):
        return self.rows[:, r, :]

    def load_srow(self, r, off, n):
        src = self.W["rows"][r:r + 1, 0:n].broadcast_to([128, n])
        self.load("sp", self.srow, self.srow[:, off:off + n], self.W["rows"], src)

    def load_mask(self, name):
        cd = self.cdram[name]
        for m_ in range(4):
            self.load("sp", self.maskbuf, self.maskbuf[:, m_, :], cd, cd[m_, :, :])

    def load_rows(self, idx_list):
        for slot, r in enumerate(idx_list):
            src = self.W["rows"][r:r + 1, :].broadcast_to([128, D])
            self.load("sp", self.rows, self.rows[:, slot, :], self.W["rows"], src)

    def transpose_tile(self, xb, xap, j, router_layer=None):
        c = self.c
        for half in range(2):
            ps = self.PS()
            for kk in range(4):
                k = half * 4 + kk
                self.tr(ps[:, kk * 128:(kk + 1) * 128], xap[:, k * 128:(k + 1) * 128], c["ident"][:, :],
                        [xb, c["ident"]], [ps])
            self.evi += 1
            for kk in range(4):
                k = half * 4 + kk
                self.cp("act" if self.evi % 2 else "dve", self.XT[k][:, j * 128:(j + 1) * 128],
                        ps[:, kk * 128:(kk + 1) * 128], [ps], [self.XT[k]])
            if router_layer is not None and not self.dbg.get('noxtf'):
                xtf = self.xtf[half]
                self.cp("dve", xtf[:, :], ps[:, :], [ps], [xtf])
        if router_layer is not None and not self.dbg.get('norouter'):
            self.router(j, router_layer)

    def router(self, j, l):
        sm = self.small
        ps = self.PS()
        for k in range(8):
            xtf = self.xtf[k // 4]
            self.mm(ps[:, 0:20], xtf[:, (k % 4) * 128:(k % 4 + 1) * 128], self.wr[:, l, k, :], k == 0, k == 7,
                    [xtf, self.wr], [ps])
        lg = sm[:, 0:20]
        rb = self.srow[:, 0:20]
        self.tt("dve", lg, ps[:, 0:20], rb, ALU.add, [ps, self.srow], [sm])
        R, Wr = [sm], [sm]
        self.sc.op("dve", lambda e: e.reduce_max(out=sm[:, 20:21], in_=sm[:, 0:4], axis=mybir.AxisListType.X), R, Wr)
        self.ts("dve", sm[:, 21:25], sm[:, 0:4], sm[:, 20:21], None, ALU.is_equal, None, R, Wr)
        self.ts("dve", sm[:, 25:29], sm[:, 0:4], sm[:, 20:21], None, ALU.subtract, None, R, Wr)
        self.act(sm[:, 25:29], sm[:, 25:29], AF.Exp, R, Wr)
        self.sc.op("dve", lambda e: e.reduce_sum(out=sm[:, 29:30], in_=sm[:, 25:29], axis=mybir.AxisListType.X), R, Wr)
        self.sc.op("dve", lambda e: e.reciprocal(out=sm[:, 30:31], in_=sm[:, 29:30]), R, Wr)
        el = sm[:, 4:20].rearrange("p (g e) -> p g e", e=4)
        ohg_b = sm[:, 21:25].unsqueeze(2).broadcast_to([128, 4, 4])
        prod = sm[:, 32:48].rearrange("p (g e) -> p g e", e=4)
        self.tt("dve", prod, el, ohg_b, ALU.mult, R, Wr)
        prod_t = sm[:, 32:48].rearrange("p (g e) -> p e g", e=4)
        self.sc.op("dve", lambda e: e.reduce_sum(out=sm[:, 48:52], in_=prod_t, axis=mybir.AxisListType.X), R, Wr)
        self.sc.op("dve", lambda e: e.reduce_max(out=sm[:, 52:53], in_=sm[:, 48:52], axis=mybir.AxisListType.X), R, Wr)
        self.ts("dve", sm[:, 53:57], sm[:, 48:52], sm[:, 52:53], None, ALU.is_equal, None, R, Wr)
        self.stt(sm[:, 57:61], sm[:, 53:57], -1e30, sm[:, 48:52], ALU.mult, ALU.add, R, Wr)
        self.sc.op("dve", lambda e: e.reduce_max(out=sm[:, 61:62], in_=sm[:, 57:61], axis=mybir.AxisListType.X), R, Wr)
        self.ts("dve", sm[:, 25:29], sm[:, 57:61], sm[:, 61:62], None, ALU.is_equal, None, R, Wr)
        self.ts("dve", sm[:, 62:63], sm[:, 61:62], sm[:, 52:53], None, ALU.subtract, None, R, Wr)
        self.act(sm[:, 62:63], sm[:, 62:63], AF.Exp, R, Wr)
        self.ts("dve", sm[:, 63:64], sm[:, 62:63], 1.0, None, ALU.add, None, R, Wr)
        self.sc.op("dve", lambda e: e.reciprocal(out=sm[:, 63:64], in_=sm[:, 63:64]), R, Wr)
        self.stt(sm[:, 63:64], sm[:, 63:64], 1.0 / ALPHA, sm[:, 30:31], ALU.mult, ALU.mult, R, Wr)
        self.tt("dve", sm[:, 62:63], sm[:, 62:63], sm[:, 63:64], ALU.mult, R, Wr)
        self.ts("dve", sm[:, 53:57], sm[:, 53:57], sm[:, 63:64], None, ALU.mult, None, R, Wr)
        self.stt(sm[:, 53:57], sm[:, 25:29], sm[:, 62:63], sm[:, 53:57], ALU.mult, ALU.add, R, Wr)
        g4_b = sm[:, 53:57].unsqueeze(1).broadcast_to([128, 4, 4])
        gout = self.gate[:, j, :].rearrange("p (g e) -> p g e", e=4)
        self.tt("dve", gout, ohg_b, g4_b, ALU.mult, R, [self.gate])

    def ln_tile(self, xb, xap, g_row, b_row, eps=LN_EPS):
        sm = self.lnsm
        R, Wr = [sm], [sm]
        for h in range(2):
            self.sc.op("dve", lambda e, h=h: e.bn_stats(out=sm[:, 6 * h:6 * h + 6], in_=xap[:, h * 512:(h + 1) * 512]),
                       [xb], Wr)
        self.sc.op("dve", lambda e: e.bn_aggr(out=sm[:, 12:14], in_=sm[:, 0:12]), R, Wr)
        self.ts("dve", sm[:, 14:15], sm[:, 13:14], eps, None, ALU.add, None, R, Wr)
        self.act(sm[:, 14:15], sm[:, 14:15], AF.Ln, R, Wr)
        self.act(sm[:, 15:16], sm[:, 14:15], AF.Exp, R, Wr, scale=-0.5)
        self.stt(xap, xap, sm[:, 12:13], g_row, ALU.subtract, ALU.mult, [xb, sm, self.rows], [xb])
        self.stt(xap, xap, sm[:, 15:16], b_row, ALU.mult, ALU.add, [xb, sm, self.rows], [xb])

    def wnext(self):
        b = self.wslot[self.wi % len(self.wslot)]
        self.wi += 1
        return b

    def wload_k(self, slot, col0, ncols, wbuf, wap2d, nk=8):
        raise NotImplementedError

    def moe(self, l, src, dst, last):
        S, NT, NG = self.S, self.NT, self.NG
        W = self.W
        XA = [self.slab[8 + j] for j in range(NT)]
        xa = [b.t[:, :].bitcast(F32) for b in XA]
        self.load_rows([2 + 4 * l, 3 + 4 * l])
        for j in range(NT):
            sb_, sap = src(j)
            self.load("sp", XA[j], xa[j], sb_, sap)
        hid4 = self.P2[4:8]
        hidv4 = [h.t[:, :].bitcast(BF16)[:, 0:512] for h in hid4]
        self.sc.rec = []
        _marks = []
        _s0 = 0
        _u = 0
        for e in range(self.dbg.get('nexp', 16)):
            sa = self.wnext()
            sv = sa.t[:, :].rearrange("p (k c) -> p k c", c=512)
            self.load("pool", sa, sv[:, :, 0:256], W["moe_w_gate"],
                      W["moe_w_gate"][l, e, :, :].rearrange("(k p) f -> p k f", p=128))
            self.load("pool", sa, sv[:, :, 256:512], W["moe_w_up"],
                      W["moe_w_up"][l, e, :, :].rearrange("(k p) f -> p k f", p=128))
            sd = self.wnext()
            dv = sd.t[:, 0:2048].rearrange("p (k c) -> p k c", c=1024)
            self.load("pool", sd, dv, W["moe_w_down"],
                      W["moe_w_down"][l, e, :, :].rearrange("(k p) f -> p k f", p=128))
            for g in range(NG):
                tsl = slice(g * 512, (g + 1) * 512)
                hid = hid4[2 * (_u % 2):2 * (_u % 2) + 2]
                hidv = hidv4[2 * (_u % 2):2 * (_u % 2) + 2]
                _u += 1
                for f in range(2):
                    gps = self.PS()
                    for k in range(8):
                        self.mm(gps[:, :], sv[:, k, f * 128:(f + 1) * 128], self.XT[k][:, tsl], k == 0, k == 7,
                                [sa, self.XT[k]], [gps])
                    ups = self.PS()
                    for k in range(8):
                        self.mm(ups[:, :], sv[:, k, 256 + f * 128:256 + (f + 1) * 128], self.XT[k][:, tsl], k == 0, k == 7,
                                [sa, self.XT[k]], [ups])
                    sg = self.sg[f]
                    self.act(sg[:, :], gps[:, :], AF.Silu, [gps], [sg])
                    self.tt("dve", hidv[f], sg[:, :], ups[:, :], ALU.mult, [sg, ups], [hid[f]])
                _mid = len(self.sc.rec)
                for tt_ in range(4):
                    j = g * 4 + tt_
                    for half in range(2):
                        ops = self.PS()
                        for f in range(2):
                            self.mm(ops[:, :], hidv[f][:, tt_ * 128:(tt_ + 1) * 128], dv[:, f, half * 512:(half + 1) * 512],
                                    f == 0, f == 1, [hid[f], sd], [ops])
                        xs_ = xa[j][:, half * 512:(half + 1) * 512]
                        self.stt(xs_, ops[:, :], self.gate[:, j, e:e + 1], xs_, ALU.mult, ALU.add,
                                 [ops, self.gate, XA[j]], [XA[j]])
                _marks.append((_s0, _mid, len(self.sc.rec)))
                _s0 = len(self.sc.rec)
        _lst, self.sc.rec = self.sc.rec, None
        if _marks:
            self.sc.replay(_lst[_marks[0][0]:_marks[0][1]])
            for _i in range(len(_marks)):
                if _i + 1 < len(_marks):
                    self.sc.replay(_lst[_marks[_i + 1][0]:_marks[_i + 1][1]])
                self.sc.replay(_lst[_marks[_i][1]:_marks[_i][2]])
        for j in range(NT):
            if not self.dbg.get('noln'):
                self.ln_tile(XA[j], xa[j], self.row(0), self.row(1), eps=LN_EPS / (ALPHA * ALPHA))
            db, dap = dst(j)
            self.load("sp", db, dap, XA[j], xa[j])
            if not last:
                self.transpose_tile(XA[j], xa[j], j)

    def prologue(self, seq, router_layer=None):
        for j in range(self.NT):
            xb = self.xres[j % 2]
            self.load("sp", xb, xb[:, :], self.x, self.x[seq, j * 128:(j + 1) * 128, :])
            self.transpose_tile(xb, xb[:, :], j, router_layer)

    def alloc_misc(self):
        self.xtf = self.P2[0:2]
        self.lnsm = self.sb("lnsm", [128, 16])
        self.sg = self.P2[2:4]
        self.hid = self.P2[4:6]

    def run(self):
        self.setup()
        self.alloc_misc()
        NT = self.NT
        for seq in range(self.NSEQ):
            def src_x(j, seq=seq):
                return self.x, self.x[seq, j * 128:(j + 1) * 128, :]

            def src_scr(j):
                return self.xscr, self.xscr[j * 128:(j + 1) * 128, :]

            def dst_out(j, seq=seq):
                return self.out, self.out[seq, j * 128:(j + 1) * 128, :]
            cur = src_x
            first = True
            subl = [(l, p) for l in self.layers for p in self.parts]
            for i, (l, p) in enumerate(subl):
                last = i == len(subl) - 1
                dst = dst_out if last else src_scr
                if p == "moe":
                    if first:
                        self.load_srow(9 + l, 0, 20)
                        self.prologue(seq, router_layer=l)
                    self.moe(l, cur, dst, last)
                else:
                    if first:
                        self.prologue(seq)
                    if l == 0:
                        self.even_mixer(cur, dst)
                    else:
                        self.odd_mixer(cur, dst)
                cur = src_scr
                first = False
        self.sc.finish_waits("sp", [self.out])
        self.sc.emit_all()
        return self.nc


NROWS = 16
NROWS_SB = 3
NCOLS = 64


def _host_inputs(S, inputs):
    f = lambda a: np.ascontiguousarray(np.asarray(a, dtype=np.float32))
    m = {}
    m["ev_w_in"] = f(inputs["ev_w_in"][0])
    m["ev_w_out"] = f(inputs["ev_w_out"][0])
    wi = f(inputs["od_w_in"][0])
    kpe = wi[:, 384:416]
    kpe_sw = np.concatenate([kpe[:, 16:32], kpe[:, 0:16]], 1)
    m["od_w_in"] = np.ascontiguousarray(np.concatenate([wi, kpe, kpe, kpe_sw, kpe_sw], 1))
    wq = f(inputs["od_w_q_up"][0]).reshape(256, 8, 96)
    nope = wq[:, :, :64].reshape(256, 512)
    pe = wq[:, :, 64:]
    pe_sw = np.concatenate([pe[:, :, 16:], pe[:, :, :16]], 2)
    m["od_w_q_up"] = np.ascontiguousarray(np.concatenate([nope, pe.reshape(256, 256), pe_sw.reshape(256, 256)], 1))
    wkv = f(inputs["od_w_kv_up"][0]).reshape(128, 8, 128)
    m["od_w_kv_up"] = np.ascontiguousarray(np.concatenate([wkv[:, :, :64].reshape(128, 512), wkv[:, :, 64:].reshape(128, 512)], 1))
    m["od_w_out"] = f(inputs["od_w_out"][0])
    m["moe_w_gate"] = f(inputs["moe_w_gate"])
    m["moe_w_up"] = f(inputs["moe_w_up"])
    m["moe_w_down"] = f(inputs["moe_w_down"])
    wr = np.concatenate([f(inputs["moe_w_group"]), f(inputs["moe_w_expert"])], 2)
    m["moe_wr"] = np.ascontiguousarray(wr.reshape(2, 8, 128, 20).transpose(0, 2, 1, 3))
    rows = np.zeros((NROWS, D), np.float32)
    for l in range(2):
        rows[4 * l + 0] = inputs["ln1_g"][l]
        rows[4 * l + 1] = inputs["ln1_b"][l]
        rows[4 * l + 2] = inputs["ln2_g"][l]
        rows[4 * l + 3] = inputs["ln2_b"][l]
        rows[9 + l, 0:4] = inputs["moe_b_group"][l]
        rows[9 + l, 4:20] = inputs["moe_b_expert"][l]
    rows[8] = inputs["ev_norm_g"][0]
    rows[11, 0:16] = inputs["ev_dt_bias"][0]
    rows[11, 16:32] = inputs["ev_a_log"][0]
    rows[11, 32:48] = inputs["ev_d_skip"][0]
    rows[12, 0:256] = inputs["od_q_norm_g"][0]
    rows[12, 256:384] = inputs["od_kv_norm_g"][0]
    rows[13, 0:8] = inputs["od_f_bias"][0]
    m["rows"] = rows
    cols = np.zeros((128, NCOLS), np.float32)
    cw = f(inputs["ev_conv_w"][0])
    cols[:, 0:48] = cw.T.reshape(12, 128, 4).transpose(1, 0, 2).reshape(128, 48)
    cols[:, 48:60] = f(inputs["ev_conv_b"][0]).reshape(12, 128).T
    cols[0:8, 60] = f(inputs["od_f_bias"][0])
    m["cols"] = cols
    for k, v in _consts(S).items():
        m["c_" + k] = v
    return m


_CACHE = {}


def kernel(**inputs):
    x = np.ascontiguousarray(np.asarray(inputs["x"], dtype=np.float32))
    B, S, _ = x.shape
    nseq = B // NCORES
    key = (S, nseq)
    if key not in _CACHE:
        _CACHE[key] = K(S, nseq).run()
    nc = _CACHE[key]
    shared = _host_inputs(S, inputs)
    in_maps = []
    for c in range(NCORES):
        m = dict(shared)
        m["x"] = np.ascontiguousarray(x[c * nseq:(c + 1) * nseq])
        in_maps.append(m)
    res = run_bass_kernel_spmd(nc, in_maps, core_ids=list(range(NCORES)))
    return np.concatenate([np.asarray(r["out"]) for r in res.results], axis=0).astype(np.float32)


def _f32v(b):
    return b.t[:, :].bitcast(F32)


def _bfv(b):
    return b.t[:, :].bitcast(BF16)


def _wview(slot, c):
    return slot.t[:, :].rearrange("p (k c) -> p k c", c=c)


def _kp(ap2d):
    return ap2d.rearrange("(k p) c -> p k c", p=128)


def _rope_tables(self, g):
    rc, rs = self.P2[3], self.P2[4]
    cc, cs_ = self.cdram["ropec"], self.cdram["ropes"]
    self.load("sp", rc, rc[:, :], cc, cc[0:128, g * 512:(g + 1) * 512])
    self.load("sp", rs, rs[:, :], cs_, cs_[0:128, g * 512:(g + 1) * 512])
    return rc, rs


def _rope(self, A, B, rc, rs, dst, dst_ap, lo=0, hi=64):
    t1, t2 = self.P2[5], self.P2[6]
    self.tt("dve", t1[lo:hi, :], A[lo:hi, :], rc[lo:hi, :], ALU.mult, [A, rc], [t1])
    self.tt("dve", t2[lo:hi, :], B[lo:hi, :], rs[lo:hi, :], ALU.mult, [B, rs], [t2])
    self.tt("pool", dst_ap, t1[lo:hi, :], t2[lo:hi, :], ALU.add, [t1, t2], [dst])


def _attn_finish(self, OT, DEN, hh, dst, dst_ap):
    rec = self.P2[6]
    lo, hi = hh * 64, hh * 64 + 64
    self.act(rec[lo:hi, :], DEN[lo:hi, :], AF.Ln, [DEN], [rec])
    self.act(rec[lo:hi, :], rec[lo:hi, :], AF.Exp, [rec], [rec], scale=-1.0)
    self.tt("dve", dst_ap, OT[lo:hi, :], rec[lo:hi, :], ALU.mult, [OT, rec], [dst])


def _replay_hp(self, recs):
    self.sc.replay(recs[0][0])
    for hp in range(len(recs)):
        if hp + 1 < len(recs):
            self.sc.replay(recs[hp + 1][0])
        self.sc.replay(recs[hp][1])


def odd_mixer(self, src, dst):
    S, NT, NG = self.S, self.NT, self.NG
    W = self.W
    Win = W["od_w_in"]
    sl = self.slab
    P2 = self.P2
    XT = self.XT
    c = self.c
    sc_mla = 1.0 / math.sqrt(96.0)
    sc_fox = 1.0 / 8.0
    self.load_rows([4, 5])
    self.load_srow(10, 0, 20)
    self.load_srow(12, 96, 384)
    CQNT, CKVNT, KPE = sl[8:10], sl[10], sl[11]
    OTF = sl[12:16]
    sm = self.small
    wl = self.wnext()
    wlv = _wview(wl, 512)
    self.load("pool", wl, wlv[:, :, 0:384], Win, _kp(Win[:, 0:384]))
    self.load("pool", wl, wlv[:, :, 384:512], Win, _kp(Win[:, 1960:2088]))
    lat, sq, latn_b = P2[0], P2[1], P2[2]
    latn = _bfv(latn_b)
    for j in range(NT):
        tsl = slice(j * 128, (j + 1) * 128)
        ps = self.PS()
        for k in range(8):
            self.mm(ps[:, 0:384], XT[k][:, tsl], wlv[:, k, 0:384], k == 0, k == 7, [XT[k], wl], [ps])
        self.cp("act", lat[:, 0:384], ps[:, 0:384], [ps], [lat])
        self.tt("pool", sq[:, 0:384], lat[:, 0:384], lat[:, 0:384], ALU.mult, [lat], [sq])
        self.sc.op("dve", lambda e: e.reduce_sum(out=sm[:, 0:1], in_=sq[:, 0:256], axis=mybir.AxisListType.X), [sq], [sm])
        self.sc.op("dve", lambda e: e.reduce_sum(out=sm[:, 1:2], in_=sq[:, 256:384], axis=mybir.AxisListType.X), [sq], [sm])
        self.ts("dve", sm[:, 0:1], sm[:, 0:1], 1.0 / 256, RMS_EPS, ALU.mult, ALU.add, [sm], [sm])
        self.ts("dve", sm[:, 1:2], sm[:, 1:2], 1.0 / 128, RMS_EPS, ALU.mult, ALU.add, [sm], [sm])
        self.act(sm[:, 0:2], sm[:, 0:2], AF.Ln, [sm], [sm])
        self.act(sm[:, 2:4], sm[:, 0:2], AF.Exp, [sm], [sm], scale=-0.5)
        self.stt(latn[:, 0:256], lat[:, 0:256], sm[:, 2:3], self.srow[:, 96:352], ALU.mult, ALU.mult,
                 [lat, sm, self.srow], [latn_b])
        self.stt(latn[:, 256:384], lat[:, 256:384], sm[:, 3:4], self.srow[:, 352:480], ALU.mult, ALU.mult,
                 [lat, sm, self.srow], [latn_b])
        pb = self.PS()
        pbv = _bfv(pb)
        for i in range(3):
            self.tr(pbv[:, i * 128:(i + 1) * 128], latn[:, i * 128:(i + 1) * 128], c["identb"][:, :], [latn_b, c["identb"]], [pb])
        for i, dstb in enumerate((CQNT[0], CQNT[1], CKVNT)):
            self.cp("dve", dstb[:, tsl], pbv[:, i * 128:(i + 1) * 128], [pb], [dstb])
    for g in range(NG):
        gsl = slice(g * 512, (g + 1) * 512)
        rc, rs = _rope_tables(self, g)
        A = self.PS()
        for k in range(8):
            self.mm(A[64:96, :], wlv[:, k, 384:416], XT[k][:, gsl], k == 0, k == 7, [wl, XT[k]], [A])
        B = self.PS()
        for k in range(8):
            self.mm(B[64:96, :], wlv[:, k, 448:480], XT[k][:, gsl], k == 0, k == 7, [wl, XT[k]], [B])
        _rope(self, A, B, rc, rs, KPE, KPE[64:96, gsl], 64, 96)
    wf = self.wnext()
    wfv = wf.t[:, 0:64].rearrange("p (k c) -> p k c", c=8)
    self.load("pool", wf, wfv, Win, _kp(Win[:, 1952:1960]))
    ones = P2[7]
    self.sc.op("pool", lambda e: e.memset(ones[:, :], 1.0), [], [ones])
    negfb = self.small2
    self.ts("pool", negfb[0:8, 0:1], self.cols[0:8, 60:61], -1.0, None, ALU.mult, None, [self.cols], [negfb])
    CSPb = [self.T4[0], self.T4[1]]
    FHQb = [self.T4[2], self.T4[3]]

    def cspg(g):
        return CSPb[g // 2], _f32v(CSPb[g // 2])[:, (g % 2) * 512:(g % 2 + 1) * 512]

    def fhqg(g):
        return FHQb[g // 2], _f32v(FHQb[g // 2])[:, (g % 2) * 512:(g % 2 + 1) * 512]
    CSPT = self.cspt
    for g in range(NG):
        gsl = slice(g * 512, (g + 1) * 512)
        ps = self.PS()
        for k in range(8):
            self.mm(ps[0:8, :], wfv[:, k, :], XT[k][:, gsl], k == 0, k == 7, [wf, XT[k]], [ps])
        e_, sp_ = P2[5], P2[6]
        self.act(e_[0:8, :], ps[0:8, :], AF.Exp, [ps, negfb], [e_], bias=negfb[0:8, 0:1], scale=-1.0)
        self.act(sp_[0:8, :], e_[0:8, :], AF.Ln, [e_], [sp_], bias=1.0)
        cb, cap = cspg(g)
        if g == 0:
            init, rds = 0.0, [ones, sp_]
        else:
            pb_, pap = cspg(g - 1)
            init, rds = pap[0:8, 511:512], [ones, sp_, pb_]
        self.sc.op("dve", lambda e, cap=cap, init=init: e.tensor_tensor_scan(
            out=cap[0:8, :], data0=ones[0:8, :], data1=sp_[0:8, :], initial=init, op0=ALU.mult, op1=ALU.add), rds, [cb])
        for tt_ in range(4):
            j = g * 4 + tt_
            pt = self.PS()
            self.tr(pt[:, 0:8], cap[0:8, tt_ * 128:(tt_ + 1) * 128], c["ident"][0:8, 0:8], [cb, c["ident"]], [pt])
            self.cp("dve", CSPT[:, j, :], pt[:, 0:8], [pt], [CSPT])
    self.load_mask("negm_fox")
    OT, DEN = self.acc
    _recs = []
    for hp in range(4):
        self.sc.rec = []
        q4 = sl[16 + 4 * (hp % 2):20 + 4 * (hp % 2)]
        FQz, FK, FV = q4[0:2], q4[2], q4[3]
        if hp < 2:
            self.sc.op("pool", lambda e, b=FQz[0]: e.memset(b[64:128, :], 0.0), [], [FQz[0]])
            self.sc.op("pool", lambda e, b=FQz[1]: e.memset(b[0:64, :], 0.0), [], [FQz[1]])
        wq = self.wnext()
        wqv = _wview(wq, 512)
        for i, c0 in enumerate((416, 928, 1440)):
            self.load("pool", wq, wqv[:, :, i * 128:(i + 1) * 128], Win, _kp(Win[:, c0 + hp * 128:c0 + (hp + 1) * 128]))
        for g in range(NG):
            gsl = slice(g * 512, (g + 1) * 512)
            for i in range(2):
                ps = self.PS()
                for k in range(8):
                    self.mm(ps[:, :], wqv[:, k, i * 128:(i + 1) * 128], XT[k][:, gsl], k == 0, k == 7, [wq, XT[k]], [ps])
                if i == 0:
                    self.cp("act", FQz[0][0:64, gsl], ps[0:64, :], [ps], [FQz[0]])
                    self.cp("act", FQz[1][64:128, gsl], ps[64:128, :], [ps], [FQz[1]])
                else:
                    self.cp("dve", FK[:, gsl], ps[:, :], [ps], [FK])
        for j in range(NT):
            tsl = slice(j * 128, (j + 1) * 128)
            ps = self.PS()
            for k in range(8):
                self.mm(ps[:, 0:128], XT[k][:, tsl], wqv[:, k, 256:384], k == 0, k == 7, [wq, XT[k]], [ps])
            self.evac(FV[:, tsl], ps[:, 0:128], [ps], [FV])
        _split = len(self.sc.rec)
        FHQs = [[self.T4[2], self.T4[3]], [self.T4[2], self.T4[3]]]
        for hh in range(2):
            h = 2 * hp + hh
            for g in range(NG):
                cb, cap = cspg(g)
                m_ = P2[5]
                self.ts("dve", m_[0:8, :], cap[0:8, :], c["ident"][0:8, h:h + 1], 1.0 / sc_fox, ALU.mult, ALU.mult, [cb, c["ident"]], [m_])
                ps = self.PS()
                self.mm(ps[:, :], ones[0:8, 0:128], m_[0:8, :], True, True, [ones, m_], [ps])
                fb = FHQs[hh][g // 2]
                self.cp("act", _f32v(fb)[:, (g % 2) * 512:(g % 2 + 1) * 512], ps[:, :], [ps], [fb])
            _fox_pipe(self, hp, hh, FQz, FK, FV, FHQs, CSPT, OTF[hp], sc_fox)
        _lst, self.sc.rec = self.sc.rec, None
        _recs.append((_lst[:_split], _lst[_split:]))
    _replay_hp(self, _recs)
    self.load_mask("mask_mla")
    wm = self.wnext()
    wq_ = wm.t[:, 0:2048].rearrange("p (k c) -> p k c", c=1024)
    wkv = wm.t[:, 2048:3072]
    self.load("pool", wm, wq_, W["od_w_q_up"], _kp(W["od_w_q_up"][:, :]))
    self.load("pool", wm, wkv, W["od_w_kv_up"], W["od_w_kv_up"][:, :])
    VM = XT[0:4]
    OTM = XT[4:8]
    for j in range(NT):
        ps = self.PS()
        self.mm(ps[:, :], CKVNT[:, j * 128:(j + 1) * 128], wkv[:, 512:1024], True, True, [CKVNT, wm], [ps])
        self.evac(VM[j // 4][:, (j % 4) * 512:(j % 4 + 1) * 512], ps[:, :], [ps], [VM[j // 4]])
    _recs = []
    for hp in range(4):
        self.sc.rec = []
        q4 = sl[16 + 4 * (hp % 2):20 + 4 * (hp % 2)]
        QH, KH = q4[0:2], q4[2:4]
        if hp < 2:
            for b in q4:
                self.sc.op("pool", lambda e, b=b: e.memset(b[64:128, :], 0.0), [], [b])
        for hh in range(2):
            h = 2 * hp + hh
            for g in range(NG):
                gsl = slice(g * 512, (g + 1) * 512)
                ps = self.PS()
                for kk in range(2):
                    self.mm(ps[0:64, :], wq_[:, kk, h * 64:(h + 1) * 64], CQNT[kk][:, gsl], kk == 0, kk == 1, [wm, CQNT[kk]], [ps])
                self.evac(QH[hh][0:64, gsl], ps[0:64, :], [ps], [QH[hh]])
                ps = self.PS()
                self.mm(ps[0:64, :], wkv[:, h * 64:(h + 1) * 64], CKVNT[:, gsl], True, True, [wm, CKVNT], [ps])
                self.evac(KH[hh][0:64, gsl], ps[0:64, :], [ps], [KH[hh]])
                rc, rs = _rope_tables(self, g)
                A = self.PS()
                for kk in range(2):
                    self.mm(A[64:96, :], wq_[:, kk, 512 + h * 32:512 + (h + 1) * 32], CQNT[kk][:, gsl], kk == 0, kk == 1, [wm, CQNT[kk]], [A])
                B = self.PS()
                for kk in range(2):
                    self.mm(B[64:96, :], wq_[:, kk, 768 + h * 32:768 + (h + 1) * 32], CQNT[kk][:, gsl], kk == 0, kk == 1, [wm, CQNT[kk]], [B])
                _rope(self, A, B, rc, rs, QH[hh], QH[hh][64:96, gsl], 64, 96)
            self.cp("act", KH[hh][64:96, 0:S], KPE[64:96, 0:S], [KPE], [KH[hh]])
        _split = len(self.sc.rec)
        _mla_pipe(self, hp, QH, KH, VM, OTM[hp], sc_mla)
        _lst, self.sc.rec = self.sc.rec, None
        _recs.append((_lst[:_split], _lst[_split:]))
    _replay_hp(self, _recs)
    wo = [self.wnext(), self.wnext()]
    wov = [_wview(w_, 1024) for w_ in wo]
    Wo = W["od_w_out"]
    for i in range(2):
        self.load("pool", wo[i], wov[i], Wo, _kp(Wo[i * 512:(i + 1) * 512, :]))
    cat = list(OTM) + list(OTF)
    ps_all6 = self.ps

    def e1(j):
        tsl = slice(j * 128, (j + 1) * 128)
        xr = self.xres[j % 2]
        sb_, sap = src(j)
        self.load("sp", xr, xr[:, :], sb_, sap)
        y = self.tmpA[j % 2]
        for half in range(2):
            hs = slice(half * 512, (half + 1) * 512)
            ps = self.PS()
            for kc in range(8):
                self.mm(ps[:, :], cat[kc][:, tsl], wov[kc // 4][:, kc % 4, hs], kc == 0, kc == 7, [cat[kc], wo[kc // 4]], [ps])
            self.stt(y[:, hs], xr[:, hs], ALPHA, ps[:, :], ALU.mult, ALU.add, [xr, ps], [y])
        self.ln_tile(y, y[:, :], self.row(0), self.row(1))
        db, dap = dst(j)
        self.load("sp", db, dap, y, y[:, :])

    def e2(j):
        y = self.tmpA[j % 2]
        self.transpose_tile(y, y[:, :], j, router_layer=1)

    def rec_(fn, j, banks):
        self.ps = banks
        return self.sc.record(lambda: fn(j))
    self.sc.replay(rec_(e1, 0, ps_all6[0:3]))
    for j in range(NT):
        lb = rec_(e2, j, ps_all6[3:6])
        la = rec_(e1, j + 1, ps_all6[0:3]) if j + 1 < NT else []
        self.sc.replay(la, lb)
    self.ps = ps_all6

K.odd_mixer = odd_mixer


def _v3(ap, inner):
    return ap.rearrange("p (a b) -> p a b", b=inner)


def _bc(ap2, n):
    return ap2.unsqueeze(2).broadcast_to([ap2.shape[0], ap2.shape[1], n])


def even_mixer(self, src, dst):
    S, NT, NG = self.S, self.NT, self.NG
    W = self.W
    Win = W["ev_w_in"]
    sl = self.slab
    P2 = self.P2
    XT = self.XT
    c = self.c
    sm = self.small
    X = mybir.AxisListType.X
    self.load_rows([0, 1, 8])
    self.load_srow(9, 0, 20)
    self.load_srow(11, 32, 48)
    OTS = sl[12:16]
    self.load_mask("mask_sb")
    OTa = self.acc
    _recs = []
    for hp in range(4):
        self.sc.rec = []
        q4 = sl[16 + 4 * (hp % 2):20 + 4 * (hp % 2)]
        QTz, KT, FV = q4[0:2], q4[2], q4[3]
        if hp < 2:
            self.sc.op("pool", lambda e, b=QTz[0]: e.memset(b[64:128, :], 0.0), [], [QTz[0]])
            self.sc.op("pool", lambda e, b=QTz[1]: e.memset(b[0:64, :], 0.0), [], [QTz[1]])
        wq = self.wnext()
        wqv = _wview(wq, 512)
        for i, c0 in enumerate((2576, 3088, 3600)):
            self.load("pool", wq, wqv[:, :, i * 128:(i + 1) * 128], Win, _kp(Win[:, c0 + hp * 128:c0 + (hp + 1) * 128]))
        for g in range(NG):
            gsl = slice(g * 512, (g + 1) * 512)
            for i in range(2):
                ps = self.PS()
                for k in range(8):
                    self.mm(ps[:, :], wqv[:, k, i * 128:(i + 1) * 128], XT[k][:, gsl], k == 0, k == 7, [wq, XT[k]], [ps])
                if i == 0:
                    self.cp("act", QTz[0][0:64, gsl], ps[0:64, :], [ps], [QTz[0]])
                    self.cp("act", QTz[1][64:128, gsl], ps[64:128, :], [ps], [QTz[1]])
                else:
                    self.cp("dve", KT[:, gsl], ps[:, :], [ps], [KT])
        for j in range(NT):
            tsl = slice(j * 128, (j + 1) * 128)
            ps = self.PS()
            for k in range(8):
                self.mm(ps[:, 0:128], XT[k][:, tsl], wqv[:, k, 256:384], k == 0, k == 7, [wq, XT[k]], [ps])
            self.evac(FV[:, tsl], ps[:, 0:128], [ps], [FV])
        _split = len(self.sc.rec)
        _sb_pipe(self, hp, QTz, KT, FV, OTS[hp])
        _lst, self.sc.rec = self.sc.rec, None
        _recs.append((_lst[:_split], _lst[_split:]))
    _replay_hp(self, _recs)
    ps_save = self.ps
    xtf_save = self.xtf
    self.xtf = [P2[0], P2[3]]
    allps = list(self.ps) + list(self.acc)
    self.ps = allps[0:4]
    YD, YO = allps[4:6], allps[6:8]
    XS_T = list(sl[8:12]) + list(sl[16:20])
    BT, CT = sl[20:22], sl[22:24]
    T4 = self.T4
    dests = XS_T + list(BT) + list(CT)
    self.sc.rec = []
    _marks = []
    _s0 = 0
    _uc = 0
    for pnl in range(3):
        wp = self.wnext()
        wpv = _wview(wp, 512)
        self.load("pool", wp, wpv, Win, _kp(Win[:, 1024 + pnl * 512:1024 + (pnl + 1) * 512]))
        for q in range(4):
            cc = pnl * 4 + q
            for g in range(NG):
                gsl = slice(g * 512, (g + 1) * 512)
                ps = self.PS()
                for k in range(8):
                    self.mm(ps[:, :], wpv[:, k, q * 128:(q + 1) * 128], XT[k][:, gsl], k == 0, k == 7, [wp, XT[k]], [ps])
                RAWb = T4[_uc % 2]
                RAW = _bfv(RAWb)
                if g == 0:
                    self.sc.op("pool", lambda e, RAW=RAW: e.memset(RAW[:, 0:3], 0.0), [], [RAWb])
                else:
                    prev = _bfv(T4[(_uc - 1) % 2])
                    self.cp("pool", RAW[:, 0:3], prev[:, 512:515], [T4[(_uc - 1) % 2]], [RAWb])
                _uc += 1
                self.cp("act", RAW[:, 3:515], ps[:, :], [ps], [RAWb])
                DGb = T4[2 + cc % 2]
                DG = _bfv(DGb)
                if g == 0:
                    for tap in range(4):
                        self.ts("dve", DG[:, tap * 128:(tap + 1) * 128], c["identb"][:, :], self.cols[:, cc * 4 + tap:cc * 4 + tap + 1],
                                None, ALU.mult, None, [c["identb"], self.cols], [DGb])
                _mid = len(self.sc.rec)
                cps = self.PS()
                for tap in range(4):
                    self.mm(cps[:, :], DG[:, tap * 128:(tap + 1) * 128], RAW[:, tap:tap + 512], tap == 0, tap == 3, [DGb, RAWb], [cps])
                self.act(dests[cc][:, gsl], cps[:, :], AF.Silu, [cps, self.cols], [dests[cc]], bias=self.cols[:, 48 + cc:49 + cc])
                _marks.append((_s0, _mid, len(self.sc.rec)))
                _s0 = len(self.sc.rec)
    _lst, self.sc.rec = self.sc.rec, None
    self.sc.replay(_lst[_marks[0][0]:_marks[0][1]])
    for _u in range(len(_marks)):
        if _u + 1 < len(_marks):
            self.sc.replay(_lst[_marks[_u + 1][0]:_marks[_u + 1][1]])
        self.sc.replay(_lst[_marks[_u][1]:_marks[_u][2]])
    wd = self.wnext()
    wdv = wd.t[:, 0:128].rearrange("p (k c) -> p k c", c=16)
    self.load("pool", wd, wdv, Win, _kp(Win[:, 2560:2576]))
    DT, DA = self.dtb, self.dab
    AROW = self.small2b
    self.act(AROW[:, 0:16], self.srow[:, 48:64], AF.Exp, [self.srow], [AROW])
    self.ts("pool", AROW[:, 0:16], AROW[:, 0:16], -1.0, None, ALU.mult, None, [AROW], [AROW])
    for j in range(NT):
        tsl = slice(j * 128, (j + 1) * 128)
        ps = self.PS()
        for k in range(8):
            self.mm(ps[:, 0:16], XT[k][:, tsl], wdv[:, k, :], k == 0, k == 7, [XT[k], wd], [ps])
        self.tt("dve", sm[:, 0:16], ps[:, 0:16], self.srow[:, 32:48], ALU.add, [ps, self.srow], [sm])
        self.act(sm[:, 0:16], sm[:, 0:16], AF.Exp, [sm], [sm])
        self.act(DT[:, j, :], sm[:, 0:16], AF.Ln, [sm], [DT], bias=1.0)
        self.tt("dve", DA[:, j, :], DT[:, j, :], AROW[:, 0:16], ALU.mult, [DT, AROW], [DA])
    for half in range(2):
        wz = self.wnext()
        wzv = _wview(wz, 512)
        self.load("pool", wz, wzv, Win, _kp(Win[:, half * 512:(half + 1) * 512]))
        for j in range(NT):
            tsl = slice(j * 128, (j + 1) * 128)
            ps = self.PS()
            for k in range(8):
                self.mm(ps[:, :], XT[k][:, tsl], wzv[:, k, :], k == 0, k == 7, [XT[k], wz], [ps])
            szb = self.tmpA[j % 2]
            self.act(szb[:, 0:512], ps[:, :], AF.Silu, [ps], [szb])
            self.load("sp", self.zscr, self.zscr[j * 128:(j + 1) * 128, half * 512:(half + 1) * 512], szb, szb[:, 0:512])
    wo = [self.wnext(), self.wnext(), self.wnext()]
    wov = [_wview(w_, 1024) for w_ in wo]
    Wo = W["ev_w_out"]
    for i in range(3):
        self.load("pool", wo[i], wov[i], Wo, _kp(Wo[i * 512:(i + 1) * 512, :]))
    XSTOKb, SEGb, GTBb = T4[0], T4[1], T4[3]
    Dgb = SEGb
    XSTOK, SEG = _f32v(XSTOKb), _f32v(SEGb)
    Dg = SEG
    GTB = _bfv(GTBb)
    XSPb, YNb = P2[0], P2[3]
    XSP, YN = _bfv(XSPb), _bfv(YNb)
    XSPPb = [P2[1], P2[2]]
    XSPP = [_bfv(b) for b in XSPPb]
    BTOKb = [self.H2[2], self.H2[3]]
    HT = [P2[5], P2[6]]
    HTbb = P2[7]
    HTb = [_bfv(HTbb)[:, 0:512], _bfv(HTbb)[:, 512:1024]]
    YNTh = [self.H2[0], self.H2[1]]
    TMPb = P2[4]
    self.xtf = [P2[4], P2[3]]
    Y0b = [self.tmpA[1], T4[2]]
    Y0 = [self.tmpA[1][:, :], _f32v(T4[2])]
    ssms = [self.ssm, self.ssm2]
    CBM = self.cbm
    self.sc.op("pool", lambda e: e.memset(GTB, 0.0), [], [GTBb])
    for g in range(2):
        self.sc.op("pool", lambda e, g=g: e.memset(HT[g][:, :], 0.0), [], [HT[g]])
    self.sc.op("pool", lambda e: e.memset(_bfv(HTbb), 0.0), [], [HTbb])

    def stage_a(j):
        tsl = slice(j * 128, (j + 1) * 128)
        ssm = ssms[j % 2]
        ACUM, EA, DTW = ssm[:, 0:16], ssm[:, 16:32], ssm[:, 32:48]
        DEC = [ssm[:, 48:64], ssm[:, 64:80]]
        BTOK = BTOKb[j % 2]
        pb = self.PS()
        pbv = _bfv(pb)
        for cc in range(8):
            self.tr(pbv[:, cc * 128:(cc + 1) * 128], XS_T[cc][:, tsl], c["identb"][:, :], [XS_T[cc], c["identb"]], [pb])
        self.cp("act", XSTOK, pbv, [pb], [XSTOKb])
        pb2 = self.PS()
        pb2v = _bfv(pb2)
        for g in range(2):
            self.tr(pb2v[:, g * 128:(g + 1) * 128], BT[g][:, tsl], c["identb"][:, :], [BT[g], c["identb"]], [pb2])
        self.cp("dve", BTOK[:, 0:256], pb2v[:, 0:256], [pb2], [BTOK])
        ps = self.PS()
        self.mm(ps[:, 0:16], c["tri2"][:, :], DA[:, j, :], True, True, [c["tri2"], DA], [ps])
        self.cp("dve", ACUM, ps[:, 0:16], [ps], [ssm])
        self.act(EA, ACUM, AF.Exp, [ssm], [ssm])
        ps = self.PS()
        self.mm(ps[:, 0:16], c["lastsel"][:, :], ACUM, True, True, [c["lastsel"], ssm], [ps])
        self.tt("dve", DTW, ps[:, 0:16], ACUM, ALU.subtract, [ps, ssm], [ssm])
        self.act(DTW, DTW, AF.Exp, [ssm], [ssm])
        self.tt("dve", DTW, DTW, DT[:, j, :], ALU.mult, [ssm, DT], [ssm])
        for c2 in range(2):
            ps = self.PS()
            self.mm(ps[:, 0:16], c["sellast"][:, c2, :], ACUM, True, True, [c["sellast"], ssm], [ps])
            self.act(DEC[c2], ps[:, 0:16], AF.Exp, [ps], [ssm])
        self.tt("dve", _v3(Dg, 64), _bc(ACUM, 64), c["i64x2"][:, :].unsqueeze(1).broadcast_to([128, 16, 64]), ALU.mult,
                [ssm, c["i64x2"]], [Dgb])
        p1s = []
        for hb in range(2):
            p1 = self.PS()
            p1s.append(p1)
            self.mm(p1[:, :], c["bd"][:, :], Dg[:, hb * 512:(hb + 1) * 512], True, True, [c["bd"], Dgb], [p1])
        for hb in range(2):
            p1 = p1s[hb]
            self.tt("dve", _v3(SEG[:, hb * 512:(hb + 1) * 512], 64), _v3(p1[:, :], 64), _bc(ssm[:, hb * 8:hb * 8 + 8], 64),
                    ALU.subtract, [p1, ssm], [SEGb])
        self.ts("pool", SEG, SEG, 0.0, None, ALU.min, None, [SEGb], [SEGb])
        self.act(SEG, SEG, AF.Exp, [SEGb], [SEGb])
        ps = self.PS()
        for c2 in range(2):
            csl = slice(j * 128 + c2 * 64, j * 128 + c2 * 64 + 64)
            for g in range(2):
                self.sc.op("pe", lambda e, ps=ps, c2=c2, g=g, csl=csl: e.matmul(
                    ps[c2 * 64:c2 * 64 + 64, g * 64:(g + 1) * 64], BT[g][:, csl], CT[g][:, csl], start=True, stop=True,
                    skip_group_check=True), [BT[g], CT[g]], [ps])
        self.tt("dve", _v3(CBM[:, :], 64), _v3(ps[:, 0:128], 64), c["trimask"][:, :].unsqueeze(1).broadcast_to([128, 2, 64]),
                ALU.mult, [ps, c["trimask"]], [CBM])
        for c2 in range(2):
            lo, hi = c2 * 64, c2 * 64 + 64
            out_ap = GTB[lo:hi, :].rearrange("p (h x) -> p h x", x=128)[:, :, lo:hi].rearrange("p (g r) l -> p g r l", g=2)
            in0 = SEG[lo:hi, :].rearrange("p (g r l) -> p g r l", g=2, r=8)
            in1 = _v3(CBM[lo:hi, :], 64).unsqueeze(2).broadcast_to([64, 2, 8, 64])
            self.tt("dve", out_ap, in0, in1, ALU.mult, [SEGb, CBM], [GTBb])
        self.tt("pool", _v3(XSP, 64), _v3(XSTOK, 64), _bc(DT[:, j, :], 64), ALU.mult, [XSTOKb, DT], [XSPb])
        self.tt("pool", _v3(XSPP[j % 2], 64), _v3(XSTOK, 64), _bc(DTW, 64), ALU.mult, [XSTOKb, ssm], [XSPPb[j % 2]])
        for h in range(16):
            self.sc.op("pe", lambda e, h=h: e.matmul(
                YD[h // 8][:, (h % 8) * 64:(h % 8 + 1) * 64], GTB[:, h * 128:(h + 1) * 128], XSP[:, h * 64:(h + 1) * 64],
                start=True, stop=True, skip_group_check=True), [GTBb, XSPb], [YD[h // 8]])
        self.tt("pool", _v3(SEG, 64), _v3(XSTOK, 64), _bc(self.srow[:, 64:80], 64), ALU.mult, [XSTOKb, self.srow], [SEGb])
        for g in range(2):
            hs = slice(g * 512, (g + 1) * 512)
            self.tt("dve", Y0[j % 2][:, hs], SEG[:, hs], YD[g][:, :], ALU.add, [SEGb, YD[g]], [Y0b[j % 2]])

    def stage_b(j):
        tsl = slice(j * 128, (j + 1) * 128)
        ssm = ssms[j % 2]
        DEC = [ssm[:, 48:64], ssm[:, 64:80]]
        BTOK = BTOKb[j % 2]
        Yb, Yap = Y0b[j % 2], Y0[j % 2]
        for c2 in range(2):
            lo, hi = c2 * 64, c2 * 64 + 64
            csl = slice(j * 128 + lo, j * 128 + hi)
            for g in range(2):
                self.sc.op("pe", lambda e, g=g, lo=lo, hi=hi, csl=csl: e.matmul(
                    YO[g][lo:hi, :], CT[g][:, csl], HTb[g], start=True, stop=True, skip_group_check=True),
                    [CT[g], HTbb], [YO[g]])
            for g in range(2):
                st = self.PS()
                self.mm(st[:, :], BTOK[lo:hi, g * 128:(g + 1) * 128], XSPP[j % 2][lo:hi, g * 512:(g + 1) * 512], True, True,
                        [BTOK, XSPPb[j % 2]], [st])
                self.tt("dve", _v3(HT[g][:, :], 64), _v3(HT[g][:, :], 64), _bc(DEC[c2][:, g * 8:(g + 1) * 8], 64), ALU.mult,
                        [HT[g], ssm], [HT[g]])
                self.tt("dve", HT[g][:, :], HT[g][:, :], st[:, :], ALU.add, [HT[g], st], [HT[g]])
                self.cp("act", HTb[g], HT[g][:, :], [HT[g]], [HTbb])
        SZb = self.tmpA[0]
        self.load("sp", SZb, SZb[:, :], self.zscr, self.zscr[tsl, :])
        for g in range(2):
            hs = slice(g * 512, (g + 1) * 512)
            self.tt("dve", _v3(TMPb[:, :], 64), _v3(YO[g][:, :], 64), _bc(ssm[:, 16 + g * 8:16 + g * 8 + 8], 64), ALU.mult,
                    [YO[g], ssm], [TMPb])
            self.tt("pool", Yap[:, hs], Yap[:, hs], TMPb[:, :], ALU.add, [Yb, TMPb], [Yb])
        self.tt("pool", Yap, Yap, SZb[:, :], ALU.mult, [Yb, SZb], [Yb])
        self.tt("pool", SZb[:, :], Yap, Yap, ALU.mult, [Yb], [SZb])
        self.sc.op("dve", lambda e: e.reduce_sum(out=sm[:, 0:1], in_=SZb[:, :], axis=X), [SZb], [sm])
        self.ts("dve", sm[:, 0:1], sm[:, 0:1], 1.0 / 1024, RMS_EPS, ALU.mult, ALU.add, [sm], [sm])
        self.act(sm[:, 0:1], sm[:, 0:1], AF.Ln, [sm], [sm])
        self.act(sm[:, 1:2], sm[:, 0:1], AF.Exp, [sm], [sm], scale=-0.5)
        self.stt(YN, Yap, sm[:, 1:2], self.rows[:, 2, :], ALU.mult, ALU.mult, [Yb, sm, self.rows], [YNb])
        pb = self.PS()
        pbv = _bfv(pb)
        for cc in range(8):
            self.tr(pbv[:, cc * 128:(cc + 1) * 128], YN[:, cc * 128:(cc + 1) * 128], c["identb"][:, :], [YNb, c["identb"]], [pb])
        self.cp("act", YNTh[0][:, :], pbv[:, 0:512], [pb], [YNTh[0]])
        self.cp("act", YNTh[1][:, :], pbv[:, 512:1024], [pb], [YNTh[1]])
        xr = self.xres[j % 2]
        sb_, sap = src(j)
        self.load("sp", xr, xr[:, :], sb_, sap)
        for half in range(2):
            hs = slice(half * 512, (half + 1) * 512)
            ps = self.PS()
            for kc in range(12):
                lhsT = YNTh[kc // 4][:, (kc % 4) * 128:(kc % 4 + 1) * 128] if kc < 8 else OTS[kc - 8][:, tsl]
                rb_ = YNTh[kc // 4] if kc < 8 else OTS[kc - 8]
                self.mm(ps[:, :], lhsT, wov[kc // 4][:, kc % 4, hs], kc == 0, kc == 11, [rb_, wo[kc // 4]], [ps])
            self.stt(SZb[:, hs], xr[:, hs], ALPHA, ps[:, :], ALU.mult, ALU.add, [xr, ps], [SZb])
        self.ln_tile(SZb, SZb[:, :], self.row(0), self.row(1))
        db, dap = dst(j)
        self.load("sp", db, dap, SZb, SZb[:, :])
        self.transpose_tile(SZb, SZb[:, :], j, router_layer=0)

    psA, psB = allps[0:2], allps[2:4]

    def rec_stage(fn, j, banks):
        self.ps = banks
        return self.sc.record(lambda: fn(j))
    self.sc.replay(rec_stage(stage_a, 0, psA))
    for j in range(NT):
        lb = rec_stage(stage_b, j, psB)
        la = rec_stage(stage_a, j + 1, psA) if j + 1 < NT else []
        self.sc.replay(la, lb)
    self.ps = ps_save
    self.xtf = xtf_save


K.even_mixer = even_mixer


def _pipe(n, stages):
    maxs = max(s for s, _ in stages)
    for t in range(n + maxs):
        for s, fn in stages:
            u = t - s
            if 0 <= u < n:
                fn(u)


def _units(NG, hhs=(0, 1)):
    return [(hh, G, idx, i, 4 * (G + 1)) for hh in hhs for G in range(NG)
            for idx, i in enumerate(range(4 * (G + 1) - 1, -1, -1))]


def _sb_pipe(self, hp, QTz, KT, FV, OTSb):
    P2, H2, c = self.P2, self.H2, self.c
    units = _units(self.NG)
    n = len(units)
    E, Ecs = P2[0:3], P2[3:5]
    Lb, Rb = H2[0:2], H2[2]
    Wb = [H2[3], P2[5]]
    Wt = [H2[3][:, :], _bfv(P2[5])[:, 0:512]]
    zb, cb = {}, {}

    def s_z(u):
        hh, G, idx, i, nkb = units[u]
        lo, hi = hh * 64, hh * 64 + 64
        z = self.PS()
        zb[u] = z
        self.mm(z[:, :], KT[:, i * 128:(i + 1) * 128], QTz[hh][:, G * 512:(G + 1) * 512], True, True, [KT, QTz[hh]], [z])

    def s_E(u):
        hh, G, idx, i, nkb = units[u]
        z = zb.pop(u)
        e = E[u % 3]
        self.act(e[:, :], z[:, :], AF.Exp, [z], [e], scale=0.125)
        if i >= 4 * G:
            self.tt("dve", e[:, :], e[:, :], self.maskbuf[:, i - 4 * G, :], ALU.mult, [e, self.maskbuf], [e])

    def s_Lb(u):
        e, l = E[u % 3], Lb[u % 2]
        self.act(l[:, :], e[:, :], AF.Ln, [e], [l], bias=1.0)

    def s_CS(u):
        hh, G, idx, i, nkb = units[u]
        l = Lb[u % 2]
        CS = self.PS()
        cb[u] = CS
        self.mm(CS[:, :], c["uincl"][:, :], l[:, :], True, idx == 0, [c["uincl"], l], [CS])
        if idx > 0:
            self.mm(CS[:, :], c["onesb"][:, :], Rb[:, :], False, True, [c["onesb"], Rb], [CS])
        if idx < nkb - 1:
            if idx == 0:
                self.cp("dve", Rb[:, :], l[:, :], [l], [Rb])
            else:
                self.tt("pool", Rb[:, :], Rb[:, :], l[:, :], ALU.add, [Rb, l], [Rb])

    def s_Ecs(u):
        CS = cb.pop(u)
        ec = Ecs[u % 2]
        self.act(ec[:, :], CS[:, :], AF.Exp, [CS], [ec], scale=-1.0)
        self.tt("dve", Wt[u % 2], E[u % 3][:, :], ec[:, :], ALU.mult, [E[u % 3], ec], [Wb[u % 2]])

    def s_PV(u):
        hh, G, idx, i, nkb = units[u]
        lo, hi = hh * 64, hh * 64 + 64
        OT = self.acc[(hh * self.NG + G) % 2]
        self.mm(OT[:, :], FV[:, i * 128:(i + 1) * 128], Wt[u % 2], idx == 0, idx == nkb - 1, [FV, Wb[u % 2]], [OT])
        if idx == nkb - 1:
            self.cp("act", OTSb[lo:hi, G * 512:(G + 1) * 512], OT[lo:hi, :], [OT], [OTSb])

    _pipe(n, [(0, s_z), (2, s_Lb), (2, s_CS), (3, s_Ecs), (1, s_E), (4, s_PV)])


def _fox_pipe(self, hp, hh_, FQz, FK, FV, FHQs, CSPT, OTb, scale):
    P2, H2, c = self.P2, self.H2, self.c
    units = _units(self.NG, (hh_,))
    n = len(units)
    A = P2[0:3]
    Wb = H2[0:2]
    OT, DEN = self.acc
    zb = {}

    def s_z(u):
        hh, G, idx, i, nkb = units[u]
        lo, hi = hh * 64, hh * 64 + 64
        z = self.PS()
        zb[u] = z
        self.mm(z[:, :], FK[:, i * 128:(i + 1) * 128], FQz[hh][:, G * 512:(G + 1) * 512], True, True, [FK, FQz[hh]], [z])

    def s_A(u):
        hh, G, idx, i, nkb = units[u]
        z = zb.pop(u)
        a = A[u % 3]
        fb = FHQs[hh][G // 2]
        fap = _f32v(fb)[:, (G % 2) * 512:(G % 2 + 1) * 512]
        self.tt("dve", a[:, :], z[:, :], fap, ALU.subtract, [z, fb], [a])
        if i >= 4 * G:
            self.tt("pool", a[:, :], a[:, :], self.maskbuf[:, i - 4 * G, :], ALU.add, [a, self.maskbuf], [a])

    def s_W(u):
        hh, G, idx, i, nkb = units[u]
        h = 2 * hp + hh
        a, w = A[u % 3], Wb[u % 2]
        self.act(w[:, :], a[:, :], AF.Exp, [a, CSPT], [w], bias=CSPT[:, i, h:h + 1], scale=scale)

    def s_PV(u):
        hh, G, idx, i, nkb = units[u]
        lo, hi = hh * 64, hh * 64 + 64
        w = Wb[u % 2]
        self.mm(OT[:, :], FV[:, i * 128:(i + 1) * 128], w[:, :], idx == 0, idx == nkb - 1, [FV, w], [OT])
        self.mm(DEN[:, :], c["onesb"][:, :], w[:, :], idx == 0, idx == nkb - 1, [c["onesb"], w], [DEN])
        if idx == nkb - 1:
            _attn_finish(self, OT, DEN, hh, OTb, OTb[lo:hi, G * 512:(G + 1) * 512])

    _pipe(n, [(0, s_z), (2, s_W), (1, s_A), (3, s_PV)])


def _mla_pipe(self, hp, QH, KH, VM, OTb, scale):
    P2, H2, c = self.P2, self.H2, self.c
    units = _units(self.NG)
    n = len(units)
    Wf = P2[0:2]
    Wb = H2[0:2]
    OT, DEN = self.acc
    zb = {}

    def s_z(u):
        hh, G, idx, i, nkb = units[u]
        lo, hi = hh * 64, hh * 64 + 64
        plo, phi = hh * 32, hh * 32 + 32
        ksl, gsl = slice(i * 128, (i + 1) * 128), slice(G * 512, (G + 1) * 512)
        z = self.PS()
        zb[u] = z
        self.mm(z[:, :], KH[hh][:, ksl], QH[hh][:, gsl], True, True, [KH[hh], QH[hh]], [z])

    def s_W(u):
        hh, G, idx, i, nkb = units[u]
        z = zb.pop(u)
        w = Wb[u % 2]
        if i >= 4 * G:
            wf = Wf[u % 2]
            self.act(wf[:, :], z[:, :], AF.Exp, [z], [wf], scale=scale)
            self.tt("dve", w[:, :], wf[:, :], self.maskbuf[:, i - 4 * G, :], ALU.mult, [wf, self.maskbuf], [w])
        else:
            self.act(w[:, :], z[:, :], AF.Exp, [z], [w], scale=scale)

    def s_PV(u):
        hh, G, idx, i, nkb = units[u]
        h = 2 * hp + hh
        lo, hi = hh * 64, hh * 64 + 64
        w = Wb[u % 2]
        vs = VM[i // 4][:, (i % 4) * 512 + hp * 128:(i % 4) * 512 + (hp + 1) * 128]
        self.mm(OT[:, :], vs, w[:, :], idx == 0, idx == nkb - 1, [VM[i // 4], w], [OT])
        self.mm(DEN[:, :], c["onesb"][:, :], w[:, :], idx == 0, idx == nkb - 1, [c["onesb"], w], [DEN])
        if idx == nkb - 1:
            _attn_finish(self, OT, DEN, hh, OTb, OTb[lo:hi, G * 512:(G + 1) * 512])

    _pipe(n, [(0, s_z), (1, s_W), (2, s_PV)])
```
